# Optimizing a Trainium2 kernel written in Bass

```python
import math
import jax
import jax.numpy as jnp
from jax import lax
import numpy as np

D_MODEL = 1024
BATCH = 8
SEQ = 4096
DEPTH = 4

GRID_W = 64
CTX_LEN = 256
EPS = 1e-6

MIX_W = D_MODEL
S5_W = D_MODEL // 4
S5_GROUP = 16
S5_GROUPS = S5_W // S5_GROUP
S5_STATE = 64
MLA_V = 64
MLA_W = D_MODEL // 2
MLA_HEADS = MLA_W // MLA_V
MLA_NOPE = 64
MLA_ROPE = 32
MLA_Q_RANK = 384
MLA_KV_RANK = 256
MLA_SCALE = 1.0 / math.sqrt(MLA_NOPE + MLA_ROPE)
ROPE_BASE = 10000.0
Q_BLOCK = 128
HY_W = D_MODEL // 4
HY_ORDER = 2
HY_POS_EMB = 33
HY_FILTER_W = 64
HY_MIN_DECAY = math.log(1e-2) / 1.5
HY_MAX_DECAY = math.log(1e-2) / 0.3
P_IN = S5_W + MLA_Q_RANK + MLA_KV_RANK + MLA_ROPE + 3 * HY_W
MOE_GROUPS = 4
MOE_PER_GROUP = 8
MOE_EXPERTS = MOE_GROUPS * MOE_PER_GROUP
MOE_TOP_K = 2
MOE_HIDDEN = 512
MOE_BLOCK = 256

kernel_name = 'hybrid_s5_mla_hyena_hmoe_diffusion'


def _rms_norm(x, g):
    xf = x.astype(jnp.float32)
    y = xf * lax.rsqrt(jnp.mean(xf * xf, axis=-1, keepdims=True) + EPS)
    return (y * g.astype(jnp.float32)).astype(x.dtype)


def _modulate(x, g, shift, scale):
    return _rms_norm(x, g) * (1.0 + scale) + shift


def _split_projection(p):
    o1 = S5_W
    o2 = o1 + MLA_Q_RANK
    o3 = o2 + MLA_KV_RANK
    o4 = o3 + MLA_ROPE
    return p[..., :o1], p[..., o1:o2], p[..., o2:o3], p[..., o3:o4], p[..., o4:]


def _s5_discretise(lam_re, lam_im, log_dt, b_re, b_im):
    lam = lax.complex(lam_re.astype(jnp.float32), lam_im.astype(jnp.float32))
    dt = jnp.exp(log_dt.astype(jnp.float32))[..., None]
    lam_bar = jnp.exp(lam * dt)
    b = lax.complex(b_re.astype(jnp.float32), b_im.astype(jnp.float32))
    b_bar = ((lam_bar - 1.0) / lam)[..., None] * b
    return lam_bar, b_bar


def _diag_scan(lam_bar, bu, reverse):
    a = jnp.broadcast_to(lam_bar, bu.shape)

    def combine(e1, e2):
        a1, b1 = e1
        a2, b2 = e2
        return a1 * a2, a2 * b1 + b2

    return lax.associative_scan(combine, (a, bu), reverse=reverse, axis=1)[1]


def _s5_mixer(u_l, u_c, lam_re, lam_im, log_dt, b_re, b_im, c_re, c_im, d, glu_w, ctx_out):
    lam_bar, b_bar = _s5_discretise(lam_re, lam_im, log_dt, b_re, b_im)
    c = lax.complex(c_re.astype(jnp.float32), c_im.astype(jnp.float32))

    def drive(u):
        ug = u.astype(jnp.float32).reshape(u.shape[0], u.shape[1], S5_GROUPS, S5_GROUP).astype(jnp.complex64)
        return [jnp.einsum('blgh,gph->blgp', ug, b_bar[k]) for k in range(2)]

    bu_c = drive(u_c)
    bu_l = drive(u_l)
    hc_f = _diag_scan(lam_bar[0], bu_c[0], False)
    hc_b = _diag_scan(lam_bar[1], bu_c[1], True)
    hl_f = _diag_scan(lam_bar[0], bu_l[0].at[:, 0].add(lam_bar[0] * hc_f[:, -1]), False)
    hl_b = _diag_scan(lam_bar[1], bu_l[1].at[:, -1].add(lam_bar[1] * hc_b[:, 0]), True)

    def readout(u, h_f, h_b):
        y = jnp.real(jnp.einsum('blgp,ghp->blgh', h_f, c[0]) + jnp.einsum('blgp,ghp->blgh', h_b, c[1]))
        y = y.reshape(u.shape) + d.astype(jnp.float32) * u.astype(jnp.float32)
        y = jax.nn.gelu(y)
        return (y * jax.nn.sigmoid(y @ glu_w.astype(jnp.float32))).astype(u.dtype)

    y_l = readout(u_l, hl_f, hl_b)
    y_c = readout(u_c, hc_f, hc_b) if ctx_out else None
    return y_l, y_c


def _axial_rope_tables(n_tokens):
    rows = n_tokens // GRID_W
    row = jnp.repeat(jnp.arange(rows), GRID_W).astype(jnp.float32)
    col = jnp.tile(jnp.arange(GRID_W), rows).astype(jnp.float32)
    half = MLA_ROPE // 2
    inv = ROPE_BASE ** (-jnp.arange(0, half, 2, dtype=jnp.float32) / half)
    ang_r = row[:, None] * inv
    ang_c = col[:, None] * inv
    return jnp.cos(ang_r), jnp.sin(ang_r), jnp.cos(ang_c), jnp.sin(ang_c)


def _rotate(x, cos, sin):
    m = x.shape[-1] // 2
    x1, x2 = x[..., :m], x[..., m:]
    return jnp.concatenate([x1 * cos - x2 * sin, x1 * sin + x2 * cos], axis=-1)


def _axial_rope(x, tables):
    cr, sr, cc, sc = (t[:, None, :].astype(x.dtype) for t in tables)
    half = MLA_ROPE // 2
    return jnp.concatenate([_rotate(x[..., :half], cr, sr), _rotate(x[..., half:], cc, sc)], axis=-1)


def _mla_attend(q_nope, q_rope, k_nope, k_rope, v):
    s = jnp.einsum('bqhd,bkhd->bhqk', q_nope, k_nope) + jnp.einsum('bqhr,bkr->bhqk', q_rope, k_rope)
    p = jax.nn.softmax(s.astype(jnp.float32) * MLA_SCALE, axis=-1).astype(v.dtype)
    return jnp.einsum('bhqk,bkhd->bqhd', p, v)


def _mla_mixer(cq_l, ckv_l, kr_l, cq_c, ckv_c, kr_c, q_norm_g, kv_norm_g, w_uq, w_ukv, ctx_out):
    def queries(cq):
        q = (_rms_norm(cq, q_norm_g) @ w_uq).reshape(cq.shape[0], cq.shape[1], MLA_HEADS, MLA_NOPE + MLA_ROPE)
        return q[..., :MLA_NOPE], q[..., MLA_NOPE:]

    def keys_values(ckv):
        kv = (_rms_norm(ckv, kv_norm_g) @ w_ukv).reshape(ckv.shape[0], ckv.shape[1], MLA_HEADS, MLA_NOPE + MLA_V)
        return kv[..., :MLA_NOPE], kv[..., MLA_NOPE:]

    bsz, n = cq_l.shape[0], cq_l.shape[1]
    tables = _axial_rope_tables(n)
    kn_c, v_c = keys_values(ckv_c)
    kn_l, v_l = keys_values(ckv_l)
    kr_lat = _axial_rope(kr_l[:, :, None, :], tables)[:, :, 0]
    qn_l, qr_l = queries(cq_l)
    qr_l = _axial_rope(qr_l, tables)
    k_nope = jnp.concatenate([kn_c, kn_l], axis=1)
    k_rope = jnp.concatenate([kr_c, kr_lat], axis=1)
    v = jnp.concatenate([v_c, v_l], axis=1)
    nblk = n // Q_BLOCK

    def to_blocks(q):
        return q.reshape(bsz, nblk, Q_BLOCK, q.shape[2], q.shape[3]).swapaxes(0, 1)

    o = lax.map(lambda qb: _mla_attend(qb[0], qb[1], k_nope, k_rope, v), (to_blocks(qn_l), to_blocks(qr_l)))
    y_l = o.swapaxes(0, 1).reshape(bsz, n, MLA_W)
    y_c = None
    if ctx_out:
        qn_c, qr_c = queries(cq_c)
        y_c = _mla_attend(qn_c, qr_c, kn_c, kr_c, v_c).reshape(bsz, kr_c.shape[1], MLA_W)
    return y_l, y_c


def _short_conv(z, w, b):
    n = z.shape[1]
    zp = jnp.pad(z, ((0, 0), (1, 1), (0, 0)))
    return zp[:, :n] * w[0] + zp[:, 1:n + 1] * w[1] + zp[:, 2:] * w[2] + b


def _hyena_filter_spectrum(n, w1, b1, w2, b2, w3, b3, freq):
    t = jnp.linspace(0.0, 1.0, n, dtype=jnp.float32)[:, None]
    bands = (HY_POS_EMB - 1) // 2
    f = jnp.linspace(1e-4, bands - 1, bands, dtype=jnp.float32)
    ang = (2.0 * math.pi * jnp.arange(n, dtype=jnp.float32) / n)[:, None] * f
    z = jnp.concatenate([t, jnp.cos(ang), -jnp.sin(ang)], axis=-1)
    h = jnp.sin(freq[0] * (z @ w1 + b1))
    h = jnp.sin(freq[1] * (h @ w2 + b2))
    h = (h @ w3 + b3).astype(jnp.float32).reshape(n, 2, HY_ORDER, HY_W)
    deltas = jnp.abs(jnp.linspace(HY_MIN_DECAY, HY_MAX_DECAY, HY_W, dtype=jnp.float32))
    h = h * jnp.exp(-t[:, :, None, None] * deltas)
    fwd, bwd = h[:, 0], h[:, 1]
    k = jnp.concatenate([fwd, jnp.zeros((1, HY_ORDER, HY_W), jnp.float32), bwd[:0:-1]], axis=0)
    return jnp.fft.rfft(k, axis=0)


def _fft_long_conv(y, k_f):
    n = y.shape[1]
    yf = jnp.fft.rfft(y, n=2 * n, axis=1)
    return jnp.fft.irfft(yf * k_f, n=2 * n, axis=1)[:, :n]


def _hyena_mixer(z, short_w, short_b, filt, bias):
    n = z.shape[1]
    z = _short_conv(z, short_w, short_b)
    v, x1, x2 = jnp.split(z, 3, axis=-1)
    k_f = _hyena_filter_spectrum(n, *filt)
    y = v.astype(jnp.float32)
    for o, gate in enumerate((x1, x2)):
        y = gate.astype(jnp.float32) * (_fft_long_conv(y, k_f[:, o]) + y * bias[o].astype(jnp.float32))
    return y.astype(z.dtype)


def _merge_groups(y_s5, y_mla, y_hy, g):
    return jnp.concatenate([
        _rms_norm(y_s5, g[:S5_W]),
        _rms_norm(y_mla, g[S5_W:S5_W + MLA_W]),
        _rms_norm(y_hy, g[S5_W + MLA_W:]),
    ], axis=-1)


def _hier_moe(h, w_group, w_expert, w1, w3, w2):
    t, d = h.shape
    g_prob = jax.nn.softmax((h @ w_group).astype(jnp.float32), axis=-1)
    g_w, g_idx = lax.top_k(g_prob, 1)
    e_logits = (h @ w_expert).astype(jnp.float32).reshape(t, MOE_GROUPS, MOE_PER_GROUP)
    in_group = jnp.take_along_axis(e_logits, g_idx[:, :, None], axis=1)[:, 0]
    top_v, top_i = lax.top_k(in_group, MOE_TOP_K)
    gate = jax.nn.softmax(top_v, axis=-1) * g_w
    eid = g_idx * MOE_PER_GROUP + top_i
    n_assign = t * MOE_TOP_K
    flat_e = eid.reshape(n_assign)
    order = jnp.argsort(flat_e)
    se = flat_e[order]
    st = (order // MOE_TOP_K).astype(jnp.int32)
    sw = gate.reshape(n_assign)[order]
    counts = jnp.bincount(flat_e, length=MOE_EXPERTS)
    padded = (counts + MOE_BLOCK - 1) // MOE_BLOCK * MOE_BLOCK
    start = jnp.cumsum(counts) - counts
    pend = jnp.cumsum(padded)
    pstart = pend - padded
    dest = pstart[se] + jnp.arange(n_assign) - start[se]
    n_blocks = -(-n_assign // MOE_BLOCK) + MOE_EXPERTS
    n_rows = n_blocks * MOE_BLOCK
    row_tok = jnp.zeros((n_rows,), jnp.int32).at[dest].set(st)
    row_w = jnp.zeros((n_rows,), jnp.float32).at[dest].set(sw)
    block_e = jnp.minimum(jnp.searchsorted(pend, jnp.arange(n_blocks) * MOE_BLOCK, side='right'), MOE_EXPERTS - 1)

    def expert_block(args):
        xb, e = args
        return (jax.nn.silu(xb @ w1[e]) * (xb @ w3[e])) @ w2[e]

    ys = lax.map(expert_block, (h[row_tok].reshape(n_blocks, MOE_BLOCK, d), block_e))
    ys = ys.reshape(n_rows, d) * row_w[:, None].astype(h.dtype)
    return jnp.zeros_like(h).at[row_tok].add(ys)


def setup_inputs(seed: int = 0) -> dict:
    key = jax.random.key(seed)
    ks = iter(jax.random.split(key, 48))

    def nrm(shape, scale):
        return scale * jax.random.normal(next(ks), shape, jnp.float32)

    D = D_MODEL
    G, P, H = S5_GROUPS, S5_STATE, S5_GROUP
    inp = {}
    inp['x'] = nrm((BATCH, SEQ, D), 1.0)
    inp['c'] = nrm((BATCH, D), 1.0)
    inp['ctx'] = nrm((BATCH, CTX_LEN, D), 1.0)
    inp['c_ctx'] = nrm((D,), 1.0)
    inp['ada_w'] = nrm((DEPTH, D, 6 * D), 0.5 * D ** -0.5)
    inp['ada_b'] = nrm((DEPTH, 6 * D), 0.02)
    inp['norm1_g'] = 1.0 + nrm((DEPTH, D), 0.02)
    inp['norm2_g'] = 1.0 + nrm((DEPTH, D), 0.02)
    inp['w_in'] = nrm((DEPTH, D, P_IN), D ** -0.5)
    inp['s5_lambda_re'] = -0.5 + nrm((DEPTH, 2, G, P), 0.01)
    inp['s5_lambda_im'] = math.pi * jnp.arange(P, dtype=jnp.float32) + nrm((DEPTH, 2, G, P), 0.01)
    inp['s5_log_dt'] = jax.random.uniform(next(ks), (DEPTH, 2, G), jnp.float32, math.log(1e-3), math.log(1e-1))
    inp['s5_b_re'] = nrm((DEPTH, 2, G, P, H), (2 * H) ** -0.5)
    inp['s5_b_im'] = nrm((DEPTH, 2, G, P, H), (2 * H) ** -0.5)
    inp['s5_c_re'] = nrm((DEPTH, 2, G, H, P), (2 * P) ** -0.5)
    inp['s5_c_im'] = nrm((DEPTH, 2, G, H, P), (2 * P) ** -0.5)
    inp['s5_d'] = nrm((DEPTH, S5_W), 1.0)
    inp['s5_glu_w'] = nrm((DEPTH, S5_W, S5_W), S5_W ** -0.5)
    inp['mla_q_norm_g'] = 1.0 + nrm((DEPTH, MLA_Q_RANK), 0.02)
    inp['mla_kv_norm_g'] = 1.0 + nrm((DEPTH, MLA_KV_RANK), 0.02)
    inp['mla_w_uq'] = nrm((DEPTH, MLA_Q_RANK, MLA_HEADS * (MLA_NOPE + MLA_ROPE)), MLA_Q_RANK ** -0.5)
    inp['mla_w_ukv'] = nrm((DEPTH, MLA_KV_RANK, MLA_HEADS * (MLA_NOPE + MLA_V)), MLA_KV_RANK ** -0.5)
    inp['hy_short_w'] = nrm((DEPTH, 3, 3 * HY_W), 3 ** -0.5)
    inp['hy_short_b'] = nrm((DEPTH, 3 * HY_W), 0.02)
    inp['hy_f_w1'] = nrm((DEPTH, HY_POS_EMB, HY_FILTER_W), HY_POS_EMB ** -0.5)
    inp['hy_f_b1'] = nrm((DEPTH, HY_FILTER_W), 0.02)
    inp['hy_f_w2'] = nrm((DEPTH, HY_FILTER_W, HY_FILTER_W), HY_FILTER_W ** -0.5)
    inp['hy_f_b2'] = nrm((DEPTH, HY_FILTER_W), 0.02)
    inp['hy_f_w3'] = nrm((DEPTH, HY_FILTER_W, 2 * HY_ORDER * HY_W), 0.1 * HY_FILTER_W ** -0.5)
    inp['hy_f_b3'] = nrm((DEPTH, 2 * HY_ORDER * HY_W), 0.002)
    inp['hy_f_freq'] = 1.0 + nrm((DEPTH, 2, HY_FILTER_W), 0.02)
    inp['hy_bias'] = nrm((DEPTH, HY_ORDER, HY_W), 1.0)
    inp['mix_norm_g'] = 1.0 + nrm((DEPTH, MIX_W), 0.02)
    inp['w_out'] = nrm((DEPTH, MIX_W, D), MIX_W ** -0.5)
    inp['moe_w_group'] = nrm((DEPTH, D, MOE_GROUPS), D ** -0.5)
    inp['moe_w_expert'] = nrm((DEPTH, D, MOE_EXPERTS), D ** -0.5)
    inp['moe_w1'] = nrm((DEPTH, MOE_EXPERTS, D, MOE_HIDDEN), D ** -0.5)
    inp['moe_w3'] = nrm((DEPTH, MOE_EXPERTS, D, MOE_HIDDEN), D ** -0.5)
    inp['moe_w2'] = nrm((DEPTH, MOE_EXPERTS, MOE_HIDDEN, D), MOE_HIDDEN ** -0.5)
    inp['final_g'] = 1.0 + nrm((D,), 0.02)
    return inp


def reference(x, c, ctx, c_ctx, ada_w, ada_b, norm1_g, norm2_g, w_in,
              s5_lambda_re, s5_lambda_im, s5_log_dt, s5_b_re, s5_b_im, s5_c_re, s5_c_im, s5_d, s5_glu_w,
              mla_q_norm_g, mla_kv_norm_g, mla_w_uq, mla_w_ukv,
              hy_short_w, hy_short_b, hy_f_w1, hy_f_b1, hy_f_w2, hy_f_b2, hy_f_w3, hy_f_b3, hy_f_freq, hy_bias,
              mix_norm_g, w_out, moe_w_group, moe_w_expert, moe_w1, moe_w3, moe_w2, final_g):
    bsz, n, d = x.shape
    n_ctx = ctx.shape[1]
    xl, xc = x, ctx
    act_l = jax.nn.silu(c)
    act_c = jax.nn.silu(c_ctx)
    for i in range(DEPTH):
        ctx_out = i < DEPTH - 1
        mod_l = jnp.split((act_l @ ada_w[i] + ada_b[i])[:, None, :], 6, axis=-1)
        mod_c = jnp.split((act_c @ ada_w[i] + ada_b[i])[None, None, :], 6, axis=-1)
        hl = _modulate(xl, norm1_g[i], mod_l[0], mod_l[1])
        hc = _modulate(xc, norm1_g[i], mod_c[0], mod_c[1])
        u_l, cq_l, ckv_l, kr_l, hz_l = _split_projection(hl @ w_in[i])
        u_c, cq_c, ckv_c, kr_c, hz_c = _split_projection(hc @ w_in[i])
        s5_l, s5_c = _s5_mixer(u_l, u_c, s5_lambda_re[i], s5_lambda_im[i], s5_log_dt[i], s5_b_re[i], s5_b_im[i],
                               s5_c_re[i], s5_c_im[i], s5_d[i], s5_glu_w[i], ctx_out)
        mla_l, mla_c = _mla_mixer(cq_l, ckv_l, kr_l, cq_c, ckv_c, kr_c, mla_q_norm_g[i], mla_kv_norm_g[i],
                                  mla_w_uq[i], mla_w_ukv[i], ctx_out)
        filt = (hy_f_w1[i], hy_f_b1[i], hy_f_w2[i], hy_f_b2[i], hy_f_w3[i], hy_f_b3[i], hy_f_freq[i])
        hy_l = _hyena_mixer(hz_l, hy_short_w[i], hy_short_b[i], filt, hy_bias[i])
        xl = xl + mod_l[2] * (_merge_groups(s5_l, mla_l, hy_l, mix_norm_g[i]) @ w_out[i])
        fl = _modulate(xl, norm2_g[i], mod_l[3], mod_l[4]).reshape(bsz * n, d)
        if ctx_out:
            hy_c = _hyena_mixer(hz_c, hy_short_w[i], hy_short_b[i], filt, hy_bias[i])
            xc = xc + mod_c[2] * (_merge_groups(s5_c, mla_c, hy_c, mix_norm_g[i]) @ w_out[i])
            fc = _modulate(xc, norm2_g[i], mod_c[3], mod_c[4]).reshape(bsz * n_ctx, d)
            y = _hier_moe(jnp.concatenate([fl, fc], axis=0), moe_w_group[i], moe_w_expert[i],
                          moe_w1[i], moe_w3[i], moe_w2[i])
            xl = xl + mod_l[5] * y[:bsz * n].reshape(bsz, n, d)
            xc = xc + mod_c[5] * y[bsz * n:].reshape(bsz, n_ctx, d)
        else:
            y = _hier_moe(fl, moe_w_group[i], moe_w_expert[i], moe_w1[i], moe_w3[i], moe_w2[i])
            xl = xl + mod_l[5] * y.reshape(bsz, n, d)
    return _rms_norm(xl, final_g)
```

```python
import math
from contextlib import ExitStack
import numpy as np
import ml_dtypes
import concourse.bass as bass
import concourse.mybir as mybir
from concourse.bass_utils import run_bass_kernel_spmd

F32 = mybir.dt.float32
BF16 = mybir.dt.bfloat16
I32 = mybir.dt.int32
U32 = mybir.dt.uint32
AF = mybir.ActivationFunctionType
OP = mybir.AluOpType
AX = mybir.AxisListType

DM = 1024
NCTX = 256
NLAT = 4096
T = NCTX + NLAT
NT = T // 128
DEPTH = 4
EPS = 1e-6
P_IN = 1696
MAGIC = 12582912.0
MLA_SCALE = 1.0 / math.sqrt(96.0)
NE = 32
NBLK = 100


class Buf:
    __slots__ = ("w", "r", "name")

    def __init__(self, name=""):
        self.w = None
        self.r = {}
        self.name = name


class KB:
    NDMA = 10

    def __init__(self):
        nc = bass.Bass("TRN2", target_bir_lowering=False)
        self.nc = nc
        self.eng = {"pe": nc.tensor, "act": nc.scalar, "dve": nc.vector, "pool": nc.gpsimd, "sp": nc.sync}
        self.sem = {}
        self.cnt = {}
        for e in ("pe", "act", "dve", "pool"):
            self.sem[e] = nc.alloc_semaphore("s_" + e)
            self.cnt[e] = 0
        self.known = {e: {} for e in self.eng}
        self.dq = {}
        for q in ("sp", "act", "pool"):
            sems = []
            for i in range(self.NDMA):
                k = ("d", q, i)
                self.sem[k] = nc.alloc_semaphore("d_%s_%d" % (q, i))
                self.cnt[k] = 0
                sems.append(k)
            self.dq[q] = [sems, 0]
        self.nalloc = 0
        self.bufs = {}
        self.ninst = 0
        self.bank = [nc.alloc_psum_tensor("bank%d" % i, [128, 512], F32) for i in range(8)]

    def sb(self, es, shape, dt=F32, name="t"):
        self.nalloc += 1
        t = es.enter_context(self.nc.sbuf_tensor("%s_%d" % (name, self.nalloc), list(shape), dt))
        return t

    def dram(self, name, shape, dt=F32, kind="Internal"):
        return self.nc.dram_tensor(name, list(shape), dt, kind=kind)

    def B(self, key):
        if isinstance(key, Buf):
            return key
        k = key if isinstance(key, (str, tuple, int)) else id(key)
        b = self.bufs.get(k)
        if b is None:
            b = Buf(str(k))
            self.bufs[k] = b
        return b

    def _waits(self, e, reads, writes):
        need = {}
        for b in reads:
            b = self.B(b)
            if b.w is not None:
                k, v = b.w
                if need.get(k, 0) < v:
                    need[k] = v
        for b in writes:
            b = self.B(b)
            if b.w is not None:
                k, v = b.w
                if need.get(k, 0) < v:
                    need[k] = v
            for k, v in b.r.items():
                if need.get(k, 0) < v:
                    need[k] = v
        kn = self.known[e]
        for k, v in need.items():
            if k == e and e == "pe":
                continue
            if kn.get(k, 0) >= v:
                continue
            self.eng[e].wait_ge(self.sem[k], v)
            kn[k] = v

    def _done(self, key, val, reads, writes):
        for b in writes:
            b = self.B(b)
            b.w = (key, val)
            b.r = {}
        for b in reads:
            b = self.B(b)
            if b.r.get(key, 0) < val:
                b.r[key] = val

    def op(self, e, fn, reads=(), writes=()):
        self._waits(e, reads, writes)
        inst = fn(self.eng[e])
        self.cnt[e] += 1
        inst.then_inc(self.sem[e], 1)
        self._done(e, self.cnt[e], reads, writes)
        self.ninst += 1
        return inst

    def dma(self, out, in_, reads=(), writes=(), q="sp", fn=None, **kw):
        sems, i = self.dq[q]
        k = sems[i % len(sems)]
        self.dq[q][1] = i + 1
        kn = self.known[q]
        if kn.get(k, 0) < self.cnt[k]:
            self.eng[q].wait_ge(self.sem[k], self.cnt[k])
            kn[k] = self.cnt[k]
        self._waits(q, reads, writes)
        if fn is not None:
            inst = fn(self.eng[q])
        else:
            inst = self.eng[q].dma_start(out=out, in_=in_, **kw)
        self.cnt[k] += 16
        inst.then_inc(self.sem[k], 16)
        self._done(k, self.cnt[k], reads, writes)
        self.ninst += 1
        return inst

    def barrier(self):
        for e in self.eng:
            kn = self.known[e]
            for k, v in self.cnt.items():
                if v == 0 or kn.get(k, 0) >= v:
                    continue
                if k == e and e == "pe":
                    continue
                self.eng[e].wait_ge(self.sem[k], v)
                kn[k] = v
        self.bufs = {k: b for k, b in self.bufs.items() if isinstance(k, (str, tuple))}
        for b in self.bufs.values():
            b.w = None
            b.r = {}

    def mm(self, out, lhsT, rhs, start=True, stop=True, reads=(), writes=(), **kw):
        return self.op("pe", lambda E: E.matmul(out, lhsT, rhs, start=start, stop=stop, **kw), reads, writes)

    def tr(self, out, in_, ident, reads=(), writes=()):
        return self.op("pe", lambda E: E.transpose(out, in_, ident), reads, writes)

    def act(self, out, in_, func, reads=(), writes=(), **kw):
        return self.op("act", lambda E: E.activation(out, in_, func, **kw), reads, writes)

    def tt(self, e, out, in0, in1, op, reads=(), writes=()):
        return self.op(e, lambda E: E.tensor_tensor(out, in0, in1, op), reads, writes)

    def ts(self, e, out, in0, s1, op0, s2=None, op1=None, reads=(), writes=(), **kw):
        if op1 is None:
            return self.op(e, lambda E: E.tensor_scalar(out, in0, s1, None, op0, **kw), reads, writes)
        return self.op(e, lambda E: E.tensor_scalar(out, in0, s1, s2, op0, op1, **kw), reads, writes)

    def stt(self, e, out, in0, scalar, in1, op0, op1, reads=(), writes=()):
        return self.op(e, lambda E: E.scalar_tensor_tensor(out, in0, scalar, in1, op0, op1), reads, writes)

    def cp(self, e, out, in_, reads=(), writes=()):
        if e == "act":
            return self.op(e, lambda E: E.copy(out, in_), reads, writes)
        return self.op(e, lambda E: E.tensor_copy(out, in_), reads, writes)

    def memset(self, e, ap, val, writes=()):
        return self.op(e, lambda E: E.memset(ap, val), (), writes)


def _rope_tables():
    half = 16
    inv = 10000.0 ** (-np.arange(0, half, 2, dtype=np.float64) / half)
    i = np.arange(NLAT)
    row = (i // 64).astype(np.float64)
    col = (i % 64).astype(np.float64)
    ar = row[None, :] * inv[:, None]
    ac = col[None, :] * inv[:, None]
    cos = np.ones((32, T), np.float64)
    sin = np.zeros((32, T), np.float64)
    cos[0:8, NCTX:] = np.cos(ar); cos[8:16, NCTX:] = np.cos(ar); cos[16:24, NCTX:] = np.cos(ac); cos[24:32, NCTX:] = np.cos(ac)
    sin[0:8, NCTX:] = np.sin(ar); sin[8:16, NCTX:] = np.sin(ar); sin[16:24, NCTX:] = np.sin(ac); sin[24:32, NCTX:] = np.sin(ac)
    return cos.astype(np.float32), sin.astype(np.float32)


def _hy_tables(n):
    N2 = 2 * n
    ntc = n // 128
    F = n + 1
    nj = (F + 127) // 128
    t = np.arange(n, dtype=np.float64)
    tt_ = t / (n - 1)
    bands = 16
    f = np.linspace(1e-4, bands - 1, bands)
    ang = (2.0 * math.pi * t / n)[:, None] * f
    z = np.concatenate([tt_[:, None], np.cos(ang), -np.sin(ang)], axis=-1)
    lo, hi = math.log(1e-2) / 1.5, math.log(1e-2) / 0.3
    deltas = np.abs(np.linspace(lo, hi, 256))
    decay = np.exp(-tt_[None, :] * deltas[:, None])
    fi = np.arange(nj * 128, dtype=np.float64)
    valid = (fi <= n).astype(np.float64)
    ph = 2.0 * math.pi * np.outer(t, fi) / N2
    cosm = np.cos(ph) * valid[None, :]
    sinm = -np.sin(ph) * valid[None, :]
    def fwd_layout(m):
        return m.reshape(ntc, 128, nj, 128).transpose(2, 1, 0, 3)
    dfwd = np.concatenate([fwd_layout(cosm), fwd_layout(sinm)], axis=0).astype(ml_dtypes.bfloat16)
    wf = np.where((fi == 0) | (fi == n), 1.0, 2.0) * valid / N2
    icos = (np.cos(ph) * wf[None, :]).T
    isin = (-np.sin(ph) * wf[None, :]).T
    def inv_layout(m):
        return m.reshape(nj, 128, ntc, 128).transpose(2, 1, 0, 3)
    dinv = np.concatenate([inv_layout(icos), inv_layout(isin)], axis=2).astype(ml_dtypes.bfloat16)
    return (np.ascontiguousarray(z.T.astype(np.float32)), np.ascontiguousarray(decay.astype(np.float32)),
            np.ascontiguousarray(dfwd), np.ascontiguousarray(dinv))


def const_inputs():
    c = {}
    c["iota_e"] = np.ascontiguousarray(np.broadcast_to(np.arange(32, dtype=np.float32)[None, :], (128, 32)))
    c["iota_b"] = np.ascontiguousarray(np.broadcast_to(np.arange(128, dtype=np.float32)[None, :], (128, 128)))
    c["iota_p"] = np.arange(128, dtype=np.float32).reshape(128, 1)
    c["ltri"] = np.triu(np.ones((128, 128), np.float32), k=1).astype(ml_dtypes.bfloat16)
    for nm, n in (("l", NLAT), ("c", NCTX)):
        z, dec, dfwd, dinv = _hy_tables(n)
        c["hy_zpos_" + nm] = z
        c["hy_decay_" + nm] = dec
        c["hy_dfwd_" + nm] = dfwd
        c["hy_dinv_" + nm] = dinv
    c["ident"] = np.eye(128, dtype=np.float32)
    cos, sin = _rope_tables()
    c["rope_cos"] = cos
    c["rope_sin"] = sin
    return c


def prep_inputs(inp):
    cst = const_inputs()
    shared = dict(cst)
    for k in ("ada_w", "ada_b", "norm1_g", "norm2_g", "w_in"):
        shared[k] = np.ascontiguousarray(inp[k], dtype=np.float32)
    def lane(a):
        a = np.asarray(a, np.float32)
        Ld = a.shape[0]
        rest = a.shape[4:]
        a = a.reshape((Ld, 2, 8, 2, 64) + rest)
        nd = a.ndim
        a = a.transpose((0, 3, 4, 1, 2) + tuple(range(5, nd)))
        return np.ascontiguousarray(a.reshape((Ld, 128, 16) + rest))
    shared["s5_lre"] = lane(inp["s5_lambda_re"])
    shared["s5_lim"] = lane(inp["s5_lambda_im"])
    shared["s5_ldt"] = lane(np.broadcast_to(np.asarray(inp["s5_log_dt"], np.float32)[..., None], (DEPTH, 2, 16, 64)))
    shared["s5_bre"] = lane(inp["s5_b_re"])
    shared["s5_bim"] = lane(inp["s5_b_im"])
    shared["s5_cre"] = lane(np.asarray(inp["s5_c_re"], np.float32).transpose(0, 1, 2, 4, 3))
    shared["s5_cim"] = lane(np.asarray(inp["s5_c_im"], np.float32).transpose(0, 1, 2, 4, 3))
    shared["s5_dcol"] = np.ascontiguousarray(np.asarray(inp["s5_d"], np.float32).reshape(DEPTH, 2, 128).transpose(0, 2, 1))
    shared["s5_glu_w"] = np.ascontiguousarray(inp["s5_glu_w"], dtype=np.float32)
    shared["mla_qg"] = np.ascontiguousarray(np.asarray(inp["mla_q_norm_g"], np.float32).reshape(DEPTH, 3, 128).transpose(0, 2, 1))
    shared["mla_kvg"] = np.ascontiguousarray(np.asarray(inp["mla_kv_norm_g"], np.float32).reshape(DEPTH, 2, 128).transpose(0, 2, 1))
    shared["mla_w_uq"] = np.ascontiguousarray(inp["mla_w_uq"], dtype=np.float32)
    shared["mla_w_ukv"] = np.ascontiguousarray(inp["mla_w_ukv"], dtype=np.float32)
    shared["mix_norm_g"] = np.ascontiguousarray(inp["mix_norm_g"], dtype=np.float32)
    shared["hy_sw"] = np.ascontiguousarray(np.asarray(inp["hy_short_w"], np.float32).reshape(DEPTH, 3, 6, 128).transpose(0, 3, 2, 1))
    shared["hy_sb"] = np.ascontiguousarray(np.asarray(inp["hy_short_b"], np.float32).reshape(DEPTH, 6, 128).transpose(0, 2, 1))
    shared["hy_cols"] = np.ascontiguousarray(np.stack([inp["hy_f_b1"], inp["hy_f_b2"], inp["hy_f_freq"][:, 0], inp["hy_f_freq"][:, 1]], axis=-1), dtype=np.float32)
    shared["hy_b3"] = np.ascontiguousarray(np.asarray(inp["hy_f_b3"], np.float32).reshape(DEPTH, 8, 128).transpose(0, 2, 1))
    for k in ("hy_f_w1", "hy_f_w2", "hy_f_w3", "hy_bias"):
        shared[k] = np.ascontiguousarray(inp[k], dtype=np.float32)
    shared["mix_g_s5col"] = np.ascontiguousarray(np.asarray(inp["mix_norm_g"], np.float32)[:, 0:256].reshape(DEPTH, 2, 128).transpose(0, 2, 1))
    shared["w_out"] = np.ascontiguousarray(inp["w_out"], dtype=np.float32)
    shared["moe_wr"] = np.ascontiguousarray(np.concatenate([inp["moe_w_group"], inp["moe_w_expert"]], axis=-1), dtype=np.float32)
    shared["moe_w1"] = np.ascontiguousarray(np.asarray(inp["moe_w1"], np.float32).reshape(DEPTH, 32, 8, 128, 512).transpose(0, 1, 3, 2, 4)).reshape(DEPTH * 32 * 128, 4096)
    shared["moe_w3"] = np.ascontiguousarray(np.asarray(inp["moe_w3"], np.float32).reshape(DEPTH, 32, 8, 128, 512).transpose(0, 1, 3, 2, 4)).reshape(DEPTH * 32 * 128, 4096)
    shared["moe_w2"] = np.ascontiguousarray(np.asarray(inp["moe_w2"], np.float32).reshape(DEPTH, 32, 4, 128, 1024).transpose(0, 1, 3, 2, 4)).reshape(DEPTH * 32 * 128, 4096)
    shared["final_g"] = np.ascontiguousarray(np.asarray(inp["final_g"], np.float32).reshape(1, DM))
    maps = []
    for b in range(8):
        m = dict(shared)
        m["x"] = np.ascontiguousarray(inp["x"][b])
        m["ctx"] = np.ascontiguousarray(inp["ctx"][b])
        cc = np.stack([inp["c"][b], inp["c_ctx"]], axis=0)
        m["cc"] = np.ascontiguousarray(cc.reshape(2, 8, 128).transpose(2, 1, 0))
        maps.append(m)
    return maps


class Prog:
    def __init__(self, debug=()):
        self.kb = KB()
        self.nc = self.kb.nc
        self.debug = set(debug)
        self.I = {}
        self.S = {}
        self.outs = []

    def inp(self, name, shape, dt=F32):
        t = self.nc.dram_tensor(name, list(shape), dt, kind="ExternalInput")
        self.I[name] = t
        return t

    def scratch(self, name, shape, dt=F32):
        kind = "ExternalOutput" if name in self.debug else "Internal"
        t = self.nc.dram_tensor(name, list(shape), dt, kind=kind)
        self.S[name] = t
        if kind == "ExternalOutput":
            self.outs.append(name)
        return t

    def declare(self):
        self.inp("x", [NLAT, DM]); self.inp("ctx", [NCTX, DM]); self.inp("cc", [128, 8, 2])
        self.inp("ada_w", [DEPTH, DM, 6 * DM]); self.inp("ada_b", [DEPTH, 6 * DM])
        self.inp("norm1_g", [DEPTH, DM]); self.inp("norm2_g", [DEPTH, DM])
        self.inp("w_in", [DEPTH, DM, P_IN])
        self.inp("ident", [128, 128]); self.inp("rope_cos", [32, T]); self.inp("rope_sin", [32, T])
        for nm in ("s5_lre", "s5_lim", "s5_ldt"):
            self.inp(nm, [DEPTH, 128, 16])
        for nm in ("s5_bre", "s5_bim", "s5_cre", "s5_cim"):
            self.inp(nm, [DEPTH, 128, 16, 16])
        self.inp("s5_dcol", [DEPTH, 128, 2]); self.inp("s5_glu_w", [DEPTH, 256, 256])
        self.inp("mla_qg", [DEPTH, 128, 3]); self.inp("mla_kvg", [DEPTH, 128, 2])
        self.inp("mla_w_uq", [DEPTH, 384, 768]); self.inp("mla_w_ukv", [DEPTH, 256, 1024])
        self.inp("mix_norm_g", [DEPTH, DM])
        self.scratch("MT", [DM, T], BF16)
        if "MLAO" in self.debug:
            self.scratch("MLAO", [T, 512])
        for nm, n in (("l", NLAT), ("c", NCTX)):
            nj = (n + 1 + 127) // 128
            self.inp("hy_zpos_" + nm, [33, n]); self.inp("hy_decay_" + nm, [256, n])
            self.inp("hy_dfwd_" + nm, [2 * nj, 128, n // 128, 128], BF16); self.inp("hy_dinv_" + nm, [n // 128, 128, 2 * nj, 128], BF16)
            self.scratch("KSPEC_" + nm, [nj, 2, 128, 512])
        self.inp("hy_sw", [DEPTH, 128, 6, 3]); self.inp("hy_sb", [DEPTH, 128, 6]); self.inp("hy_cols", [DEPTH, 64, 4]); self.inp("hy_b3", [DEPTH, 128, 8])
        self.inp("hy_f_w1", [DEPTH, 33, 64]); self.inp("hy_f_w2", [DEPTH, 64, 64]); self.inp("hy_f_w3", [DEPTH, 64, 1024]); self.inp("hy_bias", [DEPTH, 2, 256])
        self.scratch("HZTM", [T, 768]); self.scratch("HY1", [T, 256])
        if "HYO" in self.debug:
            self.scratch("HYO", [T, 256])
        self.inp("iota_e", [128, 32]); self.inp("iota_b", [128, 128]); self.inp("iota_p", [128, 1]); self.inp("ltri", [128, 128], BF16)
        self.inp("mix_g_s5col", [DEPTH, 128, 2])
        self.inp("w_out", [DEPTH, DM, DM]); self.inp("moe_wr", [DEPTH, DM, 36])
        for nm_ in ("moe_w1", "moe_w3", "moe_w2"):
            self.inp(nm_, [DEPTH * 32 * 128, 4096])
        self.inp("final_g", [1, DM])
        self.scratch("FB", [T, DM], BF16); self.scratch("XS", [NBLK * 128, DM], BF16); self.scratch("YS", [NBLK * 128, DM])
        if "XMID" in self.debug:
            self.scratch("XMID", [T, DM])
        if "YMOE" in self.debug:
            self.scratch("YMOE", [T, DM])
        self.out = self.nc.dram_tensor("out", [NLAT, DM], F32, kind="ExternalOutput")
        self.scratch("S5T", [256, T])
        self.scratch("X", [T, DM])
        self.scratch("MOD", [2, 6 * DM])
        self.scratch("UT", [256, T]); self.scratch("CQT", [384, T]); self.scratch("CKVT", [256, T])
        self.scratch("KRT", [32, T]); self.scratch("HZT", [768, T])
        if "HL" in self.debug:
            self.scratch("HL", [T, DM])

    def stage_init(self):
        kb = self.kb
        X = self.S["X"]
        kb.dma(X.ap()[0:NCTX, :], self.I["ctx"].ap(), writes=[("X", 0), ("X", 1)])
        for j in range(4):
            kb.dma(X.ap()[NCTX + j * 1024: NCTX + (j + 1) * 1024, :], self.I["x"].ap()[j * 1024:(j + 1) * 1024, :],
                   writes=[("X", 2 + 8 * j + i) for i in range(8)])

    def stage_mod(self, L):
        kb = self.kb
        with ExitStack() as es:
            cc = kb.sb(es, [128, 8, 2], F32, "cc")
            act = kb.sb(es, [128, 8, 2], F32, "act")
            bb = kb.sb(es, [2, 6 * DM], F32, "adab")
            mod = kb.sb(es, [2, 6 * DM], F32, "mod")
            wt = [kb.sb(es, [128, 8, 512], F32, "adaw%d" % i) for i in range(2)]
            kb.dma(cc[:], self.I["cc"].ap(), writes=[cc])
            kb.dma(bb[:], self.I["ada_b"].ap()[L:L + 1, :].partition_broadcast(2), writes=[bb])
            kb.act(act[:], cc[:], AF.Silu, reads=[cc], writes=[act])
            wv = self.I["ada_w"].ap()[L].rearrange("(kc p) n -> p kc n", p=128)
            for nb in range(12):
                w = wt[nb % 2]
                kb.dma(w[:], wv[:, :, nb * 512:(nb + 1) * 512], writes=[w])
                ps = kb.bank[nb % 2]
                for kc in range(8):
                    kb.mm(ps[0:2, :], act[:, kc, :], w[:, kc, :], start=(kc == 0), stop=(kc == 7),
                          reads=[act, w], writes=[ps])
                kb.tt("dve", mod[:, nb * 512:(nb + 1) * 512], ps[0:2, :], bb[:, nb * 512:(nb + 1) * 512], OP.add,
                      reads=[ps, bb], writes=[mod])
            kb.dma(self.S["MOD"].ap(), mod[:], reads=[mod], writes=["MOD"])
        kb.barrier()

    def load_mod_bc(self, es_tiles, L, which, idx_g, g_name):
        kb = self.kb
        G, S, tmp = es_tiles
        MOD = self.S["MOD"].ap()
        kb.dma(S[:], MOD[which:which + 1, idx_g * DM:(idx_g + 1) * DM].partition_broadcast(128), reads=["MOD"], writes=[S])
        kb.dma(tmp[:], MOD[which:which + 1, (idx_g + 1) * DM:(idx_g + 2) * DM].partition_broadcast(128), reads=["MOD"], writes=[tmp])
        kb.dma(G[:], self.I[g_name].ap()[L:L + 1, :].partition_broadcast(128), writes=[G])
        kb.stt("dve", G[:], tmp[:], 1.0, G[:], OP.add, OP.mult, reads=[tmp, G], writes=[G])

    def rms_modulate(self, xt, G, S, sq, ss, rstd, tmp, out, out2=None):
        kb = self.kb
        kb.act(sq[:], xt[:], AF.Square, accum_out=ss[:], reads=[xt], writes=[sq, ss])
        kb.ts("dve", rstd[:], ss[:], 1.0 / DM, OP.mult, EPS, OP.add, reads=[ss], writes=[rstd])
        kb.act(rstd[:], rstd[:], AF.Sqrt, reads=[rstd], writes=[rstd])
        kb.op("dve", lambda E: E.reciprocal(rstd[:], rstd[:]), reads=[rstd], writes=[rstd])
        kb.stt("dve", tmp[:], xt[:], rstd[:, 0:1], G[:], OP.mult, OP.mult, reads=[xt, rstd, G], writes=[tmp])
        kb.tt("pool", out[:], tmp[:], S[:], OP.add, reads=[tmp, S], writes=[out])
        if out2 is not None:
            kb.cp("act", out2[:], out[:], reads=[out], writes=[out2])

    def stage_proj(self, L):
        kb = self.kb
        X = self.S["X"].ap()
        with ExitStack() as es:
            ident_f = kb.sb(es, [128, 128], F32, "identf")
            ident = kb.sb(es, [128, 128], BF16, "identb")
            kb.dma(ident_f[:], self.I["ident"].ap(), writes=[ident_f])
            kb.cp("dve", ident[:], ident_f[:], reads=[ident_f], writes=[ident])
            wf = kb.sb(es, [128, 8, P_IN], F32, "winf")
            wb = kb.sb(es, [128, 8, P_IN + 32], BF16, "winb")
            kb.dma(wf[:], self.I["w_in"].ap()[L].rearrange("(kc p) n -> p kc n", p=128), writes=[wf])
            kb.cp("act", wb[:, :, 0:P_IN], wf[:], reads=[wf], writes=[wb])
            K0 = 896
            kb.ts("dve", wb[:, :, P_IN + 0:P_IN + 8], wf[:, :, K0 + 8:K0 + 16], -1.0, OP.mult, reads=[wf], writes=[wb])
            kb.cp("dve", wb[:, :, P_IN + 8:P_IN + 16], wf[:, :, K0 + 0:K0 + 8], reads=[wf], writes=[wb])
            kb.ts("dve", wb[:, :, P_IN + 16:P_IN + 24], wf[:, :, K0 + 24:K0 + 32], -1.0, OP.mult, reads=[wf], writes=[wb])
            kb.cp("dve", wb[:, :, P_IN + 24:P_IN + 32], wf[:, :, K0 + 16:K0 + 24], reads=[wf], writes=[wb])
            rc = kb.sb(es, [32, T], F32, "ropec")
            rs = kb.sb(es, [32, T], F32, "ropes")
            kb.dma(rc[:], self.I["rope_cos"].ap(), writes=[rc])
            kb.dma(rs[:], self.I["rope_sin"].ap(), writes=[rs])
            G = kb.sb(es, [128, DM], F32, "G"); S = kb.sb(es, [128, DM], F32, "S"); tmpb = kb.sb(es, [128, DM], F32, "tmpb")
            xt = [kb.sb(es, [128, DM], F32, "xt%d" % i) for i in range(2)]
            sq = kb.sb(es, [128, DM], BF16, "sq")
            ss = kb.sb(es, [128, 1], F32, "ss"); rstd = kb.sb(es, [128, 1], F32, "rstd")
            tmp = kb.sb(es, [128, DM], F32, "tmp")
            hb = [kb.sb(es, [128, DM], BF16, "hb%d" % i) for i in range(2)]
            hf = kb.sb(es, [128, DM], F32, "hf") if "HL" in self.debug else None
            hT = [kb.sb(es, [128, 8, 512], BF16, "hT%d" % i) for i in range(2)]
            stg = [kb.sb(es, [128, 512], F32, "stg%d" % i) for i in range(3)]
            kr1 = kb.sb(es, [32, 512], F32, "kr1")
            nstg = 0
            chunks = []
            for i in range(2):
                chunks.append(("UT", i * 128, i * 128, 128))
            for i in range(3):
                chunks.append(("CQT", i * 128, 256 + i * 128, 128))
            for i in range(2):
                chunks.append(("CKVT", i * 128, 640 + i * 128, 128))
            for i in range(6):
                chunks.append(("HZT", i * 128, 928 + i * 128, 128))
            blocks = [(0, 2)] + [(2 + 4 * i, 4) for i in range(8)]
            nmm = 0
            for bi, (t0, ntl) in enumerate(blocks):
                if bi == 0:
                    self.load_mod_bc((G, S, tmpb), L, 1, 0, "norm1_g")
                elif bi == 1:
                    self.load_mod_bc((G, S, tmpb), L, 0, 0, "norm1_g")
                hTb = hT[bi % 2]
                nb = ntl * 128
                c0 = t0 * 128
                for j in range(ntl):
                    t = t0 + j
                    x_ = xt[t % 2]
                    kb.dma(x_[:], X[t * 128:(t + 1) * 128, :], reads=[("X", t)], writes=[x_])
                    h_ = hb[t % 2]
                    if hf is not None:
                        self.rms_modulate(x_, G, S, sq, ss, rstd, tmp, hf, h_)
                        kb.dma(self.S["HL"].ap()[t * 128:(t + 1) * 128, :], hf[:], reads=[hf], writes=["HL"])
                    else:
                        self.rms_modulate(x_, G, S, sq, ss, rstd, tmp, h_)
                    pb = kb.bank[2 + (t % 2)]
                    pbv = pb[:].bitcast(BF16)
                    for kc in range(8):
                        kb.tr(pbv[:, kc * 128:(kc + 1) * 128], h_[:, kc * 128:(kc + 1) * 128], ident[:],
                              reads=[h_, ident], writes=[pb])
                    kb.cp("dve" if t % 2 else "act", hTb[:, :, j * 128:(j + 1) * 128],
                          pbv.rearrange("p (k n) -> p k n", k=8), reads=[pb], writes=[hTb])
                for (dn, r0, col0, M) in chunks:
                    ps = kb.bank[4 + (nmm % 2)]
                    nmm += 1
                    for kc in range(8):
                        kb.mm(ps[0:M, 0:nb], wb[:, kc, col0:col0 + M], hTb[:, kc, 0:nb], start=(kc == 0), stop=(kc == 7),
                              reads=[wb, hTb], writes=[ps])
                    st = stg[nstg % 3]
                    nstg += 1
                    kb.cp("act" if nstg % 2 else "dve", st[0:M, 0:nb], ps[0:M, 0:nb], reads=[ps], writes=[st])
                    kb.dma(self.S[dn].ap()[r0:r0 + M, c0:c0 + nb], st[0:M, 0:nb], reads=[st], writes=[(dn, bi)], q="pool")
                ps = kb.bank[6]
                ps2 = kb.bank[7]
                for kc in range(8):
                    kb.mm(ps[0:32, 0:nb], wb[:, kc, 896:928], hTb[:, kc, 0:nb], start=(kc == 0), stop=(kc == 7),
                          reads=[wb, hTb], writes=[ps])
                for kc in range(8):
                    kb.mm(ps2[0:32, 0:nb], wb[:, kc, P_IN:P_IN + 32], hTb[:, kc, 0:nb], start=(kc == 0), stop=(kc == 7),
                          reads=[wb, hTb], writes=[ps2])
                st = stg[nstg % 3]
                nstg += 1
                kb.tt("dve", kr1[:, 0:nb], ps[0:32, 0:nb], rc[:, c0:c0 + nb], OP.mult, reads=[ps, rc], writes=[kr1])
                kb.tt("dve", st[0:32, 0:nb], ps2[0:32, 0:nb], rs[:, c0:c0 + nb], OP.mult, reads=[ps2, rs], writes=[st])
                kb.tt("dve", st[0:32, 0:nb], st[0:32, 0:nb], kr1[:, 0:nb], OP.add, reads=[st, kr1], writes=[st])
                kb.dma(self.S["KRT"].ap()[:, c0:c0 + nb], st[0:32, 0:nb], reads=[st], writes=[("KRT", bi)], q="pool")
        kb.barrier()


    def sin_rr(self, e, out, in_, add, t1, t2):
        kb = self.kb
        kb.ts(e, t1, in_, add, OP.add, reads=[in_.tensor], writes=[t1.tensor])
        kb.ts(e, t2, t1, 1.0 / (2 * math.pi), OP.mult, MAGIC, OP.add, reads=[t1.tensor], writes=[t2.tensor])
        kb.ts(e, t2, t2, -MAGIC, OP.add, -2 * math.pi, OP.mult, reads=[t2.tensor], writes=[t2.tensor])
        kb.tt(e, t1, t1, t2, OP.add, reads=[t1.tensor, t2.tensor], writes=[t1.tensor])
        kb.ts(e, t1, t1, math.pi, OP.min, -math.pi, OP.max, reads=[t1.tensor], writes=[t1.tensor])
        kb.act(out, t1, AF.Sin, reads=[t1.tensor], writes=[out.tensor])

    def stage_s5(self, L):
        kb = self.kb
        I = self.I
        blocks = [(0, 256)] + [(256 + 512 * i, 512) for i in range(8)]
        with ExitStack() as es:
            ident_f = kb.sb(es, [128, 128], F32, "identf")
            kb.dma(ident_f[:], I["ident"].ap(), writes=[ident_f])
            sm = {}
            for nm in ("lre", "lim", "ldt"):
                sm[nm] = kb.sb(es, [128, 16], F32, nm)
                kb.dma(sm[nm][:], I["s5_" + nm].ap()[L], writes=[sm[nm]])
            for nm in ("dt", "a", "th", "r", "cs", "sn", "t1", "t2", "rc", "rsn", "den", "cre", "cim", "x1", "x2"):
                sm[nm] = kb.sb(es, [128, 16], F32, nm)
            big = {}
            for nm in ("bre", "bim", "cre", "cim"):
                big[nm] = kb.sb(es, [128, 16, 16], F32, "s5" + nm)
                kb.dma(big[nm][:], I["s5_" + nm].ap()[L], writes=[big[nm]])
            A = lambda nm: sm[nm][:]
            kb.act(A("dt"), A("ldt"), AF.Exp, reads=[sm["ldt"]], writes=[sm["dt"]])
            kb.tt("dve", A("a"), A("lre"), A("dt"), OP.mult, reads=[sm["lre"], sm["dt"]], writes=[sm["a"]])
            kb.tt("dve", A("th"), A("lim"), A("dt"), OP.mult, reads=[sm["lim"], sm["dt"]], writes=[sm["th"]])
            kb.act(A("r"), A("a"), AF.Exp, reads=[sm["a"]], writes=[sm["r"]])
            self.sin_rr("dve", A("sn"), A("th"), 0.0, A("t1"), A("t2"))
            self.sin_rr("dve", A("cs"), A("th"), math.pi / 2, A("t1"), A("t2"))
            kb.tt("dve", A("rc"), A("r"), A("cs"), OP.mult, reads=[sm["r"], sm["cs"]], writes=[sm["rc"]])
            kb.tt("dve", A("rsn"), A("r"), A("sn"), OP.mult, reads=[sm["r"], sm["sn"]], writes=[sm["rsn"]])
            kb.ts("dve", A("rc"), A("rc"), -1.0, OP.add, reads=[sm["rc"]], writes=[sm["rc"]])
            kb.tt("dve", A("den"), A("lre"), A("lre"), OP.mult, reads=[sm["lre"]], writes=[sm["den"]])
            kb.tt("dve", A("x1"), A("lim"), A("lim"), OP.mult, reads=[sm["lim"]], writes=[sm["x1"]])
            kb.tt("dve", A("den"), A("den"), A("x1"), OP.add, reads=[sm["den"], sm["x1"]], writes=[sm["den"]])
            kb.op("dve", lambda E: E.reciprocal(A("den"), A("den")), reads=[sm["den"]], writes=[sm["den"]])
            kb.tt("dve", A("x1"), A("rc"), A("lre"), OP.mult, reads=[sm["rc"], sm["lre"]], writes=[sm["x1"]])
            kb.tt("dve", A("x2"), A("rsn"), A("lim"), OP.mult, reads=[sm["rsn"], sm["lim"]], writes=[sm["x2"]])
            kb.tt("dve", A("x1"), A("x1"), A("x2"), OP.add, reads=[sm["x1"], sm["x2"]], writes=[sm["x1"]])
            kb.tt("dve", A("cre"), A("x1"), A("den"), OP.mult, reads=[sm["x1"], sm["den"]], writes=[sm["cre"]])
            kb.tt("dve", A("x1"), A("rsn"), A("lre"), OP.mult, reads=[sm["rsn"], sm["lre"]], writes=[sm["x1"]])
            kb.tt("dve", A("x2"), A("rc"), A("lim"), OP.mult, reads=[sm["rc"], sm["lim"]], writes=[sm["x2"]])
            kb.tt("dve", A("x1"), A("x1"), A("x2"), OP.subtract, reads=[sm["x1"], sm["x2"]], writes=[sm["x1"]])
            kb.tt("dve", A("cim"), A("x1"), A("den"), OP.mult, reads=[sm["x1"], sm["den"]], writes=[sm["cim"]])
            ub = kb.sb(es, [128, 2, T], BF16, "ub")
            yacc = kb.sb(es, [128, 2, T], F32, "yacc")
            Er = kb.sb(es, [128, T], F32, "Er"); Ei = kb.sb(es, [128, T], F32, "Ei")
            vr = kb.sb(es, [128, T], F32, "vr"); vi = kb.sb(es, [128, T], F32, "vi")
            for c, stg_ in enumerate((vr, vi)):
                kb.dma(stg_[:], self.S["UT"].ap()[c * 128:(c + 1) * 128, :], reads=[("UT", i) for i in range(9)], writes=[stg_])
                kb.cp("act", ub[:, c, :], stg_[:], reads=[stg_], writes=[ub])
            gr = kb.sb(es, [128, T], F32, "gr"); gi = kb.sb(es, [128, T], F32, "gi")
            ZB = [kb.sb(es, [128, 128], F32, "ZB%d" % i) for i in range(2)]
            LB = [kb.sb(es, [128, 128], BF16, "LB%d" % i) for i in range(2)]
            LC = [kb.sb(es, [128, 128], BF16, "LC%d" % i) for i in range(2)]
            bt = kb.sb(es, [128, 16], F32, "bt")
            wr = kb.sb(es, [128, 1], F32, "wr"); wi = kb.sb(es, [128, 1], F32, "wi"); wt = kb.sb(es, [128, 1], F32, "wt")
            etmp = kb.sb(es, [128, 1024], F32, "etmp"); etmp2 = kb.sb(es, [128, 1024], F32, "etmp2")
            bis = [kb.sb(es, [128, 512], F32, "bis%d" % i) for i in range(2)]
            mt = [kb.sb(es, [128, 512], F32, "mt%d" % i) for i in range(4)]
            hr = [kb.sb(es, [128, 512], BF16, "hr%d" % i) for i in range(2)]
            hi = [kb.sb(es, [128, 512], BF16, "hi%d" % i) for i in range(2)]
            first_in_chunk = {0: True, 1: True}
            for lt in range(16):
                d, gp = lt // 8, lt % 8
                ch = gp // 4
                c0 = 32 * (gp % 4)
                lsl = slice(lt, lt + 1)
                for ri, (nm_a, nm_b, cA, cB, op2) in enumerate((("bre", "bim", "cre", "cim", OP.subtract), ("bim", "bre", "cre", "cim", OP.add))):
                    Z = ZB[ri]
                    kb.memset("pool", Z[:], 0.0, writes=[Z])
                    kb.ts("dve", bt[:], big[nm_b][:, lt, :], sm["cim"][:, lsl], OP.mult, reads=[big[nm_b], sm["cim"]], writes=[bt])
                    for gl in range(2):
                        ps_ = slice(gl * 64, gl * 64 + 64)
                        kb.stt("dve", Z[ps_, c0 + gl * 16:c0 + gl * 16 + 16], big[nm_a][ps_, lt, :], sm["cre"][ps_, lsl], bt[ps_, :],
                               OP.mult, op2, reads=[big[nm_a], sm["cre"], bt], writes=[Z])
                    pb = kb.bank[6]
                    kb.tr(pb[:, 0:128], Z[:], ident_f[:], reads=[Z, ident_f], writes=[pb])
                    kb.cp("act", LB[ri][:], pb[:, 0:128], reads=[pb], writes=[LB[ri]])
                for ri, nm in enumerate(("cre", "cim")):
                    kb.memset("pool", LC[ri][:], 0.0, writes=[LC[ri]])
                    for gl in range(2):
                        ps_ = slice(gl * 64, gl * 64 + 64)
                        kb.ts("dve", LC[ri][ps_, c0 + gl * 16:c0 + gl * 16 + 16], big[nm][ps_, lt, :], (1.0 if ri == 0 else -1.0), OP.mult,
                              reads=[big[nm]], writes=[LC[ri]])
                kb.memset("pool", Er[:, 0:1], 1.0, writes=[Er])
                kb.memset("pool", Ei[:, 0:1], 0.0, writes=[Ei])
                kb.cp("pool", Er[:, 1:2], sm["cs"][:, lsl], reads=[sm["cs"]], writes=[Er])
                kb.cp("pool", Ei[:, 1:2], sm["sn"][:, lsl], reads=[sm["sn"]], writes=[Ei])
                kb.cp("pool", wr[:], sm["cs"][:, lsl], reads=[sm["cs"]], writes=[wr])
                kb.cp("pool", wi[:], sm["sn"][:, lsl], reads=[sm["sn"]], writes=[wi])
                n = 2
                while n < T:
                    m = min(n, T - n)
                    kb.tt("pool", wt[:], wi[:], wi[:], OP.mult, reads=[wi], writes=[wt])
                    kb.tt("pool", wi[:], wr[:], wi[:], OP.mult, reads=[wr, wi], writes=[wi])
                    kb.ts("pool", wi[:], wi[:], 2.0, OP.mult, reads=[wi], writes=[wi])
                    kb.tt("pool", wr[:], wr[:], wr[:], OP.mult, reads=[wr], writes=[wr])
                    kb.tt("pool", wr[:], wr[:], wt[:], OP.subtract, reads=[wr, wt], writes=[wr])
                    for o in range(0, m, 1024):
                        mm_ = min(1024, m - o)
                        kb.ts("pool", etmp[:, 0:mm_], Ei[:, o:o + mm_], wi[:, 0:1], OP.mult, reads=[Ei, wi], writes=[etmp])
                        kb.ts("pool", etmp2[:, 0:mm_], Er[:, o:o + mm_], wr[:, 0:1], OP.mult, reads=[Er, wr], writes=[etmp2])
                        kb.tt("pool", Er[:, n + o:n + o + mm_], etmp2[:, 0:mm_], etmp[:, 0:mm_], OP.subtract, reads=[etmp, etmp2, Er], writes=[Er])
                        kb.ts("pool", etmp[:, 0:mm_], Ei[:, o:o + mm_], wr[:, 0:1], OP.mult, reads=[Ei, wr], writes=[etmp])
                        kb.ts("pool", etmp2[:, 0:mm_], Er[:, o:o + mm_], wi[:, 0:1], OP.mult, reads=[Er, wi], writes=[etmp2])
                        kb.tt("pool", Ei[:, n + o:n + o + mm_], etmp2[:, 0:mm_], etmp[:, 0:mm_], OP.add, reads=[etmp, etmp2, Ei], writes=[Ei])
                    n *= 2

                def Esl(Et, s0, nn):
                    if d == 0:
                        return Et[:, s0:s0 + nn]
                    hi_ = (NCTX - 1 - s0) if s0 < NCTX else (T - 1 + NCTX - s0)
                    return Et[:, hi_ - nn + 1:hi_ + 1][:, ::-1]
                for bi, (s0, nn) in enumerate(blocks):
                    pr = kb.bank[(bi % 2) * 2]
                    pi_ = kb.bank[(bi % 2) * 2 + 1]
                    kb.mm(pr[:, 0:nn], LB[0][:], ub[:, ch, s0:s0 + nn], reads=[LB[0], ub], writes=[pr])
                    kb.mm(pi_[:, 0:nn], LB[1][:], ub[:, ch, s0:s0 + nn], reads=[LB[1], ub], writes=[pi_])
                    bs = bis[bi % 2]
                    kb.cp("act", bs[:, 0:nn], pi_[:, 0:nn], reads=[pi_], writes=[bs])
                    m1, m2, m3, m4 = mt
                    kb.tt("dve", m1[:, 0:nn], Esl(Er, s0, nn), pr[:, 0:nn], OP.mult, reads=[Er, pr], writes=[m1])
                    kb.tt("pool", m2[:, 0:nn], Esl(Ei, s0, nn), bs[:, 0:nn], OP.mult, reads=[Ei, bs], writes=[m2])
                    kb.tt("dve", vr[:, s0:s0 + nn], m1[:, 0:nn], m2[:, 0:nn], OP.add, reads=[m1, m2], writes=[vr])
                    kb.tt("pool", m3[:, 0:nn], Esl(Er, s0, nn), bs[:, 0:nn], OP.mult, reads=[Er, bs], writes=[m3])
                    kb.tt("dve", m4[:, 0:nn], Esl(Ei, s0, nn), pr[:, 0:nn], OP.mult, reads=[Ei, pr], writes=[m4])
                    kb.tt("pool", vi[:, s0:s0 + nn], m3[:, 0:nn], m4[:, 0:nn], OP.subtract, reads=[m3, m4], writes=[vi])
                rdec = sm["r"][:, lsl]
                for (v_, g_) in ((vr, gr), (vi, gi)):
                    if d == 0:
                        kb.op("dve", lambda E: E.tensor_tensor_scan(g_[:], rdec.to_broadcast([128, T]), v_[:], 0.0, OP.mult, OP.add),
                              reads=[sm["r"], v_], writes=[g_])
                    else:
                        kb.op("dve", lambda E: E.tensor_tensor_scan(g_[:, 0:NCTX][:, ::-1], rdec.to_broadcast([128, NCTX]), v_[:, 0:NCTX][:, ::-1],
                                                                    0.0, OP.mult, OP.add), reads=[sm["r"], v_], writes=[g_])
                        kb.op("dve", lambda E: E.tensor_tensor_scan(g_[:, NCTX:T][:, ::-1], rdec.to_broadcast([128, NLAT]), v_[:, NCTX:T][:, ::-1],
                                                                    g_[:, 0:1], OP.mult, OP.add), reads=[sm["r"], v_, g_], writes=[g_])
                for bi, (s0, nn) in enumerate(blocks):
                    m1, m2, m3, m4 = mt
                    hr_ = hr[bi % 2]; hi_t = hi[bi % 2]
                    kb.tt("dve", m1[:, 0:nn], Esl(Er, s0, nn), gr[:, s0:s0 + nn], OP.mult, reads=[Er, gr], writes=[m1])
                    kb.tt("pool", m2[:, 0:nn], Esl(Ei, s0, nn), gi[:, s0:s0 + nn], OP.mult, reads=[Ei, gi], writes=[m2])
                    kb.tt("dve", hr_[:, 0:nn], m1[:, 0:nn], m2[:, 0:nn], OP.subtract, reads=[m1, m2], writes=[hr_])
                    kb.tt("pool", m3[:, 0:nn], Esl(Ei, s0, nn), gr[:, s0:s0 + nn], OP.mult, reads=[Ei, gr], writes=[m3])
                    kb.tt("dve", m4[:, 0:nn], Esl(Er, s0, nn), gi[:, s0:s0 + nn], OP.mult, reads=[Er, gi], writes=[m4])
                    kb.tt("pool", hi_t[:, 0:nn], m3[:, 0:nn], m4[:, 0:nn], OP.add, reads=[m3, m4], writes=[hi_t])
                    py = kb.bank[4 + (bi % 2)]
                    kb.mm(py[:, 0:nn], LC[0][:], hr_[:, 0:nn], start=True, stop=False, reads=[LC[0], hr_], writes=[py])
                    kb.mm(py[:, 0:nn], LC[1][:], hi_t[:, 0:nn], start=False, stop=True, reads=[LC[1], hi_t], writes=[py])
                    if first_in_chunk[ch]:
                        kb.cp("act", yacc[:, ch, s0:s0 + nn], py[:, 0:nn], reads=[py], writes=[yacc])
                    else:
                        kb.tt("dve", yacc[:, ch, s0:s0 + nn], yacc[:, ch, s0:s0 + nn], py[:, 0:nn], OP.add, reads=[py, yacc], writes=[yacc])
                first_in_chunk[ch] = False
            dcol = kb.sb(es, [128, 2], F32, "dcol")
            kb.dma(dcol[:], I["s5_dcol"].ap()[L], writes=[dcol])
            uf = (vr, vi)
            for c in range(2):
                kb.dma(uf[c][:], self.S["UT"].ap()[c * 128:(c + 1) * 128, :], reads=[("UT", i) for i in range(9)], writes=[uf[c]])
            gwf = kb.sb(es, [128, 2, 256], F32, "gwf"); gwb = kb.sb(es, [128, 2, 256], BF16, "gwb")
            kb.dma(gwf[:], I["s5_glu_w"].ap()[L].rearrange("(kc p) n -> p kc n", p=128), writes=[gwf])
            kb.cp("act", gwb[:], gwf[:], reads=[gwf], writes=[gwb])
            yb = kb.sb(es, [128, 2, 512], BF16, "yb")
            yf = kb.sb(es, [128, 2, 512], F32, "yf")
            so = [kb.sb(es, [128, 512], F32, "so%d" % i) for i in range(2)]
            for bi, (s0, nn) in enumerate(blocks):
                m1, m2, m3, m4 = mt
                for c in range(2):
                    kb.stt("dve", yf[:, c, 0:nn], uf[c][:, s0:s0 + nn], dcol[:, c:c + 1], yacc[:, c, s0:s0 + nn], OP.mult, OP.add,
                           reads=[uf[c], dcol, yacc], writes=[yf])
                    kb.tt("pool", m1[:, 0:nn], yf[:, c, 0:nn], yf[:, c, 0:nn], OP.mult, reads=[yf], writes=[m1])
                    kb.ts("pool", m1[:, 0:nn], m1[:, 0:nn], 0.044715, OP.mult, 1.0, OP.add, reads=[m1], writes=[m1])
                    kb.tt("pool", m1[:, 0:nn], m1[:, 0:nn], yf[:, c, 0:nn], OP.mult, reads=[m1, yf], writes=[m1])
                    kb.act(m2[:, 0:nn], m1[:, 0:nn], AF.Sigmoid, scale=1.5957691216057308, reads=[m1], writes=[m2])
                    kb.tt("dve", yf[:, c, 0:nn], yf[:, c, 0:nn], m2[:, 0:nn], OP.mult, reads=[yf, m2], writes=[yf])
                    kb.cp("act", yb[:, c, 0:nn], yf[:, c, 0:nn], reads=[yf], writes=[yb])
                for mo in range(2):
                    pz = kb.bank[mo]
                    for kc in range(2):
                        kb.mm(pz[:, 0:nn], gwb[:, kc, mo * 128:(mo + 1) * 128], yb[:, kc, 0:nn], start=(kc == 0), stop=(kc == 1),
                              reads=[gwb, yb], writes=[pz])
                    kb.act(m3[:, 0:nn], pz[:, 0:nn], AF.Sigmoid, reads=[pz], writes=[m3])
                    so_ = so[mo]
                    kb.tt("dve", so_[:, 0:nn], yf[:, mo, 0:nn], m3[:, 0:nn], OP.mult, reads=[yf, m3], writes=[so_])
                    kb.dma(self.S["S5T"].ap()[mo * 128:(mo + 1) * 128, s0:s0 + nn], so_[:, 0:nn], reads=[so_], writes=[("S5T", bi)], q="pool")
        kb.barrier()


    def stage_mla(self, L, ctx_out):
        kb = self.kb
        I = self.I
        blocks = [(0, 256)] + [(256 + 512 * i, 512) for i in range(8)]
        with ExitStack() as es:
            ident_f = kb.sb(es, [128, 128], F32, "identf")
            ident = kb.sb(es, [128, 128], BF16, "identb")
            ones = kb.sb(es, [128, 128], BF16, "ones")
            kb.dma(ident_f[:], I["ident"].ap(), writes=[ident_f])
            kb.cp("dve", ident[:], ident_f[:], reads=[ident_f], writes=[ident])
            kb.memset("dve", ones[:], 1.0, writes=[ones])
            qg = kb.sb(es, [128, 3], F32, "qg"); kvg = kb.sb(es, [128, 2], F32, "kvg")
            kb.dma(qg[:], I["mla_qg"].ap()[L], writes=[qg]); kb.dma(kvg[:], I["mla_kvg"].ap()[L], writes=[kvg])
            wuq = kb.sb(es, [128, 3, 768], BF16, "wuq")
            wrot = kb.sb(es, [128, 3, 8, 96], BF16, "wrot")
            wukv = kb.sb(es, [128, 2, 1024], BF16, "wukv")
            KT = kb.sb(es, [97, 8, T], BF16, "KT")
            VA = kb.sb(es, [128, NT, 8, 65], BF16, "VA")
            gbc = kb.sb(es, [128, 512], F32, "gbc")
            kb.dma(gbc[:], I["mix_norm_g"].ap()[L:L + 1, 256:768].partition_broadcast(128), writes=[gbc])
            es2 = ExitStack()
            wst = kb.sb(es2, [128, 3, 1024], F32, "wst")
            kb.dma(wst[:, :, 0:768], I["mla_w_uq"].ap()[L].rearrange("(kc p) n -> p kc n", p=128), writes=[wst])
            kb.cp("act", wuq[:], wst[:, :, 0:768], reads=[wst], writes=[wuq])
            kb.memset("pool", wrot[:], 0.0, writes=[wrot])
            wv4 = wst[:, :, 0:768].rearrange("p k (h x) -> p k h x", x=96)
            kb.ts("dve", wrot[:, :, :, 64:72], wv4[:, :, :, 72:80], -1.0, OP.mult, reads=[wst], writes=[wrot])
            kb.cp("dve", wrot[:, :, :, 72:80], wv4[:, :, :, 64:72], reads=[wst], writes=[wrot])
            kb.ts("dve", wrot[:, :, :, 80:88], wv4[:, :, :, 88:96], -1.0, OP.mult, reads=[wst], writes=[wrot])
            kb.cp("dve", wrot[:, :, :, 88:96], wv4[:, :, :, 80:88], reads=[wst], writes=[wrot])
            kb.dma(wst[:, 0:2, :], I["mla_w_ukv"].ap()[L].rearrange("(kc p) n -> p kc n", p=128), reads=[wst], writes=[wst])
            kb.cp("act", wukv[:], wst[:, 0:2, :], reads=[wst], writes=[wukv])
            kb.memset("pool", KT[96:97, :, :], 1.0, writes=[KT])
            kb.memset("pool", VA[:, :, :, 64:65], 1.0, writes=[VA])
            krf = kb.sb(es2, [32, T], F32, "krf"); krb = kb.sb(es2, [32, T], BF16, "krb")
            kb.dma(krf[:], self.S["KRT"].ap(), reads=[("KRT", i) for i in range(9)], writes=[krf])
            kb.cp("act", krb[:], krf[:], reads=[krf], writes=[krb])
            for h in range(8):
                kb.dma(KT[64:96, h, :], krb[:], reads=[krb], writes=[KT])
            kb.barrier()
            es2.close()
            if "mla_stop1" in self.debug:
                return
            rcb = [kb.sb(es, [96, 512], F32, "rcb%d" % i) for i in range(2)]
            rsb = [kb.sb(es, [96, 512], F32, "rsb%d" % i) for i in range(2)]
            xin = [kb.sb(es, [128, 3, 512], F32, "xin%d" % i) for i in range(2)]
            sqb = kb.sb(es, [128, 3, 512], BF16, "sqb")
            rst = kb.sb(es, [128, 512], F32, "rst")
            xn = kb.sb(es, [128, 3, 512], BF16, "xn")
            kmax2 = kb.sb(es, [128, 8], F32, "kmax2"); kmb = kb.sb(es, [128, 8], F32, "kmb"); negk = kb.sb(es, [128, 8], F32, "negk")
            kb.memset("dve", kmax2[:], 0.0, writes=[kmax2])

            def rmsnorm_fm(src, nch, nfeat, nn, gcol, dst):
                kb.act(sqb[:, 0:nch, 0:nn], src[:, 0:nch, 0:nn], AF.Square, reads=[src], writes=[sqb])
                pn = kb.bank[6]
                for c in range(nch):
                    kb.mm(pn[:, 0:nn], ones[:], sqb[:, c, 0:nn], start=(c == 0), stop=(c == nch - 1), reads=[ones, sqb], writes=[pn])
                kb.ts("dve", rst[:, 0:nn], pn[:, 0:nn], 1.0 / nfeat, OP.mult, EPS, OP.add, reads=[pn], writes=[rst])
                kb.act(rst[:, 0:nn], rst[:, 0:nn], AF.Sqrt, reads=[rst], writes=[rst])
                kb.op("dve", lambda E: E.reciprocal(rst[:, 0:nn], rst[:, 0:nn]), reads=[rst], writes=[rst])
                for c in range(nch):
                    kb.stt("dve", dst[:, c, 0:nn], src[:, c, 0:nn], gcol[:, c:c + 1], rst[:, 0:nn], OP.mult, OP.mult,
                           reads=[src, gcol, rst], writes=[dst])

            for bi, (s0, nn) in enumerate(blocks):
                x_ = xin[bi % 2]
                kb.dma(x_[:, 0:2, 0:nn], self.S["CKVT"].ap().rearrange("(c p) t -> p c t", p=128)[:, :, s0:s0 + nn],
                       reads=[("CKVT", bi)], writes=[x_])
                rmsnorm_fm(x_, 2, 256, nn, kvg, xn)
                for h in range(8):
                    pk = kb.bank[h % 2]
                    for kc in range(2):
                        kb.mm(pk[0:64, 0:nn], wukv[:, kc, h * 128:h * 128 + 64], xn[:, kc, 0:nn], start=(kc == 0), stop=(kc == 1),
                              reads=[wukv, xn], writes=[pk])
                    kb.cp("act" if h % 2 else "dve", KT[0:64, h, s0:s0 + nn], pk[0:64, 0:nn], reads=[pk], writes=[KT])
                    kb.act(sqb[0:96, 0, 0:nn], KT[0:96, h, s0:s0 + nn], AF.Square, reads=[KT], writes=[sqb])
                    pn = kb.bank[6]
                    kb.mm(pn[:, 0:nn], ones[0:96, :], sqb[0:96, 0, 0:nn], reads=[ones, sqb], writes=[pn])
                    kb.op("dve", lambda E: E.reduce_max(kmb[:, h:h + 1], pn[:, 0:nn], AX.X), reads=[pn], writes=[kmb])
                    kb.tt("dve", kmax2[:, h:h + 1], kmax2[:, h:h + 1], kmb[:, h:h + 1], OP.max, reads=[kmb, kmax2], writes=[kmax2])
                for j in range(nn // 128):
                    ti = s0 // 128 + j
                    pv = kb.bank[2 + (j % 2)]
                    for kc in range(2):
                        kb.mm(pv[:, :].rearrange("p (h x) -> p h x", x=64), xn[:, kc, j * 128:(j + 1) * 128],
                              wukv[:, kc, :].rearrange("p (h x) -> p h x", x=128)[:, :, 64:128], start=(kc == 0), stop=(kc == 1),
                              reads=[wukv, xn], writes=[pv])
                    kb.cp("act" if j % 2 else "dve", VA[:, ti, :, 0:64], pv[:, :].rearrange("p (h x) -> p h x", x=64), reads=[pv], writes=[VA])
            if "mla_stop2" in self.debug:
                kb.barrier()
                return
            kb.act(negk[:], kmax2[:], AF.Sqrt, reads=[kmax2], writes=[negk])
            kb.ts("dve", negk[:], negk[:], -1.02, OP.mult, reads=[negk], writes=[negk])
            QT = [kb.sb(es, [97, 8, 512], BF16, "QT%d" % i) for i in range(2)]
            PT = [kb.sb(es, [128, 512], BF16, "PT%d" % i) for i in range(3)]
            tq = kb.sb(es, [96, 512], F32, "tq"); tq2 = kb.sb(es, [96, 512], F32, "tq2")
            qn1 = kb.sb(es, [97, 512], F32, "qn1")
            yt = kb.sb(es, [128, 4, 512], F32, "yt")
            ytb = kb.sb(es, [128, 4, 512], BF16, "ytb")
            rec = kb.sb(es, [128, 4], F32, "rec")
            ss4 = kb.sb(es, [128, 4], F32, "ss4")
            mts = kb.sb(es, [128, 4, 512], BF16, "mts")
            npt = 0
            for bi, (s0, nn) in enumerate(blocks):
                if bi == 0 and not ctx_out:
                    continue
                nj = nn // 128
                kchunks = list(range(2)) if bi == 0 else list(range(NT))
                x_ = xin[bi % 2]
                kb.dma(x_[:, 0:3, 0:nn], self.S["CQT"].ap().rearrange("(c p) t -> p c t", p=128)[:, :, s0:s0 + nn],
                       reads=[("CQT", bi)], writes=[x_])
                rmsnorm_fm(x_, 3, 384, nn, qg, xn)
                Q = QT[bi % 2]
                rc = rcb[bi % 2]; rs = rsb[bi % 2]
                kb.dma(rc[64:96, 0:nn], I["rope_cos"].ap()[:, s0:s0 + nn], writes=[rc])
                kb.dma(rs[64:96, 0:nn], I["rope_sin"].ap()[:, s0:s0 + nn], writes=[rs])
                for h in range(8):
                    pa = kb.bank[6]
                    pb = kb.bank[7]
                    for kc in range(3):
                        kb.mm(pa[0:96, 0:nn], wuq[:, kc, h * 96:(h + 1) * 96], xn[:, kc, 0:nn], start=(kc == 0), stop=(kc == 2),
                              reads=[wuq, xn], writes=[pa])
                    for kc in range(3):
                        kb.mm(pb[0:96, 0:nn], wrot[:, kc, h, :], xn[:, kc, 0:nn], start=(kc == 0), stop=(kc == 2),
                              reads=[wrot, xn], writes=[pb])
                    kb.cp("act", Q[0:64, h, 0:nn], pa[0:64, 0:nn], reads=[pa], writes=[Q])
                    kb.tt("dve", tq[64:96, 0:nn], pb[64:96, 0:nn], rs[64:96, 0:nn], OP.mult, reads=[pb, rs], writes=[tq])
                    kb.tt("dve", tq2[64:96, 0:nn], pa[64:96, 0:nn], rc[64:96, 0:nn], OP.mult, reads=[pa, rc], writes=[tq2])
                    kb.tt("dve", Q[64:96, h, 0:nn], tq[64:96, 0:nn], tq2[64:96, 0:nn], OP.add, reads=[tq, tq2], writes=[Q])
                    kb.act(sqb[0:96, 0, 0:nn], Q[0:96, h, 0:nn], AF.Square, reads=[Q], writes=[sqb])
                    pn = kb.bank[6]
                    kb.mm(pn[:, 0:nn], ones[0:96, :], sqb[0:96, 0, 0:nn], reads=[ones, sqb], writes=[pn])
                    kb.act(qn1[96:97, 0:nn], pn[96:97, 0:nn], AF.Sqrt, reads=[pn], writes=[qn1])
                    kb.ts("dve", Q[96:97, h, 0:nn], qn1[96:97, 0:nn], negk[96:97, h:h + 1], OP.mult, reads=[qn1, negk], writes=[Q])
                if "mla_stop3" in self.debug:
                    continue
                for h in range(8):
                    for ci, kc in enumerate(kchunks):
                        pst = kb.bank[ci % 2]
                        kb.mm(pst[:, 0:nn], KT[0:97, h, kc * 128:(kc + 1) * 128], Q[0:97, h, 0:nn], reads=[KT, Q], writes=[pst])
                        P_ = PT[npt % 3]
                        npt += 1
                        kb.act(P_[:, 0:nn], pst[:, 0:nn], AF.Exp, scale=MLA_SCALE, reads=[pst], writes=[P_])
                        for j in range(nj):
                            if "mla_nopv" in self.debug:
                                continue
                            po = kb.bank[2 + j]
                            kb.mm(po[:, 0:65], P_[:, j * 128:(j + 1) * 128], VA[:, kc, h, :], start=(ci == 0), stop=(ci == len(kchunks) - 1),
                                  reads=[P_, VA], writes=[po])
                    for j in range(nj):
                        if "mla_nonorm" in self.debug:
                            continue
                        po = kb.bank[2 + j]
                        if "mla_norec" in self.debug:
                            kb.ts("dve", yt[:, j, h * 64:(h + 1) * 64], po[:, 0:64], 0.5, OP.mult, reads=[po], writes=[yt])
                            continue
                        if "mla_recsb" in self.debug:
                            kb.cp("act", rec[:, j:j + 1], po[:, 64:65], reads=[po], writes=[rec])
                            kb.op("dve", lambda E: E.reciprocal(rec[:, j:j + 1], rec[:, j:j + 1]), reads=[rec], writes=[rec])
                        else:
                            kb.op("dve", lambda E: E.reciprocal(rec[:, j:j + 1], po[:, 64:65]), reads=[po], writes=[rec])
                        kb.ts("dve", yt[:, j, h * 64:(h + 1) * 64], po[:, 0:64], rec[:, j:j + 1], OP.mult, reads=[po, rec], writes=[yt])
                for j in range(nj):
                    if "MLAO" in self.debug:
                        kb.dma(self.S["MLAO"].ap()[s0 + j * 128:s0 + (j + 1) * 128, :], yt[:, j, :], reads=[yt], writes=["MLAO"], q="pool")
                    kb.act(mts[:, j, :], yt[:, j, :], AF.Square, accum_out=ss4[:, j:j + 1], reads=[yt], writes=[mts, ss4])
                kb.ts("dve", ss4[:, 0:nj], ss4[:, 0:nj], 1.0 / 512, OP.mult, EPS, OP.add, reads=[ss4], writes=[ss4])
                kb.act(ss4[:, 0:nj], ss4[:, 0:nj], AF.Sqrt, reads=[ss4], writes=[ss4])
                kb.op("dve", lambda E: E.reciprocal(ss4[:, 0:nj], ss4[:, 0:nj]), reads=[ss4], writes=[ss4])
                for j in range(nj):
                    kb.stt("dve", ytb[:, j, :], yt[:, j, :], ss4[:, j:j + 1], gbc[:], OP.mult, OP.mult, reads=[yt, ss4, gbc], writes=[ytb])
                    pt_ = kb.bank[6 + (j % 2)]
                    ptv = pt_[:].bitcast(BF16)
                    for fc in range(4):
                        kb.tr(ptv[:, fc * 128:(fc + 1) * 128], ytb[:, j, fc * 128:(fc + 1) * 128], ident[:], reads=[ytb, ident], writes=[pt_])
                    kb.cp("act", mts[:, :, j * 128:(j + 1) * 128], ptv[:, 0:512].rearrange("p (f q) -> p f q", f=4), reads=[pt_], writes=[mts])
                kb.dma(self.S["MT"].ap()[256:768, s0:s0 + nn].rearrange("(f p) t -> p f t", p=128), mts[:, :, 0:nn], reads=[mts],
                       writes=[("MT_mla", bi)], q="pool")
        kb.barrier()


    def stage_hyena(self, L, part):
        kb = self.kb
        I = self.I
        nm = part
        n = NLAT if part == "l" else NCTX
        r0 = NCTX if part == "l" else 0
        ntc = n // 128
        nj = (n + 1 + 127) // 128
        lagblocks = [(i * 512, min(512, n - i * 512)) for i in range((n + 511) // 512)]
        KSP = self.S["KSPEC_" + nm]
        with ExitStack() as es:
            ident_f = kb.sb(es, [128, 128], F32, "identf")
            kb.dma(ident_f[:], I["ident"].ap(), writes=[ident_f])
            w1 = kb.sb(es, [33, 64], F32, "w1"); w2 = kb.sb(es, [64, 64], F32, "w2"); w3 = kb.sb(es, [64, 1024], F32, "w3")
            cols = kb.sb(es, [64, 4], F32, "cols"); bf = kb.sb(es, [64, 2], F32, "bf"); b3 = kb.sb(es, [128, 8], F32, "b3")
            kb.dma(w1[:], I["hy_f_w1"].ap()[L], writes=[w1]); kb.dma(w2[:], I["hy_f_w2"].ap()[L], writes=[w2]); kb.dma(w3[:], I["hy_f_w3"].ap()[L], writes=[w3])
            kb.dma(cols[:], I["hy_cols"].ap()[L], writes=[cols]); kb.dma(b3[:], I["hy_b3"].ap()[L], writes=[b3])
            kb.tt("dve", bf[:, 0:1], cols[:, 0:1], cols[:, 2:3], OP.mult, reads=[cols], writes=[bf])
            kb.tt("dve", bf[:, 1:2], cols[:, 1:2], cols[:, 3:4], OP.mult, reads=[cols], writes=[bf])
            FS = kb.sb(es, [128, ntc, 512], BF16, "FS"); FD = kb.sb(es, [128, ntc, 512], BF16, "FD")
            zp = kb.sb(es, [33, 512], F32, "zp")
            h1 = kb.sb(es, [64, 512], F32, "h1"); h2 = kb.sb(es, [64, 512], F32, "h2")
            t1 = kb.sb(es, [64, 512], F32, "t1"); t2 = kb.sb(es, [64, 512], F32, "t2"); t0 = kb.sb(es, [64, 512], F32, "t0")
            dec = kb.sb(es, [128, 2, 512], F32, "dec")
            ff = kb.sb(es, [128, 512], F32, "ff"); fb = kb.sb(es, [128, 512], F32, "fb")
            fs_ = kb.sb(es, [128, 512], F32, "fs"); fd_ = kb.sb(es, [128, 512], F32, "fd")
            for (l0, ln) in lagblocks:
                kb.dma(zp[:, 0:ln], I["hy_zpos_" + nm].ap()[:, l0:l0 + ln], writes=[zp])
                kb.dma(dec[:, :, 0:ln], I["hy_decay_" + nm].ap().rearrange("(c p) t -> p c t", p=128)[:, :, l0:l0 + ln], writes=[dec])
                p1 = kb.bank[0]
                kb.mm(p1[0:64, 0:ln], w1[:], zp[:, 0:ln], reads=[w1, zp], writes=[p1])
                kb.ts("dve", t0[:, 0:ln], p1[0:64, 0:ln], cols[:, 2:3], OP.mult, bf[:, 0:1], OP.add, reads=[p1, cols, bf], writes=[t0])
                self.sin_rr("dve", h1[:, 0:ln], t0[:, 0:ln], 0.0, t1[:, 0:ln], t2[:, 0:ln])
                p2 = kb.bank[1]
                kb.mm(p2[0:64, 0:ln], w2[:], h1[:, 0:ln], reads=[w2, h1], writes=[p2])
                kb.ts("dve", t0[:, 0:ln], p2[0:64, 0:ln], cols[:, 3:4], OP.mult, bf[:, 1:2], OP.add, reads=[p2, cols, bf], writes=[t0])
                self.sin_rr("dve", h2[:, 0:ln], t0[:, 0:ln], 0.0, t1[:, 0:ln], t2[:, 0:ln])
                for o in range(2):
                    for chf in range(2):
                        jf = o * 2 + chf
                        jb = 4 + o * 2 + chf
                        pf = kb.bank[2]; pbk = kb.bank[3]
                        kb.mm(pf[:, 0:ln], w3[:, jf * 128:(jf + 1) * 128], h2[:, 0:ln], reads=[w3, h2], writes=[pf])
                        kb.mm(pbk[:, 0:ln], w3[:, jb * 128:(jb + 1) * 128], h2[:, 0:ln], reads=[w3, h2], writes=[pbk])
                        kb.stt("dve", ff[:, 0:ln], pf[:, 0:ln], b3[:, jf:jf + 1], dec[:, chf, 0:ln], OP.add, OP.mult, reads=[pf, b3, dec], writes=[ff])
                        kb.stt("dve", fb[:, 0:ln], pbk[:, 0:ln], b3[:, jb:jb + 1], dec[:, chf, 0:ln], OP.add, OP.mult, reads=[pbk, b3, dec], writes=[fb])
                        if l0 == 0:
                            kb.memset("dve", fb[:, 0:1], 0.0, writes=[fb])
                        kb.tt("dve", fs_[:, 0:ln], ff[:, 0:ln], fb[:, 0:ln], OP.add, reads=[ff, fb], writes=[fs_])
                        kb.tt("pool", fd_[:, 0:ln], ff[:, 0:ln], fb[:, 0:ln], OP.subtract, reads=[ff, fb], writes=[fd_])
                        for (src, dst, pbank) in ((fs_, FS, 4), (fd_, FD, 5)):
                            pt_ = kb.bank[pbank]
                            nq = ln // 128
                            for q in range(nq):
                                kb.tr(pt_[:, q * 128:(q + 1) * 128], src[:, q * 128:(q + 1) * 128], ident_f[:], reads=[src, ident_f], writes=[pt_])
                            tc0 = l0 // 128
                            kb.cp("act", dst[:, tc0:tc0 + nq, o * 256 + chf * 128:o * 256 + (chf + 1) * 128],
                                  pt_[:, 0:nq * 128].rearrange("p (q c) -> p q c", c=128), reads=[pt_], writes=[dst])
            Dt = [kb.sb(es, [128, ntc, 128], BF16, "Dt%d" % i) for i in range(4)]
            ks = [kb.sb(es, [128, 2, 512], F32, "ks%d" % i) for i in range(2)]
            nd = 0
            for j in range(nj):
                for ri, (fc, src) in enumerate(((j, FS), (nj + j, FD))):
                    D_ = Dt[nd % 4]
                    nd += 1
                    kb.dma(D_[:], I["hy_dfwd_" + nm].ap()[fc], writes=[D_])
                    pk = kb.bank[ri]
                    for tc in range(ntc):
                        kb.mm(pk[:, :], D_[:, tc, :], src[:, tc, :], start=(tc == 0), stop=(tc == ntc - 1), reads=[D_, src], writes=[pk])
                    kb.cp("act" if ri else "dve", ks[j % 2][:, ri, :], pk[:, :], reads=[pk], writes=[ks[j % 2]])
                kb.dma(KSP.ap()[j].rearrange("r p c -> p r c"), ks[j % 2][:], reads=[ks[j % 2]], writes=["KSPEC_" + nm], q="pool")
        kb.barrier()
        with ExitStack() as es:
            ident_f = kb.sb(es, [128, 128], F32, "identf")
            kb.dma(ident_f[:], I["ident"].ap(), writes=[ident_f])
            sw = kb.sb(es, [128, 6, 3], F32, "sw"); sbb = kb.sb(es, [128, 6], F32, "sbb")
            kb.dma(sw[:], I["hy_sw"].ap()[L], writes=[sw]); kb.dma(sbb[:], I["hy_sb"].ap()[L], writes=[sbb])
            xin = [kb.sb(es, [128, n], F32, "hzx%d" % i) for i in range(2)]
            zz = [kb.sb(es, [128, n], F32, "hzz%d" % i) for i in range(2)]
            tm = [kb.sb(es, [128, 4, 128], F32, "tm%d" % i) for i in range(2)]
            ntm = 0
            for c in range(6):
                x_ = xin[c % 2]; z_ = zz[c % 2]
                kb.dma(x_[:], self.S["HZT"].ap()[c * 128:(c + 1) * 128, r0:r0 + n], reads=[("HZT", i) for i in range(9)], writes=[x_])
                kb.ts("dve", z_[:], x_[:], sw[:, c, 1:2], OP.mult, sbb[:, c:c + 1], OP.add, reads=[x_, sw, sbb], writes=[z_])
                kb.stt("dve", z_[:, 1:n], x_[:, 0:n - 1], sw[:, c, 0:1], z_[:, 1:n], OP.mult, OP.add, reads=[x_, sw, z_], writes=[z_])
                kb.stt("dve", z_[:, 0:n - 1], x_[:, 1:n], sw[:, c, 2:3], z_[:, 0:n - 1], OP.mult, OP.add, reads=[x_, sw, z_], writes=[z_])
                for tg in range(0, ntc, 4):
                    ng = min(4, ntc - tg)
                    pt_ = kb.bank[ntm % 2]
                    t_ = tm[ntm % 2]
                    ntm += 1
                    for q in range(ng):
                        kb.tr(pt_[:, q * 128:(q + 1) * 128], z_[:, (tg + q) * 128:(tg + q + 1) * 128], ident_f[:], reads=[z_, ident_f], writes=[pt_])
                    kb.cp("act" if ntm % 2 else "dve", t_[:, 0:ng, :], pt_[:, 0:ng * 128].rearrange("p (q c) -> p q c", c=128), reads=[pt_], writes=[t_])
                    kb.dma(self.S["HZTM"].ap()[r0 + tg * 128:r0 + (tg + ng) * 128, c * 128:(c + 1) * 128].rearrange("(q p) c -> p q c", p=128),
                           t_[:, 0:ng, :], reads=[t_], writes=["HZTM"], q="pool")
        kb.barrier()
        with ExitStack() as es:
            ident = kb.sb(es, [128, 128], BF16, "identb")
            idf = kb.sb(es, [128, 128], F32, "identf")
            kb.dma(idf[:], I["ident"].ap(), writes=[idf])
            kb.cp("dve", ident[:], idf[:], reads=[idf], writes=[ident])
            Yb = [kb.sb(es, [128, ntc, 256], BF16, "Yb%d" % i) for i in range(2)]
            Z = kb.sb(es, [128, 2 * nj, 256], BF16, "Zs")
            Dt = [kb.sb(es, [128, ntc, 128], BF16, "Df%d" % i) for i in range(4)]
            Di = [kb.sb(es, [128, 2 * nj, 128], BF16, "Di%d" % i) for i in range(2)]
            kt = [kb.sb(es, [128, 2, 256], F32, "kt%d" % i) for i in range(2)]
            cm = [kb.sb(es, [128, 256], F32, "cm%d" % i) for i in range(4)]
            yo = [kb.sb(es, [128, 256], F32, "yo%d" % i) for i in range(2)]
            gt = [kb.sb(es, [128, 256], F32, "gt%d" % i) for i in range(2)]
            yn = [kb.sb(es, [128, 256], F32, "yn%d" % i) for i in range(2)]
            ynb = kb.sb(es, [128, 256], BF16, "ynb"); sqj = kb.sb(es, [128, 256], BF16, "sqj")
            ss = kb.sb(es, [128, 1], F32, "ss")
            mts = [kb.sb(es, [128, 2, 128], BF16, "mts%d" % i) for i in range(2)]
            bias_bc = kb.sb(es, [128, 2, 256], F32, "biasbc")
            gbc = kb.sb(es, [128, 256], F32, "gbc")
            kb.dma(bias_bc[:].rearrange("p o c -> p (o c)"), I["hy_bias"].ap()[L:L + 1].rearrange("a o c -> a (o c)").partition_broadcast(128), writes=[bias_bc])
            kb.dma(gbc[:], I["mix_norm_g"].ap()[L:L + 1, 768:1024].partition_broadcast(128), writes=[gbc])
            HZ = self.S["HZTM"].ap()
            for tc in range(ntc):
                y_ = yo[tc % 2]
                kb.dma(y_[:], HZ[r0 + tc * 128:r0 + (tc + 1) * 128, 0:256], reads=["HZTM"], writes=[y_])
                kb.cp("act" if tc % 2 else "dve", Yb[0][:, tc, :], y_[:], reads=[y_], writes=[Yb[0]])
            nd = 0
            for o in range(2):
                Yin = Yb[o]
                for j in range(nj):
                    k_ = kt[j % 2]
                    kb.dma(k_[:], KSP.ap()[j].rearrange("r p c -> p r c")[:, :, o * 256:(o + 1) * 256], reads=["KSPEC_" + nm], writes=[k_])
                    pr = kb.bank[(j % 2) * 2]; pi_ = kb.bank[(j % 2) * 2 + 1]
                    for ri, (fc, pk) in enumerate(((j, pr), (nj + j, pi_))):
                        D_ = Dt[nd % 4]
                        nd += 1
                        kb.dma(D_[:], I["hy_dfwd_" + nm].ap()[fc], writes=[D_])
                        for tc in range(ntc):
                            kb.mm(pk[:, 0:256], D_[:, tc, :], Yin[:, tc, :], start=(tc == 0), stop=(tc == ntc - 1), reads=[D_, Yin], writes=[pk])
                    c1, c2, c3, c4 = cm
                    kb.tt("dve", c1[:], pr[:, 0:256], k_[:, 0, :], OP.mult, reads=[pr, k_], writes=[c1])
                    kb.tt("dve", c2[:], pi_[:, 0:256], k_[:, 1, :], OP.mult, reads=[pi_, k_], writes=[c2])
                    kb.tt("pool", Z[:, j, :], c1[:], c2[:], OP.subtract, reads=[c1, c2], writes=[Z])
                    kb.tt("dve", c3[:], pr[:, 0:256], k_[:, 1, :], OP.mult, reads=[pr, k_], writes=[c3])
                    kb.tt("dve", c4[:], pi_[:, 0:256], k_[:, 0, :], OP.mult, reads=[pi_, k_], writes=[c4])
                    kb.tt("pool", Z[:, nj + j, :], c3[:], c4[:], OP.add, reads=[c3, c4], writes=[Z])
                for tc in range(ntc):
                    D_ = Di[tc % 2]
                    kb.dma(D_[:], I["hy_dinv_" + nm].ap()[tc], writes=[D_])
                    y_ = yo[tc % 2]; g_ = gt[tc % 2]; o_ = yn[tc % 2]
                    rows = slice(r0 + tc * 128, r0 + (tc + 1) * 128)
                    if o == 0:
                        kb.dma(y_[:], HZ[rows, 0:256], reads=["HZTM"], writes=[y_])
                    else:
                        kb.dma(y_[:], self.S["HY1"].ap()[rows, :], reads=[("HY1", tc)], writes=[y_])
                    kb.dma(g_[:], HZ[rows, 256 * (o + 1):256 * (o + 2)], reads=["HZTM"], writes=[g_])
                    pc = kb.bank[4 + (tc % 2)]
                    for fc in range(2 * nj):
                        kb.mm(pc[:, 0:256], D_[:, fc, :], Z[:, fc, :], start=(fc == 0), stop=(fc == 2 * nj - 1), reads=[D_, Z], writes=[pc])
                    kb.tt("pool", y_[:], y_[:], bias_bc[:, o, :], OP.mult, reads=[y_, bias_bc], writes=[y_])
                    kb.tt("dve", o_[:], pc[:, 0:256], y_[:], OP.add, reads=[pc, y_], writes=[o_])
                    kb.tt("dve", o_[:], o_[:], g_[:], OP.mult, reads=[o_, g_], writes=[o_])
                    if o == 0:
                        kb.cp("act", Yb[1][:, tc, :], o_[:], reads=[o_], writes=[Yb[1]])
                        kb.dma(self.S["HY1"].ap()[rows, :], o_[:], reads=[o_], writes=[("HY1", tc)], q="pool")
                    else:
                        if "HYO" in self.debug:
                            kb.dma(self.S["HYO"].ap()[rows, :], o_[:], reads=[o_], writes=["HYO"], q="pool")
                        kb.act(sqj[:], o_[:], AF.Square, accum_out=ss[:], reads=[o_], writes=[sqj, ss])
                        kb.ts("dve", ss[:], ss[:], 1.0 / 256, OP.mult, EPS, OP.add, reads=[ss], writes=[ss])
                        kb.act(ss[:], ss[:], AF.Sqrt, reads=[ss], writes=[ss])
                        kb.op("dve", lambda E: E.reciprocal(ss[:], ss[:]), reads=[ss], writes=[ss])
                        kb.stt("dve", ynb[:], o_[:], ss[:, 0:1], gbc[:], OP.mult, OP.mult, reads=[o_, ss, gbc], writes=[ynb])
                        pt_ = kb.bank[6 + (tc % 2)]
                        ptv = pt_[:].bitcast(BF16)
                        for fcx in range(2):
                            kb.tr(ptv[:, fcx * 128:(fcx + 1) * 128], ynb[:, fcx * 128:(fcx + 1) * 128], ident[:], reads=[ynb, ident], writes=[pt_])
                        m_ = mts[tc % 2]
                        kb.cp("act", m_[:], ptv[:, 0:256].rearrange("p (f q) -> p f q", f=2), reads=[pt_], writes=[m_])
                        kb.dma(self.S["MT"].ap()[768:1024, r0 + tc * 128:r0 + (tc + 1) * 128].rearrange("(f p) t -> p f t", p=128), m_[:],
                               reads=[m_], writes=[("MT_hy", part, tc)], q="pool")
        kb.barrier()


    def stage_s5merge(self, L):
        kb = self.kb
        blocks = [(0, 256)] + [(256 + 512 * i, 512) for i in range(8)]
        with ExitStack() as es:
            ones = kb.sb(es, [128, 128], BF16, "ones")
            kb.memset("dve", ones[:], 1.0, writes=[ones])
            gcol = kb.sb(es, [128, 2], F32, "gcol")
            kb.dma(gcol[:], self.I["mix_g_s5col"].ap()[L], writes=[gcol])
            xin = [kb.sb(es, [128, 2, 512], F32, "s5x%d" % i) for i in range(2)]
            sqb = kb.sb(es, [128, 2, 512], BF16, "sqb"); rst = kb.sb(es, [128, 512], F32, "rst")
            ob = [kb.sb(es, [128, 2, 512], BF16, "s5o%d" % i) for i in range(2)]
            for bi, (s0, nn) in enumerate(blocks):
                x_ = xin[bi % 2]; o_ = ob[bi % 2]
                kb.dma(x_[:, :, 0:nn], self.S["S5T"].ap().rearrange("(c p) t -> p c t", p=128)[:, :, s0:s0 + nn], reads=[("S5T", bi)], writes=[x_])
                kb.act(sqb[:, :, 0:nn], x_[:, :, 0:nn], AF.Square, reads=[x_], writes=[sqb])
                pn = kb.bank[bi % 2]
                for c in range(2):
                    kb.mm(pn[:, 0:nn], ones[:], sqb[:, c, 0:nn], start=(c == 0), stop=(c == 1), reads=[ones, sqb], writes=[pn])
                kb.ts("dve", rst[:, 0:nn], pn[:, 0:nn], 1.0 / 256, OP.mult, EPS, OP.add, reads=[pn], writes=[rst])
                kb.act(rst[:, 0:nn], rst[:, 0:nn], AF.Sqrt, reads=[rst], writes=[rst])
                kb.op("dve", lambda E: E.reciprocal(rst[:, 0:nn], rst[:, 0:nn]), reads=[rst], writes=[rst])
                for c in range(2):
                    kb.stt("dve", o_[:, c, 0:nn], x_[:, c, 0:nn], gcol[:, c:c + 1], rst[:, 0:nn], OP.mult, OP.mult, reads=[x_, gcol, rst], writes=[o_])
                kb.dma(self.S["MT"].ap()[0:256, s0:s0 + nn].rearrange("(c p) t -> p c t", p=128), o_[:, :, 0:nn], reads=[o_], writes=[("MT_s5", bi)], q="pool")
        kb.barrier()

    def stage_out_moe(self, L, last):
        kb = self.kb
        I = self.I
        S = self.S
        tiles = list(range(2, NT)) if last else list(range(NT))
        ntl = len(tiles)
        nblk = (2 * ntl * 128 + 127) // 128 + 32
        X = S["X"].ap()
        with ExitStack() as es:
            ident_f = kb.sb(es, [128, 128], F32, "identf")
            ident = kb.sb(es, [128, 128], BF16, "identb")
            kb.dma(ident_f[:], I["ident"].ap(), writes=[ident_f])
            kb.cp("dve", ident[:], ident_f[:], reads=[ident_f], writes=[ident])
            iota_e = kb.sb(es, [128, 32], F32, "iotae"); iota_b = kb.sb(es, [128, 128], F32, "iotab"); iota_p = kb.sb(es, [128, 1], F32, "iotap")
            ltri = kb.sb(es, [128, 128], BF16, "ltri"); ones = kb.sb(es, [128, 128], BF16, "ones")
            kb.dma(iota_e[:], I["iota_e"].ap(), writes=[iota_e]); kb.dma(iota_b[:], I["iota_b"].ap(), writes=[iota_b])
            kb.dma(iota_p[:], I["iota_p"].ap(), writes=[iota_p]); kb.dma(ltri[:], I["ltri"].ap(), writes=[ltri])
            kb.memset("dve", ones[:], 1.0, writes=[ones])
            GW = kb.sb(es, [128, NT, 2], F32, "GW"); EID = kb.sb(es, [128, NT, 2], F32, "EID"); RANK = kb.sb(es, [128, NT, 2], F32, "RANK")
            DEST = kb.sb(es, [128, NT, 2], F32, "DEST"); DESTI = kb.sb(es, [128, NT, 2], I32, "DESTI")
            base = kb.sb(es, [128, 32], F32, "base")
            BE = kb.sb(es, [128, 128], F32, "BE"); IDXW = kb.sb(es, [128, 128], I32, "IDXW")
            kb.memset("dve", base[:], 0.0, writes=[base])
            kb.memset("dve", RANK[:], 0.0, writes=[RANK])
            kb.memset("dve", DEST[:], 0.0, writes=[DEST])
            es2 = ExitStack()
            wo = kb.sb(es2, [128, 8, DM], BF16, "wo")
            wr = kb.sb(es2, [128, 8, 36], F32, "wr")
            kb.dma(wr[:], I["moe_wr"].ap()[L].rearrange("(kc p) n -> p kc n", p=128), writes=[wr])
            gate1 = kb.sb(es2, [128, DM], F32, "gate1"); G2 = kb.sb(es2, [128, DM], F32, "G2"); S2 = kb.sb(es2, [128, DM], F32, "S2")
            tmpb = kb.sb(es2, [128, DM], F32, "tmpb")
            es3 = ExitStack()
            wst = kb.sb(es3, [128, 8, DM], F32, "wst")
            kb.dma(wst[:], I["w_out"].ap()[L].rearrange("(kc p) n -> p kc n", p=128), writes=[wst])
            kb.cp("act", wo[:, 0:4, :], wst[:, 0:4, :], reads=[wst], writes=[wo])
            kb.cp("dve", wo[:, 4:8, :], wst[:, 4:8, :], reads=[wst], writes=[wo])
            kb.barrier()
            es3.close()
            mT = [kb.sb(es2, [128, 8, 512], BF16, "mT%d" % i) for i in range(2)]
            xt = [kb.sb(es2, [128, DM], F32, "xt%d" % i) for i in range(2)]
            xn = [kb.sb(es2, [128, DM], F32, "xn%d" % i) for i in range(2)]
            sq = kb.sb(es2, [128, DM], BF16, "sq"); ss = kb.sb(es2, [128, 1], F32, "ss"); rstd = kb.sb(es2, [128, 1], F32, "rstd")
            tmp = kb.sb(es2, [128, DM], F32, "tmp")
            ff = [kb.sb(es2, [128, DM], F32, "ff%d" % i) for i in range(2)]
            fbt = [kb.sb(es2, [128, DM], BF16, "fbt%d" % i) for i in range(2)]
            fT = kb.sb(es2, [128, 8, 128], F32, "fT")
            lg = kb.sb(es2, [128, 36], F32, "lg")
            sm = {k: kb.sb(es2, shp, F32, k) for k, shp in (("gmax", [128, 1]), ("ngmax", [128, 1]), ("ohg", [128, 4]), ("pen", [128, 4]), ("ex4", [128, 4]),
                                                            ("sume", [128, 1]), ("gw", [128, 1]), ("msk", [128, 32]), ("top8", [128, 8]), ("nv2", [128, 1]),
                                                            ("p1", [128, 1]), ("p2", [128, 1]), ("a0", [128, 32]), ("junk", [128, 32]), ("csum", [128, 32]))}
            idx8 = kb.sb(es2, [128, 8], U32, "idx8")
            E2 = kb.sb(es2, [128, 64], F32, "E2"); E2b = kb.sb(es2, [128, 64], BF16, "E2b")
            blocks = [(0, 2)] + [(2 + 4 * i, 4) for i in range(8)]
            if last:
                blocks = blocks[1:]
            for bi, (t0, nt_) in enumerate(blocks):
                which = 1 if t0 == 0 else 0
                if t0 in (0, 2):
                    MOD = S["MOD"].ap()
                    kb.dma(gate1[:], MOD[which:which + 1, 2 * DM:3 * DM].partition_broadcast(128), reads=["MOD"], writes=[gate1])
                    self.load_mod_bc((G2, S2, tmpb), L, which, 3, "norm2_g")
                m_ = mT[bi % 2]
                nb = nt_ * 128
                c0 = t0 * 128
                kb.dma(m_[:, :, 0:nb], S["MT"].ap().rearrange("(kc p) t -> p kc t", p=128)[:, :, c0:c0 + nb],
                       reads=[("MT_s5", i) for i in range(9)] + [("MT_mla", i) for i in range(9)] + [("MT_hy", "l", i) for i in range(32)] + [("MT_hy", "c", i) for i in range(2)],
                       writes=[m_])
                for j in range(nt_):
                    t = t0 + j
                    x_ = xt[t % 2]; xo = xn[t % 2]
                    kb.dma(x_[:], X[t * 128:(t + 1) * 128, :], reads=[("X", t)], writes=[x_])
                    for nh in range(2):
                        po = kb.bank[nh]
                        for kc in range(8):
                            kb.mm(po[:, :], m_[:, kc, j * 128:(j + 1) * 128], wo[:, kc, nh * 512:(nh + 1) * 512], start=(kc == 0), stop=(kc == 7),
                                  reads=[m_, wo], writes=[po])
                        kb.tt("dve", tmp[:, nh * 512:(nh + 1) * 512], po[:, :], gate1[:, nh * 512:(nh + 1) * 512], OP.mult, reads=[po, gate1], writes=[tmp])
                    kb.tt("pool", xo[:], tmp[:], x_[:], OP.add, reads=[tmp, x_], writes=[xo])
                    kb.dma(X[t * 128:(t + 1) * 128, :], xo[:], reads=[xo], writes=[("X", t)], q="pool")
                    if "XMID" in self.debug:
                        kb.dma(S["XMID"].ap()[t * 128:(t + 1) * 128, :], xo[:], reads=[xo], writes=["XMID"], q="pool")
                    f_ = ff[t % 2]; fb_ = fbt[t % 2]
                    self.rms_modulate(xo, G2, S2, sq, ss, rstd, tmp, f_, fb_)
                    kb.dma(S["FB"].ap()[t * 128:(t + 1) * 128, :], fb_[:], reads=[fb_], writes=[("FB", t)], q="pool")
                    for half in range(2):
                        pt_ = kb.bank[2 + half]
                        for q in range(4):
                            kc = half * 4 + q
                            kb.tr(pt_[:, q * 128:(q + 1) * 128], f_[:, kc * 128:(kc + 1) * 128], ident_f[:], reads=[f_, ident_f], writes=[pt_])
                        kb.cp("act" if half else "dve", fT[:, half * 4:(half + 1) * 4, :], pt_[:, :].rearrange("p (q c) -> p q c", c=128), reads=[pt_], writes=[fT])
                    pl = kb.bank[4]
                    for kc in range(8):
                        kb.mm(pl[:, 0:36], fT[:, kc, :], wr[:, kc, :], start=(kc == 0), stop=(kc == 7), reads=[fT, wr], writes=[pl])
                    kb.cp("dve", lg[:], pl[:, 0:36], reads=[pl], writes=[lg])
                    A = lambda k: sm[k][:]
                    kb.op("dve", lambda E: E.reduce_max(A("gmax"), lg[:, 0:4], AX.X), reads=[lg], writes=[sm["gmax"]])
                    kb.ts("dve", A("ohg"), lg[:, 0:4], A("gmax")[:, 0:1], OP.is_equal, reads=[lg, sm["gmax"]], writes=[sm["ohg"]])
                    kb.ts("dve", A("ngmax"), A("gmax"), -1.0, OP.mult, reads=[sm["gmax"]], writes=[sm["ngmax"]])
                    kb.act(A("ex4"), lg[:, 0:4], AF.Exp, bias=A("ngmax")[:, 0:1], accum_out=A("sume"), reads=[lg, sm["ngmax"]], writes=[sm["ex4"], sm["sume"]])
                    kb.op("dve", lambda E: E.reciprocal(A("gw"), A("sume")), reads=[sm["sume"]], writes=[sm["gw"]])
                    kb.ts("dve", A("pen"), A("ohg"), 1.0e30, OP.mult, -1.0e30, OP.add, reads=[sm["ohg"]], writes=[sm["pen"]])
                    for g in range(4):
                        kb.ts("dve", sm["msk"][:, g * 8:(g + 1) * 8], lg[:, 4 + g * 8:4 + (g + 1) * 8], sm["pen"][:, g:g + 1], OP.add,
                              reads=[lg, sm["pen"]], writes=[sm["msk"]])
                    kb.op("dve", lambda E: E.max(A("top8"), A("msk")), reads=[sm["msk"]], writes=[sm["top8"]])
                    kb.op("dve", lambda E: E.max_index(idx8[:], A("top8"), A("msk")), reads=[sm["msk"], sm["top8"]], writes=[idx8])
                    kb.cp("dve", EID[:, t, :], idx8[:, 0:2], reads=[idx8], writes=[EID])
                    kb.ts("dve", A("nv2"), sm["top8"][:, 1:2], -1.0, OP.mult, reads=[sm["top8"]], writes=[sm["nv2"]])
                    kb.act(A("p1"), sm["top8"][:, 0:1], AF.Sigmoid, bias=A("nv2")[:, 0:1], reads=[sm["top8"], sm["nv2"]], writes=[sm["p1"]])
                    kb.ts("dve", A("p2"), A("p1"), -1.0, OP.mult, 1.0, OP.add, reads=[sm["p1"]], writes=[sm["p2"]])
                    kb.tt("dve", GW[:, t, 0:1], A("p1"), A("gw"), OP.mult, reads=[sm["p1"], sm["gw"]], writes=[GW])
                    kb.tt("dve", GW[:, t, 1:2], A("p2"), A("gw"), OP.mult, reads=[sm["p2"], sm["gw"]], writes=[GW])
                    for k in range(2):
                        kb.ts("dve", E2[:, k * 32:(k + 1) * 32], iota_e[:], EID[:, t, k:k + 1], OP.is_equal, reads=[iota_e, EID], writes=[E2])
                    kb.cp("act", E2b[:], E2[:], reads=[E2], writes=[E2b])
                    ppre = kb.bank[5]; pcnt = kb.bank[6]
                    kb.mm(ppre[:, 0:64], ltri[:], E2b[:], reads=[ltri, E2b], writes=[ppre])
                    kb.mm(pcnt[:, 0:64], ones[:], E2b[:], reads=[ones, E2b], writes=[pcnt])
                    kb.tt("dve", A("a0"), ppre[:, 0:32], base[:], OP.add, reads=[ppre, base], writes=[sm["a0"]])
                    kb.op("dve", lambda E: E.scalar_tensor_tensor(A("junk"), A("a0"), 1.0, E2[:, 0:32], OP.mult, OP.mult, accum_out=RANK[:, t, 0:1]),
                          reads=[sm["a0"], E2], writes=[sm["junk"], RANK])
                    kb.tt("dve", A("csum"), pcnt[:, 0:32], base[:], OP.add, reads=[pcnt, base], writes=[sm["csum"]])
                    kb.tt("dve", A("a0"), ppre[:, 32:64], A("csum"), OP.add, reads=[ppre, sm["csum"]], writes=[sm["a0"]])
                    kb.op("dve", lambda E: E.scalar_tensor_tensor(A("junk"), A("a0"), 1.0, E2[:, 32:64], OP.mult, OP.mult, accum_out=RANK[:, t, 1:2]),
                          reads=[sm["a0"], E2], writes=[sm["junk"], RANK])
                    kb.tt("dve", base[:], A("csum"), pcnt[:, 32:64], OP.add, reads=[sm["csum"], pcnt], writes=[base])
            pb_ = kb.sb(es2, [128, 32], F32, "padb"); pend = kb.sb(es2, [128, 32], F32, "pend"); pst = kb.sb(es2, [128, 32], F32, "pstart")
            one32 = kb.sb(es2, [128, 32], F32, "one32")
            kb.memset("dve", one32[:], 1.0, writes=[one32])
            kb.ts("dve", pb_[:], base[:], 1.0 / 128, OP.mult, (127.0 / 128 - 0.5 + 1.0 / 256), OP.add, reads=[base], writes=[pb_])
            kb.ts("dve", pb_[:], pb_[:], MAGIC, OP.add, reads=[pb_], writes=[pb_])
            kb.ts("dve", pb_[:], pb_[:], -MAGIC, OP.add, reads=[pb_], writes=[pb_])
            kb.op("dve", lambda E: E.tensor_tensor_scan(pend[:], one32[:], pb_[:], 0.0, OP.mult, OP.add), reads=[one32, pb_], writes=[pend])
            kb.tt("dve", pst[:], pend[:], pb_[:], OP.subtract, reads=[pend, pb_], writes=[pst])
            kb.ts("dve", pst[:], pst[:], 128.0, OP.mult, reads=[pst], writes=[pst])
            kb.memset("dve", BE[:], 0.0, writes=[BE])
            for e in range(32):
                kb.stt("dve", BE[:], iota_b[:], pend[:, e:e + 1], BE[:], OP.is_ge, OP.add, reads=[iota_b, pend, BE], writes=[BE])
            kb.ts("dve", BE[:], BE[:], 31.0, OP.min, float(L * 32), OP.add, reads=[BE], writes=[BE])
            kb.ts("dve", BE[:], BE[:], 128.0, OP.mult, iota_p[:, 0:1], OP.add, reads=[BE, iota_p], writes=[BE])
            kb.cp("dve", IDXW[:], BE[:], reads=[BE], writes=[IDXW])
            for t in tiles:
                for k in range(2):
                    kb.ts("dve", E2[:, 0:32], iota_e[:], EID[:, t, k:k + 1], OP.is_equal, reads=[iota_e, EID], writes=[E2])
                    kb.op("dve", lambda E: E.scalar_tensor_tensor(sm["junk"][:], E2[:, 0:32], 1.0, pst[:], OP.mult, OP.mult, accum_out=DEST[:, t, k:k + 1]),
                          reads=[E2, pst], writes=[sm["junk"], DEST])
            kb.tt("dve", DEST[:], DEST[:], RANK[:], OP.add, reads=[DEST, RANK], writes=[DEST])
            kb.cp("dve", DESTI[:], DEST[:], reads=[DEST], writes=[DESTI])
            for t in tiles:
                fb_ = fbt[t % 2]
                kb.dma(fb_[:], S["FB"].ap()[t * 128:(t + 1) * 128, :], reads=[("FB", t)], writes=[fb_])
                for k in range(2):
                    kb.dma(None, None, reads=[fb_, DESTI], writes=["XS"], q="pool",
                           fn=lambda E: E.indirect_dma_start(out=S["XS"].ap(), out_offset=bass.IndirectOffsetOnAxis(DESTI[:, t, k:k + 1], 0),
                                                             in_=fb_[:], in_offset=None))
            kb.barrier()
            es2.close()
            es4 = ExitStack()
            wstg = kb.sb(es4, [128, 3, 4096], F32, "wstg")
            wbb = [kb.sb(es4, [128, 3, 4096], BF16, "wbb%d" % i) for i in range(2)]
            xs = [kb.sb(es4, [128, DM], BF16, "xs%d" % i) for i in range(2)]
            XT = [kb.sb(es4, [128, 8, 128], BF16, "XT%d" % i) for i in range(2)]
            h1s = kb.sb(es4, [128, 512], F32, "h1s")
            aT = [kb.sb(es4, [128, 4, 128], BF16, "aT%d" % i) for i in range(2)]
            ysb = [kb.sb(es4, [128, DM], F32, "ysb%d" % i) for i in range(2)]
            for b in range(nblk):
                for wi, wn in enumerate(("moe_w1", "moe_w3", "moe_w2")):
                    kb.dma(None, None, reads=[IDXW], writes=[wstg], q="pool",
                           fn=lambda E: E.indirect_dma_start(out=wstg[:, wi, :], out_offset=None, in_=I[wn].ap(),
                                                             in_offset=bass.IndirectOffsetOnAxis(IDXW[:, b:b + 1], 0)))
                wb = wbb[b % 2]
                kb.cp("act", wb[:, 0, :], wstg[:, 0, :], reads=[wstg], writes=[wb])
                kb.cp("dve", wb[:, 1, :], wstg[:, 1, :], reads=[wstg], writes=[wb])
                kb.cp("pool", wb[:, 2, :], wstg[:, 2, :], reads=[wstg], writes=[wb])
                x_ = xs[b % 2]
                kb.dma(x_[:], S["XS"].ap()[b * 128:(b + 1) * 128, :], reads=["XS"], writes=[x_])
                pt_ = kb.bank[0]
                ptv = pt_[:].bitcast(BF16)
                for kc in range(8):
                    kb.tr(ptv[:, kc * 128:(kc + 1) * 128], x_[:, kc * 128:(kc + 1) * 128], ident[:], reads=[x_, ident], writes=[pt_])
                XT_ = XT[b % 2]
                kb.cp("dve", XT_[:], ptv.rearrange("p (k n) -> p k n", k=8), reads=[pt_], writes=[XT_])
                ph1 = kb.bank[1]; ph3 = kb.bank[2]
                for (pp, wi) in ((ph1, 0), (ph3, 1)):
                    for hc in range(4):
                        for kc in range(8):
                            kb.mm(pp[:, hc * 128:(hc + 1) * 128], wb[:, wi, kc * 512 + hc * 128:kc * 512 + (hc + 1) * 128], XT_[:, kc, :],
                                  start=(kc == 0), stop=(kc == 7), reads=[wb, XT_], writes=[pp])
                kb.act(h1s[:], ph1[:, :], AF.Silu, reads=[ph1], writes=[h1s])
                a_ = aT[b % 2]
                kb.tt("dve", a_[:].rearrange("p h r -> p (h r)"), h1s[:], ph3[:, :], OP.mult, reads=[h1s, ph3], writes=[a_])
                y_ = ysb[b % 2]
                for nh in range(2):
                    py = kb.bank[3 + nh]
                    for hc in range(4):
                        kb.mm(py[:, :], a_[:, hc, :], wb[:, 2, hc * 1024 + nh * 512:hc * 1024 + (nh + 1) * 512], start=(hc == 0), stop=(hc == 3),
                              reads=[a_, wb], writes=[py])
                    kb.cp("act" if nh else "dve", y_[:, nh * 512:(nh + 1) * 512], py[:, :], reads=[py], writes=[y_])
                kb.dma(S["YS"].ap()[b * 128:(b + 1) * 128, :], y_[:], reads=[y_], writes=["YS"])
            kb.barrier()
            es4.close()
            es5 = ExitStack()
            gate2 = kb.sb(es5, [128, DM], F32, "gate2")
            xt = [kb.sb(es5, [128, DM], F32, "xt%d" % i) for i in range(2)]
            y0 = [kb.sb(es5, [128, DM], F32, "y0%d" % i) for i in range(2)]
            y1 = [kb.sb(es5, [128, DM], F32, "y1%d" % i) for i in range(2)]
            xo = [kb.sb(es5, [128, DM], F32, "xo%d" % i) for i in range(2)]
            if last:
                fg = kb.sb(es5, [128, DM], F32, "fg")
                kb.dma(fg[:], I["final_g"].ap().partition_broadcast(128), writes=[fg])
                sq = kb.sb(es5, [128, DM], BF16, "sq"); ss = kb.sb(es5, [128, 1], F32, "ss")
            for t in tiles:
                which = 1 if t < 2 else 0
                if t in (0, 2):
                    kb.dma(gate2[:], S["MOD"].ap()[which:which + 1, 5 * DM:6 * DM].partition_broadcast(128), reads=["MOD"], writes=[gate2])
                x_ = xt[t % 2]; a_ = y0[t % 2]; b_ = y1[t % 2]; o_ = xo[t % 2]
                kb.dma(x_[:], X[t * 128:(t + 1) * 128, :], reads=[("X", t)], writes=[x_])
                for k, yy in enumerate((a_, b_)):
                    kb.dma(None, None, reads=[DESTI, "YS"], writes=[yy], q="pool",
                           fn=lambda E: E.indirect_dma_start(out=yy[:], out_offset=None, in_=S["YS"].ap(),
                                                             in_offset=bass.IndirectOffsetOnAxis(DESTI[:, t, k:k + 1], 0)))
                kb.ts("dve", a_[:], a_[:], GW[:, t, 0:1], OP.mult, reads=[a_, GW], writes=[a_])
                kb.stt("dve", a_[:], b_[:], GW[:, t, 1:2], a_[:], OP.mult, OP.add, reads=[b_, GW, a_], writes=[a_])
                if "YMOE" in self.debug:
                    kb.dma(S["YMOE"].ap()[t * 128:(t + 1) * 128, :], a_[:], reads=[a_], writes=["YMOE"], q="pool")
                kb.tt("pool", a_[:], a_[:], gate2[:], OP.mult, reads=[a_, gate2], writes=[a_])
                kb.tt("dve", o_[:], a_[:], x_[:], OP.add, reads=[a_, x_], writes=[o_])
                if not last:
                    kb.dma(X[t * 128:(t + 1) * 128, :], o_[:], reads=[o_], writes=[("X", t)], q="pool")
                else:
                    kb.act(sq[:], o_[:], AF.Square, accum_out=ss[:], reads=[o_], writes=[sq, ss])
                    kb.ts("dve", ss[:], ss[:], 1.0 / DM, OP.mult, EPS, OP.add, reads=[ss], writes=[ss])
                    kb.act(ss[:], ss[:], AF.Sqrt, reads=[ss], writes=[ss])
                    kb.op("dve", lambda E: E.reciprocal(ss[:], ss[:]), reads=[ss], writes=[ss])
                    kb.stt("dve", o_[:], o_[:], ss[:, 0:1], fg[:], OP.mult, OP.mult, reads=[o_, ss, fg], writes=[o_])
                    kb.dma(self.out.ap()[(t - 2) * 128:(t - 1) * 128, :], o_[:], reads=[o_], writes=["OUT"], q="pool")
            kb.barrier()
            es5.close()
        kb.barrier()

    def finish(self, out_keys):
        kb = self.kb
        kb._waits("sp", out_keys, ())
        kb.barrier()


def build(debug=(), layers=DEPTH, stages=("mod", "proj")):
    P = Prog(debug)
    P.declare()
    P.stage_init()
    P.kb.barrier()
    for L in range(layers):
        if "mod" in stages:
            P.stage_mod(L)
        if "proj" in stages:
            P.stage_proj(L)
        if "s5" in stages:
            P.stage_s5(L)
        if "mla" in stages:
            P.stage_mla(L, L < DEPTH - 1)
        if "hy" in stages:
            P.stage_hyena(L, "l")
            if L < DEPTH - 1:
                P.stage_hyena(L, "c")
        if "moe" in stages:
            P.stage_s5merge(L)
            P.stage_out_moe(L, L == DEPTH - 1)
    P.finish(["OUT"])
    return P


def kernel(**inputs):
    inp = {k: np.asarray(v) for k, v in inputs.items()}
    P = build(debug=(), layers=DEPTH, stages=("mod", "proj", "s5", "mla", "hy", "moe"))
    maps = prep_inputs(inp)
    names = set(P.I.keys())
    in_maps = [{k: v for k, v in m.items() if k in names} for m in maps]
    res = run_bass_kernel_spmd(P.nc, in_maps, core_ids=list(range(8)))
    return np.stack([np.asarray(r["out"], dtype=np.float32) for r in res.results], axis=0)
```

```python
import math
from contextlib import ExitStack
import numpy as np
import ml_dtypes
import concourse.bass as bass
import concourse.mybir as mybir
from concourse.bass_utils import run_bass_kernel_spmd

F32 = mybir.dt.float32
BF16 = mybir.dt.bfloat16
I32 = mybir.dt.int32
U32 = mybir.dt.uint32
AF = mybir.ActivationFunctionType
OP = mybir.AluOpType
AX = mybir.AxisListType

DM = 1024
NCTX = 256
NLAT = 4096
T = NCTX + NLAT
NT = T // 128
DEPTH = 4
EPS = 1e-6
P_IN = 1696
MAGIC = 12582912.0
MLA_SCALE = 1.0 / math.sqrt(96.0)
NE = 32
NBLK = 100


class Buf:
    __slots__ = ("w", "r", "name")

    def __init__(self, name=""):
        self.w = None
        self.r = {}
        self.name = name


class KB:
    NDMA = 10

    def __init__(self, needed=None):
        nc = bass.Bass("TRN2", target_bir_lowering=False)
        self.nc = nc
        self.needed = needed
        self.waited = {e: set() for e in ("pe", "act", "dve", "pool")}
        self.real = {e: 0 for e in ("pe", "act", "dve", "pool")}
        self.vmap = {e: {} for e in ("pe", "act", "dve", "pool")}
        self.eng = {"pe": nc.tensor, "act": nc.scalar, "dve": nc.vector, "pool": nc.gpsimd, "sp": nc.sync}
        self.sem = {}
        self.cnt = {}
        for e in ("pe", "act", "dve", "pool"):
            self.sem[e] = nc.alloc_semaphore("s_" + e)
            self.cnt[e] = 0
        self.known = {e: {} for e in self.eng}
        self.dq = {}
        for q in ("sp", "act", "pool"):
            sems = []
            for i in range(self.NDMA):
                k = ("d", q, i)
                self.sem[k] = nc.alloc_semaphore("d_%s_%d" % (q, i))
                self.cnt[k] = 0
                sems.append(k)
            self.dq[q] = [sems, 0]
        self.nalloc = 0
        self.bufs = {}
        self.ninst = 0
        self.bank = [nc.alloc_psum_tensor("bank%d" % i, [128, 512], F32) for i in range(8)]
        self.bnd_reg = nc.gpsimd.alloc_register("bnd")
        nc.gpsimd.reg_mov(self.bnd_reg, 2 * DEPTH * 32 * 128 - 1)

    def sb(self, es, shape, dt=F32, name="t"):
        self.nalloc += 1
        t = es.enter_context(self.nc.sbuf_tensor("%s_%d" % (name, self.nalloc), list(shape), dt))
        return t

    def dram(self, name, shape, dt=F32, kind="Internal"):
        return self.nc.dram_tensor(name, list(shape), dt, kind=kind)

    def B(self, key):
        if isinstance(key, Buf):
            return key
        k = key if isinstance(key, (str, tuple, int)) else id(key)
        b = self.bufs.get(k)
        if b is None:
            b = Buf(str(k))
            self.bufs[k] = b
        return b

    def _wait(self, e, k, v):
        if isinstance(k, str):
            self.waited[k].add(v)
            rv = v if self.needed is None else self.vmap[k][v]
        else:
            rv = v
        self.eng[e].wait_ge(self.sem[k], rv)
        self.known[e][k] = v

    def _waits(self, e, reads, writes):
        need = {}
        for b in reads:
            b = self.B(b)
            if b.w is not None:
                k, v = b.w
                if need.get(k, 0) < v:
                    need[k] = v
        for b in writes:
            b = self.B(b)
            if b.w is not None:
                k, v = b.w
                if need.get(k, 0) < v:
                    need[k] = v
            for k, v in b.r.items():
                if need.get(k, 0) < v:
                    need[k] = v
        kn = self.known[e]
        for k, v in need.items():
            if k == e and e == "pe":
                continue
            if kn.get(k, 0) >= v:
                continue
            self._wait(e, k, v)

    def _done(self, key, val, reads, writes):
        for b in writes:
            b = self.B(b)
            b.w = (key, val)
            b.r = {}
        for b in reads:
            b = self.B(b)
            if b.r.get(key, 0) < val:
                b.r[key] = val

    def op(self, e, fn, reads=(), writes=()):
        self._waits(e, reads, writes)
        inst = fn(self.eng[e])
        self.cnt[e] += 1
        v = self.cnt[e]
        if self.needed is None or v in self.needed[e]:
            self.real[e] += 1
            self.vmap[e][v] = self.real[e]
            inst.then_inc(self.sem[e], 1)
        self._done(e, v, reads, writes)
        self.ninst += 1
        return inst

    def dma(self, out, in_, reads=(), writes=(), q="sp", fn=None, **kw):
        sems, i = self.dq[q]
        k = sems[i % len(sems)]
        self.dq[q][1] = i + 1
        kn = self.known[q]
        if kn.get(k, 0) < self.cnt[k]:
            self._wait(q, k, self.cnt[k])
        self._waits(q, reads, writes)
        if fn is not None:
            inst = fn(self.eng[q])
        else:
            inst = self.eng[q].dma_start(out=out, in_=in_, **kw)
        self.cnt[k] += 16
        inst.then_inc(self.sem[k], 16)
        self._done(k, self.cnt[k], reads, writes)
        self.ninst += 1
        return inst

    def barrier(self):
        for e in self.eng:
            kn = self.known[e]
            for k, v in self.cnt.items():
                if v == 0 or kn.get(k, 0) >= v:
                    continue
                if k == e and e == "pe":
                    continue
                self._wait(e, k, v)
        self.bufs = {k: b for k, b in self.bufs.items() if isinstance(k, (str, tuple))}
        for b in self.bufs.values():
            b.w = None
            b.r = {}

    def mm(self, out, lhsT, rhs, start=True, stop=True, reads=(), writes=(), **kw):
        return self.op("pe", lambda E: E.matmul(out, lhsT, rhs, start=start, stop=stop, **kw), reads, writes)

    def tr(self, out, in_, ident, reads=(), writes=()):
        return self.op("pe", lambda E: E.transpose(out, in_, ident), reads, writes)

    def act(self, out, in_, func, reads=(), writes=(), **kw):
        return self.op("act", lambda E: E.activation(out, in_, func, **kw), reads, writes)

    def tt(self, e, out, in0, in1, op, reads=(), writes=()):
        return self.op(e, lambda E: E.tensor_tensor(out, in0, in1, op), reads, writes)

    def ts(self, e, out, in0, s1, op0, s2=None, op1=None, reads=(), writes=(), **kw):
        if op1 is None:
            return self.op(e, lambda E: E.tensor_scalar(out, in0, s1, None, op0, **kw), reads, writes)
        return self.op(e, lambda E: E.tensor_scalar(out, in0, s1, s2, op0, op1, **kw), reads, writes)

    def stt(self, e, out, in0, scalar, in1, op0, op1, reads=(), writes=()):
        return self.op(e, lambda E: E.scalar_tensor_tensor(out, in0, scalar, in1, op0, op1), reads, writes)

    def cp(self, e, out, in_, reads=(), writes=()):
        if e == "act":
            return self.op(e, lambda E: E.copy(out, in_), reads, writes)
        return self.op(e, lambda E: E.tensor_copy(out, in_), reads, writes)

    def memset(self, e, ap, val, writes=()):
        return self.op(e, lambda E: E.memset(ap, val), (), writes)


def _rope_tables():
    half = 16
    inv = 10000.0 ** (-np.arange(0, half, 2, dtype=np.float64) / half)
    i = np.arange(NLAT)
    row = (i // 64).astype(np.float64)
    col = (i % 64).astype(np.float64)
    ar = row[None, :] * inv[:, None]
    ac = col[None, :] * inv[:, None]
    cos = np.ones((32, T), np.float64)
    sin = np.zeros((32, T), np.float64)
    cos[0:8, NCTX:] = np.cos(ar); cos[8:16, NCTX:] = np.cos(ar); cos[16:24, NCTX:] = np.cos(ac); cos[24:32, NCTX:] = np.cos(ac)
    sin[0:8, NCTX:] = np.sin(ar); sin[8:16, NCTX:] = np.sin(ar); sin[16:24, NCTX:] = np.sin(ac); sin[24:32, NCTX:] = np.sin(ac)
    return cos.astype(np.float32), sin.astype(np.float32)


def _hy_tables(n):
    N2 = 2 * n
    ntc = n // 128
    F = n + 1
    nj = (F + 127) // 128
    t = np.arange(n, dtype=np.float64)
    tt_ = t / (n - 1)
    bands = 16
    f = np.linspace(1e-4, bands - 1, bands)
    ang = (2.0 * math.pi * t / n)[:, None] * f
    z = np.concatenate([tt_[:, None], np.cos(ang), -np.sin(ang)], axis=-1)
    lo, hi = math.log(1e-2) / 1.5, math.log(1e-2) / 0.3
    deltas = np.abs(np.linspace(lo, hi, 256))
    decay = np.exp(-tt_[None, :] * deltas[:, None])
    fi = np.arange(nj * 128, dtype=np.float64)
    valid = (fi <= n).astype(np.float64)
    ph = 2.0 * math.pi * np.outer(t, fi) / N2
    cosm = np.cos(ph) * valid[None, :]
    sinm = -np.sin(ph) * valid[None, :]
    def fwd_layout(m):
        return m.reshape(ntc, 128, nj, 128).transpose(2, 1, 0, 3)
    dfwd = np.concatenate([fwd_layout(cosm), fwd_layout(sinm)], axis=0).astype(ml_dtypes.bfloat16)
    wf = np.where((fi == 0) | (fi == n), 1.0, 2.0) * valid / N2
    icos = (np.cos(ph) * wf[None, :]).T
    isin = (-np.sin(ph) * wf[None, :]).T
    def inv_layout(m):
        return m.reshape(nj, 128, ntc, 128).transpose(2, 1, 0, 3)
    dinv = np.concatenate([inv_layout(icos), inv_layout(isin)], axis=2).astype(ml_dtypes.bfloat16)
    return (np.ascontiguousarray(z.T.astype(np.float32)), np.ascontiguousarray(decay.astype(np.float32)),
            np.ascontiguousarray(dfwd), np.ascontiguousarray(dinv))


def const_inputs():
    c = {}
    c["iota_e"] = np.ascontiguousarray(np.broadcast_to(np.arange(32, dtype=np.float32)[None, :], (128, 32)))
    c["iota_b"] = np.ascontiguousarray(np.broadcast_to(np.arange(128, dtype=np.float32)[None, :], (128, 128)))
    c["iota_p"] = np.arange(128, dtype=np.float32).reshape(128, 1)
    c["ltri"] = np.triu(np.ones((128, 128), np.float32), k=1).astype(ml_dtypes.bfloat16)
    for nm, n in (("l", NLAT), ("c", NCTX)):
        z, dec, dfwd, dinv = _hy_tables(n)
        c["hy_zpos_" + nm] = z
        c["hy_decay_" + nm] = dec
        c["hy_dfwd_" + nm] = dfwd
        c["hy_dinv_" + nm] = dinv
    c["ident"] = np.eye(128, dtype=np.float32)
    cos, sin = _rope_tables()
    c["rope_cos"] = cos
    c["rope_sin"] = sin
    return c


def prep_inputs(inp):
    cst = const_inputs()
    shared = dict(cst)
    for k in ("ada_w", "ada_b", "norm1_g", "norm2_g", "w_in"):
        shared[k] = np.ascontiguousarray(inp[k], dtype=np.float32)
    def lane(a):
        a = np.asarray(a, np.float32)
        Ld = a.shape[0]
        rest = a.shape[4:]
        a = a.reshape((Ld, 2, 8, 2, 64) + rest)
        nd = a.ndim
        a = a.transpose((0, 3, 4, 1, 2) + tuple(range(5, nd)))
        return np.ascontiguousarray(a.reshape((Ld, 128, 16) + rest))
    shared["s5_lre"] = lane(inp["s5_lambda_re"])
    shared["s5_lim"] = lane(inp["s5_lambda_im"])
    shared["s5_ldt"] = lane(np.broadcast_to(np.asarray(inp["s5_log_dt"], np.float32)[..., None], (DEPTH, 2, 16, 64)))
    shared["s5_bre"] = lane(inp["s5_b_re"])
    shared["s5_bim"] = lane(inp["s5_b_im"])
    shared["s5_cre"] = lane(np.asarray(inp["s5_c_re"], np.float32).transpose(0, 1, 2, 4, 3))
    shared["s5_cim"] = lane(np.asarray(inp["s5_c_im"], np.float32).transpose(0, 1, 2, 4, 3))
    shared["s5_dcol"] = np.ascontiguousarray(np.asarray(inp["s5_d"], np.float32).reshape(DEPTH, 2, 128).transpose(0, 2, 1))
    shared["s5_glu_w"] = np.ascontiguousarray(inp["s5_glu_w"], dtype=np.float32)
    shared["mla_qg"] = np.ascontiguousarray(np.asarray(inp["mla_q_norm_g"], np.float32).reshape(DEPTH, 3, 128).transpose(0, 2, 1))
    shared["mla_kvg"] = np.ascontiguousarray(np.asarray(inp["mla_kv_norm_g"], np.float32).reshape(DEPTH, 2, 128).transpose(0, 2, 1))
    shared["mla_w_uq"] = np.ascontiguousarray(inp["mla_w_uq"], dtype=np.float32)
    shared["mla_w_ukv"] = np.ascontiguousarray(inp["mla_w_ukv"], dtype=np.float32)
    shared["mix_norm_g"] = np.ascontiguousarray(inp["mix_norm_g"], dtype=np.float32)
    shared["hy_sw"] = np.ascontiguousarray(np.asarray(inp["hy_short_w"], np.float32).reshape(DEPTH, 3, 6, 128).transpose(0, 3, 2, 1))
    shared["hy_sb"] = np.ascontiguousarray(np.asarray(inp["hy_short_b"], np.float32).reshape(DEPTH, 6, 128).transpose(0, 2, 1))
    shared["hy_cols"] = np.ascontiguousarray(np.stack([inp["hy_f_b1"], inp["hy_f_b2"], inp["hy_f_freq"][:, 0], inp["hy_f_freq"][:, 1]], axis=-1), dtype=np.float32)
    shared["hy_b3"] = np.ascontiguousarray(np.asarray(inp["hy_f_b3"], np.float32).reshape(DEPTH, 8, 128).transpose(0, 2, 1))
    for k in ("hy_f_w1", "hy_f_w2", "hy_f_w3", "hy_bias"):
        shared[k] = np.ascontiguousarray(inp[k], dtype=np.float32)
    shared["mix_g_s5col"] = np.ascontiguousarray(np.asarray(inp["mix_norm_g"], np.float32)[:, 0:256].reshape(DEPTH, 2, 128).transpose(0, 2, 1))
    shared["w_out"] = np.ascontiguousarray(inp["w_out"], dtype=np.float32)
    shared["moe_wr"] = np.ascontiguousarray(np.concatenate([inp["moe_w_group"], inp["moe_w_expert"]], axis=-1), dtype=np.float32)
    shared["moe_w1"] = np.ascontiguousarray(np.asarray(inp["moe_w1"], np.float32).reshape(DEPTH, 32, 8, 128, 512).transpose(0, 1, 3, 2, 4)).reshape(DEPTH * 32 * 128, 4096)
    shared["moe_w3"] = np.ascontiguousarray(np.asarray(inp["moe_w3"], np.float32).reshape(DEPTH, 32, 8, 128, 512).transpose(0, 1, 3, 2, 4)).reshape(DEPTH * 32 * 128, 4096)
    shared["moe_w2"] = np.ascontiguousarray(np.asarray(inp["moe_w2"], np.float32).reshape(DEPTH, 32, 4, 128, 1024).transpose(0, 1, 3, 2, 4)).reshape(DEPTH * 32 * 128, 4096)
    shared["final_g"] = np.ascontiguousarray(np.asarray(inp["final_g"], np.float32).reshape(1, DM))
    maps = []
    for b in range(8):
        m = dict(shared)
        m["x"] = np.ascontiguousarray(inp["x"][b])
        m["ctx"] = np.ascontiguousarray(inp["ctx"][b])
        cc = np.stack([inp["c"][b], inp["c_ctx"]], axis=0)
        m["cc"] = np.ascontiguousarray(cc.reshape(2, 8, 128).transpose(2, 1, 0))
        maps.append(m)
    return maps


class Prog:
    def __init__(self, debug=(), needed=None):
        self.kb = KB(needed)
        self.nc = self.kb.nc
        self.debug = set(debug)
        self.I = {}
        self.S = {}
        self.outs = []

    def inp(self, name, shape, dt=F32):
        t = self.nc.dram_tensor(name, list(shape), dt, kind="ExternalInput")
        self.I[name] = t
        return t

    def scratch(self, name, shape, dt=F32):
        kind = "ExternalOutput" if name in self.debug else "Internal"
        t = self.nc.dram_tensor(name, list(shape), dt, kind=kind)
        self.S[name] = t
        if kind == "ExternalOutput":
            self.outs.append(name)
        return t

    def declare(self):
        self.inp("x", [NLAT, DM]); self.inp("ctx", [NCTX, DM]); self.inp("cc", [128, 8, 2])
        self.inp("ada_w", [DEPTH, DM, 6 * DM]); self.inp("ada_b", [DEPTH, 6 * DM])
        self.inp("norm1_g", [DEPTH, DM]); self.inp("norm2_g", [DEPTH, DM])
        self.inp("w_in", [DEPTH, DM, P_IN])
        self.inp("ident", [128, 128]); self.inp("rope_cos", [32, T]); self.inp("rope_sin", [32, T])
        for nm in ("s5_lre", "s5_lim", "s5_ldt"):
            self.inp(nm, [DEPTH, 128, 16])
        for nm in ("s5_bre", "s5_bim", "s5_cre", "s5_cim"):
            self.inp(nm, [DEPTH, 128, 16, 16])
        self.inp("s5_dcol", [DEPTH, 128, 2]); self.inp("s5_glu_w", [DEPTH, 256, 256])
        self.inp("mla_qg", [DEPTH, 128, 3]); self.inp("mla_kvg", [DEPTH, 128, 2])
        self.inp("mla_w_uq", [DEPTH, 384, 768]); self.inp("mla_w_ukv", [DEPTH, 256, 1024])
        self.inp("mix_norm_g", [DEPTH, DM])
        self.scratch("MT", [DM, T], BF16)
        if "MLAO" in self.debug:
            self.scratch("MLAO", [T, 512])
        for nm, n in (("l", NLAT), ("c", NCTX)):
            nj = (n + 1 + 127) // 128
            self.inp("hy_zpos_" + nm, [33, n]); self.inp("hy_decay_" + nm, [256, n])
            self.inp("hy_dfwd_" + nm, [2 * nj, 128, n // 128, 128], BF16); self.inp("hy_dinv_" + nm, [n // 128, 128, 2 * nj, 128], BF16)
            self.scratch("KSPEC_" + nm, [nj, 2, 128, 512])
        self.inp("hy_sw", [DEPTH, 128, 6, 3]); self.inp("hy_sb", [DEPTH, 128, 6]); self.inp("hy_cols", [DEPTH, 64, 4]); self.inp("hy_b3", [DEPTH, 128, 8])
        self.inp("hy_f_w1", [DEPTH, 33, 64]); self.inp("hy_f_w2", [DEPTH, 64, 64]); self.inp("hy_f_w3", [DEPTH, 64, 1024]); self.inp("hy_bias", [DEPTH, 2, 256])
        self.scratch("HZTM", [T, 768]); self.scratch("HY1", [T, 256])
        if "HYO" in self.debug:
            self.scratch("HYO", [T, 256])
        self.inp("iota_e", [128, 32]); self.inp("iota_b", [128, 128]); self.inp("iota_p", [128, 1]); self.inp("ltri", [128, 128], BF16)
        self.inp("mix_g_s5col", [DEPTH, 128, 2])
        self.inp("w_out", [DEPTH, DM, DM]); self.inp("moe_wr", [DEPTH, DM, 36])
        for nm_ in ("moe_w1", "moe_w3", "moe_w2"):
            self.inp(nm_, [DEPTH * 32 * 128, 4096])
        self.inp("final_g", [1, DM])
        self.scratch("FB", [T, DM], BF16); self.scratch("XS", [NBLK * 128, DM], BF16); self.scratch("YS", [NBLK * 128, DM])
        if "XMID" in self.debug:
            self.scratch("XMID", [T, DM])
        if "YMOE" in self.debug:
            self.scratch("YMOE", [T, DM])
        self.out = self.nc.dram_tensor("out", [NLAT, DM], F32, kind="ExternalOutput")
        self.scratch("S5T", [256, T])
        self.scratch("X", [T, DM])
        self.scratch("MOD", [2, 6 * DM])
        self.scratch("UT", [256, T]); self.scratch("CQT", [384, T]); self.scratch("CKVT", [256, T])
        self.scratch("KRT", [32, T]); self.scratch("HZT", [768, T])
        if "HL" in self.debug:
            self.scratch("HL", [T, DM])

    def stage_init(self):
        kb = self.kb
        X = self.S["X"]
        kb.dma(X.ap()[0:NCTX, :], self.I["ctx"].ap(), writes=[("X", 0), ("X", 1)])
        for j in range(4):
            kb.dma(X.ap()[NCTX + j * 1024: NCTX + (j + 1) * 1024, :], self.I["x"].ap()[j * 1024:(j + 1) * 1024, :],
                   writes=[("X", 2 + 8 * j + i) for i in range(8)])

    def stage_mod(self, L):
        kb = self.kb
        with ExitStack() as es:
            cc = kb.sb(es, [128, 8, 2], F32, "cc")
            act = kb.sb(es, [128, 8, 2], F32, "act")
            bb = kb.sb(es, [2, 6 * DM], F32, "adab")
            mod = kb.sb(es, [2, 6 * DM], F32, "mod")
            wt = [kb.sb(es, [128, 8, 512], F32, "adaw%d" % i) for i in range(2)]
            kb.dma(cc[:], self.I["cc"].ap(), writes=[cc])
            kb.dma(bb[:], self.I["ada_b"].ap()[L:L + 1, :].partition_broadcast(2), writes=[bb])
            kb.act(act[:], cc[:], AF.Silu, reads=[cc], writes=[act])
            wv = self.I["ada_w"].ap()[L].rearrange("(kc p) n -> p kc n", p=128)
            for nb in range(12):
                w = wt[nb % 2]
                kb.dma(w[:], wv[:, :, nb * 512:(nb + 1) * 512], writes=[w])
                ps = kb.bank[nb % 2]
                for kc in range(8):
                    kb.mm(ps[0:2, :], act[:, kc, :], w[:, kc, :], start=(kc == 0), stop=(kc == 7),
                          reads=[act, w], writes=[ps])
                kb.tt("dve", mod[:, nb * 512:(nb + 1) * 512], ps[0:2, :], bb[:, nb * 512:(nb + 1) * 512], OP.add,
                      reads=[ps, bb], writes=[mod])
            kb.dma(self.S["MOD"].ap(), mod[:], reads=[mod], writes=["MOD"])
        kb.barrier()

    def load_mod_bc(self, es_tiles, L, which, idx_g, g_name):
        kb = self.kb
        G, S, tmp = es_tiles
        MOD = self.S["MOD"].ap()
        kb.dma(S[:], MOD[which:which + 1, idx_g * DM:(idx_g + 1) * DM].partition_broadcast(128), reads=["MOD"], writes=[S])
        kb.dma(tmp[:], MOD[which:which + 1, (idx_g + 1) * DM:(idx_g + 2) * DM].partition_broadcast(128), reads=["MOD"], writes=[tmp])
        kb.dma(G[:], self.I[g_name].ap()[L:L + 1, :].partition_broadcast(128), writes=[G])
        kb.stt("dve", G[:], tmp[:], 1.0, G[:], OP.add, OP.mult, reads=[tmp, G], writes=[G])

    def rms_modulate(self, xt, G, S, sq, ss, rstd, tmp, out, out2=None):
        kb = self.kb
        kb.act(sq[:], xt[:], AF.Square, accum_out=ss[:], reads=[xt], writes=[sq, ss])
        kb.ts("dve", rstd[:], ss[:], 1.0 / DM, OP.mult, EPS, OP.add, reads=[ss], writes=[rstd])
        kb.act(rstd[:], rstd[:], AF.Sqrt, reads=[rstd], writes=[rstd])
        kb.op("dve", lambda E: E.reciprocal(rstd[:], rstd[:]), reads=[rstd], writes=[rstd])
        kb.stt("dve", tmp[:], xt[:], rstd[:, 0:1], G[:], OP.mult, OP.mult, reads=[xt, rstd, G], writes=[tmp])
        kb.tt("pool", out[:], tmp[:], S[:], OP.add, reads=[tmp, S], writes=[out])
        if out2 is not None:
            kb.cp("act", out2[:], out[:], reads=[out], writes=[out2])

    def stage_proj(self, L):
        kb = self.kb
        X = self.S["X"].ap()
        with ExitStack() as es:
            ident_f = kb.sb(es, [128, 128], F32, "identf")
            ident = kb.sb(es, [128, 128], BF16, "identb")
            kb.dma(ident_f[:], self.I["ident"].ap(), writes=[ident_f])
            kb.cp("dve", ident[:], ident_f[:], reads=[ident_f], writes=[ident])
            wf = kb.sb(es, [128, 8, P_IN], F32, "winf")
            wb = kb.sb(es, [128, 8, P_IN + 32], BF16, "winb")
            kb.dma(wf[:], self.I["w_in"].ap()[L].rearrange("(kc p) n -> p kc n", p=128), writes=[wf])
            kb.cp("act", wb[:, :, 0:P_IN], wf[:], reads=[wf], writes=[wb])
            K0 = 896
            kb.ts("dve", wb[:, :, P_IN + 0:P_IN + 8], wf[:, :, K0 + 8:K0 + 16], -1.0, OP.mult, reads=[wf], writes=[wb])
            kb.cp("dve", wb[:, :, P_IN + 8:P_IN + 16], wf[:, :, K0 + 0:K0 + 8], reads=[wf], writes=[wb])
            kb.ts("dve", wb[:, :, P_IN + 16:P_IN + 24], wf[:, :, K0 + 24:K0 + 32], -1.0, OP.mult, reads=[wf], writes=[wb])
            kb.cp("dve", wb[:, :, P_IN + 24:P_IN + 32], wf[:, :, K0 + 16:K0 + 24], reads=[wf], writes=[wb])
            rc = kb.sb(es, [32, T], F32, "ropec")
            rs = kb.sb(es, [32, T], F32, "ropes")
            kb.dma(rc[:], self.I["rope_cos"].ap(), writes=[rc])
            kb.dma(rs[:], self.I["rope_sin"].ap(), writes=[rs])
            G = kb.sb(es, [128, DM], F32, "G"); S = kb.sb(es, [128, DM], F32, "S"); tmpb = kb.sb(es, [128, DM], F32, "tmpb")
            xt = [kb.sb(es, [128, DM], F32, "xt%d" % i) for i in range(2)]
            sq = kb.sb(es, [128, DM], BF16, "sq")
            ss = kb.sb(es, [128, 1], F32, "ss"); rstd = kb.sb(es, [128, 1], F32, "rstd")
            tmp = kb.sb(es, [128, DM], F32, "tmp")
            hb = [kb.sb(es, [128, DM], BF16, "hb%d" % i) for i in range(2)]
            hf = kb.sb(es, [128, DM], F32, "hf") if "HL" in self.debug else None
            hT = [kb.sb(es, [128, 8, 512], BF16, "hT%d" % i) for i in range(2)]
            stg = [kb.sb(es, [128, 512], F32, "stg%d" % i) for i in range(3)]
            kr1 = kb.sb(es, [32, 512], F32, "kr1")
            nstg = 0
            chunks = []
            for i in range(2):
                chunks.append(("UT", i * 128, i * 128, 128))
            for i in range(3):
                chunks.append(("CQT", i * 128, 256 + i * 128, 128))
            for i in range(2):
                chunks.append(("CKVT", i * 128, 640 + i * 128, 128))
            for i in range(6):
                chunks.append(("HZT", i * 128, 928 + i * 128, 128))
            blocks = [(0, 2)] + [(2 + 4 * i, 4) for i in range(8)]
            nmm = 0
            for bi, (t0, ntl) in enumerate(blocks):
                if bi == 0:
                    self.load_mod_bc((G, S, tmpb), L, 1, 0, "norm1_g")
                elif bi == 1:
                    self.load_mod_bc((G, S, tmpb), L, 0, 0, "norm1_g")
                hTb = hT[bi % 2]
                nb = ntl * 128
                c0 = t0 * 128
                for j in range(ntl):
                    t = t0 + j
                    x_ = xt[t % 2]
                    kb.dma(x_[:], X[t * 128:(t + 1) * 128, :], reads=[("X", t)], writes=[x_])
                    h_ = hb[t % 2]
                    if hf is not None:
                        self.rms_modulate(x_, G, S, sq, ss, rstd, tmp, hf, h_)
                        kb.dma(self.S["HL"].ap()[t * 128:(t + 1) * 128, :], hf[:], reads=[hf], writes=["HL"])
                    else:
                        self.rms_modulate(x_, G, S, sq, ss, rstd, tmp, h_)
                    pb = kb.bank[2 + (t % 2)]
                    pbv = pb[:].bitcast(BF16)
                    for kc in range(8):
                        kb.tr(pbv[:, kc * 128:(kc + 1) * 128], h_[:, kc * 128:(kc + 1) * 128], ident[:],
                              reads=[h_, ident], writes=[pb])
                    kb.cp("dve" if t % 2 else "act", hTb[:, :, j * 128:(j + 1) * 128],
                          pbv.rearrange("p (k n) -> p k n", k=8), reads=[pb], writes=[hTb])
                for (dn, r0, col0, M) in chunks:
                    ps = kb.bank[4 + (nmm % 2)]
                    nmm += 1
                    for kc in range(8):
                        kb.mm(ps[0:M, 0:nb], wb[:, kc, col0:col0 + M], hTb[:, kc, 0:nb], start=(kc == 0), stop=(kc == 7),
                              reads=[wb, hTb], writes=[ps])
                    st = stg[nstg % 3]
                    nstg += 1
                    kb.cp("act" if nstg % 2 else "dve", st[0:M, 0:nb], ps[0:M, 0:nb], reads=[ps], writes=[st])
                    kb.dma(self.S[dn].ap()[r0:r0 + M, c0:c0 + nb], st[0:M, 0:nb], reads=[st], writes=[(dn, bi)], q="pool")
                ps = kb.bank[6]
                ps2 = kb.bank[7]
                for kc in range(8):
                    kb.mm(ps[0:32, 0:nb], wb[:, kc, 896:928], hTb[:, kc, 0:nb], start=(kc == 0), stop=(kc == 7),
                          reads=[wb, hTb], writes=[ps])
                for kc in range(8):
                    kb.mm(ps2[0:32, 0:nb], wb[:, kc, P_IN:P_IN + 32], hTb[:, kc, 0:nb], start=(kc == 0), stop=(kc == 7),
                          reads=[wb, hTb], writes=[ps2])
                st = stg[nstg % 3]
                nstg += 1
                kb.tt("dve", kr1[:, 0:nb], ps[0:32, 0:nb], rc[:, c0:c0 + nb], OP.mult, reads=[ps, rc], writes=[kr1])
                kb.tt("dve", st[0:32, 0:nb], ps2[0:32, 0:nb], rs[:, c0:c0 + nb], OP.mult, reads=[ps2, rs], writes=[st])
                kb.tt("dve", st[0:32, 0:nb], st[0:32, 0:nb], kr1[:, 0:nb], OP.add, reads=[st, kr1], writes=[st])
                kb.dma(self.S["KRT"].ap()[:, c0:c0 + nb], st[0:32, 0:nb], reads=[st], writes=[("KRT", bi)], q="pool")
        kb.barrier()


    def sin_rr(self, e, out, in_, add, t1, t2):
        kb = self.kb
        kb.ts(e, t1, in_, add, OP.add, reads=[in_.tensor], writes=[t1.tensor])
        kb.ts(e, t2, t1, 1.0 / (2 * math.pi), OP.mult, MAGIC, OP.add, reads=[t1.tensor], writes=[t2.tensor])
        kb.ts(e, t2, t2, -MAGIC, OP.add, -2 * math.pi, OP.mult, reads=[t2.tensor], writes=[t2.tensor])
        kb.tt(e, t1, t1, t2, OP.add, reads=[t1.tensor, t2.tensor], writes=[t1.tensor])
        kb.ts(e, t1, t1, math.pi, OP.min, -math.pi, OP.max, reads=[t1.tensor], writes=[t1.tensor])
        kb.act(out, t1, AF.Sin, reads=[t1.tensor], writes=[out.tensor])

    def stage_s5(self, L):
        kb = self.kb
        I = self.I
        blocks = [(0, 256)] + [(256 + 512 * i, 512) for i in range(8)]
        with ExitStack() as es:
            ident_f = kb.sb(es, [128, 128], F32, "identf")
            kb.dma(ident_f[:], I["ident"].ap(), writes=[ident_f])
            sm = {}
            for nm in ("lre", "lim", "ldt"):
                sm[nm] = kb.sb(es, [128, 16], F32, nm)
                kb.dma(sm[nm][:], I["s5_" + nm].ap()[L], writes=[sm[nm]])
            for nm in ("dt", "a", "th", "r", "cs", "sn", "t1", "t2", "rc", "rsn", "den", "cre", "cim", "x1", "x2"):
                sm[nm] = kb.sb(es, [128, 16], F32, nm)
            big = {}
            for nm in ("bre", "bim", "cre", "cim"):
                big[nm] = kb.sb(es, [128, 16, 16], F32, "s5" + nm)
                kb.dma(big[nm][:], I["s5_" + nm].ap()[L], writes=[big[nm]])
            A = lambda nm: sm[nm][:]
            kb.act(A("dt"), A("ldt"), AF.Exp, reads=[sm["ldt"]], writes=[sm["dt"]])
            kb.tt("dve", A("a"), A("lre"), A("dt"), OP.mult, reads=[sm["lre"], sm["dt"]], writes=[sm["a"]])
            kb.tt("dve", A("th"), A("lim"), A("dt"), OP.mult, reads=[sm["lim"], sm["dt"]], writes=[sm["th"]])
            kb.act(A("r"), A("a"), AF.Exp, reads=[sm["a"]], writes=[sm["r"]])
            self.sin_rr("dve", A("sn"), A("th"), 0.0, A("t1"), A("t2"))
            self.sin_rr("dve", A("cs"), A("th"), math.pi / 2, A("t1"), A("t2"))
            kb.tt("dve", A("rc"), A("r"), A("cs"), OP.mult, reads=[sm["r"], sm["cs"]], writes=[sm["rc"]])
            kb.tt("dve", A("rsn"), A("r"), A("sn"), OP.mult, reads=[sm["r"], sm["sn"]], writes=[sm["rsn"]])
            kb.ts("dve", A("rc"), A("rc"), -1.0, OP.add, reads=[sm["rc"]], writes=[sm["rc"]])
            kb.tt("dve", A("den"), A("lre"), A("lre"), OP.mult, reads=[sm["lre"]], writes=[sm["den"]])
            kb.tt("dve", A("x1"), A("lim"), A("lim"), OP.mult, reads=[sm["lim"]], writes=[sm["x1"]])
            kb.tt("dve", A("den"), A("den"), A("x1"), OP.add, reads=[sm["den"], sm["x1"]], writes=[sm["den"]])
            kb.op("dve", lambda E: E.reciprocal(A("den"), A("den")), reads=[sm["den"]], writes=[sm["den"]])
            kb.tt("dve", A("x1"), A("rc"), A("lre"), OP.mult, reads=[sm["rc"], sm["lre"]], writes=[sm["x1"]])
            kb.tt("dve", A("x2"), A("rsn"), A("lim"), OP.mult, reads=[sm["rsn"], sm["lim"]], writes=[sm["x2"]])
            kb.tt("dve", A("x1"), A("x1"), A("x2"), OP.add, reads=[sm["x1"], sm["x2"]], writes=[sm["x1"]])
            kb.tt("dve", A("cre"), A("x1"), A("den"), OP.mult, reads=[sm["x1"], sm["den"]], writes=[sm["cre"]])
            kb.tt("dve", A("x1"), A("rsn"), A("lre"), OP.mult, reads=[sm["rsn"], sm["lre"]], writes=[sm["x1"]])
            kb.tt("dve", A("x2"), A("rc"), A("lim"), OP.mult, reads=[sm["rc"], sm["lim"]], writes=[sm["x2"]])
            kb.tt("dve", A("x1"), A("x1"), A("x2"), OP.subtract, reads=[sm["x1"], sm["x2"]], writes=[sm["x1"]])
            kb.tt("dve", A("cim"), A("x1"), A("den"), OP.mult, reads=[sm["x1"], sm["den"]], writes=[sm["cim"]])
            ub = kb.sb(es, [128, 2, T], BF16, "ub")
            yacc = kb.sb(es, [128, 2, T], F32, "yacc")
            Er = kb.sb(es, [128, T], F32, "Er"); Ei = kb.sb(es, [128, T], F32, "Ei")
            for c, stg_ in enumerate((Er, Ei)):
                kb.dma(stg_[:], self.S["UT"].ap()[c * 128:(c + 1) * 128, :], reads=[("UT", i) for i in range(9)], writes=[stg_])
                kb.cp("act", ub[:, c, :], stg_[:], reads=[stg_], writes=[ub])
            Ebr = kb.sb(es, [128, T], BF16, "Ebr"); Ebi = kb.sb(es, [128, T], BF16, "Ebi")
            bur = kb.sb(es, [128, T], BF16, "bur"); bui = kb.sb(es, [128, T], BF16, "bui")
            vr = kb.sb(es, [128, T], BF16, "vr"); vi = kb.sb(es, [128, T], BF16, "vi")
            m1 = kb.sb(es, [128, T], BF16, "m1"); m2 = kb.sb(es, [128, T], BF16, "m2")
            ZB = [kb.sb(es, [128, 128], F32, "ZB%d" % i) for i in range(2)]
            LB = [kb.sb(es, [128, 128], BF16, "LB%d" % i) for i in range(2)]
            LC = [kb.sb(es, [128, 128], BF16, "LC%d" % i) for i in range(2)]
            bt = kb.sb(es, [128, 16], F32, "bt")
            wr = kb.sb(es, [128, 1], F32, "wr"); wi = kb.sb(es, [128, 1], F32, "wi"); wt = kb.sb(es, [128, 1], F32, "wt")
            etmp = kb.sb(es, [128, 2048], F32, "etmp"); etmp2 = kb.sb(es, [128, 2048], F32, "etmp2")
            first_in_chunk = {0: True, 1: True}
            for lt in range(16):
                d, gp = lt // 8, lt % 8
                ch = gp // 4
                c0 = 32 * (gp % 4)
                lsl = slice(lt, lt + 1)
                for ri, (nm_a, nm_b, op2) in enumerate((("bre", "bim", OP.subtract), ("bim", "bre", OP.add))):
                    Z = ZB[ri]
                    kb.memset("pool", Z[:], 0.0, writes=[Z])
                    kb.ts("dve", bt[:], big[nm_b][:, lt, :], sm["cim"][:, lsl], OP.mult, reads=[big[nm_b], sm["cim"]], writes=[bt])
                    for gl in range(2):
                        ps_ = slice(gl * 64, gl * 64 + 64)
                        kb.stt("dve", Z[ps_, c0 + gl * 16:c0 + gl * 16 + 16], big[nm_a][ps_, lt, :], sm["cre"][ps_, lsl], bt[ps_, :],
                               OP.mult, op2, reads=[big[nm_a], sm["cre"], bt], writes=[Z])
                    pb = kb.bank[6]
                    kb.tr(pb[:, 0:128], Z[:], ident_f[:], reads=[Z, ident_f], writes=[pb])
                    kb.cp("act", LB[ri][:], pb[:, 0:128], reads=[pb], writes=[LB[ri]])
                for ri, nm in enumerate(("cre", "cim")):
                    kb.memset("pool", LC[ri][:], 0.0, writes=[LC[ri]])
                    for gl in range(2):
                        ps_ = slice(gl * 64, gl * 64 + 64)
                        kb.ts("dve", LC[ri][ps_, c0 + gl * 16:c0 + gl * 16 + 16], big[nm][ps_, lt, :], (1.0 if ri == 0 else -1.0), OP.mult,
                              reads=[big[nm]], writes=[LC[ri]])
                kb.memset("pool", Er[:, 0:1], 1.0, writes=[Er])
                kb.memset("pool", Ei[:, 0:1], 0.0, writes=[Ei])
                kb.cp("pool", Er[:, 1:2], sm["cs"][:, lsl], reads=[sm["cs"]], writes=[Er])
                kb.cp("pool", Ei[:, 1:2], sm["sn"][:, lsl], reads=[sm["sn"]], writes=[Ei])
                kb.cp("pool", wr[:], sm["cs"][:, lsl], reads=[sm["cs"]], writes=[wr])
                kb.cp("pool", wi[:], sm["sn"][:, lsl], reads=[sm["sn"]], writes=[wi])
                n = 2
                while n < T:
                    m = min(n, T - n)
                    kb.tt("pool", wt[:], wi[:], wi[:], OP.mult, reads=[wi], writes=[wt])
                    kb.tt("pool", wi[:], wr[:], wi[:], OP.mult, reads=[wr, wi], writes=[wi])
                    kb.ts("pool", wi[:], wi[:], 2.0, OP.mult, reads=[wi], writes=[wi])
                    kb.tt("pool", wr[:], wr[:], wr[:], OP.mult, reads=[wr], writes=[wr])
                    kb.tt("pool", wr[:], wr[:], wt[:], OP.subtract, reads=[wr, wt], writes=[wr])
                    for o in range(0, m, 2048):
                        mm_ = min(2048, m - o)
                        kb.act(etmp[:, 0:mm_], Ei[:, o:o + mm_], AF.Copy, scale=wi[:, 0:1], reads=[Ei, wi], writes=[etmp])
                        kb.act(etmp2[:, 0:mm_], Er[:, o:o + mm_], AF.Copy, scale=wr[:, 0:1], reads=[Er, wr], writes=[etmp2])
                        kb.tt("pool", Er[:, n + o:n + o + mm_], etmp2[:, 0:mm_], etmp[:, 0:mm_], OP.subtract, reads=[etmp, etmp2, Er], writes=[Er])
                        kb.act(etmp[:, 0:mm_], Ei[:, o:o + mm_], AF.Copy, scale=wr[:, 0:1], reads=[Ei, wr], writes=[etmp])
                        kb.act(etmp2[:, 0:mm_], Er[:, o:o + mm_], AF.Copy, scale=wi[:, 0:1], reads=[Er, wi], writes=[etmp2])
                        kb.tt("pool", Ei[:, n + o:n + o + mm_], etmp2[:, 0:mm_], etmp[:, 0:mm_], OP.add, reads=[etmp, etmp2, Ei], writes=[Ei])
                    n *= 2
                if d == 0:
                    kb.cp("act", Ebr[:], Er[:], reads=[Er], writes=[Ebr])
                    kb.cp("act", Ebi[:], Ei[:], reads=[Ei], writes=[Ebi])
                else:
                    for (Eb, E_) in ((Ebr, Er), (Ebi, Ei)):
                        kb.cp("act", Eb[:, 0:NCTX], E_[:, 0:NCTX][:, ::-1], reads=[E_], writes=[Eb])
                        kb.cp("act", Eb[:, NCTX:T], E_[:, NCTX:T][:, ::-1], reads=[E_], writes=[Eb])
                for bi, (s0, nn) in enumerate(blocks):
                    pr = kb.bank[(bi % 2) * 2]
                    pi_ = kb.bank[(bi % 2) * 2 + 1]
                    kb.mm(pr[:, 0:nn], LB[0][:], ub[:, ch, s0:s0 + nn], reads=[LB[0], ub], writes=[pr])
                    kb.mm(pi_[:, 0:nn], LB[1][:], ub[:, ch, s0:s0 + nn], reads=[LB[1], ub], writes=[pi_])
                    kb.cp("act", bur[:, s0:s0 + nn], pr[:, 0:nn], reads=[pr], writes=[bur])
                    kb.cp("act", bui[:, s0:s0 + nn], pi_[:, 0:nn], reads=[pi_], writes=[bui])
                kb.tt("dve", m1[:], Ebr[:], bur[:], OP.mult, reads=[Ebr, bur], writes=[m1])
                kb.tt("dve", m2[:], Ebi[:], bui[:], OP.mult, reads=[Ebi, bui], writes=[m2])
                kb.tt("dve", vr[:], m1[:], m2[:], OP.add, reads=[m1, m2], writes=[vr])
                kb.tt("dve", m1[:], Ebr[:], bui[:], OP.mult, reads=[Ebr, bui], writes=[m1])
                kb.tt("dve", m2[:], Ebi[:], bur[:], OP.mult, reads=[Ebi, bur], writes=[m2])
                kb.tt("dve", vi[:], m1[:], m2[:], OP.subtract, reads=[m1, m2], writes=[vi])
                rdec = sm["r"][:, lsl]
                for (v_, g_) in ((vr, bur), (vi, bui)):
                    if d == 0:
                        kb.op("dve", lambda E: E.tensor_tensor_scan(g_[:], rdec.to_broadcast([128, T]), v_[:], 0.0, OP.mult, OP.add),
                              reads=[sm["r"], v_], writes=[g_])
                    else:
                        kb.op("dve", lambda E: E.tensor_tensor_scan(g_[:, 0:NCTX][:, ::-1], rdec.to_broadcast([128, NCTX]), v_[:, 0:NCTX][:, ::-1],
                                                                    0.0, OP.mult, OP.add), reads=[sm["r"], v_], writes=[g_])
                        kb.op("dve", lambda E: E.tensor_tensor_scan(g_[:, NCTX:T][:, ::-1], rdec.to_broadcast([128, NLAT]), v_[:, NCTX:T][:, ::-1],
                                                                    g_[:, 0:1], OP.mult, OP.add), reads=[sm["r"], v_, g_], writes=[g_])
                kb.tt("dve", m1[:], Ebr[:], bur[:], OP.mult, reads=[Ebr, bur], writes=[m1])
                kb.tt("dve", m2[:], Ebi[:], bui[:], OP.mult, reads=[Ebi, bui], writes=[m2])
                kb.tt("dve", vr[:], m1[:], m2[:], OP.subtract, reads=[m1, m2], writes=[vr])
                kb.tt("dve", m1[:], Ebi[:], bur[:], OP.mult, reads=[Ebi, bur], writes=[m1])
                kb.tt("dve", m2[:], Ebr[:], bui[:], OP.mult, reads=[Ebr, bui], writes=[m2])
                kb.tt("dve", vi[:], m1[:], m2[:], OP.add, reads=[m1, m2], writes=[vi])
                for bi, (s0, nn) in enumerate(blocks):
                    py = kb.bank[4 + (bi % 2)]
                    kb.mm(py[:, 0:nn], LC[0][:], vr[:, s0:s0 + nn], start=True, stop=False, reads=[LC[0], vr], writes=[py])
                    kb.mm(py[:, 0:nn], LC[1][:], vi[:, s0:s0 + nn], start=False, stop=True, reads=[LC[1], vi], writes=[py])
                    if first_in_chunk[ch]:
                        kb.cp("act", yacc[:, ch, s0:s0 + nn], py[:, 0:nn], reads=[py], writes=[yacc])
                    else:
                        kb.tt("pool" if False else "dve", yacc[:, ch, s0:s0 + nn], yacc[:, ch, s0:s0 + nn], py[:, 0:nn], OP.add, reads=[py, yacc], writes=[yacc])
                first_in_chunk[ch] = False
            dcol = kb.sb(es, [128, 2], F32, "dcol")
            kb.dma(dcol[:], I["s5_dcol"].ap()[L], writes=[dcol])
            uf = (Er, Ei)
            mt = [kb.sb(es, [128, 512], F32, "mt%d" % i) for i in range(4)]
            for c in range(2):
                kb.dma(uf[c][:], self.S["UT"].ap()[c * 128:(c + 1) * 128, :], reads=[("UT", i) for i in range(9)], writes=[uf[c]])
            gwf = kb.sb(es, [128, 2, 256], F32, "gwf"); gwb = kb.sb(es, [128, 2, 256], BF16, "gwb")
            kb.dma(gwf[:], I["s5_glu_w"].ap()[L].rearrange("(kc p) n -> p kc n", p=128), writes=[gwf])
            kb.cp("act", gwb[:], gwf[:], reads=[gwf], writes=[gwb])
            yb = kb.sb(es, [128, 2, 512], BF16, "yb")
            yf = kb.sb(es, [128, 2, 512], F32, "yf")
            so = [kb.sb(es, [128, 512], F32, "so%d" % i) for i in range(2)]
            for bi, (s0, nn) in enumerate(blocks):
                m1, m2, m3, m4 = mt
                for c in range(2):
                    kb.stt("dve", yf[:, c, 0:nn], uf[c][:, s0:s0 + nn], dcol[:, c:c + 1], yacc[:, c, s0:s0 + nn], OP.mult, OP.add,
                           reads=[uf[c], dcol, yacc], writes=[yf])
                    kb.tt("pool", m1[:, 0:nn], yf[:, c, 0:nn], yf[:, c, 0:nn], OP.mult, reads=[yf], writes=[m1])
                    kb.ts("pool", m1[:, 0:nn], m1[:, 0:nn], 0.044715, OP.mult, 1.0, OP.add, reads=[m1], writes=[m1])
                    kb.tt("pool", m1[:, 0:nn], m1[:, 0:nn], yf[:, c, 0:nn], OP.mult, reads=[m1, yf], writes=[m1])
                    kb.act(m2[:, 0:nn], m1[:, 0:nn], AF.Sigmoid, scale=1.5957691216057308, reads=[m1], writes=[m2])
                    kb.tt("dve", yf[:, c, 0:nn], yf[:, c, 0:nn], m2[:, 0:nn], OP.mult, reads=[yf, m2], writes=[yf])
                    kb.cp("act", yb[:, c, 0:nn], yf[:, c, 0:nn], reads=[yf], writes=[yb])
                for mo in range(2):
                    pz = kb.bank[mo]
                    for kc in range(2):
                        kb.mm(pz[:, 0:nn], gwb[:, kc, mo * 128:(mo + 1) * 128], yb[:, kc, 0:nn], start=(kc == 0), stop=(kc == 1),
                              reads=[gwb, yb], writes=[pz])
                    kb.act(m3[:, 0:nn], pz[:, 0:nn], AF.Sigmoid, reads=[pz], writes=[m3])
                    so_ = so[mo]
                    kb.tt("dve", so_[:, 0:nn], yf[:, mo, 0:nn], m3[:, 0:nn], OP.mult, reads=[yf, m3], writes=[so_])
                    kb.dma(self.S["S5T"].ap()[mo * 128:(mo + 1) * 128, s0:s0 + nn], so_[:, 0:nn], reads=[so_], writes=[("S5T", bi)], q="pool")
        kb.barrier()


    def stage_mla(self, L, ctx_out):
        kb = self.kb
        I = self.I
        blocks = [(0, 256)] + [(256 + 512 * i, 512) for i in range(8)]
        with ExitStack() as es:
            ident_f = kb.sb(es, [128, 128], F32, "identf")
            ident = kb.sb(es, [128, 128], BF16, "identb")
            ones = kb.sb(es, [128, 128], BF16, "ones")
            kb.dma(ident_f[:], I["ident"].ap(), writes=[ident_f])
            kb.cp("dve", ident[:], ident_f[:], reads=[ident_f], writes=[ident])
            kb.memset("dve", ones[:], 1.0, writes=[ones])
            qg = kb.sb(es, [128, 3], F32, "qg"); kvg = kb.sb(es, [128, 2], F32, "kvg")
            kb.dma(qg[:], I["mla_qg"].ap()[L], writes=[qg]); kb.dma(kvg[:], I["mla_kvg"].ap()[L], writes=[kvg])
            wuq = kb.sb(es, [128, 3, 768], BF16, "wuq")
            wrot = kb.sb(es, [128, 3, 8, 96], BF16, "wrot")
            wukv = kb.sb(es, [128, 2, 1024], BF16, "wukv")
            KT = kb.sb(es, [97, 8, T], BF16, "KT")
            VA = kb.sb(es, [128, NT, 8, 65], BF16, "VA")
            gbc = kb.sb(es, [128, 512], F32, "gbc")
            kb.dma(gbc[:], I["mix_norm_g"].ap()[L:L + 1, 256:768].partition_broadcast(128), writes=[gbc])
            es2 = ExitStack()
            wst = kb.sb(es2, [128, 3, 1024], F32, "wst")
            kb.dma(wst[:, :, 0:768], I["mla_w_uq"].ap()[L].rearrange("(kc p) n -> p kc n", p=128), writes=[wst])
            kb.cp("act", wuq[:], wst[:, :, 0:768], reads=[wst], writes=[wuq])
            kb.memset("pool", wrot[:], 0.0, writes=[wrot])
            wv4 = wst[:, :, 0:768].rearrange("p k (h x) -> p k h x", x=96)
            kb.ts("dve", wrot[:, :, :, 64:72], wv4[:, :, :, 72:80], -1.0, OP.mult, reads=[wst], writes=[wrot])
            kb.cp("dve", wrot[:, :, :, 72:80], wv4[:, :, :, 64:72], reads=[wst], writes=[wrot])
            kb.ts("dve", wrot[:, :, :, 80:88], wv4[:, :, :, 88:96], -1.0, OP.mult, reads=[wst], writes=[wrot])
            kb.cp("dve", wrot[:, :, :, 88:96], wv4[:, :, :, 80:88], reads=[wst], writes=[wrot])
            kb.dma(wst[:, 0:2, :], I["mla_w_ukv"].ap()[L].rearrange("(kc p) n -> p kc n", p=128), reads=[wst], writes=[wst])
            kb.cp("act", wukv[:], wst[:, 0:2, :], reads=[wst], writes=[wukv])
            kb.memset("pool", KT[96:97, :, :], 1.0, writes=[KT])
            kb.memset("pool", VA[:, :, :, 64:65], 1.0, writes=[VA])
            krf = kb.sb(es2, [32, T], F32, "krf"); krb = kb.sb(es2, [32, T], BF16, "krb")
            kb.dma(krf[:], self.S["KRT"].ap(), reads=[("KRT", i) for i in range(9)], writes=[krf])
            kb.cp("act", krb[:], krf[:], reads=[krf], writes=[krb])
            for h in range(8):
                kb.dma(KT[64:96, h, :], krb[:], reads=[krb], writes=[KT])
            kb.barrier()
            es2.close()
            if "mla_stop1" in self.debug:
                return
            rcb = [kb.sb(es, [96, 512], F32, "rcb%d" % i) for i in range(2)]
            rsb = [kb.sb(es, [96, 512], F32, "rsb%d" % i) for i in range(2)]
            xin = [kb.sb(es, [128, 3, 512], F32, "xin%d" % i) for i in range(2)]
            sqb = kb.sb(es, [128, 3, 512], BF16, "sqb")
            rst = kb.sb(es, [128, 512], F32, "rst")
            xn = kb.sb(es, [128, 3, 512], BF16, "xn")
            kmax2 = kb.sb(es, [128, 8], F32, "kmax2"); kmb = kb.sb(es, [128, 8], F32, "kmb"); negk = kb.sb(es, [128, 8], F32, "negk")
            kb.memset("dve", kmax2[:], 0.0, writes=[kmax2])

            def rmsnorm_fm(src, nch, nfeat, nn, gcol, dst):
                kb.act(sqb[:, 0:nch, 0:nn], src[:, 0:nch, 0:nn], AF.Square, reads=[src], writes=[sqb])
                pn = kb.bank[6]
                for c in range(nch):
                    kb.mm(pn[:, 0:nn], ones[:], sqb[:, c, 0:nn], start=(c == 0), stop=(c == nch - 1), reads=[ones, sqb], writes=[pn])
                kb.ts("dve", rst[:, 0:nn], pn[:, 0:nn], 1.0 / nfeat, OP.mult, EPS, OP.add, reads=[pn], writes=[rst])
                kb.act(rst[:, 0:nn], rst[:, 0:nn], AF.Sqrt, reads=[rst], writes=[rst])
                kb.op("dve", lambda E: E.reciprocal(rst[:, 0:nn], rst[:, 0:nn]), reads=[rst], writes=[rst])
                for c in range(nch):
                    kb.stt("dve", dst[:, c, 0:nn], src[:, c, 0:nn], gcol[:, c:c + 1], rst[:, 0:nn], OP.mult, OP.mult,
                           reads=[src, gcol, rst], writes=[dst])

            for bi, (s0, nn) in enumerate(blocks):
                x_ = xin[bi % 2]
                kb.dma(x_[:, 0:2, 0:nn], self.S["CKVT"].ap().rearrange("(c p) t -> p c t", p=128)[:, :, s0:s0 + nn],
                       reads=[("CKVT", bi)], writes=[x_])
                rmsnorm_fm(x_, 2, 256, nn, kvg, xn)
                for h in range(8):
                    pk = kb.bank[h % 2]
                    for kc in range(2):
                        kb.mm(pk[0:64, 0:nn], wukv[:, kc, h * 128:h * 128 + 64], xn[:, kc, 0:nn], start=(kc == 0), stop=(kc == 1),
                              reads=[wukv, xn], writes=[pk])
                    kb.cp("act" if h % 2 else "dve", KT[0:64, h, s0:s0 + nn], pk[0:64, 0:nn], reads=[pk], writes=[KT])
                    kb.act(sqb[0:96, 0, 0:nn], KT[0:96, h, s0:s0 + nn], AF.Square, reads=[KT], writes=[sqb])
                    pn = kb.bank[6]
                    kb.mm(pn[:, 0:nn], ones[0:96, :], sqb[0:96, 0, 0:nn], reads=[ones, sqb], writes=[pn])
                    kb.op("dve", lambda E: E.reduce_max(kmb[:, h:h + 1], pn[:, 0:nn], AX.X), reads=[pn], writes=[kmb])
                    kb.tt("dve", kmax2[:, h:h + 1], kmax2[:, h:h + 1], kmb[:, h:h + 1], OP.max, reads=[kmb, kmax2], writes=[kmax2])
                for j in range(nn // 128):
                    ti = s0 // 128 + j
                    pv = kb.bank[2 + (j % 2)]
                    for kc in range(2):
                        kb.mm(pv[:, :].rearrange("p (h x) -> p h x", x=64), xn[:, kc, j * 128:(j + 1) * 128],
                              wukv[:, kc, :].rearrange("p (h x) -> p h x", x=128)[:, :, 64:128], start=(kc == 0), stop=(kc == 1),
                              reads=[wukv, xn], writes=[pv])
                    kb.cp("act" if j % 2 else "dve", VA[:, ti, :, 0:64], pv[:, :].rearrange("p (h x) -> p h x", x=64), reads=[pv], writes=[VA])
            if "mla_stop2" in self.debug:
                kb.barrier()
                return
            kb.act(negk[:], kmax2[:], AF.Sqrt, reads=[kmax2], writes=[negk])
            kb.ts("dve", negk[:], negk[:], -1.02, OP.mult, reads=[negk], writes=[negk])
            QT = [kb.sb(es, [97, 8, 512], BF16, "QT%d" % i) for i in range(2)]
            PT = [kb.sb(es, [128, 512], BF16, "PT%d" % i) for i in range(3)]
            tq = kb.sb(es, [96, 512], F32, "tq"); tq2 = kb.sb(es, [96, 512], F32, "tq2")
            qn1 = kb.sb(es, [97, 512], F32, "qn1")
            yt = kb.sb(es, [128, 4, 512], F32, "yt")
            ytb = kb.sb(es, [128, 4, 512], BF16, "ytb")
            rec = kb.sb(es, [128, 4], F32, "rec")
            ss4 = kb.sb(es, [128, 4], F32, "ss4")
            mts = kb.sb(es, [128, 4, 512], BF16, "mts")
            npt = 0
            for bi, (s0, nn) in enumerate(blocks):
                if bi == 0 and not ctx_out:
                    continue
                nj = nn // 128
                kchunks = list(range(2)) if bi == 0 else list(range(NT))
                x_ = xin[bi % 2]
                kb.dma(x_[:, 0:3, 0:nn], self.S["CQT"].ap().rearrange("(c p) t -> p c t", p=128)[:, :, s0:s0 + nn],
                       reads=[("CQT", bi)], writes=[x_])
                rmsnorm_fm(x_, 3, 384, nn, qg, xn)
                Q = QT[bi % 2]
                rc = rcb[bi % 2]; rs = rsb[bi % 2]
                kb.dma(rc[64:96, 0:nn], I["rope_cos"].ap()[:, s0:s0 + nn], writes=[rc])
                kb.dma(rs[64:96, 0:nn], I["rope_sin"].ap()[:, s0:s0 + nn], writes=[rs])
                for h in range(8):
                    pa = kb.bank[6]
                    pb = kb.bank[7]
                    for kc in range(3):
                        kb.mm(pa[0:96, 0:nn], wuq[:, kc, h * 96:(h + 1) * 96], xn[:, kc, 0:nn], start=(kc == 0), stop=(kc == 2),
                              reads=[wuq, xn], writes=[pa])
                    for kc in range(3):
                        kb.mm(pb[0:96, 0:nn], wrot[:, kc, h, :], xn[:, kc, 0:nn], start=(kc == 0), stop=(kc == 2),
                              reads=[wrot, xn], writes=[pb])
                    kb.cp("act", Q[0:64, h, 0:nn], pa[0:64, 0:nn], reads=[pa], writes=[Q])
                    kb.tt("dve", tq[64:96, 0:nn], pb[64:96, 0:nn], rs[64:96, 0:nn], OP.mult, reads=[pb, rs], writes=[tq])
                    kb.tt("dve", tq2[64:96, 0:nn], pa[64:96, 0:nn], rc[64:96, 0:nn], OP.mult, reads=[pa, rc], writes=[tq2])
                    kb.tt("dve", Q[64:96, h, 0:nn], tq[64:96, 0:nn], tq2[64:96, 0:nn], OP.add, reads=[tq, tq2], writes=[Q])
                    kb.act(sqb[0:96, 0, 0:nn], Q[0:96, h, 0:nn], AF.Square, reads=[Q], writes=[sqb])
                    pn = kb.bank[6]
                    kb.mm(pn[:, 0:nn], ones[0:96, :], sqb[0:96, 0, 0:nn], reads=[ones, sqb], writes=[pn])
                    kb.act(qn1[96:97, 0:nn], pn[96:97, 0:nn], AF.Sqrt, reads=[pn], writes=[qn1])
                    kb.ts("dve", Q[96:97, h, 0:nn], qn1[96:97, 0:nn], negk[96:97, h:h + 1], OP.mult, reads=[qn1, negk], writes=[Q])
                if "mla_stop3" in self.debug:
                    continue
                for h in range(8):
                    pend = None

                    def emit_pv(ci_, kc_, P__):
                        for j in range(nj):
                            po = kb.bank[2 + j]
                            kb.mm(po[:, 0:65], P__[:, j * 128:(j + 1) * 128], VA[:, kc_, h, :], start=(ci_ == 0), stop=(ci_ == len(kchunks) - 1),
                                  reads=[P__, VA], writes=[po])
                    for ci, kc in enumerate(kchunks):
                        pst = kb.bank[ci % 2]
                        kb.mm(pst[:, 0:nn], KT[0:97, h, kc * 128:(kc + 1) * 128], Q[0:97, h, 0:nn], reads=[KT, Q], writes=[pst])
                        P_ = PT[npt % 3]
                        npt += 1
                        kb.act(P_[:, 0:nn], pst[:, 0:nn], AF.Exp, scale=MLA_SCALE, reads=[pst], writes=[P_])
                        if pend is not None:
                            emit_pv(*pend)
                        pend = (ci, kc, P_)
                    emit_pv(*pend)
                    for j in range(nj):
                        if "mla_nonorm" in self.debug:
                            continue
                        po = kb.bank[2 + j]
                        if "mla_norec" in self.debug:
                            kb.ts("dve", yt[:, j, h * 64:(h + 1) * 64], po[:, 0:64], 0.5, OP.mult, reads=[po], writes=[yt])
                            continue
                        if "mla_recsb" in self.debug:
                            kb.cp("act", rec[:, j:j + 1], po[:, 64:65], reads=[po], writes=[rec])
                            kb.op("dve", lambda E: E.reciprocal(rec[:, j:j + 1], rec[:, j:j + 1]), reads=[rec], writes=[rec])
                        else:
                            kb.op("dve", lambda E: E.reciprocal(rec[:, j:j + 1], po[:, 64:65]), reads=[po], writes=[rec])
                        kb.ts("dve", yt[:, j, h * 64:(h + 1) * 64], po[:, 0:64], rec[:, j:j + 1], OP.mult, reads=[po, rec], writes=[yt])
                for j in range(nj):
                    if "MLAO" in self.debug:
                        kb.dma(self.S["MLAO"].ap()[s0 + j * 128:s0 + (j + 1) * 128, :], yt[:, j, :], reads=[yt], writes=["MLAO"], q="pool")
                    kb.act(mts[:, j, :], yt[:, j, :], AF.Square, accum_out=ss4[:, j:j + 1], reads=[yt], writes=[mts, ss4])
                kb.ts("dve", ss4[:, 0:nj], ss4[:, 0:nj], 1.0 / 512, OP.mult, EPS, OP.add, reads=[ss4], writes=[ss4])
                kb.act(ss4[:, 0:nj], ss4[:, 0:nj], AF.Sqrt, reads=[ss4], writes=[ss4])
                kb.op("dve", lambda E: E.reciprocal(ss4[:, 0:nj], ss4[:, 0:nj]), reads=[ss4], writes=[ss4])
                for j in range(nj):
                    kb.stt("dve", ytb[:, j, :], yt[:, j, :], ss4[:, j:j + 1], gbc[:], OP.mult, OP.mult, reads=[yt, ss4, gbc], writes=[ytb])
                    pt_ = kb.bank[6 + (j % 2)]
                    ptv = pt_[:].bitcast(BF16)
                    for fc in range(4):
                        kb.tr(ptv[:, fc * 128:(fc + 1) * 128], ytb[:, j, fc * 128:(fc + 1) * 128], ident[:], reads=[ytb, ident], writes=[pt_])
                    kb.cp("act", mts[:, :, j * 128:(j + 1) * 128], ptv[:, 0:512].rearrange("p (f q) -> p f q", f=4), reads=[pt_], writes=[mts])
                kb.dma(self.S["MT"].ap()[256:768, s0:s0 + nn].rearrange("(f p) t -> p f t", p=128), mts[:, :, 0:nn], reads=[mts],
                       writes=[("MT_mla", bi)], q="pool")
        kb.barrier()


    def stage_hyena(self, L, part):
        kb = self.kb
        I = self.I
        nm = part
        n = NLAT if part == "l" else NCTX
        r0 = NCTX if part == "l" else 0
        ntc = n // 128
        nj = (n + 1 + 127) // 128
        lagblocks = [(i * 512, min(512, n - i * 512)) for i in range((n + 511) // 512)]
        KSP = self.S["KSPEC_" + nm]
        with ExitStack() as es:
            ident_f = kb.sb(es, [128, 128], F32, "identf")
            kb.dma(ident_f[:], I["ident"].ap(), writes=[ident_f])
            w1 = kb.sb(es, [33, 64], F32, "w1"); w2 = kb.sb(es, [64, 64], F32, "w2"); w3 = kb.sb(es, [64, 1024], F32, "w3")
            cols = kb.sb(es, [64, 4], F32, "cols"); bf = kb.sb(es, [64, 2], F32, "bf"); b3 = kb.sb(es, [128, 8], F32, "b3")
            kb.dma(w1[:], I["hy_f_w1"].ap()[L], writes=[w1]); kb.dma(w2[:], I["hy_f_w2"].ap()[L], writes=[w2]); kb.dma(w3[:], I["hy_f_w3"].ap()[L], writes=[w3])
            kb.dma(cols[:], I["hy_cols"].ap()[L], writes=[cols]); kb.dma(b3[:], I["hy_b3"].ap()[L], writes=[b3])
            kb.tt("dve", bf[:, 0:1], cols[:, 0:1], cols[:, 2:3], OP.mult, reads=[cols], writes=[bf])
            kb.tt("dve", bf[:, 1:2], cols[:, 1:2], cols[:, 3:4], OP.mult, reads=[cols], writes=[bf])
            FS = kb.sb(es, [128, ntc, 512], BF16, "FS"); FD = kb.sb(es, [128, ntc, 512], BF16, "FD")
            zp = kb.sb(es, [33, 512], F32, "zp")
            h1 = kb.sb(es, [64, 512], F32, "h1"); h2 = kb.sb(es, [64, 512], F32, "h2")
            t1 = kb.sb(es, [64, 512], F32, "t1"); t2 = kb.sb(es, [64, 512], F32, "t2"); t0 = kb.sb(es, [64, 512], F32, "t0")
            dec = kb.sb(es, [128, 2, 512], F32, "dec")
            ff = kb.sb(es, [128, 512], F32, "ff"); fb = kb.sb(es, [128, 512], F32, "fb")
            fs_ = kb.sb(es, [128, 512], F32, "fs"); fd_ = kb.sb(es, [128, 512], F32, "fd")
            for (l0, ln) in lagblocks:
                kb.dma(zp[:, 0:ln], I["hy_zpos_" + nm].ap()[:, l0:l0 + ln], writes=[zp])
                kb.dma(dec[:, :, 0:ln], I["hy_decay_" + nm].ap().rearrange("(c p) t -> p c t", p=128)[:, :, l0:l0 + ln], writes=[dec])
                p1 = kb.bank[0]
                kb.mm(p1[0:64, 0:ln], w1[:], zp[:, 0:ln], reads=[w1, zp], writes=[p1])
                kb.ts("dve", t0[:, 0:ln], p1[0:64, 0:ln], cols[:, 2:3], OP.mult, bf[:, 0:1], OP.add, reads=[p1, cols, bf], writes=[t0])
                self.sin_rr("dve", h1[:, 0:ln], t0[:, 0:ln], 0.0, t1[:, 0:ln], t2[:, 0:ln])
                p2 = kb.bank[1]
                kb.mm(p2[0:64, 0:ln], w2[:], h1[:, 0:ln], reads=[w2, h1], writes=[p2])
                kb.ts("dve", t0[:, 0:ln], p2[0:64, 0:ln], cols[:, 3:4], OP.mult, bf[:, 1:2], OP.add, reads=[p2, cols, bf], writes=[t0])
                self.sin_rr("dve", h2[:, 0:ln], t0[:, 0:ln], 0.0, t1[:, 0:ln], t2[:, 0:ln])
                for o in range(2):
                    for chf in range(2):
                        jf = o * 2 + chf
                        jb = 4 + o * 2 + chf
                        pf = kb.bank[2]; pbk = kb.bank[3]
                        kb.mm(pf[:, 0:ln], w3[:, jf * 128:(jf + 1) * 128], h2[:, 0:ln], reads=[w3, h2], writes=[pf])
                        kb.mm(pbk[:, 0:ln], w3[:, jb * 128:(jb + 1) * 128], h2[:, 0:ln], reads=[w3, h2], writes=[pbk])
                        kb.stt("dve", ff[:, 0:ln], pf[:, 0:ln], b3[:, jf:jf + 1], dec[:, chf, 0:ln], OP.add, OP.mult, reads=[pf, b3, dec], writes=[ff])
                        kb.stt("dve", fb[:, 0:ln], pbk[:, 0:ln], b3[:, jb:jb + 1], dec[:, chf, 0:ln], OP.add, OP.mult, reads=[pbk, b3, dec], writes=[fb])
                        if l0 == 0:
                            kb.memset("dve", fb[:, 0:1], 0.0, writes=[fb])
                        kb.tt("dve", fs_[:, 0:ln], ff[:, 0:ln], fb[:, 0:ln], OP.add, reads=[ff, fb], writes=[fs_])
                        kb.tt("pool", fd_[:, 0:ln], ff[:, 0:ln], fb[:, 0:ln], OP.subtract, reads=[ff, fb], writes=[fd_])
                        for (src, dst, pbank) in ((fs_, FS, 4), (fd_, FD, 5)):
                            pt_ = kb.bank[pbank]
                            nq = ln // 128
                            for q in range(nq):
                                kb.tr(pt_[:, q * 128:(q + 1) * 128], src[:, q * 128:(q + 1) * 128], ident_f[:], reads=[src, ident_f], writes=[pt_])
                            tc0 = l0 // 128
                            kb.cp("act", dst[:, tc0:tc0 + nq, o * 256 + chf * 128:o * 256 + (chf + 1) * 128],
                                  pt_[:, 0:nq * 128].rearrange("p (q c) -> p q c", c=128), reads=[pt_], writes=[dst])
            Dt = [kb.sb(es, [128, ntc, 128], BF16, "Dt%d" % i) for i in range(4)]
            ks = [kb.sb(es, [128, 2, 512], F32, "ks%d" % i) for i in range(2)]
            nd = 0
            for j in range(nj):
                for ri, (fc, src) in enumerate(((j, FS), (nj + j, FD))):
                    D_ = Dt[nd % 4]
                    nd += 1
                    kb.dma(D_[:], I["hy_dfwd_" + nm].ap()[fc], writes=[D_])
                    pk = kb.bank[ri]
                    for tc in range(ntc):
                        kb.mm(pk[:, :], D_[:, tc, :], src[:, tc, :], start=(tc == 0), stop=(tc == ntc - 1), reads=[D_, src], writes=[pk])
                    kb.cp("act" if ri else "dve", ks[j % 2][:, ri, :], pk[:, :], reads=[pk], writes=[ks[j % 2]])
                kb.dma(KSP.ap()[j].rearrange("r p c -> p r c"), ks[j % 2][:], reads=[ks[j % 2]], writes=["KSPEC_" + nm], q="pool")
        kb.barrier()
        with ExitStack() as es:
            ident_f = kb.sb(es, [128, 128], F32, "identf")
            kb.dma(ident_f[:], I["ident"].ap(), writes=[ident_f])
            sw = kb.sb(es, [128, 6, 3], F32, "sw"); sbb = kb.sb(es, [128, 6], F32, "sbb")
            kb.dma(sw[:], I["hy_sw"].ap()[L], writes=[sw]); kb.dma(sbb[:], I["hy_sb"].ap()[L], writes=[sbb])
            xin = [kb.sb(es, [128, n], F32, "hzx%d" % i) for i in range(2)]
            zz = [kb.sb(es, [128, n], F32, "hzz%d" % i) for i in range(2)]
            tm = [kb.sb(es, [128, 4, 128], F32, "tm%d" % i) for i in range(2)]
            ntm = 0
            for c in range(6):
                x_ = xin[c % 2]; z_ = zz[c % 2]
                kb.dma(x_[:], self.S["HZT"].ap()[c * 128:(c + 1) * 128, r0:r0 + n], reads=[("HZT", i) for i in range(9)], writes=[x_])
                kb.ts("dve", z_[:], x_[:], sw[:, c, 1:2], OP.mult, sbb[:, c:c + 1], OP.add, reads=[x_, sw, sbb], writes=[z_])
                kb.stt("dve", z_[:, 1:n], x_[:, 0:n - 1], sw[:, c, 0:1], z_[:, 1:n], OP.mult, OP.add, reads=[x_, sw, z_], writes=[z_])
                kb.stt("dve", z_[:, 0:n - 1], x_[:, 1:n], sw[:, c, 2:3], z_[:, 0:n - 1], OP.mult, OP.add, reads=[x_, sw, z_], writes=[z_])
                for tg in range(0, ntc, 4):
                    ng = min(4, ntc - tg)
                    pt_ = kb.bank[ntm % 2]
                    t_ = tm[ntm % 2]
                    ntm += 1
                    for q in range(ng):
                        kb.tr(pt_[:, q * 128:(q + 1) * 128], z_[:, (tg + q) * 128:(tg + q + 1) * 128], ident_f[:], reads=[z_, ident_f], writes=[pt_])
                    kb.cp("act" if ntm % 2 else "dve", t_[:, 0:ng, :], pt_[:, 0:ng * 128].rearrange("p (q c) -> p q c", c=128), reads=[pt_], writes=[t_])
                    kb.dma(self.S["HZTM"].ap()[r0 + tg * 128:r0 + (tg + ng) * 128, c * 128:(c + 1) * 128].rearrange("(q p) c -> p q c", p=128),
                           t_[:, 0:ng, :], reads=[t_], writes=["HZTM"], q="pool")
        kb.barrier()
        with ExitStack() as es:
            ident = kb.sb(es, [128, 128], BF16, "identb")
            idf = kb.sb(es, [128, 128], F32, "identf")
            kb.dma(idf[:], I["ident"].ap(), writes=[idf])
            kb.cp("dve", ident[:], idf[:], reads=[idf], writes=[ident])
            Yb = [kb.sb(es, [128, ntc, 256], BF16, "Yb%d" % i) for i in range(2)]
            Z = kb.sb(es, [128, 2 * nj, 256], BF16, "Zs")
            Dt = [kb.sb(es, [128, ntc, 128], BF16, "Df%d" % i) for i in range(4)]
            Di = [kb.sb(es, [128, 2 * nj, 128], BF16, "Di%d" % i) for i in range(2)]
            kt = [kb.sb(es, [128, 2, 256], F32, "kt%d" % i) for i in range(2)]
            cm = [kb.sb(es, [128, 256], F32, "cm%d" % i) for i in range(4)]
            yo = [kb.sb(es, [128, 256], F32, "yo%d" % i) for i in range(2)]
            gt = [kb.sb(es, [128, 256], F32, "gt%d" % i) for i in range(2)]
            yn = [kb.sb(es, [128, 256], F32, "yn%d" % i) for i in range(2)]
            ynb = kb.sb(es, [128, 256], BF16, "ynb"); sqj = kb.sb(es, [128, 256], BF16, "sqj")
            ss = kb.sb(es, [128, 1], F32, "ss")
            mts = [kb.sb(es, [128, 2, 128], BF16, "mts%d" % i) for i in range(2)]
            bias_bc = kb.sb(es, [128, 2, 256], F32, "biasbc")
            gbc = kb.sb(es, [128, 256], F32, "gbc")
            kb.dma(bias_bc[:].rearrange("p o c -> p (o c)"), I["hy_bias"].ap()[L:L + 1].rearrange("a o c -> a (o c)").partition_broadcast(128), writes=[bias_bc])
            kb.dma(gbc[:], I["mix_norm_g"].ap()[L:L + 1, 768:1024].partition_broadcast(128), writes=[gbc])
            HZ = self.S["HZTM"].ap()
            for tc in range(ntc):
                y_ = yo[tc % 2]
                kb.dma(y_[:], HZ[r0 + tc * 128:r0 + (tc + 1) * 128, 0:256], reads=["HZTM"], writes=[y_])
                kb.cp("act" if tc % 2 else "dve", Yb[0][:, tc, :], y_[:], reads=[y_], writes=[Yb[0]])
            nd = 0
            for o in range(2):
                Yin = Yb[o]
                for j in range(nj):
                    k_ = kt[j % 2]
                    kb.dma(k_[:], KSP.ap()[j].rearrange("r p c -> p r c")[:, :, o * 256:(o + 1) * 256], reads=["KSPEC_" + nm], writes=[k_])
                    pr = kb.bank[(j % 2) * 2]; pi_ = kb.bank[(j % 2) * 2 + 1]
                    for ri, (fc, pk) in enumerate(((j, pr), (nj + j, pi_))):
                        D_ = Dt[nd % 4]
                        nd += 1
                        kb.dma(D_[:], I["hy_dfwd_" + nm].ap()[fc], writes=[D_])
                        for tc in range(ntc):
                            kb.mm(pk[:, 0:256], D_[:, tc, :], Yin[:, tc, :], start=(tc == 0), stop=(tc == ntc - 1), reads=[D_, Yin], writes=[pk])
                    c1, c2, c3, c4 = cm
                    kb.tt("dve", c1[:], pr[:, 0:256], k_[:, 0, :], OP.mult, reads=[pr, k_], writes=[c1])
                    kb.tt("dve", c2[:], pi_[:, 0:256], k_[:, 1, :], OP.mult, reads=[pi_, k_], writes=[c2])
                    kb.tt("pool", Z[:, j, :], c1[:], c2[:], OP.subtract, reads=[c1, c2], writes=[Z])
                    kb.tt("dve", c3[:], pr[:, 0:256], k_[:, 1, :], OP.mult, reads=[pr, k_], writes=[c3])
                    kb.tt("dve", c4[:], pi_[:, 0:256], k_[:, 0, :], OP.mult, reads=[pi_, k_], writes=[c4])
                    kb.tt("pool", Z[:, nj + j, :], c3[:], c4[:], OP.add, reads=[c3, c4], writes=[Z])
                for tc in range(ntc):
                    D_ = Di[tc % 2]
                    kb.dma(D_[:], I["hy_dinv_" + nm].ap()[tc], writes=[D_])
                    y_ = yo[tc % 2]; g_ = gt[tc % 2]; o_ = yn[tc % 2]
                    rows = slice(r0 + tc * 128, r0 + (tc + 1) * 128)
                    if o == 0:
                        kb.dma(y_[:], HZ[rows, 0:256], reads=["HZTM"], writes=[y_])
                    else:
                        kb.dma(y_[:], self.S["HY1"].ap()[rows, :], reads=[("HY1", tc)], writes=[y_])
                    kb.dma(g_[:], HZ[rows, 256 * (o + 1):256 * (o + 2)], reads=["HZTM"], writes=[g_])
                    pc = kb.bank[4 + (tc % 2)]
                    for fc in range(2 * nj):
                        kb.mm(pc[:, 0:256], D_[:, fc, :], Z[:, fc, :], start=(fc == 0), stop=(fc == 2 * nj - 1), reads=[D_, Z], writes=[pc])
                    kb.tt("pool", y_[:], y_[:], bias_bc[:, o, :], OP.mult, reads=[y_, bias_bc], writes=[y_])
                    kb.tt("dve", o_[:], pc[:, 0:256], y_[:], OP.add, reads=[pc, y_], writes=[o_])
                    kb.tt("dve", o_[:], o_[:], g_[:], OP.mult, reads=[o_, g_], writes=[o_])
                    if o == 0:
                        kb.cp("act", Yb[1][:, tc, :], o_[:], reads=[o_], writes=[Yb[1]])
                        kb.dma(self.S["HY1"].ap()[rows, :], o_[:], reads=[o_], writes=[("HY1", tc)], q="pool")
                    else:
                        if "HYO" in self.debug:
                            kb.dma(self.S["HYO"].ap()[rows, :], o_[:], reads=[o_], writes=["HYO"], q="pool")
                        kb.act(sqj[:], o_[:], AF.Square, accum_out=ss[:], reads=[o_], writes=[sqj, ss])
                        kb.ts("dve", ss[:], ss[:], 1.0 / 256, OP.mult, EPS, OP.add, reads=[ss], writes=[ss])
                        kb.act(ss[:], ss[:], AF.Sqrt, reads=[ss], writes=[ss])
                        kb.op("dve", lambda E: E.reciprocal(ss[:], ss[:]), reads=[ss], writes=[ss])
                        kb.stt("dve", ynb[:], o_[:], ss[:, 0:1], gbc[:], OP.mult, OP.mult, reads=[o_, ss, gbc], writes=[ynb])
                        pt_ = kb.bank[6 + (tc % 2)]
                        ptv = pt_[:].bitcast(BF16)
                        for fcx in range(2):
                            kb.tr(ptv[:, fcx * 128:(fcx + 1) * 128], ynb[:, fcx * 128:(fcx + 1) * 128], ident[:], reads=[ynb, ident], writes=[pt_])
                        m_ = mts[tc % 2]
                        kb.cp("act", m_[:], ptv[:, 0:256].rearrange("p (f q) -> p f q", f=2), reads=[pt_], writes=[m_])
                        kb.dma(self.S["MT"].ap()[768:1024, r0 + tc * 128:r0 + (tc + 1) * 128].rearrange("(f p) t -> p f t", p=128), m_[:],
                               reads=[m_], writes=[("MT_hy", part, tc)], q="pool")
        kb.barrier()


    def stage_s5merge(self, L):
        kb = self.kb
        blocks = [(0, 256)] + [(256 + 512 * i, 512) for i in range(8)]
        with ExitStack() as es:
            ones = kb.sb(es, [128, 128], BF16, "ones")
            kb.memset("dve", ones[:], 1.0, writes=[ones])
            gcol = kb.sb(es, [128, 2], F32, "gcol")
            kb.dma(gcol[:], self.I["mix_g_s5col"].ap()[L], writes=[gcol])
            xin = [kb.sb(es, [128, 2, 512], F32, "s5x%d" % i) for i in range(2)]
            sqb = kb.sb(es, [128, 2, 512], BF16, "sqb"); rst = kb.sb(es, [128, 512], F32, "rst")
            ob = [kb.sb(es, [128, 2, 512], BF16, "s5o%d" % i) for i in range(2)]
            for bi, (s0, nn) in enumerate(blocks):
                x_ = xin[bi % 2]; o_ = ob[bi % 2]
                kb.dma(x_[:, :, 0:nn], self.S["S5T"].ap().rearrange("(c p) t -> p c t", p=128)[:, :, s0:s0 + nn], reads=[("S5T", bi)], writes=[x_])
                kb.act(sqb[:, :, 0:nn], x_[:, :, 0:nn], AF.Square, reads=[x_], writes=[sqb])
                pn = kb.bank[bi % 2]
                for c in range(2):
                    kb.mm(pn[:, 0:nn], ones[:], sqb[:, c, 0:nn], start=(c == 0), stop=(c == 1), reads=[ones, sqb], writes=[pn])
                kb.ts("dve", rst[:, 0:nn], pn[:, 0:nn], 1.0 / 256, OP.mult, EPS, OP.add, reads=[pn], writes=[rst])
                kb.act(rst[:, 0:nn], rst[:, 0:nn], AF.Sqrt, reads=[rst], writes=[rst])
                kb.op("dve", lambda E: E.reciprocal(rst[:, 0:nn], rst[:, 0:nn]), reads=[rst], writes=[rst])
                for c in range(2):
                    kb.stt("dve", o_[:, c, 0:nn], x_[:, c, 0:nn], gcol[:, c:c + 1], rst[:, 0:nn], OP.mult, OP.mult, reads=[x_, gcol, rst], writes=[o_])
                kb.dma(self.S["MT"].ap()[0:256, s0:s0 + nn].rearrange("(c p) t -> p c t", p=128), o_[:, :, 0:nn], reads=[o_], writes=[("MT_s5", bi)], q="pool")
        kb.barrier()

    def stage_out_moe(self, L, last):
        kb = self.kb
        I = self.I
        S = self.S
        tiles = list(range(2, NT)) if last else list(range(NT))
        ntl = len(tiles)
        nblk = (2 * ntl * 128 + 127) // 128 + 32
        X = S["X"].ap()
        with ExitStack() as es:
            ident_f = kb.sb(es, [128, 128], F32, "identf")
            ident = kb.sb(es, [128, 128], BF16, "identb")
            kb.dma(ident_f[:], I["ident"].ap(), writes=[ident_f])
            kb.cp("dve", ident[:], ident_f[:], reads=[ident_f], writes=[ident])
            iota_e = kb.sb(es, [128, 32], F32, "iotae"); iota_b = kb.sb(es, [128, 128], F32, "iotab"); iota_p = kb.sb(es, [128, 1], F32, "iotap")
            ltri = kb.sb(es, [128, 128], BF16, "ltri"); ones = kb.sb(es, [128, 128], BF16, "ones")
            kb.dma(iota_e[:], I["iota_e"].ap(), writes=[iota_e]); kb.dma(iota_b[:], I["iota_b"].ap(), writes=[iota_b])
            kb.dma(iota_p[:], I["iota_p"].ap(), writes=[iota_p]); kb.dma(ltri[:], I["ltri"].ap(), writes=[ltri])
            kb.memset("dve", ones[:], 1.0, writes=[ones])
            GW = kb.sb(es, [128, NT, 2], F32, "GW"); EID = kb.sb(es, [128, NT, 2], F32, "EID"); RANK = kb.sb(es, [128, NT, 2], F32, "RANK")
            DEST = kb.sb(es, [128, NT, 2], F32, "DEST"); DESTI = kb.sb(es, [128, NT, 2], I32, "DESTI")
            base = kb.sb(es, [128, 32], F32, "base")
            BE = kb.sb(es, [128, 128], F32, "BE"); IDXW = kb.sb(es, [128, 2, 128], I32, "IDXW")
            CHG = kb.sb(es, [128, 128], F32, "CHG"); IDXF = kb.sb(es, [128, 128], F32, "IDXF")
            kb.memset("dve", base[:], 0.0, writes=[base])
            kb.memset("dve", RANK[:], 0.0, writes=[RANK])
            kb.memset("dve", DEST[:], 0.0, writes=[DEST])
            es2 = ExitStack()
            wo = kb.sb(es2, [128, 8, DM], BF16, "wo")
            wr = kb.sb(es2, [128, 8, 36], F32, "wr")
            kb.dma(wr[:], I["moe_wr"].ap()[L].rearrange("(kc p) n -> p kc n", p=128), writes=[wr])
            gate1 = kb.sb(es2, [128, DM], F32, "gate1"); G2 = kb.sb(es2, [128, DM], F32, "G2"); S2 = kb.sb(es2, [128, DM], F32, "S2")
            tmpb = kb.sb(es2, [128, DM], F32, "tmpb")
            es3 = ExitStack()
            wst = kb.sb(es3, [128, 8, DM], F32, "wst")
            kb.dma(wst[:], I["w_out"].ap()[L].rearrange("(kc p) n -> p kc n", p=128), writes=[wst])
            kb.cp("act", wo[:, 0:4, :], wst[:, 0:4, :], reads=[wst], writes=[wo])
            kb.cp("dve", wo[:, 4:8, :], wst[:, 4:8, :], reads=[wst], writes=[wo])
            kb.barrier()
            es3.close()
            mT = [kb.sb(es2, [128, 8, 512], BF16, "mT%d" % i) for i in range(2)]
            xt = [kb.sb(es2, [128, DM], F32, "xt%d" % i) for i in range(2)]
            xn = [kb.sb(es2, [128, DM], F32, "xn%d" % i) for i in range(2)]
            sq = kb.sb(es2, [128, DM], BF16, "sq"); ss = kb.sb(es2, [128, 1], F32, "ss"); rstd = kb.sb(es2, [128, 1], F32, "rstd")
            tmp = kb.sb(es2, [128, DM], F32, "tmp")
            ff = [kb.sb(es2, [128, DM], F32, "ff%d" % i) for i in range(2)]
            fbt = [kb.sb(es2, [128, DM], BF16, "fbt%d" % i) for i in range(2)]
            fT = kb.sb(es2, [128, 8, 128], F32, "fT")
            lg = kb.sb(es2, [128, 36], F32, "lg")
            sm = {k: kb.sb(es2, shp, F32, k) for k, shp in (("gmax", [128, 1]), ("ngmax", [128, 1]), ("ohg", [128, 4]), ("pen", [128, 4]), ("ex4", [128, 4]),
                                                            ("sume", [128, 1]), ("gw", [128, 1]), ("msk", [128, 32]), ("top8", [128, 8]), ("nv2", [128, 1]),
                                                            ("p1", [128, 1]), ("p2", [128, 1]), ("a0", [128, 32]), ("junk", [128, 32]), ("csum", [128, 32]))}
            idx8 = kb.sb(es2, [128, 8], U32, "idx8")
            E2 = kb.sb(es2, [128, 64], F32, "E2"); E2b = kb.sb(es2, [128, 64], BF16, "E2b")
            blocks = [(0, 2)] + [(2 + 4 * i, 4) for i in range(8)]
            if last:
                blocks = blocks[1:]
            for bi, (t0, nt_) in enumerate(blocks):
                which = 1 if t0 == 0 else 0
                if t0 in (0, 2):
                    MOD = S["MOD"].ap()
                    kb.dma(gate1[:], MOD[which:which + 1, 2 * DM:3 * DM].partition_broadcast(128), reads=["MOD"], writes=[gate1])
                    self.load_mod_bc((G2, S2, tmpb), L, which, 3, "norm2_g")
                m_ = mT[bi % 2]
                nb = nt_ * 128
                c0 = t0 * 128
                kb.dma(m_[:, :, 0:nb], S["MT"].ap().rearrange("(kc p) t -> p kc t", p=128)[:, :, c0:c0 + nb],
                       reads=[("MT_s5", i) for i in range(9)] + [("MT_mla", i) for i in range(9)] + [("MT_hy", "l", i) for i in range(32)] + [("MT_hy", "c", i) for i in range(2)],
                       writes=[m_])
                for j in range(nt_):
                    t = t0 + j
                    x_ = xt[t % 2]; xo = xn[t % 2]
                    kb.dma(x_[:], X[t * 128:(t + 1) * 128, :], reads=[("X", t)], writes=[x_])
                    for nh in range(2):
                        po = kb.bank[nh]
                        for kc in range(8):
                            kb.mm(po[:, :], m_[:, kc, j * 128:(j + 1) * 128], wo[:, kc, nh * 512:(nh + 1) * 512], start=(kc == 0), stop=(kc == 7),
                                  reads=[m_, wo], writes=[po])
                        kb.tt("dve", tmp[:, nh * 512:(nh + 1) * 512], po[:, :], gate1[:, nh * 512:(nh + 1) * 512], OP.mult, reads=[po, gate1], writes=[tmp])
                    kb.tt("pool", xo[:], tmp[:], x_[:], OP.add, reads=[tmp, x_], writes=[xo])
                    kb.dma(X[t * 128:(t + 1) * 128, :], xo[:], reads=[xo], writes=[("X", t)], q="pool")
                    if "XMID" in self.debug:
                        kb.dma(S["XMID"].ap()[t * 128:(t + 1) * 128, :], xo[:], reads=[xo], writes=["XMID"], q="pool")
                    f_ = ff[t % 2]; fb_ = fbt[t % 2]
                    self.rms_modulate(xo, G2, S2, sq, ss, rstd, tmp, f_, fb_)
                    kb.dma(S["FB"].ap()[t * 128:(t + 1) * 128, :], fb_[:], reads=[fb_], writes=[("FB", t)], q="pool")
                    for half in range(2):
                        pt_ = kb.bank[2 + half]
                        for q in range(4):
                            kc = half * 4 + q
                            kb.tr(pt_[:, q * 128:(q + 1) * 128], f_[:, kc * 128:(kc + 1) * 128], ident_f[:], reads=[f_, ident_f], writes=[pt_])
                        kb.cp("act" if half else "dve", fT[:, half * 4:(half + 1) * 4, :], pt_[:, :].rearrange("p (q c) -> p q c", c=128), reads=[pt_], writes=[fT])
                    pl = kb.bank[4]
                    for kc in range(8):
                        kb.mm(pl[:, 0:36], fT[:, kc, :], wr[:, kc, :], start=(kc == 0), stop=(kc == 7), reads=[fT, wr], writes=[pl])
                    kb.cp("dve", lg[:], pl[:, 0:36], reads=[pl], writes=[lg])
                    A = lambda k: sm[k][:]
                    kb.op("dve", lambda E: E.reduce_max(A("gmax"), lg[:, 0:4], AX.X), reads=[lg], writes=[sm["gmax"]])
                    kb.ts("dve", A("ohg"), lg[:, 0:4], A("gmax")[:, 0:1], OP.is_equal, reads=[lg, sm["gmax"]], writes=[sm["ohg"]])
                    kb.ts("dve", A("ngmax"), A("gmax"), -1.0, OP.mult, reads=[sm["gmax"]], writes=[sm["ngmax"]])
                    kb.act(A("ex4"), lg[:, 0:4], AF.Exp, bias=A("ngmax")[:, 0:1], accum_out=A("sume"), reads=[lg, sm["ngmax"]], writes=[sm["ex4"], sm["sume"]])
                    kb.op("dve", lambda E: E.reciprocal(A("gw"), A("sume")), reads=[sm["sume"]], writes=[sm["gw"]])
                    kb.ts("dve", A("pen"), A("ohg"), 1.0e30, OP.mult, -1.0e30, OP.add, reads=[sm["ohg"]], writes=[sm["pen"]])
                    for g in range(4):
                        kb.ts("dve", sm["msk"][:, g * 8:(g + 1) * 8], lg[:, 4 + g * 8:4 + (g + 1) * 8], sm["pen"][:, g:g + 1], OP.add,
                              reads=[lg, sm["pen"]], writes=[sm["msk"]])
                    kb.op("dve", lambda E: E.max(A("top8"), A("msk")), reads=[sm["msk"]], writes=[sm["top8"]])
                    kb.op("dve", lambda E: E.max_index(idx8[:], A("top8"), A("msk")), reads=[sm["msk"], sm["top8"]], writes=[idx8])
                    kb.cp("dve", EID[:, t, :], idx8[:, 0:2], reads=[idx8], writes=[EID])
                    kb.ts("dve", A("nv2"), sm["top8"][:, 1:2], -1.0, OP.mult, reads=[sm["top8"]], writes=[sm["nv2"]])
                    kb.act(A("p1"), sm["top8"][:, 0:1], AF.Sigmoid, bias=A("nv2")[:, 0:1], reads=[sm["top8"], sm["nv2"]], writes=[sm["p1"]])
                    kb.ts("dve", A("p2"), A("p1"), -1.0, OP.mult, 1.0, OP.add, reads=[sm["p1"]], writes=[sm["p2"]])
                    kb.tt("dve", GW[:, t, 0:1], A("p1"), A("gw"), OP.mult, reads=[sm["p1"], sm["gw"]], writes=[GW])
                    kb.tt("dve", GW[:, t, 1:2], A("p2"), A("gw"), OP.mult, reads=[sm["p2"], sm["gw"]], writes=[GW])
                    for k in range(2):
                        kb.ts("dve", E2[:, k * 32:(k + 1) * 32], iota_e[:], EID[:, t, k:k + 1], OP.is_equal, reads=[iota_e, EID], writes=[E2])
                    kb.cp("act", E2b[:], E2[:], reads=[E2], writes=[E2b])
                    ppre = kb.bank[5]; pcnt = kb.bank[6]
                    kb.mm(ppre[:, 0:64], ltri[:], E2b[:], reads=[ltri, E2b], writes=[ppre])
                    kb.mm(pcnt[:, 0:64], ones[:], E2b[:], reads=[ones, E2b], writes=[pcnt])
                    kb.tt("dve", A("a0"), ppre[:, 0:32], base[:], OP.add, reads=[ppre, base], writes=[sm["a0"]])
                    kb.op("dve", lambda E: E.scalar_tensor_tensor(A("junk"), A("a0"), 1.0, E2[:, 0:32], OP.mult, OP.mult, accum_out=RANK[:, t, 0:1]),
                          reads=[sm["a0"], E2], writes=[sm["junk"], RANK])
                    kb.tt("dve", A("csum"), pcnt[:, 0:32], base[:], OP.add, reads=[pcnt, base], writes=[sm["csum"]])
                    kb.tt("dve", A("a0"), ppre[:, 32:64], A("csum"), OP.add, reads=[ppre, sm["csum"]], writes=[sm["a0"]])
                    kb.op("dve", lambda E: E.scalar_tensor_tensor(A("junk"), A("a0"), 1.0, E2[:, 32:64], OP.mult, OP.mult, accum_out=RANK[:, t, 1:2]),
                          reads=[sm["a0"], E2], writes=[sm["junk"], RANK])
                    kb.tt("dve", base[:], A("csum"), pcnt[:, 32:64], OP.add, reads=[sm["csum"], pcnt], writes=[base])
            pb_ = kb.sb(es2, [128, 32], F32, "padb"); pend = kb.sb(es2, [128, 32], F32, "pend"); pst = kb.sb(es2, [128, 32], F32, "pstart")
            one32 = kb.sb(es2, [128, 32], F32, "one32")
            kb.memset("dve", one32[:], 1.0, writes=[one32])
            kb.ts("dve", pb_[:], base[:], 1.0 / 128, OP.mult, (127.0 / 128 - 0.5 + 1.0 / 256), OP.add, reads=[base], writes=[pb_])
            kb.ts("dve", pb_[:], pb_[:], MAGIC, OP.add, reads=[pb_], writes=[pb_])
            kb.ts("dve", pb_[:], pb_[:], -MAGIC, OP.add, reads=[pb_], writes=[pb_])
            kb.op("dve", lambda E: E.tensor_tensor_scan(pend[:], one32[:], pb_[:], 0.0, OP.mult, OP.add), reads=[one32, pb_], writes=[pend])
            kb.tt("dve", pst[:], pend[:], pb_[:], OP.subtract, reads=[pend, pb_], writes=[pst])
            kb.ts("dve", pst[:], pst[:], 128.0, OP.mult, reads=[pst], writes=[pst])
            kb.memset("dve", BE[:], 0.0, writes=[BE])
            for e in range(32):
                kb.stt("dve", BE[:], iota_b[:], pend[:, e:e + 1], BE[:], OP.is_ge, OP.add, reads=[iota_b, pend, BE], writes=[BE])
            NS = 4
            per = nblk // NS
            assert nblk % NS == 0
            BIGI = 1.0e6
            kb.ts("dve", BE[:], BE[:], 31.0, OP.min, reads=[BE], writes=[BE])
            kb.memset("dve", CHG[:], 1.0, writes=[CHG])
            kb.tt("dve", CHG[:, 1:nblk], BE[:, 1:nblk], BE[:, 0:nblk - 1], OP.not_equal, reads=[BE], writes=[CHG])
            for q in range(NS):
                kb.memset("dve", CHG[:, q * per:q * per + 1], 1.0, writes=[CHG])
            kb.ts("dve", BE[:], BE[:], float(L * 32), OP.add, 128.0, OP.mult, reads=[BE], writes=[BE])
            kb.ts("dve", BE[:], BE[:], iota_p[:, 0:1], OP.add, 2.0, OP.mult, reads=[BE, iota_p], writes=[BE])
            for half in range(2):
                kb.ts("dve", IDXF[:], BE[:], float(half) - BIGI, OP.add, reads=[BE], writes=[IDXF])
                kb.tt("dve", IDXF[:], IDXF[:], CHG[:], OP.mult, reads=[IDXF, CHG], writes=[IDXF])
                kb.ts("dve", IDXF[:], IDXF[:], BIGI, OP.add, reads=[IDXF], writes=[IDXF])
                kb.cp("dve", IDXW[:, half, :], IDXF[:], reads=[IDXF], writes=[IDXW])
            for t in tiles:
                for k in range(2):
                    kb.ts("dve", E2[:, 0:32], iota_e[:], EID[:, t, k:k + 1], OP.is_equal, reads=[iota_e, EID], writes=[E2])
                    kb.op("dve", lambda E: E.scalar_tensor_tensor(sm["junk"][:], E2[:, 0:32], 1.0, pst[:], OP.mult, OP.mult, accum_out=DEST[:, t, k:k + 1]),
                          reads=[E2, pst], writes=[sm["junk"], DEST])
            kb.tt("dve", DEST[:], DEST[:], RANK[:], OP.add, reads=[DEST, RANK], writes=[DEST])
            kb.cp("dve", DESTI[:], DEST[:], reads=[DEST], writes=[DESTI])
            for t in tiles:
                fb_ = fbt[t % 2]
                kb.dma(fb_[:], S["FB"].ap()[t * 128:(t + 1) * 128, :], reads=[("FB", t)], writes=[fb_])
                for k in range(2):
                    kb.dma(None, None, reads=[fb_, DESTI], writes=["XS"], q="pool",
                           fn=lambda E: E.indirect_dma_start(out=S["XS"].ap(), out_offset=bass.IndirectOffsetOnAxis(DESTI[:, t, k:k + 1], 0),
                                                             in_=fb_[:], in_offset=None))
            kb.barrier()
            es2.close()
            es4 = ExitStack()
            WQ = [kb.sb(es4, [128, 3, 4096], BF16, "WQ%d" % i) for i in range(NS)]
            xs = [kb.sb(es4, [128, DM], BF16, "xs%d" % i) for i in range(2)]
            XT = [kb.sb(es4, [128, 8, 128], BF16, "XT%d" % i) for i in range(2)]
            h1s = kb.sb(es4, [128, 512], F32, "h1s")
            aT = [kb.sb(es4, [128, 4, 128], BF16, "aT%d" % i) for i in range(2)]
            ysb = [kb.sb(es4, [128, DM], F32, "ysb%d" % i) for i in range(2)]
            nrows2 = 2 * DEPTH * 32 * 128
            for slot in range(nblk):
                qs = slot % NS
                b = qs * per + slot // NS
                wb = WQ[qs]
                for wi, wn in enumerate(("moe_w1", "moe_w3", "moe_w2")):
                    for half in range(2):
                        kb.dma(None, None, reads=[IDXW], writes=[wb], q="pool",
                               fn=lambda E: E.indirect_dma_start(out=wb[:, wi, half * 2048:(half + 1) * 2048], out_offset=None,
                                                                 in_=I[wn].ap().rearrange("r (h c) -> (r h) c", h=2),
                                                                 in_offset=bass.IndirectOffsetOnAxis(IDXW[:, half, b:b + 1], 0),
                                                                 bounds_check=kb.bnd_reg, oob_is_err=False))
                x_ = xs[slot % 2]
                kb.dma(x_[:], S["XS"].ap()[b * 128:(b + 1) * 128, :], reads=["XS"], writes=[x_])
                pt_ = kb.bank[0]
                ptv = pt_[:].bitcast(BF16)
                for kc in range(8):
                    kb.tr(ptv[:, kc * 128:(kc + 1) * 128], x_[:, kc * 128:(kc + 1) * 128], ident[:], reads=[x_, ident], writes=[pt_])
                XT_ = XT[slot % 2]
                kb.cp("dve", XT_[:], ptv.rearrange("p (k n) -> p k n", k=8), reads=[pt_], writes=[XT_])
                ph1 = kb.bank[1]; ph3 = kb.bank[2]
                for (pp, wi) in ((ph1, 0), (ph3, 1)):
                    for hc in range(4):
                        for kc in range(8):
                            kb.mm(pp[:, hc * 128:(hc + 1) * 128], wb[:, wi, kc * 512 + hc * 128:kc * 512 + (hc + 1) * 128], XT_[:, kc, :],
                                  start=(kc == 0), stop=(kc == 7), reads=[wb, XT_], writes=[pp])
                kb.act(h1s[:], ph1[:, :], AF.Silu, reads=[ph1], writes=[h1s])
                a_ = aT[slot % 2]
                kb.tt("dve", a_[:].rearrange("p h r -> p (h r)"), h1s[:], ph3[:, :], OP.mult, reads=[h1s, ph3], writes=[a_])
                y_ = ysb[slot % 2]
                for nh in range(2):
                    py = kb.bank[3 + nh]
                    for hc in range(4):
                        kb.mm(py[:, :], a_[:, hc, :], wb[:, 2, hc * 1024 + nh * 512:hc * 1024 + (nh + 1) * 512], start=(hc == 0), stop=(hc == 3),
                              reads=[a_, wb], writes=[py])
                    kb.cp("act" if nh else "dve", y_[:, nh * 512:(nh + 1) * 512], py[:, :], reads=[py], writes=[y_])
                kb.dma(S["YS"].ap()[b * 128:(b + 1) * 128, :], y_[:], reads=[y_], writes=["YS"])
            kb.barrier()
            es4.close()
            es5 = ExitStack()
            gate2 = kb.sb(es5, [128, DM], F32, "gate2")
            xt = [kb.sb(es5, [128, DM], F32, "xt%d" % i) for i in range(2)]
            y0 = [kb.sb(es5, [128, DM], F32, "y0%d" % i) for i in range(2)]
            y1 = [kb.sb(es5, [128, DM], F32, "y1%d" % i) for i in range(2)]
            xo = [kb.sb(es5, [128, DM], F32, "xo%d" % i) for i in range(2)]
            if last:
                fg = kb.sb(es5, [128, DM], F32, "fg")
                kb.dma(fg[:], I["final_g"].ap().partition_broadcast(128), writes=[fg])
                sq = kb.sb(es5, [128, DM], BF16, "sq"); ss = kb.sb(es5, [128, 1], F32, "ss")
            for t in tiles:
                which = 1 if t < 2 else 0
                if t in (0, 2):
                    kb.dma(gate2[:], S["MOD"].ap()[which:which + 1, 5 * DM:6 * DM].partition_broadcast(128), reads=["MOD"], writes=[gate2])
                x_ = xt[t % 2]; a_ = y0[t % 2]; b_ = y1[t % 2]; o_ = xo[t % 2]
                kb.dma(x_[:], X[t * 128:(t + 1) * 128, :], reads=[("X", t)], writes=[x_])
                for k, yy in enumerate((a_, b_)):
                    kb.dma(None, None, reads=[DESTI, "YS"], writes=[yy], q="pool",
                           fn=lambda E: E.indirect_dma_start(out=yy[:], out_offset=None, in_=S["YS"].ap(),
                                                             in_offset=bass.IndirectOffsetOnAxis(DESTI[:, t, k:k + 1], 0)))
                kb.ts("dve", a_[:], a_[:], GW[:, t, 0:1], OP.mult, reads=[a_, GW], writes=[a_])
                kb.stt("dve", a_[:], b_[:], GW[:, t, 1:2], a_[:], OP.mult, OP.add, reads=[b_, GW, a_], writes=[a_])
                if "YMOE" in self.debug:
                    kb.dma(S["YMOE"].ap()[t * 128:(t + 1) * 128, :], a_[:], reads=[a_], writes=["YMOE"], q="pool")
                kb.tt("pool", a_[:], a_[:], gate2[:], OP.mult, reads=[a_, gate2], writes=[a_])
                kb.tt("dve", o_[:], a_[:], x_[:], OP.add, reads=[a_, x_], writes=[o_])
                if not last:
                    kb.dma(X[t * 128:(t + 1) * 128, :], o_[:], reads=[o_], writes=[("X", t)], q="pool")
                else:
                    kb.act(sq[:], o_[:], AF.Square, accum_out=ss[:], reads=[o_], writes=[sq, ss])
                    kb.ts("dve", ss[:], ss[:], 1.0 / DM, OP.mult, EPS, OP.add, reads=[ss], writes=[ss])
                    kb.act(ss[:], ss[:], AF.Sqrt, reads=[ss], writes=[ss])
                    kb.op("dve", lambda E: E.reciprocal(ss[:], ss[:]), reads=[ss], writes=[ss])
                    kb.stt("dve", o_[:], o_[:], ss[:, 0:1], fg[:], OP.mult, OP.mult, reads=[o_, ss, fg], writes=[o_])
                    kb.dma(self.out.ap()[(t - 2) * 128:(t - 1) * 128, :], o_[:], reads=[o_], writes=["OUT"], q="pool")
            kb.barrier()
            es5.close()
        kb.barrier()

    def finish(self, out_keys):
        kb = self.kb
        kb._waits("sp", out_keys, ())
        kb.barrier()


def build(debug=(), layers=DEPTH, stages=("mod", "proj")):
    P1 = _build(debug, layers, stages, None)
    return _build(debug, layers, stages, P1.kb.waited)


def _build(debug, layers, stages, needed):
    P = Prog(debug, needed)
    P.declare()
    P.stage_init()
    P.kb.barrier()
    for L in range(layers):
        if "mod" in stages:
            P.stage_mod(L)
        if "proj" in stages:
            P.stage_proj(L)
        if "s5" in stages:
            P.stage_s5(L)
        if "mla" in stages:
            P.stage_mla(L, L < DEPTH - 1)
        if "hy" in stages:
            P.stage_hyena(L, "l")
            if L < DEPTH - 1:
                P.stage_hyena(L, "c")
        if "moe" in stages:
            P.stage_s5merge(L)
            P.stage_out_moe(L, L == DEPTH - 1)
    P.finish(["OUT"])
    return P


def kernel(**inputs):
    inp = {k: np.asarray(v) for k, v in inputs.items()}
    P = build(debug=(), layers=DEPTH, stages=("mod", "proj", "s5", "mla", "hy", "moe"))
    maps = prep_inputs(inp)
    names = set(P.I.keys())
    in_maps = [{k: v for k, v in m.items() if k in names} for m in maps]
    res = run_bass_kernel_spmd(P.nc, in_maps, core_ids=list(range(8)))
    return np.stack([np.asarray(r["out"], dtype=np.float32) for r in res.results], axis=0)
```

```python
import math
from contextlib import ExitStack
import numpy as np
import ml_dtypes
import concourse.bass as bass
import concourse.mybir as mybir
from concourse.bass_utils import run_bass_kernel_spmd

F32 = mybir.dt.float32
BF16 = mybir.dt.bfloat16
I32 = mybir.dt.int32
U32 = mybir.dt.uint32
AF = mybir.ActivationFunctionType
OP = mybir.AluOpType
AX = mybir.AxisListType

DM = 1024
NCTX = 256
NLAT = 4096
T = NCTX + NLAT
NT = T // 128
DEPTH = 4
EPS = 1e-6
P_IN = 1696
MAGIC = 12582912.0
MLA_SCALE = 1.0 / math.sqrt(96.0)
NE = 32
NBLK = 100


class Buf:
    __slots__ = ("w", "r", "name")

    def __init__(self, name=""):
        self.w = None
        self.r = {}
        self.name = name


class KB:
    NDMA = 10

    def __init__(self, needed=None):
        nc = bass.Bass("TRN2", target_bir_lowering=False)
        self.nc = nc
        self.needed = needed
        self.waited = {e: set() for e in ("pe", "act", "dve", "pool")}
        self.real = {e: 0 for e in ("pe", "act", "dve", "pool")}
        self.vmap = {e: {} for e in ("pe", "act", "dve", "pool")}
        self.eng = {"pe": nc.tensor, "act": nc.scalar, "dve": nc.vector, "pool": nc.gpsimd, "sp": nc.sync}
        self.sem = {}
        self.cnt = {}
        for e in ("pe", "act", "dve", "pool"):
            self.sem[e] = nc.alloc_semaphore("s_" + e)
            self.cnt[e] = 0
        self.known = {e: {} for e in self.eng}
        self.dq = {}
        for q in ("sp", "act", "pool"):
            sems = []
            for i in range(self.NDMA):
                k = ("d", q, i)
                self.sem[k] = nc.alloc_semaphore("d_%s_%d" % (q, i))
                self.cnt[k] = 0
                sems.append(k)
            self.dq[q] = [sems, 0]
        self.nalloc = 0
        self.bufs = {}
        self.ninst = 0
        self.bank = [nc.alloc_psum_tensor("bank%d" % i, [128, 512], F32) for i in range(8)]
        self.bnd_reg = nc.gpsimd.alloc_register("bnd")
        nc.gpsimd.reg_mov(self.bnd_reg, 2 * DEPTH * 32 * 128 - 1)

    def sb(self, es, shape, dt=F32, name="t"):
        self.nalloc += 1
        t = es.enter_context(self.nc.sbuf_tensor("%s_%d" % (name, self.nalloc), list(shape), dt))
        return t

    def dram(self, name, shape, dt=F32, kind="Internal"):
        return self.nc.dram_tensor(name, list(shape), dt, kind=kind)

    def B(self, key):
        if isinstance(key, Buf):
            return key
        k = key if isinstance(key, (str, tuple, int)) else id(key)
        b = self.bufs.get(k)
        if b is None:
            b = Buf(str(k))
            self.bufs[k] = b
        return b

    def _wait(self, e, k, v):
        if isinstance(k, str):
            self.waited[k].add(v)
            rv = v if self.needed is None else self.vmap[k][v]
        else:
            rv = v
        self.eng[e].wait_ge(self.sem[k], rv)
        self.known[e][k] = v

    def _waits(self, e, reads, writes):
        need = {}
        for b in reads:
            b = self.B(b)
            if b.w is not None:
                k, v = b.w
                if need.get(k, 0) < v:
                    need[k] = v
        for b in writes:
            b = self.B(b)
            if b.w is not None:
                k, v = b.w
                if need.get(k, 0) < v:
                    need[k] = v
            for k, v in b.r.items():
                if need.get(k, 0) < v:
                    need[k] = v
        kn = self.known[e]
        for k, v in need.items():
            if k == e and e == "pe":
                continue
            if kn.get(k, 0) >= v:
                continue
            self._wait(e, k, v)

    def _done(self, key, val, reads, writes):
        for b in writes:
            b = self.B(b)
            b.w = (key, val)
            b.r = {}
        for b in reads:
            b = self.B(b)
            if b.r.get(key, 0) < val:
                b.r[key] = val

    def op(self, e, fn, reads=(), writes=()):
        self._waits(e, reads, writes)
        inst = fn(self.eng[e])
        self.cnt[e] += 1
        v = self.cnt[e]
        if self.needed is None or v in self.needed[e]:
            self.real[e] += 1
            self.vmap[e][v] = self.real[e]
            inst.then_inc(self.sem[e], 1)
        self._done(e, v, reads, writes)
        self.ninst += 1
        return inst

    def dma(self, out, in_, reads=(), writes=(), q="sp", fn=None, **kw):
        sems, i = self.dq[q]
        k = sems[i % len(sems)]
        self.dq[q][1] = i + 1
        kn = self.known[q]
        if kn.get(k, 0) < self.cnt[k]:
            self._wait(q, k, self.cnt[k])
        self._waits(q, reads, writes)
        if fn is not None:
            inst = fn(self.eng[q])
        else:
            inst = self.eng[q].dma_start(out=out, in_=in_, **kw)
        self.cnt[k] += 16
        inst.then_inc(self.sem[k], 16)
        self._done(k, self.cnt[k], reads, writes)
        self.ninst += 1
        return inst

    def barrier(self):
        for e in self.eng:
            kn = self.known[e]
            for k, v in self.cnt.items():
                if v == 0 or kn.get(k, 0) >= v:
                    continue
                if k == e and e == "pe":
                    continue
                self._wait(e, k, v)
        self.bufs = {k: b for k, b in self.bufs.items() if isinstance(k, (str, tuple))}
        for b in self.bufs.values():
            b.w = None
            b.r = {}

    def mm(self, out, lhsT, rhs, start=True, stop=True, reads=(), writes=(), **kw):
        return self.op("pe", lambda E: E.matmul(out, lhsT, rhs, start=start, stop=stop, **kw), reads, writes)

    def tr(self, out, in_, ident, reads=(), writes=()):
        return self.op("pe", lambda E: E.transpose(out, in_, ident), reads, writes)

    def act(self, out, in_, func, reads=(), writes=(), **kw):
        return self.op("act", lambda E: E.activation(out, in_, func, **kw), reads, writes)

    def tt(self, e, out, in0, in1, op, reads=(), writes=()):
        return self.op(e, lambda E: E.tensor_tensor(out, in0, in1, op), reads, writes)

    def ts(self, e, out, in0, s1, op0, s2=None, op1=None, reads=(), writes=(), **kw):
        if op1 is None:
            return self.op(e, lambda E: E.tensor_scalar(out, in0, s1, None, op0, **kw), reads, writes)
        return self.op(e, lambda E: E.tensor_scalar(out, in0, s1, s2, op0, op1, **kw), reads, writes)

    def stt(self, e, out, in0, scalar, in1, op0, op1, reads=(), writes=()):
        return self.op(e, lambda E: E.scalar_tensor_tensor(out, in0, scalar, in1, op0, op1), reads, writes)

    def cp(self, e, out, in_, reads=(), writes=()):
        if e == "act":
            return self.op(e, lambda E: E.copy(out, in_), reads, writes)
        return self.op(e, lambda E: E.tensor_copy(out, in_), reads, writes)

    def memset(self, e, ap, val, writes=()):
        return self.op(e, lambda E: E.memset(ap, val), (), writes)


def _rope_tables():
    half = 16
    inv = 10000.0 ** (-np.arange(0, half, 2, dtype=np.float64) / half)
    i = np.arange(NLAT)
    row = (i // 64).astype(np.float64)
    col = (i % 64).astype(np.float64)
    ar = row[None, :] * inv[:, None]
    ac = col[None, :] * inv[:, None]
    cos = np.ones((32, T), np.float64)
    sin = np.zeros((32, T), np.float64)
    cos[0:8, NCTX:] = np.cos(ar); cos[8:16, NCTX:] = np.cos(ar); cos[16:24, NCTX:] = np.cos(ac); cos[24:32, NCTX:] = np.cos(ac)
    sin[0:8, NCTX:] = np.sin(ar); sin[8:16, NCTX:] = np.sin(ar); sin[16:24, NCTX:] = np.sin(ac); sin[24:32, NCTX:] = np.sin(ac)
    return cos.astype(np.float32), sin.astype(np.float32)


def _hy_tables(n):
    N2 = 2 * n
    ntc = n // 128
    F = n + 1
    nj = (F + 127) // 128
    t = np.arange(n, dtype=np.float64)
    tt_ = t / (n - 1)
    bands = 16
    f = np.linspace(1e-4, bands - 1, bands)
    ang = (2.0 * math.pi * t / n)[:, None] * f
    z = np.concatenate([tt_[:, None], np.cos(ang), -np.sin(ang)], axis=-1)
    lo, hi = math.log(1e-2) / 1.5, math.log(1e-2) / 0.3
    deltas = np.abs(np.linspace(lo, hi, 256))
    decay = np.exp(-tt_[None, :] * deltas[:, None])
    fi = np.arange(nj * 128, dtype=np.float64)
    valid = (fi <= n).astype(np.float64)
    ph = 2.0 * math.pi * np.outer(t, fi) / N2
    cosm = np.cos(ph) * valid[None, :]
    sinm = -np.sin(ph) * valid[None, :]
    def fwd_layout(m):
        return m.reshape(ntc, 128, nj, 128).transpose(2, 1, 0, 3)
    dfwd = np.concatenate([fwd_layout(cosm), fwd_layout(sinm)], axis=0).astype(ml_dtypes.bfloat16)
    wf = np.where((fi == 0) | (fi == n), 1.0, 2.0) * valid / N2
    icos = (np.cos(ph) * wf[None, :]).T
    isin = (-np.sin(ph) * wf[None, :]).T
    def inv_layout(m):
        return m.reshape(nj, 128, ntc, 128).transpose(2, 1, 0, 3)
    dinv = np.concatenate([inv_layout(icos), inv_layout(isin)], axis=2).astype(ml_dtypes.bfloat16)
    return (np.ascontiguousarray(z.T.astype(np.float32)), np.ascontiguousarray(decay.astype(np.float32)),
            np.ascontiguousarray(dfwd), np.ascontiguousarray(dinv))


def const_inputs():
    c = {}
    c["iota_e"] = np.ascontiguousarray(np.broadcast_to(np.arange(32, dtype=np.float32)[None, :], (128, 32)))
    c["iota_b"] = np.ascontiguousarray(np.broadcast_to(np.arange(128, dtype=np.float32)[None, :], (128, 128)))
    c["iota_p"] = np.arange(128, dtype=np.float32).reshape(128, 1)
    c["ltri"] = np.triu(np.ones((128, 128), np.float32), k=1).astype(ml_dtypes.bfloat16)
    for nm, n in (("l", NLAT), ("c", NCTX)):
        z, dec, dfwd, dinv = _hy_tables(n)
        c["hy_zpos_" + nm] = z
        c["hy_decay_" + nm] = dec
        c["hy_dfwd_" + nm] = dfwd
        c["hy_dinv_" + nm] = dinv
    c["ident"] = np.eye(128, dtype=np.float32)
    cos, sin = _rope_tables()
    c["rope_cos"] = cos
    c["rope_sin"] = sin
    return c


def prep_inputs(inp):
    cst = const_inputs()
    shared = dict(cst)
    for k in ("ada_w", "ada_b", "norm1_g", "norm2_g", "w_in"):
        shared[k] = np.ascontiguousarray(inp[k], dtype=np.float32)
    def lane(a):
        a = np.asarray(a, np.float32)
        Ld = a.shape[0]
        rest = a.shape[4:]
        a = a.reshape((Ld, 2, 8, 2, 64) + rest)
        nd = a.ndim
        a = a.transpose((0, 3, 4, 1, 2) + tuple(range(5, nd)))
        return np.ascontiguousarray(a.reshape((Ld, 128, 16) + rest))
    shared["s5_lre"] = lane(inp["s5_lambda_re"])
    shared["s5_lim"] = lane(inp["s5_lambda_im"])
    shared["s5_ldt"] = lane(np.broadcast_to(np.asarray(inp["s5_log_dt"], np.float32)[..., None], (DEPTH, 2, 16, 64)))
    shared["s5_bre"] = lane(inp["s5_b_re"])
    shared["s5_bim"] = lane(inp["s5_b_im"])
    shared["s5_cre"] = lane(np.asarray(inp["s5_c_re"], np.float32).transpose(0, 1, 2, 4, 3))
    shared["s5_cim"] = lane(np.asarray(inp["s5_c_im"], np.float32).transpose(0, 1, 2, 4, 3))
    shared["s5_dcol"] = np.ascontiguousarray(np.asarray(inp["s5_d"], np.float32).reshape(DEPTH, 2, 128).transpose(0, 2, 1))
    shared["s5_glu_w"] = np.ascontiguousarray(inp["s5_glu_w"], dtype=np.float32)
    shared["mla_qg"] = np.ascontiguousarray(np.asarray(inp["mla_q_norm_g"], np.float32).reshape(DEPTH, 3, 128).transpose(0, 2, 1))
    shared["mla_kvg"] = np.ascontiguousarray(np.asarray(inp["mla_kv_norm_g"], np.float32).reshape(DEPTH, 2, 128).transpose(0, 2, 1))
    shared["mla_w_uq"] = np.ascontiguousarray(inp["mla_w_uq"], dtype=np.float32)
    shared["mla_w_ukv"] = np.ascontiguousarray(inp["mla_w_ukv"], dtype=np.float32)
    shared["mix_norm_g"] = np.ascontiguousarray(inp["mix_norm_g"], dtype=np.float32)
    shared["hy_sw"] = np.ascontiguousarray(np.asarray(inp["hy_short_w"], np.float32).reshape(DEPTH, 3, 6, 128).transpose(0, 3, 2, 1))
    shared["hy_sb"] = np.ascontiguousarray(np.asarray(inp["hy_short_b"], np.float32).reshape(DEPTH, 6, 128).transpose(0, 2, 1))
    shared["hy_cols"] = np.ascontiguousarray(np.stack([inp["hy_f_b1"], inp["hy_f_b2"], inp["hy_f_freq"][:, 0], inp["hy_f_freq"][:, 1]], axis=-1), dtype=np.float32)
    shared["hy_b3"] = np.ascontiguousarray(np.asarray(inp["hy_f_b3"], np.float32).reshape(DEPTH, 8, 128).transpose(0, 2, 1))
    for k in ("hy_f_w1", "hy_f_w2", "hy_f_w3", "hy_bias"):
        shared[k] = np.ascontiguousarray(inp[k], dtype=np.float32)
    shared["mix_g_s5col"] = np.ascontiguousarray(np.asarray(inp["mix_norm_g"], np.float32)[:, 0:256].reshape(DEPTH, 2, 128).transpose(0, 2, 1))
    shared["w_out"] = np.ascontiguousarray(inp["w_out"], dtype=np.float32)
    shared["moe_wr"] = np.ascontiguousarray(np.concatenate([inp["moe_w_group"], inp["moe_w_expert"]], axis=-1), dtype=np.float32)
    shared["moe_w1"] = np.ascontiguousarray(np.asarray(inp["moe_w1"], np.float32).reshape(DEPTH, 32, 8, 128, 512).transpose(0, 1, 3, 2, 4)).reshape(DEPTH * 32 * 128, 4096)
    shared["moe_w3"] = np.ascontiguousarray(np.asarray(inp["moe_w3"], np.float32).reshape(DEPTH, 32, 8, 128, 512).transpose(0, 1, 3, 2, 4)).reshape(DEPTH * 32 * 128, 4096)
    shared["moe_w2"] = np.ascontiguousarray(np.asarray(inp["moe_w2"], np.float32).reshape(DEPTH, 32, 4, 128, 1024).transpose(0, 1, 3, 2, 4)).reshape(DEPTH * 32 * 128, 4096)
    shared["final_g"] = np.ascontiguousarray(np.asarray(inp["final_g"], np.float32).reshape(1, DM))
    maps = []
    for b in range(8):
        m = dict(shared)
        m["x"] = np.ascontiguousarray(inp["x"][b])
        m["ctx"] = np.ascontiguousarray(inp["ctx"][b])
        cc = np.stack([inp["c"][b], inp["c_ctx"]], axis=0)
        m["cc"] = np.ascontiguousarray(cc.reshape(2, 8, 128).transpose(2, 1, 0))
        maps.append(m)
    return maps


class Prog:
    def __init__(self, debug=(), needed=None):
        self.kb = KB(needed)
        self.nc = self.kb.nc
        self.debug = set(debug)
        self.I = {}
        self.S = {}
        self.outs = []

    def inp(self, name, shape, dt=F32):
        t = self.nc.dram_tensor(name, list(shape), dt, kind="ExternalInput")
        self.I[name] = t
        return t

    def scratch(self, name, shape, dt=F32):
        kind = "ExternalOutput" if name in self.debug else "Internal"
        t = self.nc.dram_tensor(name, list(shape), dt, kind=kind)
        self.S[name] = t
        if kind == "ExternalOutput":
            self.outs.append(name)
        return t

    def declare(self):
        self.inp("x", [NLAT, DM]); self.inp("ctx", [NCTX, DM]); self.inp("cc", [128, 8, 2])
        self.inp("ada_w", [DEPTH, DM, 6 * DM]); self.inp("ada_b", [DEPTH, 6 * DM])
        self.inp("norm1_g", [DEPTH, DM]); self.inp("norm2_g", [DEPTH, DM])
        self.inp("w_in", [DEPTH, DM, P_IN])
        self.inp("ident", [128, 128]); self.inp("rope_cos", [32, T]); self.inp("rope_sin", [32, T])
        for nm in ("s5_lre", "s5_lim", "s5_ldt"):
            self.inp(nm, [DEPTH, 128, 16])
        for nm in ("s5_bre", "s5_bim", "s5_cre", "s5_cim"):
            self.inp(nm, [DEPTH, 128, 16, 16])
        self.inp("s5_dcol", [DEPTH, 128, 2]); self.inp("s5_glu_w", [DEPTH, 256, 256])
        self.inp("mla_qg", [DEPTH, 128, 3]); self.inp("mla_kvg", [DEPTH, 128, 2])
        self.inp("mla_w_uq", [DEPTH, 384, 768]); self.inp("mla_w_ukv", [DEPTH, 256, 1024])
        self.inp("mix_norm_g", [DEPTH, DM])
        self.scratch("MT", [DM, T], BF16)
        if "MLAO" in self.debug:
            self.scratch("MLAO", [T, 512])
        for nm, n in (("l", NLAT), ("c", NCTX)):
            nj = (n + 1 + 127) // 128
            self.inp("hy_zpos_" + nm, [33, n]); self.inp("hy_decay_" + nm, [256, n])
            self.inp("hy_dfwd_" + nm, [2 * nj, 128, n // 128, 128], BF16); self.inp("hy_dinv_" + nm, [n // 128, 128, 2 * nj, 128], BF16)
            self.scratch("KSPEC_" + nm, [nj, 2, 128, 512])
        self.inp("hy_sw", [DEPTH, 128, 6, 3]); self.inp("hy_sb", [DEPTH, 128, 6]); self.inp("hy_cols", [DEPTH, 64, 4]); self.inp("hy_b3", [DEPTH, 128, 8])
        self.inp("hy_f_w1", [DEPTH, 33, 64]); self.inp("hy_f_w2", [DEPTH, 64, 64]); self.inp("hy_f_w3", [DEPTH, 64, 1024]); self.inp("hy_bias", [DEPTH, 2, 256])
        self.scratch("HZTM", [T, 768]); self.scratch("HY1", [T, 256])
        if "HYO" in self.debug:
            self.scratch("HYO", [T, 256])
        self.inp("iota_e", [128, 32]); self.inp("iota_b", [128, 128]); self.inp("iota_p", [128, 1]); self.inp("ltri", [128, 128], BF16)
        self.inp("mix_g_s5col", [DEPTH, 128, 2])
        self.inp("w_out", [DEPTH, DM, DM]); self.inp("moe_wr", [DEPTH, DM, 36])
        for nm_ in ("moe_w1", "moe_w3", "moe_w2"):
            self.inp(nm_, [DEPTH * 32 * 128, 4096])
        self.inp("final_g", [1, DM])
        self.scratch("FB", [T, DM], BF16); self.scratch("XS", [NBLK * 128, DM], BF16); self.scratch("YS", [NBLK * 128, DM])
        if "XMID" in self.debug:
            self.scratch("XMID", [T, DM])
        if "YMOE" in self.debug:
            self.scratch("YMOE", [T, DM])
        if "DBGR" in self.debug:
            self.scratch("DBGR", [128, NT * 6 + 64])
        self.out = self.nc.dram_tensor("out", [NLAT, DM], F32, kind="ExternalOutput")
        self.scratch("S5T", [256, T])
        self.scratch("X", [T, DM])
        self.scratch("MOD", [2, 6 * DM])
        self.scratch("UT", [256, T]); self.scratch("CQT", [384, T]); self.scratch("CKVT", [256, T])
        self.scratch("KRT", [32, T]); self.scratch("HZT", [768, T])
        if "HL" in self.debug:
            self.scratch("HL", [T, DM])

    def stage_init(self):
        kb = self.kb
        X = self.S["X"]
        kb.dma(X.ap()[0:NCTX, :], self.I["ctx"].ap(), writes=[("X", 0), ("X", 1)])
        for j in range(4):
            kb.dma(X.ap()[NCTX + j * 1024: NCTX + (j + 1) * 1024, :], self.I["x"].ap()[j * 1024:(j + 1) * 1024, :],
                   writes=[("X", 2 + 8 * j + i) for i in range(8)])

    def stage_mod(self, L):
        kb = self.kb
        with ExitStack() as es:
            cc = kb.sb(es, [128, 8, 2], F32, "cc")
            act = kb.sb(es, [128, 8, 2], F32, "act")
            bb = kb.sb(es, [2, 6 * DM], F32, "adab")
            mod = kb.sb(es, [2, 6 * DM], F32, "mod")
            wt = [kb.sb(es, [128, 8, 512], F32, "adaw%d" % i) for i in range(2)]
            kb.dma(cc[:], self.I["cc"].ap(), writes=[cc])
            kb.dma(bb[:], self.I["ada_b"].ap()[L:L + 1, :].partition_broadcast(2), writes=[bb])
            kb.act(act[:], cc[:], AF.Silu, reads=[cc], writes=[act])
            wv = self.I["ada_w"].ap()[L].rearrange("(kc p) n -> p kc n", p=128)
            for nb in range(12):
                w = wt[nb % 2]
                kb.dma(w[:], wv[:, :, nb * 512:(nb + 1) * 512], writes=[w])
                ps = kb.bank[nb % 2]
                for kc in range(8):
                    kb.mm(ps[0:2, :], act[:, kc, :], w[:, kc, :], start=(kc == 0), stop=(kc == 7),
                          reads=[act, w], writes=[ps])
                kb.tt("dve", mod[:, nb * 512:(nb + 1) * 512], ps[0:2, :], bb[:, nb * 512:(nb + 1) * 512], OP.add,
                      reads=[ps, bb], writes=[mod])
            kb.dma(self.S["MOD"].ap(), mod[:], reads=[mod], writes=["MOD"])
        kb.barrier()

    def load_mod_bc(self, es_tiles, L, which, idx_g, g_name):
        kb = self.kb
        G, S, tmp = es_tiles
        MOD = self.S["MOD"].ap()
        kb.dma(S[:], MOD[which:which + 1, idx_g * DM:(idx_g + 1) * DM].partition_broadcast(128), reads=["MOD"], writes=[S])
        kb.dma(tmp[:], MOD[which:which + 1, (idx_g + 1) * DM:(idx_g + 2) * DM].partition_broadcast(128), reads=["MOD"], writes=[tmp])
        kb.dma(G[:], self.I[g_name].ap()[L:L + 1, :].partition_broadcast(128), writes=[G])
        kb.stt("dve", G[:], tmp[:], 1.0, G[:], OP.add, OP.mult, reads=[tmp, G], writes=[G])

    def rms_modulate(self, xt, G, S, sq, ss, rstd, tmp, out, out2=None):
        kb = self.kb
        kb.act(sq[:], xt[:], AF.Square, accum_out=ss[:], reads=[xt], writes=[sq, ss])
        kb.ts("dve", rstd[:], ss[:], 1.0 / DM, OP.mult, EPS, OP.add, reads=[ss], writes=[rstd])
        kb.act(rstd[:], rstd[:], AF.Sqrt, reads=[rstd], writes=[rstd])
        kb.op("dve", lambda E: E.reciprocal(rstd[:], rstd[:]), reads=[rstd], writes=[rstd])
        kb.stt("dve", tmp[:], xt[:], rstd[:, 0:1], G[:], OP.mult, OP.mult, reads=[xt, rstd, G], writes=[tmp])
        kb.tt("pool", out[:], tmp[:], S[:], OP.add, reads=[tmp, S], writes=[out])
        if out2 is not None:
            kb.cp("act", out2[:], out[:], reads=[out], writes=[out2])

    def stage_proj(self, L):
        kb = self.kb
        X = self.S["X"].ap()
        with ExitStack() as es:
            ident_f = kb.sb(es, [128, 128], F32, "identf")
            ident = kb.sb(es, [128, 128], BF16, "identb")
            kb.dma(ident_f[:], self.I["ident"].ap(), writes=[ident_f])
            kb.cp("dve", ident[:], ident_f[:], reads=[ident_f], writes=[ident])
            wf = kb.sb(es, [128, 8, P_IN], F32, "winf")
            wb = kb.sb(es, [128, 8, P_IN + 32], BF16, "winb")
            kb.dma(wf[:], self.I["w_in"].ap()[L].rearrange("(kc p) n -> p kc n", p=128), writes=[wf])
            kb.cp("act", wb[:, :, 0:P_IN], wf[:], reads=[wf], writes=[wb])
            K0 = 896
            kb.ts("dve", wb[:, :, P_IN + 0:P_IN + 8], wf[:, :, K0 + 8:K0 + 16], -1.0, OP.mult, reads=[wf], writes=[wb])
            kb.cp("dve", wb[:, :, P_IN + 8:P_IN + 16], wf[:, :, K0 + 0:K0 + 8], reads=[wf], writes=[wb])
            kb.ts("dve", wb[:, :, P_IN + 16:P_IN + 24], wf[:, :, K0 + 24:K0 + 32], -1.0, OP.mult, reads=[wf], writes=[wb])
            kb.cp("dve", wb[:, :, P_IN + 24:P_IN + 32], wf[:, :, K0 + 16:K0 + 24], reads=[wf], writes=[wb])
            rc = kb.sb(es, [32, T], F32, "ropec")
            rs = kb.sb(es, [32, T], F32, "ropes")
            kb.dma(rc[:], self.I["rope_cos"].ap(), writes=[rc])
            kb.dma(rs[:], self.I["rope_sin"].ap(), writes=[rs])
            G = kb.sb(es, [128, DM], F32, "G"); S = kb.sb(es, [128, DM], F32, "S"); tmpb = kb.sb(es, [128, DM], F32, "tmpb")
            xt = [kb.sb(es, [128, DM], F32, "xt%d" % i) for i in range(2)]
            sq = kb.sb(es, [128, DM], BF16, "sq")
            ss = kb.sb(es, [128, 1], F32, "ss"); rstd = kb.sb(es, [128, 1], F32, "rstd")
            tmp = kb.sb(es, [128, DM], F32, "tmp")
            hb = [kb.sb(es, [128, DM], BF16, "hb%d" % i) for i in range(2)]
            hf = kb.sb(es, [128, DM], F32, "hf") if "HL" in self.debug else None
            hT = [kb.sb(es, [128, 8, 512], BF16, "hT%d" % i) for i in range(2)]
            stg = [kb.sb(es, [128, 512], F32, "stg%d" % i) for i in range(3)]
            kr1 = kb.sb(es, [32, 512], F32, "kr1")
            nstg = 0
            chunks = []
            for i in range(2):
                chunks.append(("UT", i * 128, i * 128, 128))
            for i in range(3):
                chunks.append(("CQT", i * 128, 256 + i * 128, 128))
            for i in range(2):
                chunks.append(("CKVT", i * 128, 640 + i * 128, 128))
            for i in range(6):
                chunks.append(("HZT", i * 128, 928 + i * 128, 128))
            blocks = [(0, 2)] + [(2 + 4 * i, 4) for i in range(8)]
            nmm = 0
            for bi, (t0, ntl) in enumerate(blocks):
                if bi == 0:
                    self.load_mod_bc((G, S, tmpb), L, 1, 0, "norm1_g")
                elif bi == 1:
                    self.load_mod_bc((G, S, tmpb), L, 0, 0, "norm1_g")
                hTb = hT[bi % 2]
                nb = ntl * 128
                c0 = t0 * 128
                for j in range(ntl):
                    t = t0 + j
                    x_ = xt[t % 2]
                    kb.dma(x_[:], X[t * 128:(t + 1) * 128, :], reads=[("X", t)], writes=[x_])
                    h_ = hb[t % 2]
                    if hf is not None:
                        self.rms_modulate(x_, G, S, sq, ss, rstd, tmp, hf, h_)
                        kb.dma(self.S["HL"].ap()[t * 128:(t + 1) * 128, :], hf[:], reads=[hf], writes=["HL"])
                    else:
                        self.rms_modulate(x_, G, S, sq, ss, rstd, tmp, h_)
                    pb = kb.bank[2 + (t % 2)]
                    pbv = pb[:].bitcast(BF16)
                    for kc in range(8):
                        kb.tr(pbv[:, kc * 128:(kc + 1) * 128], h_[:, kc * 128:(kc + 1) * 128], ident[:],
                              reads=[h_, ident], writes=[pb])
                    kb.cp("dve" if t % 2 else "act", hTb[:, :, j * 128:(j + 1) * 128],
                          pbv.rearrange("p (k n) -> p k n", k=8), reads=[pb], writes=[hTb])
                for (dn, r0, col0, M) in chunks:
                    ps = kb.bank[4 + (nmm % 2)]
                    nmm += 1
                    for kc in range(8):
                        kb.mm(ps[0:M, 0:nb], wb[:, kc, col0:col0 + M], hTb[:, kc, 0:nb], start=(kc == 0), stop=(kc == 7),
                              reads=[wb, hTb], writes=[ps])
                    st = stg[nstg % 3]
                    nstg += 1
                    kb.cp("act" if nstg % 2 else "dve", st[0:M, 0:nb], ps[0:M, 0:nb], reads=[ps], writes=[st])
                    kb.dma(self.S[dn].ap()[r0:r0 + M, c0:c0 + nb], st[0:M, 0:nb], reads=[st], writes=[(dn, bi)], q="pool")
                ps = kb.bank[6]
                ps2 = kb.bank[7]
                for kc in range(8):
                    kb.mm(ps[0:32, 0:nb], wb[:, kc, 896:928], hTb[:, kc, 0:nb], start=(kc == 0), stop=(kc == 7),
                          reads=[wb, hTb], writes=[ps])
                for kc in range(8):
                    kb.mm(ps2[0:32, 0:nb], wb[:, kc, P_IN:P_IN + 32], hTb[:, kc, 0:nb], start=(kc == 0), stop=(kc == 7),
                          reads=[wb, hTb], writes=[ps2])
                st = stg[nstg % 3]
                nstg += 1
                kb.tt("dve", kr1[:, 0:nb], ps[0:32, 0:nb], rc[:, c0:c0 + nb], OP.mult, reads=[ps, rc], writes=[kr1])
                kb.tt("dve", st[0:32, 0:nb], ps2[0:32, 0:nb], rs[:, c0:c0 + nb], OP.mult, reads=[ps2, rs], writes=[st])
                kb.tt("dve", st[0:32, 0:nb], st[0:32, 0:nb], kr1[:, 0:nb], OP.add, reads=[st, kr1], writes=[st])
                kb.dma(self.S["KRT"].ap()[:, c0:c0 + nb], st[0:32, 0:nb], reads=[st], writes=[("KRT", bi)], q="pool")
        kb.barrier()


    def sin_rr(self, e, out, in_, add, t1, t2):
        kb = self.kb
        kb.ts(e, t1, in_, add, OP.add, reads=[in_.tensor], writes=[t1.tensor])
        kb.ts(e, t2, t1, 1.0 / (2 * math.pi), OP.mult, MAGIC, OP.add, reads=[t1.tensor], writes=[t2.tensor])
        kb.ts(e, t2, t2, -MAGIC, OP.add, -2 * math.pi, OP.mult, reads=[t2.tensor], writes=[t2.tensor])
        kb.tt(e, t1, t1, t2, OP.add, reads=[t1.tensor, t2.tensor], writes=[t1.tensor])
        kb.ts(e, t1, t1, math.pi, OP.min, -math.pi, OP.max, reads=[t1.tensor], writes=[t1.tensor])
        kb.act(out, t1, AF.Sin, reads=[t1.tensor], writes=[out.tensor])

    def stage_s5(self, L):
        kb = self.kb
        I = self.I
        blocks = [(0, 256)] + [(256 + 512 * i, 512) for i in range(8)]
        with ExitStack() as es:
            ident_f = kb.sb(es, [128, 128], F32, "identf")
            kb.dma(ident_f[:], I["ident"].ap(), writes=[ident_f])
            sm = {}
            for nm in ("lre", "lim", "ldt"):
                sm[nm] = kb.sb(es, [128, 16], F32, nm)
                kb.dma(sm[nm][:], I["s5_" + nm].ap()[L], writes=[sm[nm]])
            for nm in ("dt", "a", "th", "r", "cs", "sn", "t1", "t2", "rc", "rsn", "den", "cre", "cim", "x1", "x2"):
                sm[nm] = kb.sb(es, [128, 16], F32, nm)
            big = {}
            for nm in ("bre", "bim", "cre", "cim"):
                big[nm] = kb.sb(es, [128, 16, 16], F32, "s5" + nm)
                kb.dma(big[nm][:], I["s5_" + nm].ap()[L], writes=[big[nm]])
            A = lambda nm: sm[nm][:]
            kb.act(A("dt"), A("ldt"), AF.Exp, reads=[sm["ldt"]], writes=[sm["dt"]])
            kb.tt("dve", A("a"), A("lre"), A("dt"), OP.mult, reads=[sm["lre"], sm["dt"]], writes=[sm["a"]])
            kb.tt("dve", A("th"), A("lim"), A("dt"), OP.mult, reads=[sm["lim"], sm["dt"]], writes=[sm["th"]])
            kb.act(A("r"), A("a"), AF.Exp, reads=[sm["a"]], writes=[sm["r"]])
            self.sin_rr("dve", A("sn"), A("th"), 0.0, A("t1"), A("t2"))
            self.sin_rr("dve", A("cs"), A("th"), math.pi / 2, A("t1"), A("t2"))
            kb.tt("dve", A("rc"), A("r"), A("cs"), OP.mult, reads=[sm["r"], sm["cs"]], writes=[sm["rc"]])
            kb.tt("dve", A("rsn"), A("r"), A("sn"), OP.mult, reads=[sm["r"], sm["sn"]], writes=[sm["rsn"]])
            kb.ts("dve", A("rc"), A("rc"), -1.0, OP.add, reads=[sm["rc"]], writes=[sm["rc"]])
            kb.tt("dve", A("den"), A("lre"), A("lre"), OP.mult, reads=[sm["lre"]], writes=[sm["den"]])
            kb.tt("dve", A("x1"), A("lim"), A("lim"), OP.mult, reads=[sm["lim"]], writes=[sm["x1"]])
            kb.tt("dve", A("den"), A("den"), A("x1"), OP.add, reads=[sm["den"], sm["x1"]], writes=[sm["den"]])
            kb.op("dve", lambda E: E.reciprocal(A("den"), A("den")), reads=[sm["den"]], writes=[sm["den"]])
            kb.tt("dve", A("x1"), A("rc"), A("lre"), OP.mult, reads=[sm["rc"], sm["lre"]], writes=[sm["x1"]])
            kb.tt("dve", A("x2"), A("rsn"), A("lim"), OP.mult, reads=[sm["rsn"], sm["lim"]], writes=[sm["x2"]])
            kb.tt("dve", A("x1"), A("x1"), A("x2"), OP.add, reads=[sm["x1"], sm["x2"]], writes=[sm["x1"]])
            kb.tt("dve", A("cre"), A("x1"), A("den"), OP.mult, reads=[sm["x1"], sm["den"]], writes=[sm["cre"]])
            kb.tt("dve", A("x1"), A("rsn"), A("lre"), OP.mult, reads=[sm["rsn"], sm["lre"]], writes=[sm["x1"]])
            kb.tt("dve", A("x2"), A("rc"), A("lim"), OP.mult, reads=[sm["rc"], sm["lim"]], writes=[sm["x2"]])
            kb.tt("dve", A("x1"), A("x1"), A("x2"), OP.subtract, reads=[sm["x1"], sm["x2"]], writes=[sm["x1"]])
            kb.tt("dve", A("cim"), A("x1"), A("den"), OP.mult, reads=[sm["x1"], sm["den"]], writes=[sm["cim"]])
            ub = kb.sb(es, [128, 2, T], BF16, "ub")
            yacc = kb.sb(es, [128, 2, T], F32, "yacc")
            Er = kb.sb(es, [128, T], F32, "Er"); Ei = kb.sb(es, [128, T], F32, "Ei")
            for c, stg_ in enumerate((Er, Ei)):
                kb.dma(stg_[:], self.S["UT"].ap()[c * 128:(c + 1) * 128, :], reads=[("UT", i) for i in range(9)], writes=[stg_])
                kb.cp("act", ub[:, c, :], stg_[:], reads=[stg_], writes=[ub])
            esm = ExitStack()
            Ebr = [kb.sb(esm, [128, T], BF16, "Ebr%d" % i) for i in range(2)]
            Ebi = [kb.sb(esm, [128, T], BF16, "Ebi%d" % i) for i in range(2)]
            bur = kb.sb(esm, [128, T], BF16, "bur"); bui = kb.sb(esm, [128, T], BF16, "bui")
            vr = kb.sb(esm, [128, T], BF16, "vr"); vi = kb.sb(esm, [128, T], BF16, "vi")
            m1 = kb.sb(esm, [128, T], BF16, "m1"); m2 = kb.sb(esm, [128, T], BF16, "m2")
            ZB = [kb.sb(esm, [128, 128], F32, "ZB%d" % i) for i in range(2)]
            LBs = [[kb.sb(esm, [128, 128], BF16, "LB%d_%d" % (p_, i)) for i in range(2)] for p_ in range(2)]
            LCs = [[kb.sb(esm, [128, 128], BF16, "LC%d_%d" % (p_, i)) for i in range(3)] for p_ in range(2)]
            ytmp = [kb.sb(esm, [128, 512], F32, "ytmp%d" % i) for i in range(2)]
            bt = kb.sb(esm, [128, 16], F32, "bt")
            etmp = kb.sb(esm, [128, 2048], F32, "etmp"); etmp2 = kb.sb(esm, [128, 2048], F32, "etmp2")
            NPW = 13
            WR = kb.sb(esm, [128, NPW, 16], F32, "WR"); WI = kb.sb(esm, [128, NPW, 16], F32, "WI"); wt = kb.sb(esm, [128, 16], F32, "wt")
            kb.cp("dve", WR[:, 0, :], sm["cs"][:], reads=[sm["cs"]], writes=[WR])
            kb.cp("dve", WI[:, 0, :], sm["sn"][:], reads=[sm["sn"]], writes=[WI])
            for k in range(1, NPW):
                kb.tt("dve", wt[:], WI[:, k - 1, :], WI[:, k - 1, :], OP.mult, reads=[WI], writes=[wt])
                kb.tt("dve", WI[:, k, :], WR[:, k - 1, :], WI[:, k - 1, :], OP.mult, reads=[WR, WI], writes=[WI])
                kb.ts("dve", WI[:, k, :], WI[:, k, :], 2.0, OP.mult, reads=[WI], writes=[WI])
                kb.tt("dve", WR[:, k, :], WR[:, k - 1, :], WR[:, k - 1, :], OP.mult, reads=[WR], writes=[WR])
                kb.tt("dve", WR[:, k, :], WR[:, k, :], wt[:], OP.subtract, reads=[WR, wt], writes=[WR])
            first_in_chunk = {0: True, 1: True}

            def prep(lt):
                d, gp = lt // 8, lt % 8
                c0 = 32 * (gp % 4)
                LB = LBs[lt % 2]; LC = LCs[lt % 2]
                for ri, (nm_a, nm_b, op2) in enumerate((("bre", "bim", OP.subtract), ("bim", "bre", OP.add))):
                    Z = ZB[ri]
                    kb.memset("pool", Z[:], 0.0, writes=[Z])
                    kb.ts("pool", bt[:], big[nm_b][:, lt, :], sm["cim"][:, lt:lt + 1], OP.mult, reads=[big[nm_b], sm["cim"]], writes=[bt])
                    for gl in range(2):
                        ps_ = slice(gl * 64, gl * 64 + 64)
                        kb.ts("pool", Z[ps_, c0 + gl * 16:c0 + gl * 16 + 16], big[nm_a][ps_, lt, :], sm["cre"][ps_, lt:lt + 1], OP.mult,
                              reads=[big[nm_a], sm["cre"]], writes=[Z])
                        kb.tt("pool", Z[ps_, c0 + gl * 16:c0 + gl * 16 + 16], Z[ps_, c0 + gl * 16:c0 + gl * 16 + 16], bt[ps_, :], op2,
                              reads=[Z, bt], writes=[Z])
                    pb = kb.bank[6 + ri]
                    kb.tr(pb[:, 0:128], Z[:], ident_f[:], reads=[Z, ident_f], writes=[pb])
                    kb.cp("pool" if False else "act", LB[ri][:], pb[:, 0:128], reads=[pb], writes=[LB[ri]])
                for ri, (nm, sgn) in enumerate((("cre", 1.0), ("cim", -1.0), ("cre", -1.0))):
                    kb.memset("pool", LC[ri][:], 0.0, writes=[LC[ri]])
                    for gl in range(2):
                        ps_ = slice(gl * 64, gl * 64 + 64)
                        kb.ts("pool", LC[ri][ps_, c0 + gl * 16:c0 + gl * 16 + 16], big[nm][ps_, lt, :], sgn, OP.mult,
                              reads=[big[nm]], writes=[LC[ri]])

            def tgen(lt):
                d = lt // 8
                lsl = slice(lt, lt + 1)
                kb.memset("pool", Er[:, 0:1], 1.0, writes=[Er])
                kb.memset("pool", Ei[:, 0:1], 0.0, writes=[Ei])
                kb.cp("pool", Er[:, 1:2], sm["cs"][:, lsl], reads=[sm["cs"]], writes=[Er])
                kb.cp("pool", Ei[:, 1:2], sm["sn"][:, lsl], reads=[sm["sn"]], writes=[Ei])
                n = 2
                k = 1
                while n < T:
                    m = min(n, T - n)
                    wr_ = WR[:, k, lsl]; wi_ = WI[:, k, lsl]
                    for o in range(0, m, 2048):
                        mm_ = min(2048, m - o)
                        kb.act(etmp[:, 0:mm_], Ei[:, o:o + mm_], AF.Copy, scale=wi_, reads=[Ei, WI], writes=[etmp])
                        kb.act(etmp2[:, 0:mm_], Er[:, o:o + mm_], AF.Copy, scale=wr_, reads=[Er, WR], writes=[etmp2])
                        kb.tt("pool", Er[:, n + o:n + o + mm_], etmp2[:, 0:mm_], etmp[:, 0:mm_], OP.subtract, reads=[etmp, etmp2, Er], writes=[Er])
                        kb.act(etmp[:, 0:mm_], Ei[:, o:o + mm_], AF.Copy, scale=wr_, reads=[Ei, WR], writes=[etmp])
                        kb.act(etmp2[:, 0:mm_], Er[:, o:o + mm_], AF.Copy, scale=wi_, reads=[Er, WI], writes=[etmp2])
                        kb.tt("pool", Ei[:, n + o:n + o + mm_], etmp2[:, 0:mm_], etmp[:, 0:mm_], OP.add, reads=[etmp, etmp2, Ei], writes=[Ei])
                    n *= 2
                    k += 1
                br_, bi_ = Ebr[lt % 2], Ebi[lt % 2]
                if d == 0:
                    kb.cp("act", br_[:], Er[:], reads=[Er], writes=[br_])
                    kb.cp("act", bi_[:], Ei[:], reads=[Ei], writes=[bi_])
                else:
                    for (Eb, E_) in ((br_, Er), (bi_, Ei)):
                        kb.cp("act", Eb[:, 0:NCTX], E_[:, 0:NCTX][:, ::-1], reads=[E_], writes=[Eb])
                        kb.cp("act", Eb[:, NCTX:T], E_[:, NCTX:T][:, ::-1], reads=[E_], writes=[Eb])

            def drive(lt):
                gp = lt % 8
                ch = gp // 4
                LB = LBs[lt % 2]
                for bi, (s0, nn) in enumerate(blocks):
                    pr = kb.bank[(bi % 2) * 2]
                    pi_ = kb.bank[(bi % 2) * 2 + 1]
                    kb.mm(pr[:, 0:nn], LB[0][:], ub[:, ch, s0:s0 + nn], reads=[LB[0], ub], writes=[pr])
                    kb.mm(pi_[:, 0:nn], LB[1][:], ub[:, ch, s0:s0 + nn], reads=[LB[1], ub], writes=[pi_])
                    kb.cp("act", bur[:, s0:s0 + nn], pr[:, 0:nn], reads=[pr], writes=[bur])
                    kb.cp("act", bui[:, s0:s0 + nn], pi_[:, 0:nn], reads=[pi_], writes=[bui])

            def rot_scan(lt):
                d = lt // 8
                br_, bi_ = Ebr[lt % 2], Ebi[lt % 2]
                kb.tt("dve", m1[:], br_[:], bur[:], OP.mult, reads=[br_, bur], writes=[m1])
                kb.tt("dve", m2[:], bi_[:], bui[:], OP.mult, reads=[bi_, bui], writes=[m2])
                kb.tt("dve", vr[:], m1[:], m2[:], OP.add, reads=[m1, m2], writes=[vr])
                kb.tt("dve", m1[:], br_[:], bui[:], OP.mult, reads=[br_, bui], writes=[m1])
                kb.tt("dve", m2[:], bi_[:], bur[:], OP.mult, reads=[bi_, bur], writes=[m2])
                kb.tt("dve", vi[:], m1[:], m2[:], OP.subtract, reads=[m1, m2], writes=[vi])
                rdec = sm["r"][:, lt:lt + 1]
                for (v_, g_) in ((vr, bur), (vi, bui)):
                    if d == 0:
                        kb.op("dve", lambda E: E.tensor_tensor_scan(g_[:], rdec.to_broadcast([128, T]), v_[:], 0.0, OP.mult, OP.add),
                              reads=[sm["r"], v_], writes=[g_])
                    else:
                        kb.op("dve", lambda E: E.tensor_tensor_scan(g_[:, 0:NCTX][:, ::-1], rdec.to_broadcast([128, NCTX]), v_[:, 0:NCTX][:, ::-1],
                                                                    0.0, OP.mult, OP.add), reads=[sm["r"], v_], writes=[g_])
                        kb.op("dve", lambda E: E.tensor_tensor_scan(g_[:, NCTX:T][:, ::-1], rdec.to_broadcast([128, NLAT]), v_[:, NCTX:T][:, ::-1],
                                                                    g_[:, 0:1], OP.mult, OP.add), reads=[sm["r"], v_, g_], writes=[g_])
                kb.tt("dve", m1[:], br_[:], bur[:], OP.mult, reads=[br_, bur], writes=[m1])
                kb.tt("dve", m2[:], bi_[:], bui[:], OP.mult, reads=[bi_, bui], writes=[m2])
                kb.tt("dve", vr[:], bi_[:], bur[:], OP.mult, reads=[bi_, bur], writes=[vr])
                kb.tt("dve", vi[:], br_[:], bui[:], OP.mult, reads=[br_, bui], writes=[vi])

            def readout(lt):
                gp = lt % 8
                ch = gp // 4
                LC = LCs[lt % 2]
                for bi, (s0, nn) in enumerate(blocks):
                    py = kb.bank[4 + (bi % 2)]
                    kb.mm(py[:, 0:nn], LC[0][:], m1[:, s0:s0 + nn], start=True, stop=False, reads=[LC[0], m1], writes=[py])
                    kb.mm(py[:, 0:nn], LC[2][:], m2[:, s0:s0 + nn], start=False, stop=False, reads=[LC[2], m2], writes=[py])
                    kb.mm(py[:, 0:nn], LC[1][:], vr[:, s0:s0 + nn], start=False, stop=False, reads=[LC[1], vr], writes=[py])
                    kb.mm(py[:, 0:nn], LC[1][:], vi[:, s0:s0 + nn], start=False, stop=True, reads=[LC[1], vi], writes=[py])
                    if first_in_chunk[ch]:
                        kb.cp("act", yacc[:, ch, s0:s0 + nn], py[:, 0:nn], reads=[py], writes=[yacc])
                    else:
                        yt_ = ytmp[bi % 2]
                        kb.cp("act", yt_[:, 0:nn], py[:, 0:nn], reads=[py], writes=[yt_])
                        kb.tt("pool", yacc[:, ch, s0:s0 + nn], yacc[:, ch, s0:s0 + nn], yt_[:, 0:nn], OP.add, reads=[yt_, yacc], writes=[yacc])
                first_in_chunk[ch] = False

            prep(0)
            tgen(0)
            for lt in range(16):
                drive(lt)
                if lt + 1 < 16:
                    prep(lt + 1)
                    tgen(lt + 1)
                rot_scan(lt)
                readout(lt)
            kb.barrier()
            esm.close()
            dcol = kb.sb(es, [128, 2], F32, "dcol")
            kb.dma(dcol[:], I["s5_dcol"].ap()[L], writes=[dcol])
            uf = (Er, Ei)
            mt = [kb.sb(es, [128, 512], F32, "mt%d" % i) for i in range(4)]
            for c in range(2):
                kb.dma(uf[c][:], self.S["UT"].ap()[c * 128:(c + 1) * 128, :], reads=[("UT", i) for i in range(9)], writes=[uf[c]])
            gwf = kb.sb(es, [128, 2, 256], F32, "gwf"); gwb = kb.sb(es, [128, 2, 256], BF16, "gwb")
            kb.dma(gwf[:], I["s5_glu_w"].ap()[L].rearrange("(kc p) n -> p kc n", p=128), writes=[gwf])
            kb.cp("act", gwb[:], gwf[:], reads=[gwf], writes=[gwb])
            yb = kb.sb(es, [128, 2, 512], BF16, "yb")
            yf = kb.sb(es, [128, 2, 512], F32, "yf")
            so = [kb.sb(es, [128, 512], F32, "so%d" % i) for i in range(2)]
            for bi, (s0, nn) in enumerate(blocks):
                m1, m2, m3, m4 = mt
                for c in range(2):
                    kb.stt("dve", yf[:, c, 0:nn], uf[c][:, s0:s0 + nn], dcol[:, c:c + 1], yacc[:, c, s0:s0 + nn], OP.mult, OP.add,
                           reads=[uf[c], dcol, yacc], writes=[yf])
                    kb.tt("pool", m1[:, 0:nn], yf[:, c, 0:nn], yf[:, c, 0:nn], OP.mult, reads=[yf], writes=[m1])
                    kb.ts("pool", m1[:, 0:nn], m1[:, 0:nn], 0.044715, OP.mult, 1.0, OP.add, reads=[m1], writes=[m1])
                    kb.tt("pool", m1[:, 0:nn], m1[:, 0:nn], yf[:, c, 0:nn], OP.mult, reads=[m1, yf], writes=[m1])
                    kb.act(m2[:, 0:nn], m1[:, 0:nn], AF.Sigmoid, scale=1.5957691216057308, reads=[m1], writes=[m2])
                    kb.tt("dve", yf[:, c, 0:nn], yf[:, c, 0:nn], m2[:, 0:nn], OP.mult, reads=[yf, m2], writes=[yf])
                    kb.cp("act", yb[:, c, 0:nn], yf[:, c, 0:nn], reads=[yf], writes=[yb])
                for mo in range(2):
                    pz = kb.bank[mo]
                    for kc in range(2):
                        kb.mm(pz[:, 0:nn], gwb[:, kc, mo * 128:(mo + 1) * 128], yb[:, kc, 0:nn], start=(kc == 0), stop=(kc == 1),
                              reads=[gwb, yb], writes=[pz])
                    kb.act(m3[:, 0:nn], pz[:, 0:nn], AF.Sigmoid, reads=[pz], writes=[m3])
                    so_ = so[mo]
                    kb.tt("dve", so_[:, 0:nn], yf[:, mo, 0:nn], m3[:, 0:nn], OP.mult, reads=[yf, m3], writes=[so_])
                    kb.dma(self.S["S5T"].ap()[mo * 128:(mo + 1) * 128, s0:s0 + nn], so_[:, 0:nn], reads=[so_], writes=[("S5T", bi)], q="pool")
        kb.barrier()


    def stage_mla(self, L, ctx_out):
        kb = self.kb
        I = self.I
        blocks = [(0, 256)] + [(256 + 512 * i, 512) for i in range(8)]
        with ExitStack() as es:
            ident_f = kb.sb(es, [128, 128], F32, "identf")
            ident = kb.sb(es, [128, 128], BF16, "identb")
            ones = kb.sb(es, [128, 128], BF16, "ones")
            kb.dma(ident_f[:], I["ident"].ap(), writes=[ident_f])
            kb.cp("dve", ident[:], ident_f[:], reads=[ident_f], writes=[ident])
            kb.memset("dve", ones[:], 1.0, writes=[ones])
            qg = kb.sb(es, [128, 3], F32, "qg"); kvg = kb.sb(es, [128, 2], F32, "kvg")
            kb.dma(qg[:], I["mla_qg"].ap()[L], writes=[qg]); kb.dma(kvg[:], I["mla_kvg"].ap()[L], writes=[kvg])
            wuq = kb.sb(es, [128, 3, 768], BF16, "wuq")
            wrot = kb.sb(es, [128, 3, 8, 96], BF16, "wrot")
            wukv = kb.sb(es, [128, 2, 1024], BF16, "wukv")
            KT = kb.sb(es, [97, 8, T], BF16, "KT")
            VA = kb.sb(es, [128, NT, 8, 65], BF16, "VA")
            gbc = kb.sb(es, [128, 512], F32, "gbc")
            kb.dma(gbc[:], I["mix_norm_g"].ap()[L:L + 1, 256:768].partition_broadcast(128), writes=[gbc])
            es2 = ExitStack()
            wst = kb.sb(es2, [128, 3, 1024], F32, "wst")
            kb.dma(wst[:, :, 0:768], I["mla_w_uq"].ap()[L].rearrange("(kc p) n -> p kc n", p=128), writes=[wst])
            kb.cp("act", wuq[:], wst[:, :, 0:768], reads=[wst], writes=[wuq])
            kb.memset("pool", wrot[:], 0.0, writes=[wrot])
            wv4 = wst[:, :, 0:768].rearrange("p k (h x) -> p k h x", x=96)
            kb.ts("dve", wrot[:, :, :, 64:72], wv4[:, :, :, 72:80], -1.0, OP.mult, reads=[wst], writes=[wrot])
            kb.cp("dve", wrot[:, :, :, 72:80], wv4[:, :, :, 64:72], reads=[wst], writes=[wrot])
            kb.ts("dve", wrot[:, :, :, 80:88], wv4[:, :, :, 88:96], -1.0, OP.mult, reads=[wst], writes=[wrot])
            kb.cp("dve", wrot[:, :, :, 88:96], wv4[:, :, :, 80:88], reads=[wst], writes=[wrot])
            kb.dma(wst[:, 0:2, :], I["mla_w_ukv"].ap()[L].rearrange("(kc p) n -> p kc n", p=128), reads=[wst], writes=[wst])
            kb.cp("act", wukv[:], wst[:, 0:2, :], reads=[wst], writes=[wukv])
            kb.memset("pool", KT[96:97, :, :], 1.0, writes=[KT])
            kb.memset("pool", VA[:, :, :, 64:65], 1.0, writes=[VA])
            krf = kb.sb(es2, [32, T], F32, "krf"); krb = kb.sb(es2, [32, T], BF16, "krb")
            kb.dma(krf[:], self.S["KRT"].ap(), reads=[("KRT", i) for i in range(9)], writes=[krf])
            kb.cp("act", krb[:], krf[:], reads=[krf], writes=[krb])
            for h in range(8):
                kb.dma(KT[64:96, h, :], krb[:], reads=[krb], writes=[KT])
            kb.barrier()
            es2.close()
            if "mla_stop1" in self.debug:
                return
            rcb = [kb.sb(es, [96, 512], F32, "rcb%d" % i) for i in range(2)]
            rsb = [kb.sb(es, [96, 512], F32, "rsb%d" % i) for i in range(2)]
            xin = [kb.sb(es, [128, 3, 512], F32, "xin%d" % i) for i in range(2)]
            sqb = kb.sb(es, [128, 3, 512], BF16, "sqb")
            sqk = [kb.sb(es, [96, 512], BF16, "sqk%d" % i) for i in range(2)]
            rst = kb.sb(es, [128, 512], F32, "rst")
            xn = kb.sb(es, [128, 3, 512], BF16, "xn")
            xnq = [kb.sb(es, [128, 3, 512], BF16, "xnq%d" % i) for i in range(2)]
            kmax2 = kb.sb(es, [128, 8], F32, "kmax2"); kmb = kb.sb(es, [128, 8], F32, "kmb"); negk = kb.sb(es, [128, 8], F32, "negk")
            kb.memset("dve", kmax2[:], 0.0, writes=[kmax2])

            def rmsnorm_fm(src, nch, nfeat, nn, gcol, dst):
                kb.tt("pool", sqb[:, 0:nch, 0:nn], src[:, 0:nch, 0:nn], src[:, 0:nch, 0:nn], OP.mult, reads=[src], writes=[sqb])
                pn = kb.bank[6]
                for c in range(nch):
                    kb.mm(pn[:, 0:nn], ones[:], sqb[:, c, 0:nn], start=(c == 0), stop=(c == nch - 1), reads=[ones, sqb], writes=[pn])
                kb.ts("dve", rst[:, 0:nn], pn[:, 0:nn], 1.0 / nfeat, OP.mult, EPS, OP.add, reads=[pn], writes=[rst])
                kb.act(rst[:, 0:nn], rst[:, 0:nn], AF.Sqrt, reads=[rst], writes=[rst])
                kb.op("dve", lambda E: E.reciprocal(rst[:, 0:nn], rst[:, 0:nn]), reads=[rst], writes=[rst])
                for c in range(nch):
                    kb.stt("dve", dst[:, c, 0:nn], src[:, c, 0:nn], gcol[:, c:c + 1], rst[:, 0:nn], OP.mult, OP.mult,
                           reads=[src, gcol, rst], writes=[dst])

            for bi, (s0, nn) in enumerate(blocks):
                x_ = xin[bi % 2]
                kb.dma(x_[:, 0:2, 0:nn], self.S["CKVT"].ap().rearrange("(c p) t -> p c t", p=128)[:, :, s0:s0 + nn],
                       reads=[("CKVT", bi)], writes=[x_])
                rmsnorm_fm(x_, 2, 256, nn, kvg, xn)
                for h in range(8):
                    pk = kb.bank[h % 2]
                    for kc in range(2):
                        kb.mm(pk[0:64, 0:nn], wukv[:, kc, h * 128:h * 128 + 64], xn[:, kc, 0:nn], start=(kc == 0), stop=(kc == 1),
                              reads=[wukv, xn], writes=[pk])
                    kb.cp("dve", KT[0:64, h, s0:s0 + nn], pk[0:64, 0:nn], reads=[pk], writes=[KT])
                    kb.tt("pool", sqk[h % 2][0:96, 0:nn], KT[0:96, h, s0:s0 + nn], KT[0:96, h, s0:s0 + nn], OP.mult, reads=[KT], writes=[sqk[h % 2]])
                    pn = kb.bank[6]
                    kb.mm(pn[:, 0:nn], ones[0:96, :], sqk[h % 2][0:96, 0:nn], reads=[ones, sqk[h % 2]], writes=[pn])
                    kb.op("dve", lambda E: E.reduce_max(kmb[:, h:h + 1], pn[:, 0:nn], AX.X), reads=[pn], writes=[kmb])
                    kb.tt("dve", kmax2[:, h:h + 1], kmax2[:, h:h + 1], kmb[:, h:h + 1], OP.max, reads=[kmb, kmax2], writes=[kmax2])
                for j in range(nn // 128):
                    ti = s0 // 128 + j
                    pv = kb.bank[2 + (j % 2)]
                    for kc in range(2):
                        kb.mm(pv[:, :].rearrange("p (h x) -> p h x", x=64), xn[:, kc, j * 128:(j + 1) * 128],
                              wukv[:, kc, :].rearrange("p (h x) -> p h x", x=128)[:, :, 64:128], start=(kc == 0), stop=(kc == 1),
                              reads=[wukv, xn], writes=[pv])
                    kb.cp("act" if j % 2 else "dve", VA[:, ti, :, 0:64], pv[:, :].rearrange("p (h x) -> p h x", x=64), reads=[pv], writes=[VA])
            if "mla_stop2" in self.debug:
                kb.barrier()
                return
            kb.act(negk[:], kmax2[:], AF.Sqrt, reads=[kmax2], writes=[negk])
            kb.ts("dve", negk[:], negk[:], -1.02, OP.mult, reads=[negk], writes=[negk])
            QT = [kb.sb(es, [97, 8, 512], BF16, "QT%d" % i) for i in range(2)]
            PT = [kb.sb(es, [128, 512], BF16, "PT%d" % i) for i in range(3)]
            tq = kb.sb(es, [96, 512], F32, "tq"); tq2 = kb.sb(es, [96, 512], F32, "tq2")
            qn1 = kb.sb(es, [97, 512], F32, "qn1")
            yt = kb.sb(es, [128, 4, 512], F32, "yt")
            ytb = kb.sb(es, [128, 4, 512], BF16, "ytb")
            rec = kb.sb(es, [128, 4], F32, "rec")
            ss4 = kb.sb(es, [128, 4], F32, "ss4")
            mts = kb.sb(es, [128, 4, 512], BF16, "mts")
            npt = 0
            qblocks = [(bi, s0, nn) for bi, (s0, nn) in enumerate(blocks) if not (bi == 0 and not ctx_out)]

            def qphase(bi, s0, nn):
                x_ = xin[bi % 2]
                xq = xnq[bi % 2]
                kb.dma(x_[:, 0:3, 0:nn], self.S["CQT"].ap().rearrange("(c p) t -> p c t", p=128)[:, :, s0:s0 + nn],
                       reads=[("CQT", bi)], writes=[x_])
                rmsnorm_fm(x_, 3, 384, nn, qg, xq)
                Q = QT[bi % 2]
                rc = rcb[bi % 2]; rs = rsb[bi % 2]
                kb.dma(rc[64:96, 0:nn], I["rope_cos"].ap()[:, s0:s0 + nn], writes=[rc])
                kb.dma(rs[64:96, 0:nn], I["rope_sin"].ap()[:, s0:s0 + nn], writes=[rs])
                for h in range(8):
                    pa = kb.bank[6]
                    pb = kb.bank[7]
                    for kc in range(3):
                        kb.mm(pa[0:96, 0:nn], wuq[:, kc, h * 96:(h + 1) * 96], xq[:, kc, 0:nn], start=(kc == 0), stop=(kc == 2),
                              reads=[wuq, xq], writes=[pa])
                    for kc in range(3):
                        kb.mm(pb[0:96, 0:nn], wrot[:, kc, h, :], xq[:, kc, 0:nn], start=(kc == 0), stop=(kc == 2),
                              reads=[wrot, xq], writes=[pb])
                    kb.cp("dve", Q[0:64, h, 0:nn], pa[0:64, 0:nn], reads=[pa], writes=[Q])
                    kb.tt("dve", tq[64:96, 0:nn], pb[64:96, 0:nn], rs[64:96, 0:nn], OP.mult, reads=[pb, rs], writes=[tq])
                    kb.tt("dve", tq2[64:96, 0:nn], pa[64:96, 0:nn], rc[64:96, 0:nn], OP.mult, reads=[pa, rc], writes=[tq2])
                    kb.tt("dve", Q[64:96, h, 0:nn], tq[64:96, 0:nn], tq2[64:96, 0:nn], OP.add, reads=[tq, tq2], writes=[Q])
                    sk = sqk[h % 2]
                    kb.tt("pool", sk[0:96, 0:nn], Q[0:96, h, 0:nn], Q[0:96, h, 0:nn], OP.mult, reads=[Q], writes=[sk])
                    pn = kb.bank[6]
                    kb.mm(pn[:, 0:nn], ones[0:96, :], sk[0:96, 0:nn], reads=[ones, sk], writes=[pn])
                    kb.act(qn1[96:97, 0:nn], pn[96:97, 0:nn], AF.Sqrt, reads=[pn], writes=[qn1])
                    kb.ts("dve", Q[96:97, h, 0:nn], qn1[96:97, 0:nn], negk[96:97, h:h + 1], OP.mult, reads=[qn1, negk], writes=[Q])

            qphase(*qblocks[0])
            for qi, (bi, s0, nn) in enumerate(qblocks):
                nj = nn // 128
                kchunks = list(range(2)) if bi == 0 else list(range(NT))
                Q = QT[bi % 2]
                if qi + 1 < len(qblocks):
                    qphase(*qblocks[qi + 1])
                if "mla_stop3" in self.debug:
                    continue
                for h in range(8):
                    pend = None

                    def emit_pv(ci_, kc_, P__):
                        for j in range(nj):
                            po = kb.bank[2 + j]
                            kb.mm(po[:, 0:65], P__[:, j * 128:(j + 1) * 128], VA[:, kc_, h, :], start=(ci_ == 0), stop=(ci_ == len(kchunks) - 1),
                                  reads=[P__, VA], writes=[po])
                    for ci, kc in enumerate(kchunks):
                        pst = kb.bank[ci % 2]
                        kb.mm(pst[:, 0:nn], KT[0:97, h, kc * 128:(kc + 1) * 128], Q[0:97, h, 0:nn], reads=[KT, Q], writes=[pst])
                        P_ = PT[npt % 3]
                        npt += 1
                        kb.act(P_[:, 0:nn], pst[:, 0:nn], AF.Exp, scale=MLA_SCALE, reads=[pst], writes=[P_])
                        if pend is not None:
                            emit_pv(*pend)
                        pend = (ci, kc, P_)
                    emit_pv(*pend)
                    for j in range(nj):
                        if "mla_nonorm" in self.debug:
                            continue
                        po = kb.bank[2 + j]
                        if "mla_norec" in self.debug:
                            kb.ts("dve", yt[:, j, h * 64:(h + 1) * 64], po[:, 0:64], 0.5, OP.mult, reads=[po], writes=[yt])
                            continue
                        if "mla_recsb" in self.debug:
                            kb.cp("act", rec[:, j:j + 1], po[:, 64:65], reads=[po], writes=[rec])
                            kb.op("dve", lambda E: E.reciprocal(rec[:, j:j + 1], rec[:, j:j + 1]), reads=[rec], writes=[rec])
                        else:
                            kb.op("dve", lambda E: E.reciprocal(rec[:, j:j + 1], po[:, 64:65]), reads=[po], writes=[rec])
                        kb.ts("dve", yt[:, j, h * 64:(h + 1) * 64], po[:, 0:64], rec[:, j:j + 1], OP.mult, reads=[po, rec], writes=[yt])
                for j in range(nj):
                    if "MLAO" in self.debug:
                        kb.dma(self.S["MLAO"].ap()[s0 + j * 128:s0 + (j + 1) * 128, :], yt[:, j, :], reads=[yt], writes=["MLAO"], q="pool")
                    kb.act(mts[:, j, :], yt[:, j, :], AF.Square, accum_out=ss4[:, j:j + 1], reads=[yt], writes=[mts, ss4])
                kb.ts("dve", ss4[:, 0:nj], ss4[:, 0:nj], 1.0 / 512, OP.mult, EPS, OP.add, reads=[ss4], writes=[ss4])
                kb.act(ss4[:, 0:nj], ss4[:, 0:nj], AF.Sqrt, reads=[ss4], writes=[ss4])
                kb.op("dve", lambda E: E.reciprocal(ss4[:, 0:nj], ss4[:, 0:nj]), reads=[ss4], writes=[ss4])
                for j in range(nj):
                    kb.stt("dve", ytb[:, j, :], yt[:, j, :], ss4[:, j:j + 1], gbc[:], OP.mult, OP.mult, reads=[yt, ss4, gbc], writes=[ytb])
                    pt_ = kb.bank[6 + (j % 2)]
                    ptv = pt_[:].bitcast(BF16)
                    for fc in range(4):
                        kb.tr(ptv[:, fc * 128:(fc + 1) * 128], ytb[:, j, fc * 128:(fc + 1) * 128], ident[:], reads=[ytb, ident], writes=[pt_])
                    kb.cp("act", mts[:, :, j * 128:(j + 1) * 128], ptv[:, 0:512].rearrange("p (f q) -> p f q", f=4), reads=[pt_], writes=[mts])
                kb.dma(self.S["MT"].ap()[256:768, s0:s0 + nn].rearrange("(f p) t -> p f t", p=128), mts[:, :, 0:nn], reads=[mts],
                       writes=[("MT_mla", bi)], q="pool")
        kb.barrier()


    def stage_hyena(self, L, part):
        kb = self.kb
        I = self.I
        nm = part
        n = NLAT if part == "l" else NCTX
        r0 = NCTX if part == "l" else 0
        ntc = n // 128
        nj = (n + 1 + 127) // 128
        lagblocks = [(i * 512, min(512, n - i * 512)) for i in range((n + 511) // 512)]
        KSP = self.S["KSPEC_" + nm]
        with ExitStack() as es:
            ident_f = kb.sb(es, [128, 128], F32, "identf")
            kb.dma(ident_f[:], I["ident"].ap(), writes=[ident_f])
            w1 = kb.sb(es, [33, 64], F32, "w1"); w2 = kb.sb(es, [64, 64], F32, "w2"); w3 = kb.sb(es, [64, 1024], F32, "w3")
            cols = kb.sb(es, [64, 4], F32, "cols"); bf = kb.sb(es, [64, 2], F32, "bf"); b3 = kb.sb(es, [128, 8], F32, "b3")
            kb.dma(w1[:], I["hy_f_w1"].ap()[L], writes=[w1]); kb.dma(w2[:], I["hy_f_w2"].ap()[L], writes=[w2]); kb.dma(w3[:], I["hy_f_w3"].ap()[L], writes=[w3])
            kb.dma(cols[:], I["hy_cols"].ap()[L], writes=[cols]); kb.dma(b3[:], I["hy_b3"].ap()[L], writes=[b3])
            kb.tt("dve", bf[:, 0:1], cols[:, 0:1], cols[:, 2:3], OP.mult, reads=[cols], writes=[bf])
            kb.tt("dve", bf[:, 1:2], cols[:, 1:2], cols[:, 3:4], OP.mult, reads=[cols], writes=[bf])
            FS = kb.sb(es, [128, ntc, 512], BF16, "FS"); FD = kb.sb(es, [128, ntc, 512], BF16, "FD")
            zp = kb.sb(es, [33, 512], F32, "zp")
            h1 = kb.sb(es, [64, 512], F32, "h1"); h2 = kb.sb(es, [64, 512], F32, "h2")
            t1 = kb.sb(es, [64, 512], F32, "t1"); t2 = kb.sb(es, [64, 512], F32, "t2"); t0 = kb.sb(es, [64, 512], F32, "t0")
            dec = kb.sb(es, [128, 2, 512], F32, "dec")
            ff = kb.sb(es, [128, 512], F32, "ff"); fb = kb.sb(es, [128, 512], F32, "fb")
            fs_ = kb.sb(es, [128, 512], F32, "fs"); fd_ = kb.sb(es, [128, 512], F32, "fd")
            for (l0, ln) in lagblocks:
                kb.dma(zp[:, 0:ln], I["hy_zpos_" + nm].ap()[:, l0:l0 + ln], writes=[zp])
                kb.dma(dec[:, :, 0:ln], I["hy_decay_" + nm].ap().rearrange("(c p) t -> p c t", p=128)[:, :, l0:l0 + ln], writes=[dec])
                p1 = kb.bank[0]
                kb.mm(p1[0:64, 0:ln], w1[:], zp[:, 0:ln], reads=[w1, zp], writes=[p1])
                kb.ts("dve", t0[:, 0:ln], p1[0:64, 0:ln], cols[:, 2:3], OP.mult, bf[:, 0:1], OP.add, reads=[p1, cols, bf], writes=[t0])
                self.sin_rr("dve", h1[:, 0:ln], t0[:, 0:ln], 0.0, t1[:, 0:ln], t2[:, 0:ln])
                p2 = kb.bank[1]
                kb.mm(p2[0:64, 0:ln], w2[:], h1[:, 0:ln], reads=[w2, h1], writes=[p2])
                kb.ts("dve", t0[:, 0:ln], p2[0:64, 0:ln], cols[:, 3:4], OP.mult, bf[:, 1:2], OP.add, reads=[p2, cols, bf], writes=[t0])
                self.sin_rr("dve", h2[:, 0:ln], t0[:, 0:ln], 0.0, t1[:, 0:ln], t2[:, 0:ln])
                for o in range(2):
                    for chf in range(2):
                        jf = o * 2 + chf
                        jb = 4 + o * 2 + chf
                        pf = kb.bank[2]; pbk = kb.bank[3]
                        kb.mm(pf[:, 0:ln], w3[:, jf * 128:(jf + 1) * 128], h2[:, 0:ln], reads=[w3, h2], writes=[pf])
                        kb.mm(pbk[:, 0:ln], w3[:, jb * 128:(jb + 1) * 128], h2[:, 0:ln], reads=[w3, h2], writes=[pbk])
                        kb.stt("dve", ff[:, 0:ln], pf[:, 0:ln], b3[:, jf:jf + 1], dec[:, chf, 0:ln], OP.add, OP.mult, reads=[pf, b3, dec], writes=[ff])
                        kb.stt("dve", fb[:, 0:ln], pbk[:, 0:ln], b3[:, jb:jb + 1], dec[:, chf, 0:ln], OP.add, OP.mult, reads=[pbk, b3, dec], writes=[fb])
                        if l0 == 0:
                            kb.memset("dve", fb[:, 0:1], 0.0, writes=[fb])
                        kb.tt("dve", fs_[:, 0:ln], ff[:, 0:ln], fb[:, 0:ln], OP.add, reads=[ff, fb], writes=[fs_])
                        kb.tt("pool", fd_[:, 0:ln], ff[:, 0:ln], fb[:, 0:ln], OP.subtract, reads=[ff, fb], writes=[fd_])
                        for (src, dst, pbank) in ((fs_, FS, 4), (fd_, FD, 5)):
                            pt_ = kb.bank[pbank]
                            nq = ln // 128
                            for q in range(nq):
                                kb.tr(pt_[:, q * 128:(q + 1) * 128], src[:, q * 128:(q + 1) * 128], ident_f[:], reads=[src, ident_f], writes=[pt_])
                            tc0 = l0 // 128
                            kb.cp("act", dst[:, tc0:tc0 + nq, o * 256 + chf * 128:o * 256 + (chf + 1) * 128],
                                  pt_[:, 0:nq * 128].rearrange("p (q c) -> p q c", c=128), reads=[pt_], writes=[dst])
            Dt = [kb.sb(es, [128, ntc, 128], BF16, "Dt%d" % i) for i in range(4)]
            ks = [kb.sb(es, [128, 2, 512], F32, "ks%d" % i) for i in range(2)]
            nd = 0
            for j in range(nj):
                for ri, (fc, src) in enumerate(((j, FS), (nj + j, FD))):
                    D_ = Dt[nd % 4]
                    nd += 1
                    kb.dma(D_[:], I["hy_dfwd_" + nm].ap()[fc], writes=[D_])
                    pk = kb.bank[ri]
                    for tc in range(ntc):
                        kb.mm(pk[:, :], D_[:, tc, :], src[:, tc, :], start=(tc == 0), stop=(tc == ntc - 1), reads=[D_, src], writes=[pk])
                    kb.cp("act" if ri else "dve", ks[j % 2][:, ri, :], pk[:, :], reads=[pk], writes=[ks[j % 2]])
                kb.dma(KSP.ap()[j].rearrange("r p c -> p r c"), ks[j % 2][:], reads=[ks[j % 2]], writes=["KSPEC_" + nm], q="pool")
        kb.barrier()
        with ExitStack() as es:
            ident_f = kb.sb(es, [128, 128], F32, "identf")
            kb.dma(ident_f[:], I["ident"].ap(), writes=[ident_f])
            sw = kb.sb(es, [128, 6, 3], F32, "sw"); sbb = kb.sb(es, [128, 6], F32, "sbb")
            kb.dma(sw[:], I["hy_sw"].ap()[L], writes=[sw]); kb.dma(sbb[:], I["hy_sb"].ap()[L], writes=[sbb])
            xin = [kb.sb(es, [128, n], F32, "hzx%d" % i) for i in range(2)]
            zz = [kb.sb(es, [128, n], F32, "hzz%d" % i) for i in range(2)]
            tm = [kb.sb(es, [128, 4, 128], F32, "tm%d" % i) for i in range(2)]
            ntm = 0
            for c in range(6):
                x_ = xin[c % 2]; z_ = zz[c % 2]
                kb.dma(x_[:], self.S["HZT"].ap()[c * 128:(c + 1) * 128, r0:r0 + n], reads=[("HZT", i) for i in range(9)], writes=[x_])
                kb.ts("dve", z_[:], x_[:], sw[:, c, 1:2], OP.mult, sbb[:, c:c + 1], OP.add, reads=[x_, sw, sbb], writes=[z_])
                kb.stt("dve", z_[:, 1:n], x_[:, 0:n - 1], sw[:, c, 0:1], z_[:, 1:n], OP.mult, OP.add, reads=[x_, sw, z_], writes=[z_])
                kb.stt("dve", z_[:, 0:n - 1], x_[:, 1:n], sw[:, c, 2:3], z_[:, 0:n - 1], OP.mult, OP.add, reads=[x_, sw, z_], writes=[z_])
                for tg in range(0, ntc, 4):
                    ng = min(4, ntc - tg)
                    pt_ = kb.bank[ntm % 2]
                    t_ = tm[ntm % 2]
                    ntm += 1
                    for q in range(ng):
                        kb.tr(pt_[:, q * 128:(q + 1) * 128], z_[:, (tg + q) * 128:(tg + q + 1) * 128], ident_f[:], reads=[z_, ident_f], writes=[pt_])
                    kb.cp("act" if ntm % 2 else "dve", t_[:, 0:ng, :], pt_[:, 0:ng * 128].rearrange("p (q c) -> p q c", c=128), reads=[pt_], writes=[t_])
                    kb.dma(self.S["HZTM"].ap()[r0 + tg * 128:r0 + (tg + ng) * 128, c * 128:(c + 1) * 128].rearrange("(q p) c -> p q c", p=128),
                           t_[:, 0:ng, :], reads=[t_], writes=["HZTM"], q="pool")
        kb.barrier()
        with ExitStack() as es:
            ident = kb.sb(es, [128, 128], BF16, "identb")
            idf = kb.sb(es, [128, 128], F32, "identf")
            kb.dma(idf[:], I["ident"].ap(), writes=[idf])
            kb.cp("dve", ident[:], idf[:], reads=[idf], writes=[ident])
            Yb = [kb.sb(es, [128, ntc, 256], BF16, "Yb%d" % i) for i in range(2)]
            Z = kb.sb(es, [128, 2 * nj, 256], BF16, "Zs")
            Dt = [kb.sb(es, [128, ntc, 128], BF16, "Df%d" % i) for i in range(4)]
            Di = [kb.sb(es, [128, 2 * nj, 128], BF16, "Di%d" % i) for i in range(2)]
            kt = [kb.sb(es, [128, 2, 256], F32, "kt%d" % i) for i in range(2)]
            cm = [kb.sb(es, [128, 256], F32, "cm%d" % i) for i in range(4)]
            yo = [kb.sb(es, [128, 256], F32, "yo%d" % i) for i in range(2)]
            gt = [kb.sb(es, [128, 256], F32, "gt%d" % i) for i in range(2)]
            yn = [kb.sb(es, [128, 256], F32, "yn%d" % i) for i in range(2)]
            ynb = kb.sb(es, [128, 256], BF16, "ynb"); sqj = kb.sb(es, [128, 256], BF16, "sqj")
            ss = kb.sb(es, [128, 1], F32, "ss")
            mts = [kb.sb(es, [128, 2, 128], BF16, "mts%d" % i) for i in range(2)]
            bias_bc = kb.sb(es, [128, 2, 256], F32, "biasbc")
            gbc = kb.sb(es, [128, 256], F32, "gbc")
            kb.dma(bias_bc[:].rearrange("p o c -> p (o c)"), I["hy_bias"].ap()[L:L + 1].rearrange("a o c -> a (o c)").partition_broadcast(128), writes=[bias_bc])
            kb.dma(gbc[:], I["mix_norm_g"].ap()[L:L + 1, 768:1024].partition_broadcast(128), writes=[gbc])
            HZ = self.S["HZTM"].ap()
            for tc in range(ntc):
                y_ = yo[tc % 2]
                kb.dma(y_[:], HZ[r0 + tc * 128:r0 + (tc + 1) * 128, 0:256], reads=["HZTM"], writes=[y_])
                kb.cp("act" if tc % 2 else "dve", Yb[0][:, tc, :], y_[:], reads=[y_], writes=[Yb[0]])
            nd = 0
            for o in range(2):
                Yin = Yb[o]
                for j in range(nj):
                    k_ = kt[j % 2]
                    kb.dma(k_[:], KSP.ap()[j].rearrange("r p c -> p r c")[:, :, o * 256:(o + 1) * 256], reads=["KSPEC_" + nm], writes=[k_])
                    pr = kb.bank[(j % 2) * 2]; pi_ = kb.bank[(j % 2) * 2 + 1]
                    for ri, (fc, pk) in enumerate(((j, pr), (nj + j, pi_))):
                        D_ = Dt[nd % 4]
                        nd += 1
                        kb.dma(D_[:], I["hy_dfwd_" + nm].ap()[fc], writes=[D_])
                        for tc in range(ntc):
                            kb.mm(pk[:, 0:256], D_[:, tc, :], Yin[:, tc, :], start=(tc == 0), stop=(tc == ntc - 1), reads=[D_, Yin], writes=[pk])
                    c1, c2, c3, c4 = cm
                    kb.tt("dve", c1[:], pr[:, 0:256], k_[:, 0, :], OP.mult, reads=[pr, k_], writes=[c1])
                    kb.tt("dve", c2[:], pi_[:, 0:256], k_[:, 1, :], OP.mult, reads=[pi_, k_], writes=[c2])
                    kb.tt("pool", Z[:, j, :], c1[:], c2[:], OP.subtract, reads=[c1, c2], writes=[Z])
                    kb.tt("dve", c3[:], pr[:, 0:256], k_[:, 1, :], OP.mult, reads=[pr, k_], writes=[c3])
                    kb.tt("dve", c4[:], pi_[:, 0:256], k_[:, 0, :], OP.mult, reads=[pi_, k_], writes=[c4])
                    kb.tt("pool", Z[:, nj + j, :], c3[:], c4[:], OP.add, reads=[c3, c4], writes=[Z])
                for tc in range(ntc):
                    D_ = Di[tc % 2]
                    kb.dma(D_[:], I["hy_dinv_" + nm].ap()[tc], writes=[D_])
                    y_ = yo[tc % 2]; g_ = gt[tc % 2]; o_ = yn[tc % 2]
                    rows = slice(r0 + tc * 128, r0 + (tc + 1) * 128)
                    if o == 0:
                        kb.dma(y_[:], HZ[rows, 0:256], reads=["HZTM"], writes=[y_])
                    else:
                        kb.dma(y_[:], self.S["HY1"].ap()[rows, :], reads=[("HY1", tc)], writes=[y_])
                    kb.dma(g_[:], HZ[rows, 256 * (o + 1):256 * (o + 2)], reads=["HZTM"], writes=[g_])
                    pc = kb.bank[4 + (tc % 2)]
                    for fc in range(2 * nj):
                        kb.mm(pc[:, 0:256], D_[:, fc, :], Z[:, fc, :], start=(fc == 0), stop=(fc == 2 * nj - 1), reads=[D_, Z], writes=[pc])
                    kb.tt("pool", y_[:], y_[:], bias_bc[:, o, :], OP.mult, reads=[y_, bias_bc], writes=[y_])
                    kb.tt("dve", o_[:], pc[:, 0:256], y_[:], OP.add, reads=[pc, y_], writes=[o_])
                    kb.tt("dve", o_[:], o_[:], g_[:], OP.mult, reads=[o_, g_], writes=[o_])
                    if o == 0:
                        kb.cp("act", Yb[1][:, tc, :], o_[:], reads=[o_], writes=[Yb[1]])
                        kb.dma(self.S["HY1"].ap()[rows, :], o_[:], reads=[o_], writes=[("HY1", tc)], q="pool")
                    else:
                        if "HYO" in self.debug:
                            kb.dma(self.S["HYO"].ap()[rows, :], o_[:], reads=[o_], writes=["HYO"], q="pool")
                        kb.act(sqj[:], o_[:], AF.Square, accum_out=ss[:], reads=[o_], writes=[sqj, ss])
                        kb.ts("dve", ss[:], ss[:], 1.0 / 256, OP.mult, EPS, OP.add, reads=[ss], writes=[ss])
                        kb.act(ss[:], ss[:], AF.Sqrt, reads=[ss], writes=[ss])
                        kb.op("dve", lambda E: E.reciprocal(ss[:], ss[:]), reads=[ss], writes=[ss])
                        kb.stt("dve", ynb[:], o_[:], ss[:, 0:1], gbc[:], OP.mult, OP.mult, reads=[o_, ss, gbc], writes=[ynb])
                        pt_ = kb.bank[6 + (tc % 2)]
                        ptv = pt_[:].bitcast(BF16)
                        for fcx in range(2):
                            kb.tr(ptv[:, fcx * 128:(fcx + 1) * 128], ynb[:, fcx * 128:(fcx + 1) * 128], ident[:], reads=[ynb, ident], writes=[pt_])
                        m_ = mts[tc % 2]
                        kb.cp("act", m_[:], ptv[:, 0:256].rearrange("p (f q) -> p f q", f=2), reads=[pt_], writes=[m_])
                        kb.dma(self.S["MT"].ap()[768:1024, r0 + tc * 128:r0 + (tc + 1) * 128].rearrange("(f p) t -> p f t", p=128), m_[:],
                               reads=[m_], writes=[("MT_hy", part, tc)], q="pool")
        kb.barrier()


    def stage_s5merge(self, L):
        kb = self.kb
        blocks = [(0, 256)] + [(256 + 512 * i, 512) for i in range(8)]
        with ExitStack() as es:
            ones = kb.sb(es, [128, 128], BF16, "ones")
            kb.memset("dve", ones[:], 1.0, writes=[ones])
            gcol = kb.sb(es, [128, 2], F32, "gcol")
            kb.dma(gcol[:], self.I["mix_g_s5col"].ap()[L], writes=[gcol])
            xin = [kb.sb(es, [128, 2, 512], F32, "s5x%d" % i) for i in range(2)]
            sqb = kb.sb(es, [128, 2, 512], BF16, "sqb"); rst = kb.sb(es, [128, 512], F32, "rst")
            ob = [kb.sb(es, [128, 2, 512], BF16, "s5o%d" % i) for i in range(2)]
            for bi, (s0, nn) in enumerate(blocks):
                x_ = xin[bi % 2]; o_ = ob[bi % 2]
                kb.dma(x_[:, :, 0:nn], self.S["S5T"].ap().rearrange("(c p) t -> p c t", p=128)[:, :, s0:s0 + nn], reads=[("S5T", bi)], writes=[x_])
                kb.act(sqb[:, :, 0:nn], x_[:, :, 0:nn], AF.Square, reads=[x_], writes=[sqb])
                pn = kb.bank[bi % 2]
                for c in range(2):
                    kb.mm(pn[:, 0:nn], ones[:], sqb[:, c, 0:nn], start=(c == 0), stop=(c == 1), reads=[ones, sqb], writes=[pn])
                kb.ts("dve", rst[:, 0:nn], pn[:, 0:nn], 1.0 / 256, OP.mult, EPS, OP.add, reads=[pn], writes=[rst])
                kb.act(rst[:, 0:nn], rst[:, 0:nn], AF.Sqrt, reads=[rst], writes=[rst])
                kb.op("dve", lambda E: E.reciprocal(rst[:, 0:nn], rst[:, 0:nn]), reads=[rst], writes=[rst])
                for c in range(2):
                    kb.stt("dve", o_[:, c, 0:nn], x_[:, c, 0:nn], gcol[:, c:c + 1], rst[:, 0:nn], OP.mult, OP.mult, reads=[x_, gcol, rst], writes=[o_])
                kb.dma(self.S["MT"].ap()[0:256, s0:s0 + nn].rearrange("(c p) t -> p c t", p=128), o_[:, :, 0:nn], reads=[o_], writes=[("MT_s5", bi)], q="pool")
        kb.barrier()

    def stage_out_moe(self, L, last):
        kb = self.kb
        I = self.I
        S = self.S
        tiles = list(range(2, NT)) if last else list(range(NT))
        ntl = len(tiles)
        nblk = (2 * ntl * 128 + 127) // 128 + 32
        X = S["X"].ap()
        with ExitStack() as es:
            ident_f = kb.sb(es, [128, 128], F32, "identf")
            ident = kb.sb(es, [128, 128], BF16, "identb")
            kb.dma(ident_f[:], I["ident"].ap(), writes=[ident_f])
            kb.cp("dve", ident[:], ident_f[:], reads=[ident_f], writes=[ident])
            iota_e = kb.sb(es, [128, 32], F32, "iotae"); iota_b = kb.sb(es, [128, 128], F32, "iotab"); iota_p = kb.sb(es, [128, 1], F32, "iotap")
            ltri = kb.sb(es, [128, 128], BF16, "ltri"); ones = kb.sb(es, [128, 128], BF16, "ones")
            kb.dma(iota_e[:], I["iota_e"].ap(), writes=[iota_e]); kb.dma(iota_b[:], I["iota_b"].ap(), writes=[iota_b])
            kb.dma(iota_p[:], I["iota_p"].ap(), writes=[iota_p]); kb.dma(ltri[:], I["ltri"].ap(), writes=[ltri])
            kb.memset("dve", ones[:], 1.0, writes=[ones])
            GW = kb.sb(es, [128, NT, 2], F32, "GW"); EID = kb.sb(es, [128, NT, 2], F32, "EID"); RANK = kb.sb(es, [128, NT, 2], F32, "RANK")
            DEST = kb.sb(es, [128, NT, 2], F32, "DEST"); DESTI = kb.sb(es, [128, NT, 2], I32, "DESTI")
            base = kb.sb(es, [128, 32], F32, "base")
            BE = kb.sb(es, [128, 128], F32, "BE"); IDXW = kb.sb(es, [128, 2, 128], I32, "IDXW")
            CHG = kb.sb(es, [128, 128], F32, "CHG"); IDXF = kb.sb(es, [128, 128], F32, "IDXF")
            kb.memset("dve", base[:], 0.0, writes=[base])
            kb.memset("dve", RANK[:], 0.0, writes=[RANK])
            kb.memset("dve", DEST[:], 0.0, writes=[DEST])
            es2 = ExitStack()
            wo = kb.sb(es2, [128, 8, DM], BF16, "wo")
            wr = kb.sb(es2, [128, 8, 36], F32, "wr")
            kb.dma(wr[:], I["moe_wr"].ap()[L].rearrange("(kc p) n -> p kc n", p=128), writes=[wr])
            gate1 = kb.sb(es2, [128, DM], F32, "gate1"); G2 = kb.sb(es2, [128, DM], F32, "G2"); S2 = kb.sb(es2, [128, DM], F32, "S2")
            tmpb = kb.sb(es2, [128, DM], F32, "tmpb")
            es3 = ExitStack()
            wst = kb.sb(es3, [128, 8, DM], F32, "wst")
            kb.dma(wst[:], I["w_out"].ap()[L].rearrange("(kc p) n -> p kc n", p=128), writes=[wst])
            kb.cp("act", wo[:, 0:4, :], wst[:, 0:4, :], reads=[wst], writes=[wo])
            kb.cp("dve", wo[:, 4:8, :], wst[:, 4:8, :], reads=[wst], writes=[wo])
            kb.barrier()
            es3.close()
            mT = [kb.sb(es2, [128, 8, 512], BF16, "mT%d" % i) for i in range(2)]
            xt = [kb.sb(es2, [128, DM], F32, "xt%d" % i) for i in range(2)]
            xn = [kb.sb(es2, [128, DM], F32, "xn%d" % i) for i in range(2)]
            sq = kb.sb(es2, [128, DM], BF16, "sq"); ss = kb.sb(es2, [128, 1], F32, "ss"); rstd = kb.sb(es2, [128, 1], F32, "rstd")
            tmp = kb.sb(es2, [128, DM], F32, "tmp")
            ff = [kb.sb(es2, [128, DM], F32, "ff%d" % i) for i in range(2)]
            fbt = [kb.sb(es2, [128, DM], BF16, "fbt%d" % i) for i in range(2)]
            fT = kb.sb(es2, [128, 8, 128], F32, "fT")
            LG = kb.sb(es2, [128, NT, 36], F32, "LG")
            sm = {k: kb.sb(es2, shp, F32, k) for k, shp in (("gmax", [128, 1]), ("ngmax", [128, 1]), ("ohg", [128, 4]), ("pen", [128, 4]), ("ex4", [128, 4]),
                                                            ("sume", [128, 1]), ("gw", [128, 1]), ("msk", [128, 32]), ("top8", [128, 8]), ("nv2", [128, 1]),
                                                            ("p1", [128, 1]), ("p2", [128, 1]), ("a0", [128, 32]), ("junk", [128, 32]), ("csum", [128, 32]))}
            idx8 = kb.sb(es2, [128, 8], U32, "idx8")
            E2 = kb.sb(es2, [128, 64], F32, "E2"); E2b = kb.sb(es2, [128, 64], BF16, "E2b")
            blocks = [(0, 2)] + [(2 + 4 * i, 4) for i in range(8)]
            if last:
                blocks = blocks[1:]
            for bi, (t0, nt_) in enumerate(blocks):
                which = 1 if t0 == 0 else 0
                if t0 in (0, 2):
                    MOD = S["MOD"].ap()
                    kb.dma(gate1[:], MOD[which:which + 1, 2 * DM:3 * DM].partition_broadcast(128), reads=["MOD"], writes=[gate1])
                    self.load_mod_bc((G2, S2, tmpb), L, which, 3, "norm2_g")
                m_ = mT[bi % 2]
                nb = nt_ * 128
                c0 = t0 * 128
                kb.dma(m_[:, :, 0:nb], S["MT"].ap().rearrange("(kc p) t -> p kc t", p=128)[:, :, c0:c0 + nb],
                       reads=[("MT_s5", i) for i in range(9)] + [("MT_mla", i) for i in range(9)] + [("MT_hy", "l", i) for i in range(32)] + [("MT_hy", "c", i) for i in range(2)],
                       writes=[m_])
                for j in range(nt_):
                    t = t0 + j
                    x_ = xt[t % 2]; xo = xn[t % 2]
                    kb.dma(x_[:], X[t * 128:(t + 1) * 128, :], reads=[("X", t)], writes=[x_])
                    for nh in range(2):
                        po = kb.bank[nh]
                        for kc in range(8):
                            kb.mm(po[:, :], m_[:, kc, j * 128:(j + 1) * 128], wo[:, kc, nh * 512:(nh + 1) * 512], start=(kc == 0), stop=(kc == 7),
                                  reads=[m_, wo], writes=[po])
                        kb.tt("dve", tmp[:, nh * 512:(nh + 1) * 512], po[:, :], gate1[:, nh * 512:(nh + 1) * 512], OP.mult, reads=[po, gate1], writes=[tmp])
                    kb.tt("pool", xo[:], tmp[:], x_[:], OP.add, reads=[tmp, x_], writes=[xo])
                    kb.dma(X[t * 128:(t + 1) * 128, :], xo[:], reads=[xo], writes=[("X", t)], q="pool")
                    if "XMID" in self.debug:
                        kb.dma(S["XMID"].ap()[t * 128:(t + 1) * 128, :], xo[:], reads=[xo], writes=["XMID"], q="pool")
                    f_ = ff[t % 2]; fb_ = fbt[t % 2]
                    self.rms_modulate(xo, G2, S2, sq, ss, rstd, tmp, f_, fb_)
                    kb.dma(S["FB"].ap()[t * 128:(t + 1) * 128, :], fb_[:], reads=[fb_], writes=[("FB", t)], q="pool")
                    for half in range(2):
                        pt_ = kb.bank[2 + half]
                        for q in range(4):
                            kc = half * 4 + q
                            kb.tr(pt_[:, q * 128:(q + 1) * 128], f_[:, kc * 128:(kc + 1) * 128], ident_f[:], reads=[f_, ident_f], writes=[pt_])
                        kb.cp("act" if half else "dve", fT[:, half * 4:(half + 1) * 4, :], pt_[:, :].rearrange("p (q c) -> p q c", c=128), reads=[pt_], writes=[fT])
                    pl = kb.bank[4]
                    for kc in range(8):
                        kb.mm(pl[:, 0:36], fT[:, kc, :], wr[:, kc, :], start=(kc == 0), stop=(kc == 7), reads=[fT, wr], writes=[pl])
                    kb.cp("act", LG[:, t, :], pl[:, 0:36], reads=[pl], writes=[LG])
            tl = tiles[0]
            NTL = len(tiles)
            TS = slice(tl, NT)

            def bcl(ap2, n_):
                return ap2.unsqueeze(2).to_broadcast([128, NTL, n_])
            R = {k: kb.sb(es2, shp, F32, k) for k, shp in (("gm", [128, NT]), ("d4", [128, NT, 4]), ("ohg", [128, NT, 4]), ("sume", [128, NT]), ("gwv", [128, NT]),
                                                           ("pen", [128, NT, 4]), ("msk", [128, NT, 32]), ("msk2", [128, NT, 32]), ("v1", [128, NT]), ("v2", [128, NT]),
                                                           ("p1", [128, NT]), ("EE", [128, NT, 2, 32]), ("PRE", [128, NT, 64]), ("CNT", [128, NT, 64]),
                                                           ("CNTS", [128, NT, 32]), ("BASEI", [128, NT, 32]), ("A0", [128, NT, 32]), ("onesT", [128, NT]))}
            E2b = kb.sb(es2, [128, NT, 64], BF16, "E2b")
            LG4 = LG[:, TS, 0:4]
            kb.op("dve", lambda E: E.tensor_reduce(R["gm"][:, TS], LG4, AX.X, OP.max), reads=[LG], writes=[R["gm"]])
            kb.tt("dve", R["ohg"][:, TS, :], LG4, bcl(R["gm"][:, TS], 4), OP.is_equal, reads=[LG, R["gm"]], writes=[R["ohg"]])
            kb.tt("dve", R["d4"][:, TS, :], LG4, bcl(R["gm"][:, TS], 4), OP.subtract, reads=[LG, R["gm"]], writes=[R["d4"]])
            kb.act(R["d4"][:, TS, :], R["d4"][:, TS, :], AF.Exp, reads=[R["d4"]], writes=[R["d4"]])
            kb.op("dve", lambda E: E.tensor_reduce(R["sume"][:, TS], R["d4"][:, TS, :], AX.X, OP.add), reads=[R["d4"]], writes=[R["sume"]])
            kb.op("dve", lambda E: E.reciprocal(R["gwv"][:, TS], R["sume"][:, TS]), reads=[R["sume"]], writes=[R["gwv"]])
            kb.ts("dve", R["pen"][:, TS, :], R["ohg"][:, TS, :], 1.0e30, OP.mult, -1.0e30, OP.add, reads=[R["ohg"]], writes=[R["pen"]])
            kb.tt("dve", R["msk"][:, TS, :].rearrange("p t (g e) -> p t g e", g=4), LG[:, TS, 4:36].rearrange("p t (g e) -> p t g e", g=4),
                  R["pen"][:, TS, :].unsqueeze(3).to_broadcast([128, NTL, 4, 8]), OP.add, reads=[LG, R["pen"]], writes=[R["msk"]])
            kb.op("dve", lambda E: E.tensor_reduce(R["v1"][:, TS], R["msk"][:, TS, :], AX.X, OP.max), reads=[R["msk"]], writes=[R["v1"]])
            kb.tt("dve", R["EE"][:, TS, 0, :], R["msk"][:, TS, :], bcl(R["v1"][:, TS], 32), OP.is_equal, reads=[R["msk"], R["v1"]], writes=[R["EE"]])
            kb.stt("dve", R["msk2"][:, TS, :], R["EE"][:, TS, 0, :], -2.0e30, R["msk"][:, TS, :], OP.mult, OP.add, reads=[R["EE"], R["msk"]], writes=[R["msk2"]])
            kb.op("dve", lambda E: E.tensor_reduce(R["v2"][:, TS], R["msk2"][:, TS, :], AX.X, OP.max), reads=[R["msk2"]], writes=[R["v2"]])
            kb.tt("dve", R["EE"][:, TS, 1, :], R["msk2"][:, TS, :], bcl(R["v2"][:, TS], 32), OP.is_equal, reads=[R["msk2"], R["v2"]], writes=[R["EE"]])
            kb.tt("dve", R["p1"][:, TS], R["v1"][:, TS], R["v2"][:, TS], OP.subtract, reads=[R["v1"], R["v2"]], writes=[R["p1"]])
            kb.act(R["p1"][:, TS], R["p1"][:, TS], AF.Sigmoid, reads=[R["p1"]], writes=[R["p1"]])
            kb.tt("dve", GW[:, TS, 0], R["p1"][:, TS], R["gwv"][:, TS], OP.mult, reads=[R["p1"], R["gwv"]], writes=[GW])
            kb.ts("dve", R["p1"][:, TS], R["p1"][:, TS], -1.0, OP.mult, 1.0, OP.add, reads=[R["p1"]], writes=[R["p1"]])
            kb.tt("dve", GW[:, TS, 1], R["p1"][:, TS], R["gwv"][:, TS], OP.mult, reads=[R["p1"], R["gwv"]], writes=[GW])
            kb.cp("act", E2b[:, TS, :], R["EE"][:, TS, :, :].rearrange("p t k e -> p t (k e)"), reads=[R["EE"]], writes=[E2b])
            for g0 in range(tl, NT, 8):
                g1 = min(NT, g0 + 8)
                ppre = kb.bank[5]; pcnt = kb.bank[6]
                for t in range(g0, g1):
                    kb.mm(ppre[:, (t - g0) * 64:(t - g0 + 1) * 64], ltri[:], E2b[:, t, :], reads=[ltri, E2b], writes=[ppre])
                    kb.mm(pcnt[:, (t - g0) * 64:(t - g0 + 1) * 64], ones[:], E2b[:, t, :], reads=[ones, E2b], writes=[pcnt])
                kb.cp("act", R["PRE"][:, g0:g1, :], ppre[:, 0:(g1 - g0) * 64].rearrange("p (t c) -> p t c", c=64), reads=[ppre], writes=[R["PRE"]])
                kb.cp("dve", R["CNT"][:, g0:g1, :], pcnt[:, 0:(g1 - g0) * 64].rearrange("p (t c) -> p t c", c=64), reads=[pcnt], writes=[R["CNT"]])
            kb.tt("dve", R["CNTS"][:, TS, :], R["CNT"][:, TS, 0:32], R["CNT"][:, TS, 32:64], OP.add, reads=[R["CNT"]], writes=[R["CNTS"]])
            kb.memset("dve", R["onesT"][:], 1.0, writes=[R["onesT"]])
            for e in range(32):
                kb.op("dve", lambda E: E.tensor_tensor_scan(R["BASEI"][:, TS, e], R["onesT"][:, TS], R["CNTS"][:, TS, e], 0.0, OP.mult, OP.add),
                      reads=[R["onesT"], R["CNTS"]], writes=[R["BASEI"]])
            kb.cp("dve", base[:], R["BASEI"][:, NT - 1, :], reads=[R["BASEI"]], writes=[base])
            kb.tt("dve", R["BASEI"][:, TS, :], R["BASEI"][:, TS, :], R["CNTS"][:, TS, :], OP.subtract, reads=[R["BASEI"], R["CNTS"]], writes=[R["BASEI"]])
            kb.tt("dve", R["A0"][:, TS, :], R["PRE"][:, TS, 0:32], R["BASEI"][:, TS, :], OP.add, reads=[R["PRE"], R["BASEI"]], writes=[R["A0"]])
            kb.tt("dve", R["A0"][:, TS, :], R["A0"][:, TS, :], R["EE"][:, TS, 0, :], OP.mult, reads=[R["A0"], R["EE"]], writes=[R["A0"]])
            kb.op("dve", lambda E: E.tensor_reduce(RANK[:, TS, 0], R["A0"][:, TS, :], AX.X, OP.add), reads=[R["A0"]], writes=[RANK])
            kb.tt("dve", R["A0"][:, TS, :], R["PRE"][:, TS, 32:64], R["BASEI"][:, TS, :], OP.add, reads=[R["PRE"], R["BASEI"]], writes=[R["A0"]])
            kb.tt("dve", R["A0"][:, TS, :], R["A0"][:, TS, :], R["CNT"][:, TS, 0:32], OP.add, reads=[R["A0"], R["CNT"]], writes=[R["A0"]])
            kb.tt("dve", R["A0"][:, TS, :], R["A0"][:, TS, :], R["EE"][:, TS, 1, :], OP.mult, reads=[R["A0"], R["EE"]], writes=[R["A0"]])
            kb.op("dve", lambda E: E.tensor_reduce(RANK[:, TS, 1], R["A0"][:, TS, :], AX.X, OP.add), reads=[R["A0"]], writes=[RANK])
            pb_ = kb.sb(es2, [128, 32], F32, "padb"); pend = kb.sb(es2, [128, 32], F32, "pend"); pst = kb.sb(es2, [128, 32], F32, "pstart")
            one32 = kb.sb(es2, [128, 32], F32, "one32")
            kb.memset("dve", one32[:], 1.0, writes=[one32])
            kb.ts("dve", pb_[:], base[:], 1.0 / 128, OP.mult, (127.0 / 128 - 0.5 + 1.0 / 256), OP.add, reads=[base], writes=[pb_])
            kb.ts("dve", pb_[:], pb_[:], MAGIC, OP.add, reads=[pb_], writes=[pb_])
            kb.ts("dve", pb_[:], pb_[:], -MAGIC, OP.add, reads=[pb_], writes=[pb_])
            kb.op("dve", lambda E: E.tensor_tensor_scan(pend[:], one32[:], pb_[:], 0.0, OP.mult, OP.add), reads=[one32, pb_], writes=[pend])
            kb.tt("dve", pst[:], pend[:], pb_[:], OP.subtract, reads=[pend, pb_], writes=[pst])
            kb.ts("dve", pst[:], pst[:], 128.0, OP.mult, reads=[pst], writes=[pst])
            kb.memset("dve", BE[:], 0.0, writes=[BE])
            for e in range(32):
                kb.stt("dve", BE[:], iota_b[:], pend[:, e:e + 1], BE[:], OP.is_ge, OP.add, reads=[iota_b, pend, BE], writes=[BE])
            NS = 4
            per = nblk // NS
            assert nblk % NS == 0
            BIGI = 1.0e6
            kb.ts("dve", BE[:], BE[:], 31.0, OP.min, reads=[BE], writes=[BE])
            kb.memset("dve", CHG[:], 1.0, writes=[CHG])
            kb.tt("dve", CHG[:, 1:nblk], BE[:, 1:nblk], BE[:, 0:nblk - 1], OP.not_equal, reads=[BE], writes=[CHG])
            for q in range(NS):
                kb.memset("dve", CHG[:, q * per:q * per + 1], 1.0, writes=[CHG])
            kb.ts("dve", BE[:], BE[:], float(L * 32), OP.add, 128.0, OP.mult, reads=[BE], writes=[BE])
            kb.ts("dve", BE[:], BE[:], iota_p[:, 0:1], OP.add, 2.0, OP.mult, reads=[BE, iota_p], writes=[BE])
            for half in range(2):
                kb.ts("dve", IDXF[:], BE[:], float(half) - BIGI, OP.add, reads=[BE], writes=[IDXF])
                kb.tt("dve", IDXF[:], IDXF[:], CHG[:], OP.mult, reads=[IDXF, CHG], writes=[IDXF])
                kb.ts("dve", IDXF[:], IDXF[:], BIGI, OP.add, reads=[IDXF], writes=[IDXF])
                kb.cp("dve", IDXW[:, half, :], IDXF[:], reads=[IDXF], writes=[IDXW])
            for k in range(2):
                kb.tt("dve", R["A0"][:, TS, :], R["EE"][:, TS, k, :], pst[:, :].unsqueeze(1).to_broadcast([128, NTL, 32]), OP.mult,
                      reads=[R["EE"], pst], writes=[R["A0"]])
                kb.op("dve", lambda E: E.tensor_reduce(DEST[:, TS, k], R["A0"][:, TS, :], AX.X, OP.add), reads=[R["A0"]], writes=[DEST])
            kb.tt("dve", DEST[:], DEST[:], RANK[:], OP.add, reads=[DEST, RANK], writes=[DEST])
            kb.ts("dve", DEST[:], DEST[:], float(nblk * 128 - 1), OP.min, 0.0, OP.max, reads=[DEST], writes=[DEST])
            if "DBGR" in self.debug:
                kb.dma(S["DBGR"].ap()[:, 0:NT * 2], DEST[:].rearrange("p t k -> p (t k)"), reads=[DEST], writes=["DBGR"])
                kb.dma(S["DBGR"].ap()[:, NT * 2:NT * 4], RANK[:].rearrange("p t k -> p (t k)"), reads=[RANK], writes=["DBGR"])
                kb.dma(S["DBGR"].ap()[:, NT * 4:NT * 6], GW[:].rearrange("p t k -> p (t k)"), reads=[GW], writes=["DBGR"])
                kb.dma(S["DBGR"].ap()[:, NT * 6:NT * 6 + 32], base[:], reads=[base], writes=["DBGR"])
                kb.dma(S["DBGR"].ap()[:, NT * 6 + 32:NT * 6 + 64], pst[:], reads=[pst], writes=["DBGR"])
            kb.cp("dve", DESTI[:], DEST[:], reads=[DEST], writes=[DESTI])
            for t in tiles:
                fb_ = fbt[t % 2]
                kb.dma(fb_[:], S["FB"].ap()[t * 128:(t + 1) * 128, :], reads=[("FB", t)], writes=[fb_])
                for k in range(2):
                    kb.dma(None, None, reads=[fb_, DESTI], writes=["XS"], q="pool",
                           fn=lambda E: E.indirect_dma_start(out=S["XS"].ap(), out_offset=bass.IndirectOffsetOnAxis(DESTI[:, t, k:k + 1], 0),
                                                             in_=fb_[:], in_offset=None))
            kb.barrier()
            es2.close()
            es4 = ExitStack()
            WQ = [kb.sb(es4, [128, 3, 4096], BF16, "WQ%d" % i) for i in range(NS)]
            xs = [kb.sb(es4, [128, DM], BF16, "xs%d" % i) for i in range(2)]
            XT = [kb.sb(es4, [128, 8, 128], BF16, "XT%d" % i) for i in range(2)]
            h1s = kb.sb(es4, [128, 512], F32, "h1s")
            aT = [kb.sb(es4, [128, 4, 128], BF16, "aT%d" % i) for i in range(2)]
            ysb = [kb.sb(es4, [128, DM], F32, "ysb%d" % i) for i in range(2)]
            nrows2 = 2 * DEPTH * 32 * 128
            for slot in range(nblk):
                if "moe_noB" in self.debug:
                    break
                qs = slot % NS
                b = qs * per + slot // NS
                wb = WQ[qs]
                for wi, wn in enumerate(("moe_w1", "moe_w3", "moe_w2")):
                    for half in range(2):
                        kb.dma(None, None, reads=[IDXW], writes=[wb], q="pool",
                               fn=lambda E: E.indirect_dma_start(out=wb[:, wi, half * 2048:(half + 1) * 2048], out_offset=None,
                                                                 in_=I[wn].ap().rearrange("r (h c) -> (r h) c", h=2),
                                                                 in_offset=bass.IndirectOffsetOnAxis(IDXW[:, half, b:b + 1], 0),
                                                                 bounds_check=kb.bnd_reg, oob_is_err=False))
                x_ = xs[slot % 2]
                kb.dma(x_[:], S["XS"].ap()[b * 128:(b + 1) * 128, :], reads=["XS"], writes=[x_])
                pt_ = kb.bank[0]
                ptv = pt_[:].bitcast(BF16)
                for kc in range(8):
                    kb.tr(ptv[:, kc * 128:(kc + 1) * 128], x_[:, kc * 128:(kc + 1) * 128], ident[:], reads=[x_, ident], writes=[pt_])
                XT_ = XT[slot % 2]
                kb.cp("dve", XT_[:], ptv.rearrange("p (k n) -> p k n", k=8), reads=[pt_], writes=[XT_])
                ph1 = kb.bank[1]; ph3 = kb.bank[2]
                for (pp, wi) in ((ph1, 0), (ph3, 1)):
                    for hc in range(4):
                        for kc in range(8):
                            kb.mm(pp[:, hc * 128:(hc + 1) * 128], wb[:, wi, kc * 512 + hc * 128:kc * 512 + (hc + 1) * 128], XT_[:, kc, :],
                                  start=(kc == 0), stop=(kc == 7), reads=[wb, XT_], writes=[pp])
                kb.act(h1s[:], ph1[:, :], AF.Silu, reads=[ph1], writes=[h1s])
                a_ = aT[slot % 2]
                kb.tt("dve", a_[:].rearrange("p h r -> p (h r)"), h1s[:], ph3[:, :], OP.mult, reads=[h1s, ph3], writes=[a_])
                y_ = ysb[slot % 2]
                for nh in range(2):
                    py = kb.bank[3 + nh]
                    for hc in range(4):
                        kb.mm(py[:, :], a_[:, hc, :], wb[:, 2, hc * 1024 + nh * 512:hc * 1024 + (nh + 1) * 512], start=(hc == 0), stop=(hc == 3),
                              reads=[a_, wb], writes=[py])
                    kb.cp("act" if nh else "dve", y_[:, nh * 512:(nh + 1) * 512], py[:, :], reads=[py], writes=[y_])
                kb.dma(S["YS"].ap()[b * 128:(b + 1) * 128, :], y_[:], reads=[y_], writes=["YS"])
            kb.barrier()
            es4.close()
            es5 = ExitStack()
            gate2 = kb.sb(es5, [128, DM], F32, "gate2")
            xt = [kb.sb(es5, [128, DM], F32, "xt%d" % i) for i in range(2)]
            y0 = [kb.sb(es5, [128, DM], F32, "y0%d" % i) for i in range(2)]
            y1 = [kb.sb(es5, [128, DM], F32, "y1%d" % i) for i in range(2)]
            xo = [kb.sb(es5, [128, DM], F32, "xo%d" % i) for i in range(2)]
            if last:
                fg = kb.sb(es5, [128, DM], F32, "fg")
                kb.dma(fg[:], I["final_g"].ap().partition_broadcast(128), writes=[fg])
                sq = kb.sb(es5, [128, DM], BF16, "sq"); ss = kb.sb(es5, [128, 1], F32, "ss")
            for t in tiles:
                if "moe_noC" in self.debug:
                    break
                which = 1 if t < 2 else 0
                if t in (0, 2):
                    kb.dma(gate2[:], S["MOD"].ap()[which:which + 1, 5 * DM:6 * DM].partition_broadcast(128), reads=["MOD"], writes=[gate2])
                x_ = xt[t % 2]; a_ = y0[t % 2]; b_ = y1[t % 2]; o_ = xo[t % 2]
                kb.dma(x_[:], X[t * 128:(t + 1) * 128, :], reads=[("X", t)], writes=[x_])
                for k, yy in enumerate((a_, b_)):
                    kb.dma(None, None, reads=[DESTI, "YS"], writes=[yy], q="pool",
                           fn=lambda E: E.indirect_dma_start(out=yy[:], out_offset=None, in_=S["YS"].ap(),
                                                             in_offset=bass.IndirectOffsetOnAxis(DESTI[:, t, k:k + 1], 0)))
                kb.ts("dve", a_[:], a_[:], GW[:, t, 0:1], OP.mult, reads=[a_, GW], writes=[a_])
                kb.stt("dve", a_[:], b_[:], GW[:, t, 1:2], a_[:], OP.mult, OP.add, reads=[b_, GW, a_], writes=[a_])
                if "YMOE" in self.debug:
                    kb.dma(S["YMOE"].ap()[t * 128:(t + 1) * 128, :], a_[:], reads=[a_], writes=["YMOE"], q="pool")
                kb.tt("pool", a_[:], a_[:], gate2[:], OP.mult, reads=[a_, gate2], writes=[a_])
                kb.tt("dve", o_[:], a_[:], x_[:], OP.add, reads=[a_, x_], writes=[o_])
                if not last:
                    kb.dma(X[t * 128:(t + 1) * 128, :], o_[:], reads=[o_], writes=[("X", t)], q="pool")
                else:
                    kb.act(sq[:], o_[:], AF.Square, accum_out=ss[:], reads=[o_], writes=[sq, ss])
                    kb.ts("dve", ss[:], ss[:], 1.0 / DM, OP.mult, EPS, OP.add, reads=[ss], writes=[ss])
                    kb.act(ss[:], ss[:], AF.Sqrt, reads=[ss], writes=[ss])
                    kb.op("dve", lambda E: E.reciprocal(ss[:], ss[:]), reads=[ss], writes=[ss])
                    kb.stt("dve", o_[:], o_[:], ss[:, 0:1], fg[:], OP.mult, OP.mult, reads=[o_, ss, fg], writes=[o_])
                    kb.dma(self.out.ap()[(t - 2) * 128:(t - 1) * 128, :], o_[:], reads=[o_], writes=["OUT"], q="pool")
            kb.barrier()
            es5.close()
        kb.barrier()

    def finish(self, out_keys):
        kb = self.kb
        kb._waits("sp", out_keys, ())
        kb.barrier()


def build(debug=(), layers=DEPTH, stages=("mod", "proj")):
    P1 = _build(debug, layers, stages, None)
    return _build(debug, layers, stages, P1.kb.waited)


def _build(debug, layers, stages, needed):
    P = Prog(debug, needed)
    P.declare()
    P.stage_init()
    P.kb.barrier()
    for L in range(layers):
        if "mod" in stages:
            P.stage_mod(L)
        if "proj" in stages:
            P.stage_proj(L)
        if "s5" in stages:
            P.stage_s5(L)
        if "mla" in stages:
            P.stage_mla(L, L < DEPTH - 1)
        if "hy" in stages:
            P.stage_hyena(L, "l")
            if L < DEPTH - 1:
                P.stage_hyena(L, "c")
        if "moe" in stages:
            P.stage_s5merge(L)
            P.stage_out_moe(L, L == DEPTH - 1)
    P.finish(["OUT"])
    return P


def kernel(**inputs):
    inp = {k: np.asarray(v) for k, v in inputs.items()}
    P = build(debug=(), layers=DEPTH, stages=("mod", "proj", "s5", "mla", "hy", "moe"))
    maps = prep_inputs(inp)
    names = set(P.I.keys())
    in_maps = [{k: v for k, v in m.items() if k in names} for m in maps]
    res = run_bass_kernel_spmd(P.nc, in_maps, core_ids=list(range(8)))
    return np.stack([np.asarray(r["out"], dtype=np.float32) for r in res.results], axis=0)
```

```python
import math
from contextlib import ExitStack
import numpy as np
import ml_dtypes
import concourse.bass as bass
import concourse.mybir as mybir
from concourse.bass_utils import run_bass_kernel_spmd

F32 = mybir.dt.float32
BF16 = mybir.dt.bfloat16
I32 = mybir.dt.int32
U32 = mybir.dt.uint32
AF = mybir.ActivationFunctionType
OP = mybir.AluOpType
AX = mybir.AxisListType

DM = 1024
NCTX = 256
NLAT = 4096
T = NCTX + NLAT
NT = T // 128
DEPTH = 4
EPS = 1e-6
P_IN = 1696
MAGIC = 12582912.0
MLA_SCALE = 1.0 / math.sqrt(96.0)
NE = 32
NBLK = 100


class Buf:
    __slots__ = ("w", "r", "name")

    def __init__(self, name=""):
        self.w = None
        self.r = {}
        self.name = name


class KB:
    NDMA = 10

    def __init__(self, needed=None):
        nc = bass.Bass("TRN2", target_bir_lowering=False)
        self.nc = nc
        self.needed = needed
        self.waited = {e: set() for e in ("pe", "act", "dve", "pool")}
        self.real = {e: 0 for e in ("pe", "act", "dve", "pool")}
        self.vmap = {e: {} for e in ("pe", "act", "dve", "pool")}
        self.eng = {"pe": nc.tensor, "act": nc.scalar, "dve": nc.vector, "pool": nc.gpsimd, "sp": nc.sync}
        self.sem = {}
        self.cnt = {}
        for e in ("pe", "act", "dve", "pool"):
            self.sem[e] = nc.alloc_semaphore("s_" + e)
            self.cnt[e] = 0
        self.known = {e: {} for e in self.eng}
        self.dq = {}
        for q in ("sp", "act", "pool"):
            sems = []
            for i in range(self.NDMA):
                k = ("d", q, i)
                self.sem[k] = nc.alloc_semaphore("d_%s_%d" % (q, i))
                self.cnt[k] = 0
                sems.append(k)
            self.dq[q] = [sems, 0]
        self.nalloc = 0
        self.bufs = {}
        self.ninst = 0
        self.bank = [nc.alloc_psum_tensor("bank%d" % i, [128, 512], F32) for i in range(8)]
        self.bnd_reg = nc.gpsimd.alloc_register("bnd")
        nc.gpsimd.reg_mov(self.bnd_reg, 2 * DEPTH * 32 * 128 - 1)

    def sb(self, es, shape, dt=F32, name="t"):
        self.nalloc += 1
        t = es.enter_context(self.nc.sbuf_tensor("%s_%d" % (name, self.nalloc), list(shape), dt))
        return t

    def dram(self, name, shape, dt=F32, kind="Internal"):
        return self.nc.dram_tensor(name, list(shape), dt, kind=kind)

    def B(self, key):
        if isinstance(key, Buf):
            return key
        k = key if isinstance(key, (str, tuple, int)) else id(key)
        b = self.bufs.get(k)
        if b is None:
            b = Buf(str(k))
            self.bufs[k] = b
        return b

    def _wait(self, e, k, v):
        if isinstance(k, str):
            self.waited[k].add(v)
            rv = v if self.needed is None else self.vmap[k][v]
        else:
            rv = v
        self.eng[e].wait_ge(self.sem[k], rv)
        self.known[e][k] = v

    def _waits(self, e, reads, writes):
        need = {}
        for b in reads:
            b = self.B(b)
            if b.w is not None:
                k, v = b.w
                if need.get(k, 0) < v:
                    need[k] = v
        for b in writes:
            b = self.B(b)
            if b.w is not None:
                k, v = b.w
                if need.get(k, 0) < v:
                    need[k] = v
            for k, v in b.r.items():
                if need.get(k, 0) < v:
                    need[k] = v
        kn = self.known[e]
        for k, v in need.items():
            if k == e and e == "pe":
                continue
            if kn.get(k, 0) >= v:
                continue
            self._wait(e, k, v)

    def _done(self, key, val, reads, writes):
        for b in writes:
            b = self.B(b)
            b.w = (key, val)
            b.r = {}
        for b in reads:
            b = self.B(b)
            if b.r.get(key, 0) < val:
                b.r[key] = val

    def op(self, e, fn, reads=(), writes=()):
        self._waits(e, reads, writes)
        inst = fn(self.eng[e])
        self.cnt[e] += 1
        v = self.cnt[e]
        if self.needed is None or v in self.needed[e]:
            self.real[e] += 1
            self.vmap[e][v] = self.real[e]
            inst.then_inc(self.sem[e], 1)
        self._done(e, v, reads, writes)
        self.ninst += 1
        return inst

    def dma(self, out, in_, reads=(), writes=(), q="sp", fn=None, **kw):
        sems, i = self.dq[q]
        k = sems[i % len(sems)]
        self.dq[q][1] = i + 1
        kn = self.known[q]
        if kn.get(k, 0) < self.cnt[k]:
            self._wait(q, k, self.cnt[k])
        self._waits(q, reads, writes)
        if fn is not None:
            inst = fn(self.eng[q])
        else:
            inst = self.eng[q].dma_start(out=out, in_=in_, **kw)
        self.cnt[k] += 16
        inst.then_inc(self.sem[k], 16)
        self._done(k, self.cnt[k], reads, writes)
        self.ninst += 1
        return inst

    def barrier(self):
        for e in self.eng:
            kn = self.known[e]
            for k, v in self.cnt.items():
                if v == 0 or kn.get(k, 0) >= v:
                    continue
                if k == e and e == "pe":
                    continue
                self._wait(e, k, v)
        self.bufs = {k: b for k, b in self.bufs.items() if isinstance(k, (str, tuple))}
        for b in self.bufs.values():
            b.w = None
            b.r = {}

    def mm(self, out, lhsT, rhs, start=True, stop=True, reads=(), writes=(), **kw):
        return self.op("pe", lambda E: E.matmul(out, lhsT, rhs, start=start, stop=stop, **kw), reads, writes)

    def tr(self, out, in_, ident, reads=(), writes=()):
        return self.op("pe", lambda E: E.transpose(out, in_, ident), reads, writes)

    def act(self, out, in_, func, reads=(), writes=(), **kw):
        return self.op("act", lambda E: E.activation(out, in_, func, **kw), reads, writes)

    def tt(self, e, out, in0, in1, op, reads=(), writes=()):
        return self.op(e, lambda E: E.tensor_tensor(out, in0, in1, op), reads, writes)

    def ts(self, e, out, in0, s1, op0, s2=None, op1=None, reads=(), writes=(), **kw):
        if op1 is None:
            return self.op(e, lambda E: E.tensor_scalar(out, in0, s1, None, op0, **kw), reads, writes)
        return self.op(e, lambda E: E.tensor_scalar(out, in0, s1, s2, op0, op1, **kw), reads, writes)

    def stt(self, e, out, in0, scalar, in1, op0, op1, reads=(), writes=()):
        return self.op(e, lambda E: E.scalar_tensor_tensor(out, in0, scalar, in1, op0, op1), reads, writes)

    def cp(self, e, out, in_, reads=(), writes=()):
        if e == "act":
            return self.op(e, lambda E: E.copy(out, in_), reads, writes)
        return self.op(e, lambda E: E.tensor_copy(out, in_), reads, writes)

    def memset(self, e, ap, val, writes=()):
        return self.op(e, lambda E: E.memset(ap, val), (), writes)


def _rope_tables():
    half = 16
    inv = 10000.0 ** (-np.arange(0, half, 2, dtype=np.float64) / half)
    i = np.arange(NLAT)
    row = (i // 64).astype(np.float64)
    col = (i % 64).astype(np.float64)
    ar = row[None, :] * inv[:, None]
    ac = col[None, :] * inv[:, None]
    cos = np.ones((32, T), np.float64)
    sin = np.zeros((32, T), np.float64)
    cos[0:8, NCTX:] = np.cos(ar); cos[8:16, NCTX:] = np.cos(ar); cos[16:24, NCTX:] = np.cos(ac); cos[24:32, NCTX:] = np.cos(ac)
    sin[0:8, NCTX:] = np.sin(ar); sin[8:16, NCTX:] = np.sin(ar); sin[16:24, NCTX:] = np.sin(ac); sin[24:32, NCTX:] = np.sin(ac)
    return cos.astype(np.float32), sin.astype(np.float32)


def _hy_tables(n):
    N2 = 2 * n
    ntc = n // 128
    F = n + 1
    nj = (F + 127) // 128
    t = np.arange(n, dtype=np.float64)
    tt_ = t / (n - 1)
    bands = 16
    f = np.linspace(1e-4, bands - 1, bands)
    ang = (2.0 * math.pi * t / n)[:, None] * f
    z = np.concatenate([tt_[:, None], np.cos(ang), -np.sin(ang)], axis=-1)
    lo, hi = math.log(1e-2) / 1.5, math.log(1e-2) / 0.3
    deltas = np.abs(np.linspace(lo, hi, 256))
    decay = np.exp(-tt_[None, :] * deltas[:, None])
    fi = np.arange(nj * 128, dtype=np.float64)
    valid = (fi <= n).astype(np.float64)
    ph = 2.0 * math.pi * np.outer(t, fi) / N2
    cosm = np.cos(ph) * valid[None, :]
    sinm = -np.sin(ph) * valid[None, :]
    def fwd_layout(m):
        return m.reshape(ntc, 128, nj, 128).transpose(2, 1, 0, 3)
    dfwd = np.concatenate([fwd_layout(cosm), fwd_layout(sinm)], axis=0).astype(ml_dtypes.bfloat16)
    wf = np.where((fi == 0) | (fi == n), 1.0, 2.0) * valid / N2
    icos = (np.cos(ph) * wf[None, :]).T
    isin = (-np.sin(ph) * wf[None, :]).T
    def inv_layout(m):
        return m.reshape(nj, 128, ntc, 128).transpose(2, 1, 0, 3)
    dinv = np.concatenate([inv_layout(icos), inv_layout(isin)], axis=2).astype(ml_dtypes.bfloat16)
    return (np.ascontiguousarray(z.T.astype(np.float32)), np.ascontiguousarray(decay.astype(np.float32)),
            np.ascontiguousarray(dfwd), np.ascontiguousarray(dinv))


def const_inputs():
    c = {}
    c["iota_e"] = np.ascontiguousarray(np.broadcast_to(np.arange(32, dtype=np.float32)[None, :], (128, 32)))
    c["iota_b"] = np.ascontiguousarray(np.broadcast_to(np.arange(128, dtype=np.float32)[None, :], (128, 128)))
    c["iota_p"] = np.arange(128, dtype=np.float32).reshape(128, 1)
    c["ltri"] = np.triu(np.ones((128, 128), np.float32), k=1).astype(ml_dtypes.bfloat16)
    for nm, n in (("l", NLAT), ("c", NCTX)):
        z, dec, dfwd, dinv = _hy_tables(n)
        c["hy_zpos_" + nm] = z
        c["hy_decay_" + nm] = dec
        c["hy_dfwd_" + nm] = dfwd
        c["hy_dinv_" + nm] = dinv
    c["ident"] = np.eye(128, dtype=np.float32)
    cos, sin = _rope_tables()
    c["rope_cos"] = cos
    c["rope_sin"] = sin
    return c


def prep_inputs(inp):
    cst = const_inputs()
    shared = dict(cst)
    for k in ("ada_w", "ada_b", "norm1_g", "norm2_g", "w_in"):
        shared[k] = np.ascontiguousarray(inp[k], dtype=np.float32)
    def lane(a):
        a = np.asarray(a, np.float32)
        Ld = a.shape[0]
        rest = a.shape[4:]
        a = a.reshape((Ld, 2, 8, 2, 64) + rest)
        nd = a.ndim
        a = a.transpose((0, 3, 4, 1, 2) + tuple(range(5, nd)))
        return np.ascontiguousarray(a.reshape((Ld, 128, 16) + rest))
    shared["s5_lre"] = lane(inp["s5_lambda_re"])
    shared["s5_lim"] = lane(inp["s5_lambda_im"])
    shared["s5_ldt"] = lane(np.broadcast_to(np.asarray(inp["s5_log_dt"], np.float32)[..., None], (DEPTH, 2, 16, 64)))
    shared["s5_bre"] = lane(inp["s5_b_re"])
    shared["s5_bim"] = lane(inp["s5_b_im"])
    shared["s5_cre"] = lane(np.asarray(inp["s5_c_re"], np.float32).transpose(0, 1, 2, 4, 3))
    shared["s5_cim"] = lane(np.asarray(inp["s5_c_im"], np.float32).transpose(0, 1, 2, 4, 3))
    shared["s5_dcol"] = np.ascontiguousarray(np.asarray(inp["s5_d"], np.float32).reshape(DEPTH, 2, 128).transpose(0, 2, 1))
    shared["s5_glu_w"] = np.ascontiguousarray(inp["s5_glu_w"], dtype=np.float32)
    shared["mla_qg"] = np.ascontiguousarray(np.asarray(inp["mla_q_norm_g"], np.float32).reshape(DEPTH, 3, 128).transpose(0, 2, 1))
    shared["mla_kvg"] = np.ascontiguousarray(np.asarray(inp["mla_kv_norm_g"], np.float32).reshape(DEPTH, 2, 128).transpose(0, 2, 1))
    shared["mla_w_uq"] = np.ascontiguousarray(inp["mla_w_uq"], dtype=np.float32)
    shared["mla_w_ukv"] = np.ascontiguousarray(inp["mla_w_ukv"], dtype=np.float32)
    shared["mix_norm_g"] = np.ascontiguousarray(inp["mix_norm_g"], dtype=np.float32)
    shared["hy_sw"] = np.ascontiguousarray(np.asarray(inp["hy_short_w"], np.float32).reshape(DEPTH, 3, 6, 128).transpose(0, 3, 2, 1))
    shared["hy_sb"] = np.ascontiguousarray(np.asarray(inp["hy_short_b"], np.float32).reshape(DEPTH, 6, 128).transpose(0, 2, 1))
    shared["hy_cols"] = np.ascontiguousarray(np.stack([inp["hy_f_b1"], inp["hy_f_b2"], inp["hy_f_freq"][:, 0], inp["hy_f_freq"][:, 1]], axis=-1), dtype=np.float32)
    shared["hy_b3"] = np.ascontiguousarray(np.asarray(inp["hy_f_b3"], np.float32).reshape(DEPTH, 8, 128).transpose(0, 2, 1))
    for k in ("hy_f_w1", "hy_f_w2", "hy_f_w3", "hy_bias"):
        shared[k] = np.ascontiguousarray(inp[k], dtype=np.float32)
    shared["mix_g_s5col"] = np.ascontiguousarray(np.asarray(inp["mix_norm_g"], np.float32)[:, 0:256].reshape(DEPTH, 2, 128).transpose(0, 2, 1))
    shared["w_out"] = np.ascontiguousarray(inp["w_out"], dtype=np.float32)
    shared["moe_wr"] = np.ascontiguousarray(np.concatenate([inp["moe_w_group"], inp["moe_w_expert"]], axis=-1), dtype=np.float32)
    shared["moe_w1"] = np.ascontiguousarray(np.asarray(inp["moe_w1"], np.float32).reshape(DEPTH, 32, 8, 128, 512).transpose(0, 1, 3, 2, 4)).reshape(DEPTH * 32 * 128, 4096)
    shared["moe_w3"] = np.ascontiguousarray(np.asarray(inp["moe_w3"], np.float32).reshape(DEPTH, 32, 8, 128, 512).transpose(0, 1, 3, 2, 4)).reshape(DEPTH * 32 * 128, 4096)
    shared["moe_w2"] = np.ascontiguousarray(np.asarray(inp["moe_w2"], np.float32).reshape(DEPTH, 32, 4, 128, 1024).transpose(0, 1, 3, 2, 4)).reshape(DEPTH * 32 * 128, 4096)
    shared["final_g"] = np.ascontiguousarray(np.asarray(inp["final_g"], np.float32).reshape(1, DM))
    maps = []
    for b in range(8):
        m = dict(shared)
        m["x"] = np.ascontiguousarray(inp["x"][b])
        m["ctx"] = np.ascontiguousarray(inp["ctx"][b])
        cc = np.stack([inp["c"][b], inp["c_ctx"]], axis=0)
        m["cc"] = np.ascontiguousarray(cc.reshape(2, 8, 128).transpose(2, 1, 0))
        maps.append(m)
    return maps


class Prog:
    def __init__(self, debug=(), needed=None):
        self.kb = KB(needed)
        self.nc = self.kb.nc
        self.debug = set(debug)
        self.I = {}
        self.S = {}
        self.outs = []

    def inp(self, name, shape, dt=F32):
        t = self.nc.dram_tensor(name, list(shape), dt, kind="ExternalInput")
        self.I[name] = t
        return t

    def scratch(self, name, shape, dt=F32):
        kind = "ExternalOutput" if name in self.debug else "Internal"
        t = self.nc.dram_tensor(name, list(shape), dt, kind=kind)
        self.S[name] = t
        if kind == "ExternalOutput":
            self.outs.append(name)
        return t

    def declare(self):
        self.inp("x", [NLAT, DM]); self.inp("ctx", [NCTX, DM]); self.inp("cc", [128, 8, 2])
        self.inp("ada_w", [DEPTH, DM, 6 * DM]); self.inp("ada_b", [DEPTH, 6 * DM])
        self.inp("norm1_g", [DEPTH, DM]); self.inp("norm2_g", [DEPTH, DM])
        self.inp("w_in", [DEPTH, DM, P_IN])
        self.inp("ident", [128, 128]); self.inp("rope_cos", [32, T]); self.inp("rope_sin", [32, T])
        for nm in ("s5_lre", "s5_lim", "s5_ldt"):
            self.inp(nm, [DEPTH, 128, 16])
        for nm in ("s5_bre", "s5_bim", "s5_cre", "s5_cim"):
            self.inp(nm, [DEPTH, 128, 16, 16])
        self.inp("s5_dcol", [DEPTH, 128, 2]); self.inp("s5_glu_w", [DEPTH, 256, 256])
        self.inp("mla_qg", [DEPTH, 128, 3]); self.inp("mla_kvg", [DEPTH, 128, 2])
        self.inp("mla_w_uq", [DEPTH, 384, 768]); self.inp("mla_w_ukv", [DEPTH, 256, 1024])
        self.inp("mix_norm_g", [DEPTH, DM])
        self.scratch("MT", [DM, T], BF16)
        if "MLAO" in self.debug:
            self.scratch("MLAO", [T, 512])
        for nm, n in (("l", NLAT), ("c", NCTX)):
            nj = (n + 1 + 127) // 128
            self.inp("hy_zpos_" + nm, [33, n]); self.inp("hy_decay_" + nm, [256, n])
            self.inp("hy_dfwd_" + nm, [2 * nj, 128, n // 128, 128], BF16); self.inp("hy_dinv_" + nm, [n // 128, 128, 2 * nj, 128], BF16)
            self.scratch("KSPEC_" + nm, [nj, 2, 128, 512])
        self.inp("hy_sw", [DEPTH, 128, 6, 3]); self.inp("hy_sb", [DEPTH, 128, 6]); self.inp("hy_cols", [DEPTH, 64, 4]); self.inp("hy_b3", [DEPTH, 128, 8])
        self.inp("hy_f_w1", [DEPTH, 33, 64]); self.inp("hy_f_w2", [DEPTH, 64, 64]); self.inp("hy_f_w3", [DEPTH, 64, 1024]); self.inp("hy_bias", [DEPTH, 2, 256])
        self.scratch("HZTM", [T, 768]); self.scratch("HY1", [T, 256])
        if "HYO" in self.debug:
            self.scratch("HYO", [T, 256])
        self.inp("iota_e", [128, 32]); self.inp("iota_b", [128, 128]); self.inp("iota_p", [128, 1]); self.inp("ltri", [128, 128], BF16)
        self.inp("mix_g_s5col", [DEPTH, 128, 2])
        self.inp("w_out", [DEPTH, DM, DM]); self.inp("moe_wr", [DEPTH, DM, 36])
        for nm_ in ("moe_w1", "moe_w3", "moe_w2"):
            self.inp(nm_, [DEPTH * 32 * 128, 4096])
        self.inp("final_g", [1, DM])
        self.scratch("FB", [T, DM], BF16); self.scratch("XS", [NBLK * 128, DM], BF16); self.scratch("YS", [NBLK * 128, DM])
        if "XMID" in self.debug:
            self.scratch("XMID", [T, DM])
        if "YMOE" in self.debug:
            self.scratch("YMOE", [T, DM])
        if "DBGR" in self.debug:
            self.scratch("DBGR", [128, NT * 6 + 64])
        self.out = self.nc.dram_tensor("out", [NLAT, DM], F32, kind="ExternalOutput")
        self.scratch("S5T", [256, T])
        self.scratch("X", [T, DM])
        self.scratch("MOD", [2, 6 * DM])
        self.scratch("UT", [256, T]); self.scratch("CQT", [384, T]); self.scratch("CKVT", [256, T])
        self.scratch("KRT", [32, T]); self.scratch("HZT", [768, T])
        if "HL" in self.debug:
            self.scratch("HL", [T, DM])

    def stage_init(self):
        kb = self.kb
        X = self.S["X"]
        kb.dma(X.ap()[0:NCTX, :], self.I["ctx"].ap(), writes=[("X", 0), ("X", 1)])
        for j in range(4):
            kb.dma(X.ap()[NCTX + j * 1024: NCTX + (j + 1) * 1024, :], self.I["x"].ap()[j * 1024:(j + 1) * 1024, :],
                   writes=[("X", 2 + 8 * j + i) for i in range(8)])

    def stage_mod(self, L):
        kb = self.kb
        with ExitStack() as es:
            cc = kb.sb(es, [128, 8, 2], F32, "cc")
            act = kb.sb(es, [128, 8, 2], F32, "act")
            bb = kb.sb(es, [2, 6 * DM], F32, "adab")
            mod = kb.sb(es, [2, 6 * DM], F32, "mod")
            wt = [kb.sb(es, [128, 8, 512], F32, "adaw%d" % i) for i in range(2)]
            kb.dma(cc[:], self.I["cc"].ap(), writes=[cc])
            kb.dma(bb[:], self.I["ada_b"].ap()[L:L + 1, :].partition_broadcast(2), writes=[bb])
            kb.act(act[:], cc[:], AF.Silu, reads=[cc], writes=[act])
            wv = self.I["ada_w"].ap()[L].rearrange("(kc p) n -> p kc n", p=128)
            for nb in range(12):
                w = wt[nb % 2]
                kb.dma(w[:], wv[:, :, nb * 512:(nb + 1) * 512], writes=[w])
                ps = kb.bank[nb % 2]
                for kc in range(8):
                    kb.mm(ps[0:2, :], act[:, kc, :], w[:, kc, :], start=(kc == 0), stop=(kc == 7),
                          reads=[act, w], writes=[ps])
                kb.tt("dve", mod[:, nb * 512:(nb + 1) * 512], ps[0:2, :], bb[:, nb * 512:(nb + 1) * 512], OP.add,
                      reads=[ps, bb], writes=[mod])
            kb.dma(self.S["MOD"].ap(), mod[:], reads=[mod], writes=["MOD"])
        kb.barrier()

    def load_mod_bc(self, es_tiles, L, which, idx_g, g_name):
        kb = self.kb
        G, S, tmp = es_tiles
        MOD = self.S["MOD"].ap()
        kb.dma(S[:], MOD[which:which + 1, idx_g * DM:(idx_g + 1) * DM].partition_broadcast(128), reads=["MOD"], writes=[S])
        kb.dma(tmp[:], MOD[which:which + 1, (idx_g + 1) * DM:(idx_g + 2) * DM].partition_broadcast(128), reads=["MOD"], writes=[tmp])
        kb.dma(G[:], self.I[g_name].ap()[L:L + 1, :].partition_broadcast(128), writes=[G])
        kb.stt("dve", G[:], tmp[:], 1.0, G[:], OP.add, OP.mult, reads=[tmp, G], writes=[G])

    def rms_modulate(self, xt, G, S, sq, ss, rstd, tmp, out, out2=None):
        kb = self.kb
        kb.act(sq[:], xt[:], AF.Square, accum_out=ss[:], reads=[xt], writes=[sq, ss])
        kb.ts("dve", rstd[:], ss[:], 1.0 / DM, OP.mult, EPS, OP.add, reads=[ss], writes=[rstd])
        kb.act(rstd[:], rstd[:], AF.Sqrt, reads=[rstd], writes=[rstd])
        kb.op("dve", lambda E: E.reciprocal(rstd[:], rstd[:]), reads=[rstd], writes=[rstd])
        kb.stt("dve", tmp[:], xt[:], rstd[:, 0:1], G[:], OP.mult, OP.mult, reads=[xt, rstd, G], writes=[tmp])
        kb.tt("pool", out[:], tmp[:], S[:], OP.add, reads=[tmp, S], writes=[out])
        if out2 is not None:
            kb.cp("act", out2[:], out[:], reads=[out], writes=[out2])

    def stage_proj(self, L):
        kb = self.kb
        X = self.S["X"].ap()
        with ExitStack() as es:
            ident_f = kb.sb(es, [128, 128], F32, "identf")
            ident = kb.sb(es, [128, 128], BF16, "identb")
            kb.dma(ident_f[:], self.I["ident"].ap(), writes=[ident_f])
            kb.cp("dve", ident[:], ident_f[:], reads=[ident_f], writes=[ident])
            wf = kb.sb(es, [128, 8, P_IN], F32, "winf")
            wb = kb.sb(es, [128, 8, P_IN + 32], BF16, "winb")
            kb.dma(wf[:], self.I["w_in"].ap()[L].rearrange("(kc p) n -> p kc n", p=128), writes=[wf])
            kb.cp("act", wb[:, :, 0:P_IN], wf[:], reads=[wf], writes=[wb])
            K0 = 896
            kb.ts("dve", wb[:, :, P_IN + 0:P_IN + 8], wf[:, :, K0 + 8:K0 + 16], -1.0, OP.mult, reads=[wf], writes=[wb])
            kb.cp("dve", wb[:, :, P_IN + 8:P_IN + 16], wf[:, :, K0 + 0:K0 + 8], reads=[wf], writes=[wb])
            kb.ts("dve", wb[:, :, P_IN + 16:P_IN + 24], wf[:, :, K0 + 24:K0 + 32], -1.0, OP.mult, reads=[wf], writes=[wb])
            kb.cp("dve", wb[:, :, P_IN + 24:P_IN + 32], wf[:, :, K0 + 16:K0 + 24], reads=[wf], writes=[wb])
            rc = kb.sb(es, [32, T], F32, "ropec")
            rs = kb.sb(es, [32, T], F32, "ropes")
            kb.dma(rc[:], self.I["rope_cos"].ap(), writes=[rc])
            kb.dma(rs[:], self.I["rope_sin"].ap(), writes=[rs])
            G = kb.sb(es, [128, DM], F32, "G"); S = kb.sb(es, [128, DM], F32, "S"); tmpb = kb.sb(es, [128, DM], F32, "tmpb")
            xt = [kb.sb(es, [128, DM], F32, "xt%d" % i) for i in range(2)]
            sq = kb.sb(es, [128, DM], BF16, "sq")
            ss = kb.sb(es, [128, 1], F32, "ss"); rstd = kb.sb(es, [128, 1], F32, "rstd")
            tmp = kb.sb(es, [128, DM], F32, "tmp")
            hb = [kb.sb(es, [128, DM], BF16, "hb%d" % i) for i in range(2)]
            hf = kb.sb(es, [128, DM], F32, "hf") if "HL" in self.debug else None
            hT = [kb.sb(es, [128, 8, 512], BF16, "hT%d" % i) for i in range(2)]
            stg = [kb.sb(es, [128, 512], F32, "stg%d" % i) for i in range(3)]
            kr1 = kb.sb(es, [32, 512], F32, "kr1")
            nstg = 0
            chunks = []
            for i in range(2):
                chunks.append(("UT", i * 128, i * 128, 128))
            for i in range(3):
                chunks.append(("CQT", i * 128, 256 + i * 128, 128))
            for i in range(2):
                chunks.append(("CKVT", i * 128, 640 + i * 128, 128))
            for i in range(6):
                chunks.append(("HZT", i * 128, 928 + i * 128, 128))
            blocks = [(0, 2)] + [(2 + 4 * i, 4) for i in range(8)]
            nmm = 0
            for bi, (t0, ntl) in enumerate(blocks):
                if bi == 0:
                    self.load_mod_bc((G, S, tmpb), L, 1, 0, "norm1_g")
                elif bi == 1:
                    self.load_mod_bc((G, S, tmpb), L, 0, 0, "norm1_g")
                hTb = hT[bi % 2]
                nb = ntl * 128
                c0 = t0 * 128
                for j in range(ntl):
                    t = t0 + j
                    x_ = xt[t % 2]
                    kb.dma(x_[:], X[t * 128:(t + 1) * 128, :], reads=[("X", t)], writes=[x_])
                    h_ = hb[t % 2]
                    if hf is not None:
                        self.rms_modulate(x_, G, S, sq, ss, rstd, tmp, hf, h_)
                        kb.dma(self.S["HL"].ap()[t * 128:(t + 1) * 128, :], hf[:], reads=[hf], writes=["HL"])
                    else:
                        self.rms_modulate(x_, G, S, sq, ss, rstd, tmp, h_)
                    pb = kb.bank[2 + (t % 2)]
                    pbv = pb[:].bitcast(BF16)
                    for kc in range(8):
                        kb.tr(pbv[:, kc * 128:(kc + 1) * 128], h_[:, kc * 128:(kc + 1) * 128], ident[:],
                              reads=[h_, ident], writes=[pb])
                    kb.cp("dve" if t % 2 else "act", hTb[:, :, j * 128:(j + 1) * 128],
                          pbv.rearrange("p (k n) -> p k n", k=8), reads=[pb], writes=[hTb])
                for (dn, r0, col0, M) in chunks:
                    ps = kb.bank[4 + (nmm % 2)]
                    nmm += 1
                    for kc in range(8):
                        kb.mm(ps[0:M, 0:nb], wb[:, kc, col0:col0 + M], hTb[:, kc, 0:nb], start=(kc == 0), stop=(kc == 7),
                              reads=[wb, hTb], writes=[ps])
                    st = stg[nstg % 3]
                    nstg += 1
                    kb.cp("act" if nstg % 2 else "dve", st[0:M, 0:nb], ps[0:M, 0:nb], reads=[ps], writes=[st])
                    kb.dma(self.S[dn].ap()[r0:r0 + M, c0:c0 + nb], st[0:M, 0:nb], reads=[st], writes=[(dn, bi)], q="pool")
                ps = kb.bank[6]
                ps2 = kb.bank[7]
                for kc in range(8):
                    kb.mm(ps[0:32, 0:nb], wb[:, kc, 896:928], hTb[:, kc, 0:nb], start=(kc == 0), stop=(kc == 7),
                          reads=[wb, hTb], writes=[ps])
                for kc in range(8):
                    kb.mm(ps2[0:32, 0:nb], wb[:, kc, P_IN:P_IN + 32], hTb[:, kc, 0:nb], start=(kc == 0), stop=(kc == 7),
                          reads=[wb, hTb], writes=[ps2])
                st = stg[nstg % 3]
                nstg += 1
                kb.tt("dve", kr1[:, 0:nb], ps[0:32, 0:nb], rc[:, c0:c0 + nb], OP.mult, reads=[ps, rc], writes=[kr1])
                kb.tt("dve", st[0:32, 0:nb], ps2[0:32, 0:nb], rs[:, c0:c0 + nb], OP.mult, reads=[ps2, rs], writes=[st])
                kb.tt("dve", st[0:32, 0:nb], st[0:32, 0:nb], kr1[:, 0:nb], OP.add, reads=[st, kr1], writes=[st])
                kb.dma(self.S["KRT"].ap()[:, c0:c0 + nb], st[0:32, 0:nb], reads=[st], writes=[("KRT", bi)], q="pool")
        kb.barrier()


    def sin_rr(self, e, out, in_, add, t1, t2):
        kb = self.kb
        kb.ts(e, t1, in_, add, OP.add, reads=[in_.tensor], writes=[t1.tensor])
        kb.ts(e, t2, t1, 1.0 / (2 * math.pi), OP.mult, MAGIC, OP.add, reads=[t1.tensor], writes=[t2.tensor])
        kb.ts(e, t2, t2, -MAGIC, OP.add, -2 * math.pi, OP.mult, reads=[t2.tensor], writes=[t2.tensor])
        kb.tt(e, t1, t1, t2, OP.add, reads=[t1.tensor, t2.tensor], writes=[t1.tensor])
        kb.ts(e, t1, t1, math.pi, OP.min, -math.pi, OP.max, reads=[t1.tensor], writes=[t1.tensor])
        kb.act(out, t1, AF.Sin, reads=[t1.tensor], writes=[out.tensor])

    def stage_s5(self, L):
        kb = self.kb
        I = self.I
        blocks = [(0, 256)] + [(256 + 512 * i, 512) for i in range(8)]
        with ExitStack() as es:
            ident_f = kb.sb(es, [128, 128], F32, "identf")
            kb.dma(ident_f[:], I["ident"].ap(), writes=[ident_f])
            sm = {}
            for nm in ("lre", "lim", "ldt"):
                sm[nm] = kb.sb(es, [128, 16], F32, nm)
                kb.dma(sm[nm][:], I["s5_" + nm].ap()[L], writes=[sm[nm]])
            for nm in ("dt", "a", "th", "r", "cs", "sn", "t1", "t2", "rc", "rsn", "den", "cre", "cim", "x1", "x2"):
                sm[nm] = kb.sb(es, [128, 16], F32, nm)
            big = {}
            for nm in ("bre", "bim", "cre", "cim"):
                big[nm] = kb.sb(es, [128, 16, 16], F32, "s5" + nm)
                kb.dma(big[nm][:], I["s5_" + nm].ap()[L], writes=[big[nm]])
            A = lambda nm: sm[nm][:]
            kb.act(A("dt"), A("ldt"), AF.Exp, reads=[sm["ldt"]], writes=[sm["dt"]])
            kb.tt("dve", A("a"), A("lre"), A("dt"), OP.mult, reads=[sm["lre"], sm["dt"]], writes=[sm["a"]])
            kb.tt("dve", A("th"), A("lim"), A("dt"), OP.mult, reads=[sm["lim"], sm["dt"]], writes=[sm["th"]])
            kb.act(A("r"), A("a"), AF.Exp, reads=[sm["a"]], writes=[sm["r"]])
            self.sin_rr("dve", A("sn"), A("th"), 0.0, A("t1"), A("t2"))
            self.sin_rr("dve", A("cs"), A("th"), math.pi / 2, A("t1"), A("t2"))
            kb.tt("dve", A("rc"), A("r"), A("cs"), OP.mult, reads=[sm["r"], sm["cs"]], writes=[sm["rc"]])
            kb.tt("dve", A("rsn"), A("r"), A("sn"), OP.mult, reads=[sm["r"], sm["sn"]], writes=[sm["rsn"]])
            kb.ts("dve", A("rc"), A("rc"), -1.0, OP.add, reads=[sm["rc"]], writes=[sm["rc"]])
            kb.tt("dve", A("den"), A("lre"), A("lre"), OP.mult, reads=[sm["lre"]], writes=[sm["den"]])
            kb.tt("dve", A("x1"), A("lim"), A("lim"), OP.mult, reads=[sm["lim"]], writes=[sm["x1"]])
            kb.tt("dve", A("den"), A("den"), A("x1"), OP.add, reads=[sm["den"], sm["x1"]], writes=[sm["den"]])
            kb.op("dve", lambda E: E.reciprocal(A("den"), A("den")), reads=[sm["den"]], writes=[sm["den"]])
            kb.tt("dve", A("x1"), A("rc"), A("lre"), OP.mult, reads=[sm["rc"], sm["lre"]], writes=[sm["x1"]])
            kb.tt("dve", A("x2"), A("rsn"), A("lim"), OP.mult, reads=[sm["rsn"], sm["lim"]], writes=[sm["x2"]])
            kb.tt("dve", A("x1"), A("x1"), A("x2"), OP.add, reads=[sm["x1"], sm["x2"]], writes=[sm["x1"]])
            kb.tt("dve", A("cre"), A("x1"), A("den"), OP.mult, reads=[sm["x1"], sm["den"]], writes=[sm["cre"]])
            kb.tt("dve", A("x1"), A("rsn"), A("lre"), OP.mult, reads=[sm["rsn"], sm["lre"]], writes=[sm["x1"]])
            kb.tt("dve", A("x2"), A("rc"), A("lim"), OP.mult, reads=[sm["rc"], sm["lim"]], writes=[sm["x2"]])
            kb.tt("dve", A("x1"), A("x1"), A("x2"), OP.subtract, reads=[sm["x1"], sm["x2"]], writes=[sm["x1"]])
            kb.tt("dve", A("cim"), A("x1"), A("den"), OP.mult, reads=[sm["x1"], sm["den"]], writes=[sm["cim"]])
            ub = kb.sb(es, [128, 2, T], BF16, "ub")
            yacc = kb.sb(es, [128, 2, T], F32, "yacc")
            Er = kb.sb(es, [128, T], F32, "Er"); Ei = kb.sb(es, [128, T], F32, "Ei")
            for c, stg_ in enumerate((Er, Ei)):
                kb.dma(stg_[:], self.S["UT"].ap()[c * 128:(c + 1) * 128, :], reads=[("UT", i) for i in range(9)], writes=[stg_])
                kb.cp("act", ub[:, c, :], stg_[:], reads=[stg_], writes=[ub])
            esm = ExitStack()
            Ebr = [kb.sb(esm, [128, T], BF16, "Ebr%d" % i) for i in range(2)]
            Ebi = [kb.sb(esm, [128, T], BF16, "Ebi%d" % i) for i in range(2)]
            bur = kb.sb(esm, [128, T], BF16, "bur"); bui = kb.sb(esm, [128, T], BF16, "bui")
            vr = kb.sb(esm, [128, T], BF16, "vr"); vi = kb.sb(esm, [128, T], BF16, "vi")
            m1 = kb.sb(esm, [128, T], BF16, "m1"); m2 = kb.sb(esm, [128, T], BF16, "m2")
            ZB = [kb.sb(esm, [128, 128], F32, "ZB%d" % i) for i in range(2)]
            LBs = [[kb.sb(esm, [128, 128], BF16, "LB%d_%d" % (p_, i)) for i in range(2)] for p_ in range(2)]
            LCs = [[kb.sb(esm, [128, 128], BF16, "LC%d_%d" % (p_, i)) for i in range(3)] for p_ in range(2)]
            ytmp = [kb.sb(esm, [128, 512], F32, "ytmp%d" % i) for i in range(2)]
            bt = kb.sb(esm, [128, 16], F32, "bt")
            etmp = kb.sb(esm, [128, 2048], F32, "etmp"); etmp2 = kb.sb(esm, [128, 2048], F32, "etmp2")
            NPW = 13
            WR = kb.sb(esm, [128, NPW, 16], F32, "WR"); WI = kb.sb(esm, [128, NPW, 16], F32, "WI"); wt = kb.sb(esm, [128, 16], F32, "wt")
            kb.cp("dve", WR[:, 0, :], sm["cs"][:], reads=[sm["cs"]], writes=[WR])
            kb.cp("dve", WI[:, 0, :], sm["sn"][:], reads=[sm["sn"]], writes=[WI])
            for k in range(1, NPW):
                kb.tt("dve", wt[:], WI[:, k - 1, :], WI[:, k - 1, :], OP.mult, reads=[WI], writes=[wt])
                kb.tt("dve", WI[:, k, :], WR[:, k - 1, :], WI[:, k - 1, :], OP.mult, reads=[WR, WI], writes=[WI])
                kb.ts("dve", WI[:, k, :], WI[:, k, :], 2.0, OP.mult, reads=[WI], writes=[WI])
                kb.tt("dve", WR[:, k, :], WR[:, k - 1, :], WR[:, k - 1, :], OP.mult, reads=[WR], writes=[WR])
                kb.tt("dve", WR[:, k, :], WR[:, k, :], wt[:], OP.subtract, reads=[WR, wt], writes=[WR])
            first_in_chunk = {0: True, 1: True}

            def prep(lt):
                d, gp = lt // 8, lt % 8
                c0 = 32 * (gp % 4)
                LB = LBs[lt % 2]; LC = LCs[lt % 2]
                for ri, (nm_a, nm_b, op2) in enumerate((("bre", "bim", OP.subtract), ("bim", "bre", OP.add))):
                    Z = ZB[ri]
                    kb.memset("pool", Z[:], 0.0, writes=[Z])
                    kb.ts("pool", bt[:], big[nm_b][:, lt, :], sm["cim"][:, lt:lt + 1], OP.mult, reads=[big[nm_b], sm["cim"]], writes=[bt])
                    for gl in range(2):
                        ps_ = slice(gl * 64, gl * 64 + 64)
                        kb.ts("pool", Z[ps_, c0 + gl * 16:c0 + gl * 16 + 16], big[nm_a][ps_, lt, :], sm["cre"][ps_, lt:lt + 1], OP.mult,
                              reads=[big[nm_a], sm["cre"]], writes=[Z])
                        kb.tt("pool", Z[ps_, c0 + gl * 16:c0 + gl * 16 + 16], Z[ps_, c0 + gl * 16:c0 + gl * 16 + 16], bt[ps_, :], op2,
                              reads=[Z, bt], writes=[Z])
                    pb = kb.bank[6 + ri]
                    kb.tr(pb[:, 0:128], Z[:], ident_f[:], reads=[Z, ident_f], writes=[pb])
                    kb.cp("pool" if False else "act", LB[ri][:], pb[:, 0:128], reads=[pb], writes=[LB[ri]])
                for ri, (nm, sgn) in enumerate((("cre", 1.0), ("cim", -1.0), ("cre", -1.0))):
                    kb.memset("pool", LC[ri][:], 0.0, writes=[LC[ri]])
                    for gl in range(2):
                        ps_ = slice(gl * 64, gl * 64 + 64)
                        kb.ts("pool", LC[ri][ps_, c0 + gl * 16:c0 + gl * 16 + 16], big[nm][ps_, lt, :], sgn, OP.mult,
                              reads=[big[nm]], writes=[LC[ri]])

            def tgen(lt):
                d = lt // 8
                lsl = slice(lt, lt + 1)
                kb.memset("pool", Er[:, 0:1], 1.0, writes=[Er])
                kb.memset("pool", Ei[:, 0:1], 0.0, writes=[Ei])
                kb.cp("pool", Er[:, 1:2], sm["cs"][:, lsl], reads=[sm["cs"]], writes=[Er])
                kb.cp("pool", Ei[:, 1:2], sm["sn"][:, lsl], reads=[sm["sn"]], writes=[Ei])
                n = 2
                k = 1
                while n < T:
                    m = min(n, T - n)
                    wr_ = WR[:, k, lsl]; wi_ = WI[:, k, lsl]
                    for o in range(0, m, 2048):
                        mm_ = min(2048, m - o)
                        kb.act(etmp[:, 0:mm_], Ei[:, o:o + mm_], AF.Copy, scale=wi_, reads=[Ei, WI], writes=[etmp])
                        kb.act(etmp2[:, 0:mm_], Er[:, o:o + mm_], AF.Copy, scale=wr_, reads=[Er, WR], writes=[etmp2])
                        kb.tt("pool", Er[:, n + o:n + o + mm_], etmp2[:, 0:mm_], etmp[:, 0:mm_], OP.subtract, reads=[etmp, etmp2, Er], writes=[Er])
                        kb.act(etmp[:, 0:mm_], Ei[:, o:o + mm_], AF.Copy, scale=wr_, reads=[Ei, WR], writes=[etmp])
                        kb.act(etmp2[:, 0:mm_], Er[:, o:o + mm_], AF.Copy, scale=wi_, reads=[Er, WI], writes=[etmp2])
                        kb.tt("pool", Ei[:, n + o:n + o + mm_], etmp2[:, 0:mm_], etmp[:, 0:mm_], OP.add, reads=[etmp, etmp2, Ei], writes=[Ei])
                    n *= 2
                    k += 1
                br_, bi_ = Ebr[lt % 2], Ebi[lt % 2]
                if d == 0:
                    kb.cp("act", br_[:], Er[:], reads=[Er], writes=[br_])
                    kb.cp("act", bi_[:], Ei[:], reads=[Ei], writes=[bi_])
                else:
                    for (Eb, E_) in ((br_, Er), (bi_, Ei)):
                        kb.cp("act", Eb[:, 0:NCTX], E_[:, 0:NCTX][:, ::-1], reads=[E_], writes=[Eb])
                        kb.cp("act", Eb[:, NCTX:T], E_[:, NCTX:T][:, ::-1], reads=[E_], writes=[Eb])

            def drive(lt):
                gp = lt % 8
                ch = gp // 4
                LB = LBs[lt % 2]
                for bi, (s0, nn) in enumerate(blocks):
                    pr = kb.bank[(bi % 2) * 2]
                    pi_ = kb.bank[(bi % 2) * 2 + 1]
                    kb.mm(pr[:, 0:nn], LB[0][:], ub[:, ch, s0:s0 + nn], reads=[LB[0], ub], writes=[pr])
                    kb.mm(pi_[:, 0:nn], LB[1][:], ub[:, ch, s0:s0 + nn], reads=[LB[1], ub], writes=[pi_])
                    kb.cp("act", bur[:, s0:s0 + nn], pr[:, 0:nn], reads=[pr], writes=[bur])
                    kb.cp("act", bui[:, s0:s0 + nn], pi_[:, 0:nn], reads=[pi_], writes=[bui])

            def rot_scan(lt):
                d = lt // 8
                br_, bi_ = Ebr[lt % 2], Ebi[lt % 2]
                kb.tt("dve", m1[:], br_[:], bur[:], OP.mult, reads=[br_, bur], writes=[m1])
                kb.tt("dve", m2[:], bi_[:], bui[:], OP.mult, reads=[bi_, bui], writes=[m2])
                kb.tt("dve", vr[:], m1[:], m2[:], OP.add, reads=[m1, m2], writes=[vr])
                kb.tt("dve", m1[:], br_[:], bui[:], OP.mult, reads=[br_, bui], writes=[m1])
                kb.tt("dve", m2[:], bi_[:], bur[:], OP.mult, reads=[bi_, bur], writes=[m2])
                kb.tt("dve", vi[:], m1[:], m2[:], OP.subtract, reads=[m1, m2], writes=[vi])
                rdec = sm["r"][:, lt:lt + 1]
                for (v_, g_) in ((vr, bur), (vi, bui)):
                    if d == 0:
                        kb.op("dve", lambda E: E.tensor_tensor_scan(g_[:], rdec.to_broadcast([128, T]), v_[:], 0.0, OP.mult, OP.add),
                              reads=[sm["r"], v_], writes=[g_])
                    else:
                        kb.op("dve", lambda E: E.tensor_tensor_scan(g_[:, 0:NCTX][:, ::-1], rdec.to_broadcast([128, NCTX]), v_[:, 0:NCTX][:, ::-1],
                                                                    0.0, OP.mult, OP.add), reads=[sm["r"], v_], writes=[g_])
                        kb.op("dve", lambda E: E.tensor_tensor_scan(g_[:, NCTX:T][:, ::-1], rdec.to_broadcast([128, NLAT]), v_[:, NCTX:T][:, ::-1],
                                                                    g_[:, 0:1], OP.mult, OP.add), reads=[sm["r"], v_, g_], writes=[g_])
                kb.tt("dve", m1[:], br_[:], bur[:], OP.mult, reads=[br_, bur], writes=[m1])
                kb.tt("dve", m2[:], bi_[:], bui[:], OP.mult, reads=[bi_, bui], writes=[m2])
                kb.tt("dve", vr[:], bi_[:], bur[:], OP.mult, reads=[bi_, bur], writes=[vr])
                kb.tt("dve", vi[:], br_[:], bui[:], OP.mult, reads=[br_, bui], writes=[vi])

            def readout(lt):
                gp = lt % 8
                ch = gp // 4
                LC = LCs[lt % 2]
                for bi, (s0, nn) in enumerate(blocks):
                    py = kb.bank[4 + (bi % 2)]
                    kb.mm(py[:, 0:nn], LC[0][:], m1[:, s0:s0 + nn], start=True, stop=False, reads=[LC[0], m1], writes=[py])
                    kb.mm(py[:, 0:nn], LC[2][:], m2[:, s0:s0 + nn], start=False, stop=False, reads=[LC[2], m2], writes=[py])
                    kb.mm(py[:, 0:nn], LC[1][:], vr[:, s0:s0 + nn], start=False, stop=False, reads=[LC[1], vr], writes=[py])
                    kb.mm(py[:, 0:nn], LC[1][:], vi[:, s0:s0 + nn], start=False, stop=True, reads=[LC[1], vi], writes=[py])
                    if first_in_chunk[ch]:
                        kb.cp("act", yacc[:, ch, s0:s0 + nn], py[:, 0:nn], reads=[py], writes=[yacc])
                    else:
                        yt_ = ytmp[bi % 2]
                        kb.cp("act", yt_[:, 0:nn], py[:, 0:nn], reads=[py], writes=[yt_])
                        kb.tt("pool", yacc[:, ch, s0:s0 + nn], yacc[:, ch, s0:s0 + nn], yt_[:, 0:nn], OP.add, reads=[yt_, yacc], writes=[yacc])
                first_in_chunk[ch] = False

            prep(0)
            tgen(0)
            for lt in range(16):
                drive(lt)
                if lt + 1 < 16:
                    prep(lt + 1)
                    tgen(lt + 1)
                rot_scan(lt)
                readout(lt)
            kb.barrier()
            esm.close()
            dcol = kb.sb(es, [128, 2], F32, "dcol")
            kb.dma(dcol[:], I["s5_dcol"].ap()[L], writes=[dcol])
            uf = (Er, Ei)
            mt = [kb.sb(es, [128, 512], F32, "mt%d" % i) for i in range(4)]
            for c in range(2):
                kb.dma(uf[c][:], self.S["UT"].ap()[c * 128:(c + 1) * 128, :], reads=[("UT", i) for i in range(9)], writes=[uf[c]])
            gwf = kb.sb(es, [128, 2, 256], F32, "gwf"); gwb = kb.sb(es, [128, 2, 256], BF16, "gwb")
            kb.dma(gwf[:], I["s5_glu_w"].ap()[L].rearrange("(kc p) n -> p kc n", p=128), writes=[gwf])
            kb.cp("act", gwb[:], gwf[:], reads=[gwf], writes=[gwb])
            yb = kb.sb(es, [128, 2, 512], BF16, "yb")
            yf = kb.sb(es, [128, 2, 512], F32, "yf")
            so = [kb.sb(es, [128, 512], F32, "so%d" % i) for i in range(2)]
            for bi, (s0, nn) in enumerate(blocks):
                m1, m2, m3, m4 = mt
                for c in range(2):
                    kb.stt("dve", yf[:, c, 0:nn], uf[c][:, s0:s0 + nn], dcol[:, c:c + 1], yacc[:, c, s0:s0 + nn], OP.mult, OP.add,
                           reads=[uf[c], dcol, yacc], writes=[yf])
                    kb.tt("pool", m1[:, 0:nn], yf[:, c, 0:nn], yf[:, c, 0:nn], OP.mult, reads=[yf], writes=[m1])
                    kb.ts("pool", m1[:, 0:nn], m1[:, 0:nn], 0.044715, OP.mult, 1.0, OP.add, reads=[m1], writes=[m1])
                    kb.tt("pool", m1[:, 0:nn], m1[:, 0:nn], yf[:, c, 0:nn], OP.mult, reads=[m1, yf], writes=[m1])
                    kb.act(m2[:, 0:nn], m1[:, 0:nn], AF.Sigmoid, scale=1.5957691216057308, reads=[m1], writes=[m2])
                    kb.tt("dve", yf[:, c, 0:nn], yf[:, c, 0:nn], m2[:, 0:nn], OP.mult, reads=[yf, m2], writes=[yf])
                    kb.cp("act", yb[:, c, 0:nn], yf[:, c, 0:nn], reads=[yf], writes=[yb])
                for mo in range(2):
                    pz = kb.bank[mo]
                    for kc in range(2):
                        kb.mm(pz[:, 0:nn], gwb[:, kc, mo * 128:(mo + 1) * 128], yb[:, kc, 0:nn], start=(kc == 0), stop=(kc == 1),
                              reads=[gwb, yb], writes=[pz])
                    kb.act(m3[:, 0:nn], pz[:, 0:nn], AF.Sigmoid, reads=[pz], writes=[m3])
                    so_ = so[mo]
                    kb.tt("dve", so_[:, 0:nn], yf[:, mo, 0:nn], m3[:, 0:nn], OP.mult, reads=[yf, m3], writes=[so_])
                    kb.dma(self.S["S5T"].ap()[mo * 128:(mo + 1) * 128, s0:s0 + nn], so_[:, 0:nn], reads=[so_], writes=[("S5T", bi)], q="pool")
        kb.barrier()


    def stage_mla(self, L, ctx_out):
        kb = self.kb
        I = self.I
        blocks = [(0, 256)] + [(256 + 512 * i, 512) for i in range(8)]
        with ExitStack() as es:
            ident_f = kb.sb(es, [128, 128], F32, "identf")
            ident = kb.sb(es, [128, 128], BF16, "identb")
            ones = kb.sb(es, [128, 128], BF16, "ones")
            kb.dma(ident_f[:], I["ident"].ap(), writes=[ident_f])
            kb.cp("dve", ident[:], ident_f[:], reads=[ident_f], writes=[ident])
            kb.memset("dve", ones[:], 1.0, writes=[ones])
            qg = kb.sb(es, [128, 3], F32, "qg"); kvg = kb.sb(es, [128, 2], F32, "kvg")
            kb.dma(qg[:], I["mla_qg"].ap()[L], writes=[qg]); kb.dma(kvg[:], I["mla_kvg"].ap()[L], writes=[kvg])
            wuq = kb.sb(es, [128, 3, 768], BF16, "wuq")
            wrot = kb.sb(es, [128, 3, 8, 96], BF16, "wrot")
            wukv = kb.sb(es, [128, 2, 1024], BF16, "wukv")
            KT = kb.sb(es, [97, 8, T], BF16, "KT")
            VA = kb.sb(es, [128, NT, 8, 65], BF16, "VA")
            gbc = kb.sb(es, [128, 512], F32, "gbc")
            kb.dma(gbc[:], I["mix_norm_g"].ap()[L:L + 1, 256:768].partition_broadcast(128), writes=[gbc])
            es2 = ExitStack()
            wst = kb.sb(es2, [128, 3, 1024], F32, "wst")
            kb.dma(wst[:, :, 0:768], I["mla_w_uq"].ap()[L].rearrange("(kc p) n -> p kc n", p=128), writes=[wst])
            kb.cp("act", wuq[:], wst[:, :, 0:768], reads=[wst], writes=[wuq])
            kb.memset("pool", wrot[:], 0.0, writes=[wrot])
            wv4 = wst[:, :, 0:768].rearrange("p k (h x) -> p k h x", x=96)
            kb.ts("dve", wrot[:, :, :, 64:72], wv4[:, :, :, 72:80], -1.0, OP.mult, reads=[wst], writes=[wrot])
            kb.cp("dve", wrot[:, :, :, 72:80], wv4[:, :, :, 64:72], reads=[wst], writes=[wrot])
            kb.ts("dve", wrot[:, :, :, 80:88], wv4[:, :, :, 88:96], -1.0, OP.mult, reads=[wst], writes=[wrot])
            kb.cp("dve", wrot[:, :, :, 88:96], wv4[:, :, :, 80:88], reads=[wst], writes=[wrot])
            kb.dma(wst[:, 0:2, :], I["mla_w_ukv"].ap()[L].rearrange("(kc p) n -> p kc n", p=128), reads=[wst], writes=[wst])
            kb.cp("act", wukv[:], wst[:, 0:2, :], reads=[wst], writes=[wukv])
            kb.memset("pool", KT[96:97, :, :], 1.0, writes=[KT])
            kb.memset("pool", VA[:, :, :, 64:65], 1.0, writes=[VA])
            krf = kb.sb(es2, [32, T], F32, "krf"); krb = kb.sb(es2, [32, T], BF16, "krb")
            kb.dma(krf[:], self.S["KRT"].ap(), reads=[("KRT", i) for i in range(9)], writes=[krf])
            kb.cp("act", krb[:], krf[:], reads=[krf], writes=[krb])
            for h in range(8):
                kb.dma(KT[64:96, h, :], krb[:], reads=[krb], writes=[KT])
            kb.barrier()
            es2.close()
            if "mla_stop1" in self.debug:
                return
            rcb = [kb.sb(es, [96, 512], F32, "rcb%d" % i) for i in range(2)]
            rsb = [kb.sb(es, [96, 512], F32, "rsb%d" % i) for i in range(2)]
            xin = [kb.sb(es, [128, 3, 512], F32, "xin%d" % i) for i in range(2)]
            sqb = kb.sb(es, [128, 3, 512], BF16, "sqb")
            sqk = [kb.sb(es, [96, 512], BF16, "sqk%d" % i) for i in range(2)]
            rst = kb.sb(es, [128, 512], F32, "rst")
            xn = kb.sb(es, [128, 3, 512], BF16, "xn")
            xnq = [kb.sb(es, [128, 3, 512], BF16, "xnq%d" % i) for i in range(2)]
            kmax2 = kb.sb(es, [128, 8], F32, "kmax2"); kmb = kb.sb(es, [128, 8], F32, "kmb"); negk = kb.sb(es, [128, 8], F32, "negk")
            kb.memset("dve", kmax2[:], 0.0, writes=[kmax2])

            def rmsnorm_fm(src, nch, nfeat, nn, gcol, dst):
                kb.tt("pool", sqb[:, 0:nch, 0:nn], src[:, 0:nch, 0:nn], src[:, 0:nch, 0:nn], OP.mult, reads=[src], writes=[sqb])
                pn = kb.bank[6]
                for c in range(nch):
                    kb.mm(pn[:, 0:nn], ones[:], sqb[:, c, 0:nn], start=(c == 0), stop=(c == nch - 1), reads=[ones, sqb], writes=[pn])
                kb.ts("dve", rst[:, 0:nn], pn[:, 0:nn], 1.0 / nfeat, OP.mult, EPS, OP.add, reads=[pn], writes=[rst])
                kb.act(rst[:, 0:nn], rst[:, 0:nn], AF.Sqrt, reads=[rst], writes=[rst])
                kb.op("dve", lambda E: E.reciprocal(rst[:, 0:nn], rst[:, 0:nn]), reads=[rst], writes=[rst])
                for c in range(nch):
                    kb.stt("dve", dst[:, c, 0:nn], src[:, c, 0:nn], gcol[:, c:c + 1], rst[:, 0:nn], OP.mult, OP.mult,
                           reads=[src, gcol, rst], writes=[dst])

            for bi, (s0, nn) in enumerate(blocks):
                x_ = xin[bi % 2]
                kb.dma(x_[:, 0:2, 0:nn], self.S["CKVT"].ap().rearrange("(c p) t -> p c t", p=128)[:, :, s0:s0 + nn],
                       reads=[("CKVT", bi)], writes=[x_])
                rmsnorm_fm(x_, 2, 256, nn, kvg, xn)
                for h in range(8):
                    pk = kb.bank[h % 2]
                    for kc in range(2):
                        kb.mm(pk[0:64, 0:nn], wukv[:, kc, h * 128:h * 128 + 64], xn[:, kc, 0:nn], start=(kc == 0), stop=(kc == 1),
                              reads=[wukv, xn], writes=[pk])
                    kb.cp("dve", KT[0:64, h, s0:s0 + nn], pk[0:64, 0:nn], reads=[pk], writes=[KT])
                    kb.tt("pool", sqk[h % 2][0:96, 0:nn], KT[0:96, h, s0:s0 + nn], KT[0:96, h, s0:s0 + nn], OP.mult, reads=[KT], writes=[sqk[h % 2]])
                    pn = kb.bank[6]
                    kb.mm(pn[:, 0:nn], ones[0:96, :], sqk[h % 2][0:96, 0:nn], reads=[ones, sqk[h % 2]], writes=[pn])
                    kb.op("dve", lambda E: E.reduce_max(kmb[:, h:h + 1], pn[:, 0:nn], AX.X), reads=[pn], writes=[kmb])
                    kb.tt("dve", kmax2[:, h:h + 1], kmax2[:, h:h + 1], kmb[:, h:h + 1], OP.max, reads=[kmb, kmax2], writes=[kmax2])
                for j in range(nn // 128):
                    ti = s0 // 128 + j
                    pv = kb.bank[2 + (j % 2)]
                    for kc in range(2):
                        kb.mm(pv[:, :].rearrange("p (h x) -> p h x", x=64), xn[:, kc, j * 128:(j + 1) * 128],
                              wukv[:, kc, :].rearrange("p (h x) -> p h x", x=128)[:, :, 64:128], start=(kc == 0), stop=(kc == 1),
                              reads=[wukv, xn], writes=[pv])
                    kb.cp("act" if j % 2 else "dve", VA[:, ti, :, 0:64], pv[:, :].rearrange("p (h x) -> p h x", x=64), reads=[pv], writes=[VA])
            if "mla_stop2" in self.debug:
                kb.barrier()
                return
            kb.act(negk[:], kmax2[:], AF.Sqrt, reads=[kmax2], writes=[negk])
            kb.ts("dve", negk[:], negk[:], -1.02, OP.mult, reads=[negk], writes=[negk])
            QT = [kb.sb(es, [97, 8, 512], BF16, "QT%d" % i) for i in range(2)]
            PT = [kb.sb(es, [128, 512], BF16, "PT%d" % i) for i in range(6)]
            tq = kb.sb(es, [96, 512], F32, "tq"); tq2 = kb.sb(es, [96, 512], F32, "tq2")
            qn1 = kb.sb(es, [97, 512], F32, "qn1")
            yt = kb.sb(es, [128, 4, 512], F32, "yt")
            ytb = kb.sb(es, [128, 4, 512], BF16, "ytb")
            rec = kb.sb(es, [128, 4], F32, "rec")
            ss4 = kb.sb(es, [128, 4], F32, "ss4")
            mts = kb.sb(es, [128, 4, 512], BF16, "mts")
            npt = 0
            qblocks = [(bi, s0, nn) for bi, (s0, nn) in enumerate(blocks) if not (bi == 0 and not ctx_out)]

            def qphase(bi, s0, nn):
                x_ = xin[bi % 2]
                xq = xnq[bi % 2]
                kb.dma(x_[:, 0:3, 0:nn], self.S["CQT"].ap().rearrange("(c p) t -> p c t", p=128)[:, :, s0:s0 + nn],
                       reads=[("CQT", bi)], writes=[x_])
                rmsnorm_fm(x_, 3, 384, nn, qg, xq)
                Q = QT[bi % 2]
                rc = rcb[bi % 2]; rs = rsb[bi % 2]
                kb.dma(rc[64:96, 0:nn], I["rope_cos"].ap()[:, s0:s0 + nn], writes=[rc])
                kb.dma(rs[64:96, 0:nn], I["rope_sin"].ap()[:, s0:s0 + nn], writes=[rs])
                for h in range(8):
                    pa = kb.bank[6]
                    pb = kb.bank[7]
                    for kc in range(3):
                        kb.mm(pa[0:96, 0:nn], wuq[:, kc, h * 96:(h + 1) * 96], xq[:, kc, 0:nn], start=(kc == 0), stop=(kc == 2),
                              reads=[wuq, xq], writes=[pa])
                    for kc in range(3):
                        kb.mm(pb[0:96, 0:nn], wrot[:, kc, h, :], xq[:, kc, 0:nn], start=(kc == 0), stop=(kc == 2),
                              reads=[wrot, xq], writes=[pb])
                    kb.cp("dve", Q[0:64, h, 0:nn], pa[0:64, 0:nn], reads=[pa], writes=[Q])
                    kb.tt("dve", tq[64:96, 0:nn], pb[64:96, 0:nn], rs[64:96, 0:nn], OP.mult, reads=[pb, rs], writes=[tq])
                    kb.tt("dve", tq2[64:96, 0:nn], pa[64:96, 0:nn], rc[64:96, 0:nn], OP.mult, reads=[pa, rc], writes=[tq2])
                    kb.tt("dve", Q[64:96, h, 0:nn], tq[64:96, 0:nn], tq2[64:96, 0:nn], OP.add, reads=[tq, tq2], writes=[Q])
                    sk = sqk[h % 2]
                    kb.tt("pool", sk[0:96, 0:nn], Q[0:96, h, 0:nn], Q[0:96, h, 0:nn], OP.mult, reads=[Q], writes=[sk])
                    pn = kb.bank[6]
                    kb.mm(pn[:, 0:nn], ones[0:96, :], sk[0:96, 0:nn], reads=[ones, sk], writes=[pn])
                    kb.act(qn1[96:97, 0:nn], pn[96:97, 0:nn], AF.Sqrt, reads=[pn], writes=[qn1])
                    kb.ts("dve", Q[96:97, h, 0:nn], qn1[96:97, 0:nn], negk[96:97, h:h + 1], OP.mult, reads=[qn1, negk], writes=[Q])

            qphase(*qblocks[0])
            for qi, (bi, s0, nn) in enumerate(qblocks):
                nj = nn // 128
                kchunks = list(range(2)) if bi == 0 else list(range(NT))
                Q = QT[bi % 2]
                if qi + 1 < len(qblocks):
                    qphase(*qblocks[qi + 1])
                if "mla_stop3" in self.debug:
                    continue
                SB = [0, 1, 3, 4, 5]
                DEPTH_P = 4
                for h in range(8):
                    pend = []
                    po = kb.bank[2]

                    def emit_pv(ci_, kc_, P__):
                        for j in range(nj):
                            kb.mm(po[:, j * 128:j * 128 + 65], P__[:, j * 128:(j + 1) * 128], VA[:, kc_, h, :], start=(ci_ == 0 and j == 0),
                                  stop=(ci_ == len(kchunks) - 1 and j == nj - 1), reads=[P__, VA], writes=[po], skip_group_check=True)
                    for ci, kc in enumerate(kchunks):
                        pst = kb.bank[SB[ci % 5]]
                        kb.mm(pst[:, 0:nn], KT[0:97, h, kc * 128:(kc + 1) * 128], Q[0:97, h, 0:nn], reads=[KT, Q], writes=[pst])
                        P_ = PT[npt % 6]
                        npt += 1
                        kb.act(P_[:, 0:nn], pst[:, 0:nn], AF.Exp, scale=MLA_SCALE, reads=[pst], writes=[P_])
                        pend.append((ci, kc, P_))
                        if len(pend) >= DEPTH_P:
                            emit_pv(*pend.pop(0))
                    while pend:
                        emit_pv(*pend.pop(0))
                    for j in range(nj):
                        kb.op("dve", lambda E: E.reciprocal(rec[:, j:j + 1], po[:, j * 128 + 64:j * 128 + 65]), reads=[po], writes=[rec])
                        kb.ts("dve", yt[:, j, h * 64:(h + 1) * 64], po[:, j * 128:j * 128 + 64], rec[:, j:j + 1], OP.mult, reads=[po, rec], writes=[yt])
                for j in range(nj):
                    if "MLAO" in self.debug:
                        kb.dma(self.S["MLAO"].ap()[s0 + j * 128:s0 + (j + 1) * 128, :], yt[:, j, :], reads=[yt], writes=["MLAO"], q="pool")
                    kb.act(mts[:, j, :], yt[:, j, :], AF.Square, accum_out=ss4[:, j:j + 1], reads=[yt], writes=[mts, ss4])
                kb.ts("dve", ss4[:, 0:nj], ss4[:, 0:nj], 1.0 / 512, OP.mult, EPS, OP.add, reads=[ss4], writes=[ss4])
                kb.act(ss4[:, 0:nj], ss4[:, 0:nj], AF.Sqrt, reads=[ss4], writes=[ss4])
                kb.op("dve", lambda E: E.reciprocal(ss4[:, 0:nj], ss4[:, 0:nj]), reads=[ss4], writes=[ss4])
                for j in range(nj):
                    kb.stt("dve", ytb[:, j, :], yt[:, j, :], ss4[:, j:j + 1], gbc[:], OP.mult, OP.mult, reads=[yt, ss4, gbc], writes=[ytb])
                    pt_ = kb.bank[6 + (j % 2)]
                    ptv = pt_[:].bitcast(BF16)
                    for fc in range(4):
                        kb.tr(ptv[:, fc * 128:(fc + 1) * 128], ytb[:, j, fc * 128:(fc + 1) * 128], ident[:], reads=[ytb, ident], writes=[pt_])
                    kb.cp("act", mts[:, :, j * 128:(j + 1) * 128], ptv[:, 0:512].rearrange("p (f q) -> p f q", f=4), reads=[pt_], writes=[mts])
                kb.dma(self.S["MT"].ap()[256:768, s0:s0 + nn].rearrange("(f p) t -> p f t", p=128), mts[:, :, 0:nn], reads=[mts],
                       writes=[("MT_mla", bi)], q="pool")
        kb.barrier()


    def stage_hyena(self, L, part):
        kb = self.kb
        I = self.I
        nm = part
        n = NLAT if part == "l" else NCTX
        r0 = NCTX if part == "l" else 0
        ntc = n // 128
        nj = (n + 1 + 127) // 128
        lagblocks = [(i * 512, min(512, n - i * 512)) for i in range((n + 511) // 512)]
        KSP = self.S["KSPEC_" + nm]
        with ExitStack() as es:
            ident_f = kb.sb(es, [128, 128], F32, "identf")
            kb.dma(ident_f[:], I["ident"].ap(), writes=[ident_f])
            w1 = kb.sb(es, [33, 64], F32, "w1"); w2 = kb.sb(es, [64, 64], F32, "w2"); w3 = kb.sb(es, [64, 1024], F32, "w3")
            cols = kb.sb(es, [64, 4], F32, "cols"); bf = kb.sb(es, [64, 2], F32, "bf"); b3 = kb.sb(es, [128, 8], F32, "b3")
            kb.dma(w1[:], I["hy_f_w1"].ap()[L], writes=[w1]); kb.dma(w2[:], I["hy_f_w2"].ap()[L], writes=[w2]); kb.dma(w3[:], I["hy_f_w3"].ap()[L], writes=[w3])
            kb.dma(cols[:], I["hy_cols"].ap()[L], writes=[cols]); kb.dma(b3[:], I["hy_b3"].ap()[L], writes=[b3])
            kb.tt("dve", bf[:, 0:1], cols[:, 0:1], cols[:, 2:3], OP.mult, reads=[cols], writes=[bf])
            kb.tt("dve", bf[:, 1:2], cols[:, 1:2], cols[:, 3:4], OP.mult, reads=[cols], writes=[bf])
            FS = kb.sb(es, [128, ntc, 512], BF16, "FS"); FD = kb.sb(es, [128, ntc, 512], BF16, "FD")
            zp = kb.sb(es, [33, 512], F32, "zp")
            h1 = kb.sb(es, [64, 512], F32, "h1"); h2 = kb.sb(es, [64, 512], F32, "h2")
            t1 = kb.sb(es, [64, 512], F32, "t1"); t2 = kb.sb(es, [64, 512], F32, "t2"); t0 = kb.sb(es, [64, 512], F32, "t0")
            dec = kb.sb(es, [128, 2, 512], F32, "dec")
            ff = kb.sb(es, [128, 512], F32, "ff"); fb = kb.sb(es, [128, 512], F32, "fb")
            fs_ = kb.sb(es, [128, 512], F32, "fs"); fd_ = kb.sb(es, [128, 512], F32, "fd")
            for (l0, ln) in lagblocks:
                kb.dma(zp[:, 0:ln], I["hy_zpos_" + nm].ap()[:, l0:l0 + ln], writes=[zp])
                kb.dma(dec[:, :, 0:ln], I["hy_decay_" + nm].ap().rearrange("(c p) t -> p c t", p=128)[:, :, l0:l0 + ln], writes=[dec])
                p1 = kb.bank[0]
                kb.mm(p1[0:64, 0:ln], w1[:], zp[:, 0:ln], reads=[w1, zp], writes=[p1])
                kb.ts("dve", t0[:, 0:ln], p1[0:64, 0:ln], cols[:, 2:3], OP.mult, bf[:, 0:1], OP.add, reads=[p1, cols, bf], writes=[t0])
                self.sin_rr("dve", h1[:, 0:ln], t0[:, 0:ln], 0.0, t1[:, 0:ln], t2[:, 0:ln])
                p2 = kb.bank[1]
                kb.mm(p2[0:64, 0:ln], w2[:], h1[:, 0:ln], reads=[w2, h1], writes=[p2])
                kb.ts("dve", t0[:, 0:ln], p2[0:64, 0:ln], cols[:, 3:4], OP.mult, bf[:, 1:2], OP.add, reads=[p2, cols, bf], writes=[t0])
                self.sin_rr("dve", h2[:, 0:ln], t0[:, 0:ln], 0.0, t1[:, 0:ln], t2[:, 0:ln])
                for o in range(2):
                    for chf in range(2):
                        jf = o * 2 + chf
                        jb = 4 + o * 2 + chf
                        pf = kb.bank[2]; pbk = kb.bank[3]
                        kb.mm(pf[:, 0:ln], w3[:, jf * 128:(jf + 1) * 128], h2[:, 0:ln], reads=[w3, h2], writes=[pf])
                        kb.mm(pbk[:, 0:ln], w3[:, jb * 128:(jb + 1) * 128], h2[:, 0:ln], reads=[w3, h2], writes=[pbk])
                        kb.stt("dve", ff[:, 0:ln], pf[:, 0:ln], b3[:, jf:jf + 1], dec[:, chf, 0:ln], OP.add, OP.mult, reads=[pf, b3, dec], writes=[ff])
                        kb.stt("dve", fb[:, 0:ln], pbk[:, 0:ln], b3[:, jb:jb + 1], dec[:, chf, 0:ln], OP.add, OP.mult, reads=[pbk, b3, dec], writes=[fb])
                        if l0 == 0:
                            kb.memset("dve", fb[:, 0:1], 0.0, writes=[fb])
                        kb.tt("dve", fs_[:, 0:ln], ff[:, 0:ln], fb[:, 0:ln], OP.add, reads=[ff, fb], writes=[fs_])
                        kb.tt("pool", fd_[:, 0:ln], ff[:, 0:ln], fb[:, 0:ln], OP.subtract, reads=[ff, fb], writes=[fd_])
                        for (src, dst, pbank) in ((fs_, FS, 4), (fd_, FD, 5)):
                            pt_ = kb.bank[pbank]
                            nq = ln // 128
                            for q in range(nq):
                                kb.tr(pt_[:, q * 128:(q + 1) * 128], src[:, q * 128:(q + 1) * 128], ident_f[:], reads=[src, ident_f], writes=[pt_])
                            tc0 = l0 // 128
                            kb.cp("act", dst[:, tc0:tc0 + nq, o * 256 + chf * 128:o * 256 + (chf + 1) * 128],
                                  pt_[:, 0:nq * 128].rearrange("p (q c) -> p q c", c=128), reads=[pt_], writes=[dst])
            Dt = [kb.sb(es, [128, ntc, 128], BF16, "Dt%d" % i) for i in range(4)]
            ks = [kb.sb(es, [128, 2, 512], F32, "ks%d" % i) for i in range(2)]
            nd = 0
            for j in range(nj):
                for ri, (fc, src) in enumerate(((j, FS), (nj + j, FD))):
                    D_ = Dt[nd % 4]
                    nd += 1
                    kb.dma(D_[:], I["hy_dfwd_" + nm].ap()[fc], writes=[D_])
                    pk = kb.bank[ri]
                    for tc in range(ntc):
                        kb.mm(pk[:, :], D_[:, tc, :], src[:, tc, :], start=(tc == 0), stop=(tc == ntc - 1), reads=[D_, src], writes=[pk])
                    kb.cp("act" if ri else "dve", ks[j % 2][:, ri, :], pk[:, :], reads=[pk], writes=[ks[j % 2]])
                kb.dma(KSP.ap()[j].rearrange("r p c -> p r c"), ks[j % 2][:], reads=[ks[j % 2]], writes=["KSPEC_" + nm], q="pool")
        kb.barrier()
        with ExitStack() as es:
            ident_f = kb.sb(es, [128, 128], F32, "identf")
            kb.dma(ident_f[:], I["ident"].ap(), writes=[ident_f])
            sw = kb.sb(es, [128, 6, 3], F32, "sw"); sbb = kb.sb(es, [128, 6], F32, "sbb")
            kb.dma(sw[:], I["hy_sw"].ap()[L], writes=[sw]); kb.dma(sbb[:], I["hy_sb"].ap()[L], writes=[sbb])
            xin = [kb.sb(es, [128, n], F32, "hzx%d" % i) for i in range(2)]
            zz = [kb.sb(es, [128, n], F32, "hzz%d" % i) for i in range(2)]
            tm = [kb.sb(es, [128, 4, 128], F32, "tm%d" % i) for i in range(2)]
            ntm = 0
            for c in range(6):
                x_ = xin[c % 2]; z_ = zz[c % 2]
                kb.dma(x_[:], self.S["HZT"].ap()[c * 128:(c + 1) * 128, r0:r0 + n], reads=[("HZT", i) for i in range(9)], writes=[x_])
                kb.ts("dve", z_[:], x_[:], sw[:, c, 1:2], OP.mult, sbb[:, c:c + 1], OP.add, reads=[x_, sw, sbb], writes=[z_])
                kb.stt("dve", z_[:, 1:n], x_[:, 0:n - 1], sw[:, c, 0:1], z_[:, 1:n], OP.mult, OP.add, reads=[x_, sw, z_], writes=[z_])
                kb.stt("dve", z_[:, 0:n - 1], x_[:, 1:n], sw[:, c, 2:3], z_[:, 0:n - 1], OP.mult, OP.add, reads=[x_, sw, z_], writes=[z_])
                for tg in range(0, ntc, 4):
                    ng = min(4, ntc - tg)
                    pt_ = kb.bank[ntm % 2]
                    t_ = tm[ntm % 2]
                    ntm += 1
                    for q in range(ng):
                        kb.tr(pt_[:, q * 128:(q + 1) * 128], z_[:, (tg + q) * 128:(tg + q + 1) * 128], ident_f[:], reads=[z_, ident_f], writes=[pt_])
                    kb.cp("act" if ntm % 2 else "dve", t_[:, 0:ng, :], pt_[:, 0:ng * 128].rearrange("p (q c) -> p q c", c=128), reads=[pt_], writes=[t_])
                    kb.dma(self.S["HZTM"].ap()[r0 + tg * 128:r0 + (tg + ng) * 128, c * 128:(c + 1) * 128].rearrange("(q p) c -> p q c", p=128),
                           t_[:, 0:ng, :], reads=[t_], writes=["HZTM"], q="pool")
        kb.barrier()
        with ExitStack() as es:
            ident = kb.sb(es, [128, 128], BF16, "identb")
            idf = kb.sb(es, [128, 128], F32, "identf")
            kb.dma(idf[:], I["ident"].ap(), writes=[idf])
            kb.cp("dve", ident[:], idf[:], reads=[idf], writes=[ident])
            Yb = [kb.sb(es, [128, ntc, 256], BF16, "Yb%d" % i) for i in range(2)]
            Z = kb.sb(es, [128, 2 * nj, 256], BF16, "Zs")
            Dt = [kb.sb(es, [128, ntc, 128], BF16, "Df%d" % i) for i in range(4)]
            Di = [kb.sb(es, [128, 2 * nj, 128], BF16, "Di%d" % i) for i in range(2)]
            kt = [kb.sb(es, [128, 2, 256], F32, "kt%d" % i) for i in range(2)]
            cm = [kb.sb(es, [128, 256], F32, "cm%d" % i) for i in range(4)]
            yo = [kb.sb(es, [128, 256], F32, "yo%d" % i) for i in range(2)]
            gt = [kb.sb(es, [128, 256], F32, "gt%d" % i) for i in range(2)]
            yn = [kb.sb(es, [128, 256], F32, "yn%d" % i) for i in range(2)]
            ynb = kb.sb(es, [128, 256], BF16, "ynb"); sqj = kb.sb(es, [128, 256], BF16, "sqj")
            ss = kb.sb(es, [128, 1], F32, "ss")
            mts = [kb.sb(es, [128, 2, 128], BF16, "mts%d" % i) for i in range(2)]
            bias_bc = kb.sb(es, [128, 2, 256], F32, "biasbc")
            gbc = kb.sb(es, [128, 256], F32, "gbc")
            kb.dma(bias_bc[:].rearrange("p o c -> p (o c)"), I["hy_bias"].ap()[L:L + 1].rearrange("a o c -> a (o c)").partition_broadcast(128), writes=[bias_bc])
            kb.dma(gbc[:], I["mix_norm_g"].ap()[L:L + 1, 768:1024].partition_broadcast(128), writes=[gbc])
            HZ = self.S["HZTM"].ap()
            for tc in range(ntc):
                y_ = yo[tc % 2]
                kb.dma(y_[:], HZ[r0 + tc * 128:r0 + (tc + 1) * 128, 0:256], reads=["HZTM"], writes=[y_])
                kb.cp("act" if tc % 2 else "dve", Yb[0][:, tc, :], y_[:], reads=[y_], writes=[Yb[0]])
            nd = 0
            for o in range(2):
                Yin = Yb[o]
                for j in range(nj):
                    k_ = kt[j % 2]
                    kb.dma(k_[:], KSP.ap()[j].rearrange("r p c -> p r c")[:, :, o * 256:(o + 1) * 256], reads=["KSPEC_" + nm], writes=[k_])
                    pr = kb.bank[(j % 2) * 2]; pi_ = kb.bank[(j % 2) * 2 + 1]
                    for ri, (fc, pk) in enumerate(((j, pr), (nj + j, pi_))):
                        D_ = Dt[nd % 4]
                        nd += 1
                        kb.dma(D_[:], I["hy_dfwd_" + nm].ap()[fc], writes=[D_])
                        for tc in range(ntc):
                            kb.mm(pk[:, 0:256], D_[:, tc, :], Yin[:, tc, :], start=(tc == 0), stop=(tc == ntc - 1), reads=[D_, Yin], writes=[pk])
                    c1, c2, c3, c4 = cm
                    kb.tt("dve", c1[:], pr[:, 0:256], k_[:, 0, :], OP.mult, reads=[pr, k_], writes=[c1])
                    kb.tt("dve", c2[:], pi_[:, 0:256], k_[:, 1, :], OP.mult, reads=[pi_, k_], writes=[c2])
                    kb.tt("pool", Z[:, j, :], c1[:], c2[:], OP.subtract, reads=[c1, c2], writes=[Z])
                    kb.tt("dve", c3[:], pr[:, 0:256], k_[:, 1, :], OP.mult, reads=[pr, k_], writes=[c3])
                    kb.tt("dve", c4[:], pi_[:, 0:256], k_[:, 0, :], OP.mult, reads=[pi_, k_], writes=[c4])
                    kb.tt("pool", Z[:, nj + j, :], c3[:], c4[:], OP.add, reads=[c3, c4], writes=[Z])
                for tc in range(ntc):
                    D_ = Di[tc % 2]
                    kb.dma(D_[:], I["hy_dinv_" + nm].ap()[tc], writes=[D_])
                    y_ = yo[tc % 2]; g_ = gt[tc % 2]; o_ = yn[tc % 2]
                    rows = slice(r0 + tc * 128, r0 + (tc + 1) * 128)
                    if o == 0:
                        kb.dma(y_[:], HZ[rows, 0:256], reads=["HZTM"], writes=[y_])
                    else:
                        kb.dma(y_[:], self.S["HY1"].ap()[rows, :], reads=[("HY1", tc)], writes=[y_])
                    kb.dma(g_[:], HZ[rows, 256 * (o + 1):256 * (o + 2)], reads=["HZTM"], writes=[g_])
                    pc = kb.bank[4 + (tc % 2)]
                    for fc in range(2 * nj):
                        kb.mm(pc[:, 0:256], D_[:, fc, :], Z[:, fc, :], start=(fc == 0), stop=(fc == 2 * nj - 1), reads=[D_, Z], writes=[pc])
                    kb.tt("pool", y_[:], y_[:], bias_bc[:, o, :], OP.mult, reads=[y_, bias_bc], writes=[y_])
                    kb.tt("dve", o_[:], pc[:, 0:256], y_[:], OP.add, reads=[pc, y_], writes=[o_])
                    kb.tt("dve", o_[:], o_[:], g_[:], OP.mult, reads=[o_, g_], writes=[o_])
                    if o == 0:
                        kb.cp("act", Yb[1][:, tc, :], o_[:], reads=[o_], writes=[Yb[1]])
                        kb.dma(self.S["HY1"].ap()[rows, :], o_[:], reads=[o_], writes=[("HY1", tc)], q="pool")
                    else:
                        if "HYO" in self.debug:
                            kb.dma(self.S["HYO"].ap()[rows, :], o_[:], reads=[o_], writes=["HYO"], q="pool")
                        kb.act(sqj[:], o_[:], AF.Square, accum_out=ss[:], reads=[o_], writes=[sqj, ss])
                        kb.ts("dve", ss[:], ss[:], 1.0 / 256, OP.mult, EPS, OP.add, reads=[ss], writes=[ss])
                        kb.act(ss[:], ss[:], AF.Sqrt, reads=[ss], writes=[ss])
                        kb.op("dve", lambda E: E.reciprocal(ss[:], ss[:]), reads=[ss], writes=[ss])
                        kb.stt("dve", ynb[:], o_[:], ss[:, 0:1], gbc[:], OP.mult, OP.mult, reads=[o_, ss, gbc], writes=[ynb])
                        pt_ = kb.bank[6 + (tc % 2)]
                        ptv = pt_[:].bitcast(BF16)
                        for fcx in range(2):
                            kb.tr(ptv[:, fcx * 128:(fcx + 1) * 128], ynb[:, fcx * 128:(fcx + 1) * 128], ident[:], reads=[ynb, ident], writes=[pt_])
                        m_ = mts[tc % 2]
                        kb.cp("act", m_[:], ptv[:, 0:256].rearrange("p (f q) -> p f q", f=2), reads=[pt_], writes=[m_])
                        kb.dma(self.S["MT"].ap()[768:1024, r0 + tc * 128:r0 + (tc + 1) * 128].rearrange("(f p) t -> p f t", p=128), m_[:],
                               reads=[m_], writes=[("MT_hy", part, tc)], q="pool")
        kb.barrier()


    def stage_s5merge(self, L):
        kb = self.kb
        blocks = [(0, 256)] + [(256 + 512 * i, 512) for i in range(8)]
        with ExitStack() as es:
            ones = kb.sb(es, [128, 128], BF16, "ones")
            kb.memset("dve", ones[:], 1.0, writes=[ones])
            gcol = kb.sb(es, [128, 2], F32, "gcol")
            kb.dma(gcol[:], self.I["mix_g_s5col"].ap()[L], writes=[gcol])
            xin = [kb.sb(es, [128, 2, 512], F32, "s5x%d" % i) for i in range(2)]
            sqb = kb.sb(es, [128, 2, 512], BF16, "sqb"); rst = kb.sb(es, [128, 512], F32, "rst")
            ob = [kb.sb(es, [128, 2, 512], BF16, "s5o%d" % i) for i in range(2)]
            for bi, (s0, nn) in enumerate(blocks):
                x_ = xin[bi % 2]; o_ = ob[bi % 2]
                kb.dma(x_[:, :, 0:nn], self.S["S5T"].ap().rearrange("(c p) t -> p c t", p=128)[:, :, s0:s0 + nn], reads=[("S5T", bi)], writes=[x_])
                kb.act(sqb[:, :, 0:nn], x_[:, :, 0:nn], AF.Square, reads=[x_], writes=[sqb])
                pn = kb.bank[bi % 2]
                for c in range(2):
                    kb.mm(pn[:, 0:nn], ones[:], sqb[:, c, 0:nn], start=(c == 0), stop=(c == 1), reads=[ones, sqb], writes=[pn])
                kb.ts("dve", rst[:, 0:nn], pn[:, 0:nn], 1.0 / 256, OP.mult, EPS, OP.add, reads=[pn], writes=[rst])
                kb.act(rst[:, 0:nn], rst[:, 0:nn], AF.Sqrt, reads=[rst], writes=[rst])
                kb.op("dve", lambda E: E.reciprocal(rst[:, 0:nn], rst[:, 0:nn]), reads=[rst], writes=[rst])
                for c in range(2):
                    kb.stt("dve", o_[:, c, 0:nn], x_[:, c, 0:nn], gcol[:, c:c + 1], rst[:, 0:nn], OP.mult, OP.mult, reads=[x_, gcol, rst], writes=[o_])
                kb.dma(self.S["MT"].ap()[0:256, s0:s0 + nn].rearrange("(c p) t -> p c t", p=128), o_[:, :, 0:nn], reads=[o_], writes=[("MT_s5", bi)], q="pool")
        kb.barrier()

    def stage_out_moe(self, L, last):
        kb = self.kb
        I = self.I
        S = self.S
        tiles = list(range(2, NT)) if last else list(range(NT))
        ntl = len(tiles)
        nblk = (2 * ntl * 128 + 127) // 128 + 32
        X = S["X"].ap()
        with ExitStack() as es:
            ident_f = kb.sb(es, [128, 128], F32, "identf")
            ident = kb.sb(es, [128, 128], BF16, "identb")
            kb.dma(ident_f[:], I["ident"].ap(), writes=[ident_f])
            kb.cp("dve", ident[:], ident_f[:], reads=[ident_f], writes=[ident])
            iota_e = kb.sb(es, [128, 32], F32, "iotae"); iota_b = kb.sb(es, [128, 128], F32, "iotab"); iota_p = kb.sb(es, [128, 1], F32, "iotap")
            ltri = kb.sb(es, [128, 128], BF16, "ltri"); ones = kb.sb(es, [128, 128], BF16, "ones")
            kb.dma(iota_e[:], I["iota_e"].ap(), writes=[iota_e]); kb.dma(iota_b[:], I["iota_b"].ap(), writes=[iota_b])
            kb.dma(iota_p[:], I["iota_p"].ap(), writes=[iota_p]); kb.dma(ltri[:], I["ltri"].ap(), writes=[ltri])
            kb.memset("dve", ones[:], 1.0, writes=[ones])
            GW = kb.sb(es, [128, NT, 2], F32, "GW"); EID = kb.sb(es, [128, NT, 2], F32, "EID"); RANK = kb.sb(es, [128, NT, 2], F32, "RANK")
            DEST = kb.sb(es, [128, NT, 2], F32, "DEST"); DESTI = kb.sb(es, [128, NT, 2], I32, "DESTI")
            base = kb.sb(es, [128, 32], F32, "base")
            BE = kb.sb(es, [128, 128], F32, "BE"); IDXW = kb.sb(es, [128, 2, 128], I32, "IDXW")
            CHG = kb.sb(es, [128, 128], F32, "CHG"); IDXF = kb.sb(es, [128, 128], F32, "IDXF")
            kb.memset("dve", base[:], 0.0, writes=[base])
            kb.memset("dve", RANK[:], 0.0, writes=[RANK])
            kb.memset("dve", DEST[:], 0.0, writes=[DEST])
            es2 = ExitStack()
            wo = kb.sb(es2, [128, 8, DM], BF16, "wo")
            wr = kb.sb(es2, [128, 8, 36], F32, "wr")
            kb.dma(wr[:], I["moe_wr"].ap()[L].rearrange("(kc p) n -> p kc n", p=128), writes=[wr])
            gate1 = kb.sb(es2, [128, DM], F32, "gate1"); G2 = kb.sb(es2, [128, DM], F32, "G2"); S2 = kb.sb(es2, [128, DM], F32, "S2")
            tmpb = kb.sb(es2, [128, DM], F32, "tmpb")
            es3 = ExitStack()
            wst = kb.sb(es3, [128, 8, DM], F32, "wst")
            kb.dma(wst[:], I["w_out"].ap()[L].rearrange("(kc p) n -> p kc n", p=128), writes=[wst])
            kb.cp("act", wo[:, 0:4, :], wst[:, 0:4, :], reads=[wst], writes=[wo])
            kb.cp("dve", wo[:, 4:8, :], wst[:, 4:8, :], reads=[wst], writes=[wo])
            kb.barrier()
            es3.close()
            mT = [kb.sb(es2, [128, 8, 512], BF16, "mT%d" % i) for i in range(2)]
            xt = [kb.sb(es2, [128, DM], F32, "xt%d" % i) for i in range(2)]
            xn = [kb.sb(es2, [128, DM], F32, "xn%d" % i) for i in range(2)]
            sq = kb.sb(es2, [128, DM], BF16, "sq"); ss = kb.sb(es2, [128, 1], F32, "ss"); rstd = kb.sb(es2, [128, 1], F32, "rstd")
            tmp = kb.sb(es2, [128, DM], F32, "tmp")
            ff = [kb.sb(es2, [128, DM], F32, "ff%d" % i) for i in range(2)]
            fbt = [kb.sb(es2, [128, DM], BF16, "fbt%d" % i) for i in range(2)]
            fT = kb.sb(es2, [128, 8, 128], F32, "fT")
            LG = kb.sb(es2, [128, NT, 36], F32, "LG")
            sm = {k: kb.sb(es2, shp, F32, k) for k, shp in (("gmax", [128, 1]), ("ngmax", [128, 1]), ("ohg", [128, 4]), ("pen", [128, 4]), ("ex4", [128, 4]),
                                                            ("sume", [128, 1]), ("gw", [128, 1]), ("msk", [128, 32]), ("top8", [128, 8]), ("nv2", [128, 1]),
                                                            ("p1", [128, 1]), ("p2", [128, 1]), ("a0", [128, 32]), ("junk", [128, 32]), ("csum", [128, 32]))}
            idx8 = kb.sb(es2, [128, 8], U32, "idx8")
            E2 = kb.sb(es2, [128, 64], F32, "E2"); E2b = kb.sb(es2, [128, 64], BF16, "E2b")
            blocks = [(0, 2)] + [(2 + 4 * i, 4) for i in range(8)]
            if last:
                blocks = blocks[1:]
            for bi, (t0, nt_) in enumerate(blocks):
                which = 1 if t0 == 0 else 0
                if t0 in (0, 2):
                    MOD = S["MOD"].ap()
                    kb.dma(gate1[:], MOD[which:which + 1, 2 * DM:3 * DM].partition_broadcast(128), reads=["MOD"], writes=[gate1])
                    self.load_mod_bc((G2, S2, tmpb), L, which, 3, "norm2_g")
                m_ = mT[bi % 2]
                nb = nt_ * 128
                c0 = t0 * 128
                kb.dma(m_[:, :, 0:nb], S["MT"].ap().rearrange("(kc p) t -> p kc t", p=128)[:, :, c0:c0 + nb],
                       reads=[("MT_s5", i) for i in range(9)] + [("MT_mla", i) for i in range(9)] + [("MT_hy", "l", i) for i in range(32)] + [("MT_hy", "c", i) for i in range(2)],
                       writes=[m_])
                for j in range(nt_):
                    t = t0 + j
                    x_ = xt[t % 2]; xo = xn[t % 2]
                    kb.dma(x_[:], X[t * 128:(t + 1) * 128, :], reads=[("X", t)], writes=[x_])
                    for nh in range(2):
                        po = kb.bank[nh]
                        for kc in range(8):
                            kb.mm(po[:, :], m_[:, kc, j * 128:(j + 1) * 128], wo[:, kc, nh * 512:(nh + 1) * 512], start=(kc == 0), stop=(kc == 7),
                                  reads=[m_, wo], writes=[po])
                        kb.tt("dve", tmp[:, nh * 512:(nh + 1) * 512], po[:, :], gate1[:, nh * 512:(nh + 1) * 512], OP.mult, reads=[po, gate1], writes=[tmp])
                    kb.tt("pool", xo[:], tmp[:], x_[:], OP.add, reads=[tmp, x_], writes=[xo])
                    kb.dma(X[t * 128:(t + 1) * 128, :], xo[:], reads=[xo], writes=[("X", t)], q="pool")
                    if "XMID" in self.debug:
                        kb.dma(S["XMID"].ap()[t * 128:(t + 1) * 128, :], xo[:], reads=[xo], writes=["XMID"], q="pool")
                    f_ = ff[t % 2]; fb_ = fbt[t % 2]
                    self.rms_modulate(xo, G2, S2, sq, ss, rstd, tmp, f_, fb_)
                    kb.dma(S["FB"].ap()[t * 128:(t + 1) * 128, :], fb_[:], reads=[fb_], writes=[("FB", t)], q="pool")
                    for half in range(2):
                        pt_ = kb.bank[2 + half]
                        for q in range(4):
                            kc = half * 4 + q
                            kb.tr(pt_[:, q * 128:(q + 1) * 128], f_[:, kc * 128:(kc + 1) * 128], ident_f[:], reads=[f_, ident_f], writes=[pt_])
                        kb.cp("act" if half else "dve", fT[:, half * 4:(half + 1) * 4, :], pt_[:, :].rearrange("p (q c) -> p q c", c=128), reads=[pt_], writes=[fT])
                    pl = kb.bank[4]
                    for kc in range(8):
                        kb.mm(pl[:, 0:36], fT[:, kc, :], wr[:, kc, :], start=(kc == 0), stop=(kc == 7), reads=[fT, wr], writes=[pl])
                    kb.cp("act", LG[:, t, :], pl[:, 0:36], reads=[pl], writes=[LG])
            tl = tiles[0]
            NTL = len(tiles)
            TS = slice(tl, NT)

            def bcl(ap2, n_):
                return ap2.unsqueeze(2).to_broadcast([128, NTL, n_])
            R = {k: kb.sb(es2, shp, F32, k) for k, shp in (("gm", [128, NT]), ("d4", [128, NT, 4]), ("ohg", [128, NT, 4]), ("sume", [128, NT]), ("gwv", [128, NT]),
                                                           ("pen", [128, NT, 4]), ("msk", [128, NT, 32]), ("msk2", [128, NT, 32]), ("v1", [128, NT]), ("v2", [128, NT]),
                                                           ("p1", [128, NT]), ("EE", [128, NT, 2, 32]), ("PRE", [128, NT, 64]), ("CNT", [128, NT, 64]),
                                                           ("CNTS", [128, NT, 32]), ("BASEI", [128, NT, 32]), ("A0", [128, NT, 32]), ("onesT", [128, NT]))}
            E2b = kb.sb(es2, [128, NT, 64], BF16, "E2b")
            LG4 = LG[:, TS, 0:4]
            kb.op("dve", lambda E: E.tensor_reduce(R["gm"][:, TS], LG4, AX.X, OP.max), reads=[LG], writes=[R["gm"]])
            kb.tt("dve", R["ohg"][:, TS, :], LG4, bcl(R["gm"][:, TS], 4), OP.is_equal, reads=[LG, R["gm"]], writes=[R["ohg"]])
            kb.tt("dve", R["d4"][:, TS, :], LG4, bcl(R["gm"][:, TS], 4), OP.subtract, reads=[LG, R["gm"]], writes=[R["d4"]])
            kb.act(R["d4"][:, TS, :], R["d4"][:, TS, :], AF.Exp, reads=[R["d4"]], writes=[R["d4"]])
            kb.op("dve", lambda E: E.tensor_reduce(R["sume"][:, TS], R["d4"][:, TS, :], AX.X, OP.add), reads=[R["d4"]], writes=[R["sume"]])
            kb.op("dve", lambda E: E.reciprocal(R["gwv"][:, TS], R["sume"][:, TS]), reads=[R["sume"]], writes=[R["gwv"]])
            kb.ts("dve", R["pen"][:, TS, :], R["ohg"][:, TS, :], 1.0e30, OP.mult, -1.0e30, OP.add, reads=[R["ohg"]], writes=[R["pen"]])
            kb.tt("dve", R["msk"][:, TS, :].rearrange("p t (g e) -> p t g e", g=4), LG[:, TS, 4:36].rearrange("p t (g e) -> p t g e", g=4),
                  R["pen"][:, TS, :].unsqueeze(3).to_broadcast([128, NTL, 4, 8]), OP.add, reads=[LG, R["pen"]], writes=[R["msk"]])
            kb.op("dve", lambda E: E.tensor_reduce(R["v1"][:, TS], R["msk"][:, TS, :], AX.X, OP.max), reads=[R["msk"]], writes=[R["v1"]])
            kb.tt("dve", R["EE"][:, TS, 0, :], R["msk"][:, TS, :], bcl(R["v1"][:, TS], 32), OP.is_equal, reads=[R["msk"], R["v1"]], writes=[R["EE"]])
            kb.stt("dve", R["msk2"][:, TS, :], R["EE"][:, TS, 0, :], -2.0e30, R["msk"][:, TS, :], OP.mult, OP.add, reads=[R["EE"], R["msk"]], writes=[R["msk2"]])
            kb.op("dve", lambda E: E.tensor_reduce(R["v2"][:, TS], R["msk2"][:, TS, :], AX.X, OP.max), reads=[R["msk2"]], writes=[R["v2"]])
            kb.tt("dve", R["EE"][:, TS, 1, :], R["msk2"][:, TS, :], bcl(R["v2"][:, TS], 32), OP.is_equal, reads=[R["msk2"], R["v2"]], writes=[R["EE"]])
            kb.tt("dve", R["p1"][:, TS], R["v1"][:, TS], R["v2"][:, TS], OP.subtract, reads=[R["v1"], R["v2"]], writes=[R["p1"]])
            kb.act(R["p1"][:, TS], R["p1"][:, TS], AF.Sigmoid, reads=[R["p1"]], writes=[R["p1"]])
            kb.tt("dve", GW[:, TS, 0], R["p1"][:, TS], R["gwv"][:, TS], OP.mult, reads=[R["p1"], R["gwv"]], writes=[GW])
            kb.ts("dve", R["p1"][:, TS], R["p1"][:, TS], -1.0, OP.mult, 1.0, OP.add, reads=[R["p1"]], writes=[R["p1"]])
            kb.tt("dve", GW[:, TS, 1], R["p1"][:, TS], R["gwv"][:, TS], OP.mult, reads=[R["p1"], R["gwv"]], writes=[GW])
            kb.cp("act", E2b[:, TS, :], R["EE"][:, TS, :, :].rearrange("p t k e -> p t (k e)"), reads=[R["EE"]], writes=[E2b])
            for g0 in range(tl, NT, 8):
                g1 = min(NT, g0 + 8)
                ppre = kb.bank[5]; pcnt = kb.bank[6]
                for t in range(g0, g1):
                    kb.mm(ppre[:, (t - g0) * 64:(t - g0 + 1) * 64], ltri[:], E2b[:, t, :], reads=[ltri, E2b], writes=[ppre])
                    kb.mm(pcnt[:, (t - g0) * 64:(t - g0 + 1) * 64], ones[:], E2b[:, t, :], reads=[ones, E2b], writes=[pcnt])
                kb.cp("act", R["PRE"][:, g0:g1, :], ppre[:, 0:(g1 - g0) * 64].rearrange("p (t c) -> p t c", c=64), reads=[ppre], writes=[R["PRE"]])
                kb.cp("dve", R["CNT"][:, g0:g1, :], pcnt[:, 0:(g1 - g0) * 64].rearrange("p (t c) -> p t c", c=64), reads=[pcnt], writes=[R["CNT"]])
            kb.tt("dve", R["CNTS"][:, TS, :], R["CNT"][:, TS, 0:32], R["CNT"][:, TS, 32:64], OP.add, reads=[R["CNT"]], writes=[R["CNTS"]])
            kb.memset("dve", R["onesT"][:], 1.0, writes=[R["onesT"]])
            for e in range(32):
                kb.op("dve", lambda E: E.tensor_tensor_scan(R["BASEI"][:, TS, e], R["onesT"][:, TS], R["CNTS"][:, TS, e], 0.0, OP.mult, OP.add),
                      reads=[R["onesT"], R["CNTS"]], writes=[R["BASEI"]])
            kb.cp("dve", base[:], R["BASEI"][:, NT - 1, :], reads=[R["BASEI"]], writes=[base])
            kb.tt("dve", R["BASEI"][:, TS, :], R["BASEI"][:, TS, :], R["CNTS"][:, TS, :], OP.subtract, reads=[R["BASEI"], R["CNTS"]], writes=[R["BASEI"]])
            kb.tt("dve", R["A0"][:, TS, :], R["PRE"][:, TS, 0:32], R["BASEI"][:, TS, :], OP.add, reads=[R["PRE"], R["BASEI"]], writes=[R["A0"]])
            kb.tt("dve", R["A0"][:, TS, :], R["A0"][:, TS, :], R["EE"][:, TS, 0, :], OP.mult, reads=[R["A0"], R["EE"]], writes=[R["A0"]])
            kb.op("dve", lambda E: E.tensor_reduce(RANK[:, TS, 0], R["A0"][:, TS, :], AX.X, OP.add), reads=[R["A0"]], writes=[RANK])
            kb.tt("dve", R["A0"][:, TS, :], R["PRE"][:, TS, 32:64], R["BASEI"][:, TS, :], OP.add, reads=[R["PRE"], R["BASEI"]], writes=[R["A0"]])
            kb.tt("dve", R["A0"][:, TS, :], R["A0"][:, TS, :], R["CNT"][:, TS, 0:32], OP.add, reads=[R["A0"], R["CNT"]], writes=[R["A0"]])
            kb.tt("dve", R["A0"][:, TS, :], R["A0"][:, TS, :], R["EE"][:, TS, 1, :], OP.mult, reads=[R["A0"], R["EE"]], writes=[R["A0"]])
            kb.op("dve", lambda E: E.tensor_reduce(RANK[:, TS, 1], R["A0"][:, TS, :], AX.X, OP.add), reads=[R["A0"]], writes=[RANK])
            pb_ = kb.sb(es2, [128, 32], F32, "padb"); pend = kb.sb(es2, [128, 32], F32, "pend"); pst = kb.sb(es2, [128, 32], F32, "pstart")
            one32 = kb.sb(es2, [128, 32], F32, "one32")
            kb.memset("dve", one32[:], 1.0, writes=[one32])
            kb.ts("dve", pb_[:], base[:], 1.0 / 128, OP.mult, (127.0 / 128 - 0.5 + 1.0 / 256), OP.add, reads=[base], writes=[pb_])
            kb.ts("dve", pb_[:], pb_[:], MAGIC, OP.add, reads=[pb_], writes=[pb_])
            kb.ts("dve", pb_[:], pb_[:], -MAGIC, OP.add, reads=[pb_], writes=[pb_])
            kb.op("dve", lambda E: E.tensor_tensor_scan(pend[:], one32[:], pb_[:], 0.0, OP.mult, OP.add), reads=[one32, pb_], writes=[pend])
            kb.tt("dve", pst[:], pend[:], pb_[:], OP.subtract, reads=[pend, pb_], writes=[pst])
            kb.ts("dve", pst[:], pst[:], 128.0, OP.mult, reads=[pst], writes=[pst])
            kb.memset("dve", BE[:], 0.0, writes=[BE])
            for e in range(32):
                kb.stt("dve", BE[:], iota_b[:], pend[:, e:e + 1], BE[:], OP.is_ge, OP.add, reads=[iota_b, pend, BE], writes=[BE])
            NS = 4
            per = nblk // NS
            assert nblk % NS == 0
            BIGI = 1.0e6
            kb.ts("dve", BE[:], BE[:], 31.0, OP.min, reads=[BE], writes=[BE])
            kb.memset("dve", CHG[:], 1.0, writes=[CHG])
            kb.tt("dve", CHG[:, 1:nblk], BE[:, 1:nblk], BE[:, 0:nblk - 1], OP.not_equal, reads=[BE], writes=[CHG])
            for q in range(NS):
                kb.memset("dve", CHG[:, q * per:q * per + 1], 1.0, writes=[CHG])
            kb.ts("dve", BE[:], BE[:], float(L * 32), OP.add, 128.0, OP.mult, reads=[BE], writes=[BE])
            kb.ts("dve", BE[:], BE[:], iota_p[:, 0:1], OP.add, 2.0, OP.mult, reads=[BE, iota_p], writes=[BE])
            for half in range(2):
                kb.ts("dve", IDXF[:], BE[:], float(half) - BIGI, OP.add, reads=[BE], writes=[IDXF])
                kb.tt("dve", IDXF[:], IDXF[:], CHG[:], OP.mult, reads=[IDXF, CHG], writes=[IDXF])
                kb.ts("dve", IDXF[:], IDXF[:], BIGI, OP.add, reads=[IDXF], writes=[IDXF])
                kb.cp("dve", IDXW[:, half, :], IDXF[:], reads=[IDXF], writes=[IDXW])
            for k in range(2):
                kb.tt("dve", R["A0"][:, TS, :], R["EE"][:, TS, k, :], pst[:, :].unsqueeze(1).to_broadcast([128, NTL, 32]), OP.mult,
                      reads=[R["EE"], pst], writes=[R["A0"]])
                kb.op("dve", lambda E: E.tensor_reduce(DEST[:, TS, k], R["A0"][:, TS, :], AX.X, OP.add), reads=[R["A0"]], writes=[DEST])
            kb.tt("dve", DEST[:], DEST[:], RANK[:], OP.add, reads=[DEST, RANK], writes=[DEST])
            kb.ts("dve", DEST[:], DEST[:], float(nblk * 128 - 1), OP.min, 0.0, OP.max, reads=[DEST], writes=[DEST])
            if "DBGR" in self.debug:
                kb.dma(S["DBGR"].ap()[:, 0:NT * 2], DEST[:].rearrange("p t k -> p (t k)"), reads=[DEST], writes=["DBGR"])
                kb.dma(S["DBGR"].ap()[:, NT * 2:NT * 4], RANK[:].rearrange("p t k -> p (t k)"), reads=[RANK], writes=["DBGR"])
                kb.dma(S["DBGR"].ap()[:, NT * 4:NT * 6], GW[:].rearrange("p t k -> p (t k)"), reads=[GW], writes=["DBGR"])
                kb.dma(S["DBGR"].ap()[:, NT * 6:NT * 6 + 32], base[:], reads=[base], writes=["DBGR"])
                kb.dma(S["DBGR"].ap()[:, NT * 6 + 32:NT * 6 + 64], pst[:], reads=[pst], writes=["DBGR"])
            kb.cp("dve", DESTI[:], DEST[:], reads=[DEST], writes=[DESTI])
            for t in tiles:
                fb_ = fbt[t % 2]
                kb.dma(fb_[:], S["FB"].ap()[t * 128:(t + 1) * 128, :], reads=[("FB", t)], writes=[fb_])
                for k in range(2):
                    kb.dma(None, None, reads=[fb_, DESTI], writes=["XS"], q="pool",
                           fn=lambda E: E.indirect_dma_start(out=S["XS"].ap(), out_offset=bass.IndirectOffsetOnAxis(DESTI[:, t, k:k + 1], 0),
                                                             in_=fb_[:], in_offset=None))
            kb.barrier()
            es2.close()
            es4 = ExitStack()
            WQ = [kb.sb(es4, [128, 3, 4096], BF16, "WQ%d" % i) for i in range(NS)]
            xs = [kb.sb(es4, [128, DM], BF16, "xs%d" % i) for i in range(2)]
            XT = [kb.sb(es4, [128, 8, 128], BF16, "XT%d" % i) for i in range(2)]
            h1s = [kb.sb(es4, [128, 512], F32, "h1s%d" % i) for i in range(2)]
            aT = [kb.sb(es4, [128, 4, 128], BF16, "aT%d" % i) for i in range(2)]
            ysb = [kb.sb(es4, [128, DM], F32, "ysb%d" % i) for i in range(2)]
            nrows2 = 2 * DEPTH * 32 * 128
            def slot_blk(slot):
                return (slot % NS) * per + slot // NS

            def Gs(slot):
                b = slot_blk(slot)
                wb = WQ[slot % NS]
                for wi, wn in enumerate(("moe_w1", "moe_w3", "moe_w2")):
                    for half in range(2):
                        kb.dma(None, None, reads=[IDXW], writes=[wb], q="pool",
                               fn=lambda E: E.indirect_dma_start(out=wb[:, wi, half * 2048:(half + 1) * 2048], out_offset=None,
                                                                 in_=I[wn].ap().rearrange("r (h c) -> (r h) c", h=2),
                                                                 in_offset=bass.IndirectOffsetOnAxis(IDXW[:, half, b:b + 1], 0),
                                                                 bounds_check=kb.bnd_reg, oob_is_err=False))

            def Ts(slot):
                b = slot_blk(slot)
                x_ = xs[slot % 2]
                kb.dma(x_[:], S["XS"].ap()[b * 128:(b + 1) * 128, :], reads=["XS"], writes=[x_])
                pt_ = kb.bank[0 if slot % 2 == 0 else 7]
                ptv = pt_[:].bitcast(BF16)
                for kc in range(8):
                    kb.tr(ptv[:, kc * 128:(kc + 1) * 128], x_[:, kc * 128:(kc + 1) * 128], ident[:], reads=[x_, ident], writes=[pt_])
                XT_ = XT[slot % 2]
                kb.cp("dve", XT_[:], ptv.rearrange("p (k n) -> p k n", k=8), reads=[pt_], writes=[XT_])

            def Hs(slot):
                wb = WQ[slot % NS]
                XT_ = XT[slot % 2]
                ph1 = kb.bank[1 + 2 * (slot % 2)]; ph3 = kb.bank[2 + 2 * (slot % 2)]
                for (pp, wi) in ((ph1, 0), (ph3, 1)):
                    for hc in range(4):
                        for kc in range(8):
                            kb.mm(pp[:, hc * 128:(hc + 1) * 128], wb[:, wi, kc * 512 + hc * 128:kc * 512 + (hc + 1) * 128], XT_[:, kc, :],
                                  start=(kc == 0), stop=(kc == 7), reads=[wb, XT_], writes=[pp])
                h_ = h1s[slot % 2]
                kb.act(h_[:], ph1[:, :], AF.Silu, reads=[ph1], writes=[h_])
                a_ = aT[slot % 2]
                kb.tt("dve", a_[:].rearrange("p h r -> p (h r)"), h_[:], ph3[:, :], OP.mult, reads=[h_, ph3], writes=[a_])

            def Ys(slot):
                b = slot_blk(slot)
                wb = WQ[slot % NS]
                a_ = aT[slot % 2]
                y_ = ysb[slot % 2]
                for nh in range(2):
                    py = kb.bank[5 + nh]
                    for hc in range(4):
                        kb.mm(py[:, :], a_[:, hc, :], wb[:, 2, hc * 1024 + nh * 512:hc * 1024 + (nh + 1) * 512], start=(hc == 0), stop=(hc == 3),
                              reads=[a_, wb], writes=[py])
                    kb.cp("act", y_[:, nh * 512:(nh + 1) * 512], py[:, :], reads=[py], writes=[y_])
                kb.dma(S["YS"].ap()[b * 128:(b + 1) * 128, :], y_[:], reads=[y_], writes=["YS"])

            if "moe_noB" not in self.debug:
                for s_ in range(min(NS, nblk)):
                    Gs(s_)
                Ts(0); Hs(0); Ts(1)
                for s_ in range(nblk):
                    if s_ + 1 < nblk:
                        Hs(s_ + 1)
                    Ys(s_)
                    if s_ + NS < nblk:
                        Gs(s_ + NS)
                    if s_ + 2 < nblk:
                        Ts(s_ + 2)
            kb.barrier()
            es4.close()
            es5 = ExitStack()
            gate2 = kb.sb(es5, [128, DM], F32, "gate2")
            xt = [kb.sb(es5, [128, DM], F32, "xt%d" % i) for i in range(2)]
            y0 = [kb.sb(es5, [128, DM], F32, "y0%d" % i) for i in range(2)]
            y1 = [kb.sb(es5, [128, DM], F32, "y1%d" % i) for i in range(2)]
            xo = [kb.sb(es5, [128, DM], F32, "xo%d" % i) for i in range(2)]
            if last:
                fg = kb.sb(es5, [128, DM], F32, "fg")
                kb.dma(fg[:], I["final_g"].ap().partition_broadcast(128), writes=[fg])
                sq = kb.sb(es5, [128, DM], BF16, "sq"); ss = kb.sb(es5, [128, 1], F32, "ss")
            for t in tiles:
                if "moe_noC" in self.debug:
                    break
                which = 1 if t < 2 else 0
                if t in (0, 2):
                    kb.dma(gate2[:], S["MOD"].ap()[which:which + 1, 5 * DM:6 * DM].partition_broadcast(128), reads=["MOD"], writes=[gate2])
                x_ = xt[t % 2]; a_ = y0[t % 2]; b_ = y1[t % 2]; o_ = xo[t % 2]
                kb.dma(x_[:], X[t * 128:(t + 1) * 128, :], reads=[("X", t)], writes=[x_])
                for k, yy in enumerate((a_, b_)):
                    kb.dma(None, None, reads=[DESTI, "YS"], writes=[yy], q="pool",
                           fn=lambda E: E.indirect_dma_start(out=yy[:], out_offset=None, in_=S["YS"].ap(),
                                                             in_offset=bass.IndirectOffsetOnAxis(DESTI[:, t, k:k + 1], 0)))
                kb.ts("dve", a_[:], a_[:], GW[:, t, 0:1], OP.mult, reads=[a_, GW], writes=[a_])
                kb.stt("dve", a_[:], b_[:], GW[:, t, 1:2], a_[:], OP.mult, OP.add, reads=[b_, GW, a_], writes=[a_])
                if "YMOE" in self.debug:
                    kb.dma(S["YMOE"].ap()[t * 128:(t + 1) * 128, :], a_[:], reads=[a_], writes=["YMOE"], q="pool")
                kb.tt("pool", a_[:], a_[:], gate2[:], OP.mult, reads=[a_, gate2], writes=[a_])
                kb.tt("dve", o_[:], a_[:], x_[:], OP.add, reads=[a_, x_], writes=[o_])
                if not last:
                    kb.dma(X[t * 128:(t + 1) * 128, :], o_[:], reads=[o_], writes=[("X", t)], q="pool")
                else:
                    kb.act(sq[:], o_[:], AF.Square, accum_out=ss[:], reads=[o_], writes=[sq, ss])
                    kb.ts("dve", ss[:], ss[:], 1.0 / DM, OP.mult, EPS, OP.add, reads=[ss], writes=[ss])
                    kb.act(ss[:], ss[:], AF.Sqrt, reads=[ss], writes=[ss])
                    kb.op("dve", lambda E: E.reciprocal(ss[:], ss[:]), reads=[ss], writes=[ss])
                    kb.stt("dve", o_[:], o_[:], ss[:, 0:1], fg[:], OP.mult, OP.mult, reads=[o_, ss, fg], writes=[o_])
                    kb.dma(self.out.ap()[(t - 2) * 128:(t - 1) * 128, :], o_[:], reads=[o_], writes=["OUT"], q="pool")
            kb.barrier()
            es5.close()
        kb.barrier()

    def finish(self, out_keys):
        kb = self.kb
        kb._waits("sp", out_keys, ())
        kb.barrier()


def build(debug=(), layers=DEPTH, stages=("mod", "proj")):
    P1 = _build(debug, layers, stages, None)
    return _build(debug, layers, stages, P1.kb.waited)


def _build(debug, layers, stages, needed):
    P = Prog(debug, needed)
    P.declare()
    P.stage_init()
    P.kb.barrier()
    for L in range(layers):
        if "mod" in stages:
            P.stage_mod(L)
        if "proj" in stages:
            P.stage_proj(L)
        if "s5" in stages:
            P.stage_s5(L)
        if "mla" in stages:
            P.stage_mla(L, L < DEPTH - 1)
        if "hy" in stages:
            P.stage_hyena(L, "l")
            if L < DEPTH - 1:
                P.stage_hyena(L, "c")
        if "moe" in stages:
            P.stage_s5merge(L)
            P.stage_out_moe(L, L == DEPTH - 1)
    P.finish(["OUT"])
    return P


def kernel(**inputs):
    inp = {k: np.asarray(v) for k, v in inputs.items()}
    P = build(debug=(), layers=DEPTH, stages=("mod", "proj", "s5", "mla", "hy", "moe"))
    maps = prep_inputs(inp)
    names = set(P.I.keys())
    in_maps = [{k: v for k, v in m.items() if k in names} for m in maps]
    res = run_bass_kernel_spmd(P.nc, in_maps, core_ids=list(range(8)))
    return np.stack([np.asarray(r["out"], dtype=np.float32) for r in res.results], axis=0)
```

```python
import math
from contextlib import ExitStack
import numpy as np
import ml_dtypes
import concourse.bass as bass
import concourse.mybir as mybir
from concourse.bass_utils import run_bass_kernel_spmd

F32 = mybir.dt.float32
BF16 = mybir.dt.bfloat16
I32 = mybir.dt.int32
U32 = mybir.dt.uint32
AF = mybir.ActivationFunctionType
OP = mybir.AluOpType
AX = mybir.AxisListType

DM = 1024
NCTX = 256
NLAT = 4096
T = NCTX + NLAT
NT = T // 128
DEPTH = 4
EPS = 1e-6
P_IN = 1696
MAGIC = 12582912.0
MLA_SCALE = 1.0 / math.sqrt(96.0)
NE = 32
NBLK = 132


class Buf:
    __slots__ = ("w", "r", "name")

    def __init__(self, name=""):
        self.w = None
        self.r = {}
        self.name = name


class KB:
    NDMA = 10

    def __init__(self, needed=None):
        nc = bass.Bass("TRN2", target_bir_lowering=False)
        self.nc = nc
        self.needed = needed
        self.waited = {e: set() for e in ("pe", "act", "dve", "pool")}
        self.real = {e: 0 for e in ("pe", "act", "dve", "pool")}
        self.vmap = {e: {} for e in ("pe", "act", "dve", "pool")}
        self.eng = {"pe": nc.tensor, "act": nc.scalar, "dve": nc.vector, "pool": nc.gpsimd, "sp": nc.sync}
        self.sem = {}
        self.cnt = {}
        for e in ("pe", "act", "dve", "pool"):
            self.sem[e] = nc.alloc_semaphore("s_" + e)
            self.cnt[e] = 0
        self.known = {e: {} for e in self.eng}
        self.dq = {}
        for q in ("sp", "act", "pool"):
            sems = []
            for i in range(self.NDMA):
                k = ("d", q, i)
                self.sem[k] = nc.alloc_semaphore("d_%s_%d" % (q, i))
                self.cnt[k] = 0
                sems.append(k)
            self.dq[q] = [sems, 0]
        self.nalloc = 0
        self.bufs = {}
        self.ninst = 0
        self.bank = [nc.alloc_psum_tensor("bank%d" % i, [128, 512], F32) for i in range(8)]
        self.bnd_reg = nc.gpsimd.alloc_register("bnd")
        nc.gpsimd.reg_mov(self.bnd_reg, 2 * DEPTH * 32 * 128 - 1)

    def sb(self, es, shape, dt=F32, name="t"):
        self.nalloc += 1
        t = es.enter_context(self.nc.sbuf_tensor("%s_%d" % (name, self.nalloc), list(shape), dt))
        return t

    def dram(self, name, shape, dt=F32, kind="Internal"):
        return self.nc.dram_tensor(name, list(shape), dt, kind=kind)

    def B(self, key):
        if isinstance(key, Buf):
            return key
        k = key if isinstance(key, (str, tuple, int)) else id(key)
        b = self.bufs.get(k)
        if b is None:
            b = Buf(str(k))
            self.bufs[k] = b
        return b

    def _wait(self, e, k, v):
        if isinstance(k, str):
            self.waited[k].add(v)
            rv = v if self.needed is None else self.vmap[k][v]
        else:
            rv = v
        self.eng[e].wait_ge(self.sem[k], rv)
        self.known[e][k] = v

    def _waits(self, e, reads, writes):
        need = {}
        for b in reads:
            b = self.B(b)
            if b.w is not None:
                k, v = b.w
                if need.get(k, 0) < v:
                    need[k] = v
        for b in writes:
            b = self.B(b)
            if b.w is not None:
                k, v = b.w
                if need.get(k, 0) < v:
                    need[k] = v
            for k, v in b.r.items():
                if need.get(k, 0) < v:
                    need[k] = v
        kn = self.known[e]
        for k, v in need.items():
            if k == e and e == "pe":
                continue
            if kn.get(k, 0) >= v:
                continue
            self._wait(e, k, v)

    def _done(self, key, val, reads, writes):
        for b in writes:
            b = self.B(b)
            b.w = (key, val)
            b.r = {}
        for b in reads:
            b = self.B(b)
            if b.r.get(key, 0) < val:
                b.r[key] = val

    def op(self, e, fn, reads=(), writes=()):
        self._waits(e, reads, writes)
        inst = fn(self.eng[e])
        self.cnt[e] += 1
        v = self.cnt[e]
        if self.needed is None or v in self.needed[e]:
            self.real[e] += 1
            self.vmap[e][v] = self.real[e]
            inst.then_inc(self.sem[e], 1)
        self._done(e, v, reads, writes)
        self.ninst += 1
        return inst

    def dma(self, out, in_, reads=(), writes=(), q="sp", fn=None, **kw):
        sems, i = self.dq[q]
        k = sems[i % len(sems)]
        self.dq[q][1] = i + 1
        kn = self.known[q]
        if kn.get(k, 0) < self.cnt[k]:
            self._wait(q, k, self.cnt[k])
        self._waits(q, reads, writes)
        if fn is not None:
            inst = fn(self.eng[q])
        else:
            inst = self.eng[q].dma_start(out=out, in_=in_, **kw)
        self.cnt[k] += 16
        inst.then_inc(self.sem[k], 16)
        self._done(k, self.cnt[k], reads, writes)
        self.ninst += 1
        return inst

    def barrier(self):
        for e in self.eng:
            kn = self.known[e]
            for k, v in self.cnt.items():
                if v == 0 or kn.get(k, 0) >= v:
                    continue
                if k == e and e == "pe":
                    continue
                self._wait(e, k, v)
        self.bufs = {k: b for k, b in self.bufs.items() if isinstance(k, (str, tuple))}
        for b in self.bufs.values():
            b.w = None
            b.r = {}

    def mm(self, out, lhsT, rhs, start=True, stop=True, reads=(), writes=(), **kw):
        return self.op("pe", lambda E: E.matmul(out, lhsT, rhs, start=start, stop=stop, **kw), reads, writes)

    def tr(self, out, in_, ident, reads=(), writes=()):
        return self.op("pe", lambda E: E.transpose(out, in_, ident), reads, writes)

    def act(self, out, in_, func, reads=(), writes=(), **kw):
        return self.op("act", lambda E: E.activation(out, in_, func, **kw), reads, writes)

    def tt(self, e, out, in0, in1, op, reads=(), writes=()):
        return self.op(e, lambda E: E.tensor_tensor(out, in0, in1, op), reads, writes)

    def ts(self, e, out, in0, s1, op0, s2=None, op1=None, reads=(), writes=(), **kw):
        if op1 is None:
            return self.op(e, lambda E: E.tensor_scalar(out, in0, s1, None, op0, **kw), reads, writes)
        return self.op(e, lambda E: E.tensor_scalar(out, in0, s1, s2, op0, op1, **kw), reads, writes)

    def stt(self, e, out, in0, scalar, in1, op0, op1, reads=(), writes=()):
        return self.op(e, lambda E: E.scalar_tensor_tensor(out, in0, scalar, in1, op0, op1), reads, writes)

    def cp(self, e, out, in_, reads=(), writes=()):
        if e == "act":
            return self.op(e, lambda E: E.copy(out, in_), reads, writes)
        return self.op(e, lambda E: E.tensor_copy(out, in_), reads, writes)

    def memset(self, e, ap, val, writes=()):
        return self.op(e, lambda E: E.memset(ap, val), (), writes)


def _rope_tables():
    half = 16
    inv = 10000.0 ** (-np.arange(0, half, 2, dtype=np.float64) / half)
    i = np.arange(NLAT)
    row = (i // 64).astype(np.float64)
    col = (i % 64).astype(np.float64)
    ar = row[None, :] * inv[:, None]
    ac = col[None, :] * inv[:, None]
    cos = np.ones((32, T), np.float64)
    sin = np.zeros((32, T), np.float64)
    cos[0:8, NCTX:] = np.cos(ar); cos[8:16, NCTX:] = np.cos(ar); cos[16:24, NCTX:] = np.cos(ac); cos[24:32, NCTX:] = np.cos(ac)
    sin[0:8, NCTX:] = np.sin(ar); sin[8:16, NCTX:] = np.sin(ar); sin[16:24, NCTX:] = np.sin(ac); sin[24:32, NCTX:] = np.sin(ac)
    return cos.astype(np.float32), sin.astype(np.float32)


def _hy_tables(n):
    N2 = 2 * n
    ntc = n // 128
    F = n + 1
    nj = (F + 127) // 128
    t = np.arange(n, dtype=np.float64)
    tt_ = t / (n - 1)
    bands = 16
    f = np.linspace(1e-4, bands - 1, bands)
    ang = (2.0 * math.pi * t / n)[:, None] * f
    z = np.concatenate([tt_[:, None], np.cos(ang), -np.sin(ang)], axis=-1)
    lo, hi = math.log(1e-2) / 1.5, math.log(1e-2) / 0.3
    deltas = np.abs(np.linspace(lo, hi, 256))
    decay = np.exp(-tt_[None, :] * deltas[:, None])
    fi = np.arange(nj * 128, dtype=np.float64)
    valid = (fi <= n).astype(np.float64)
    ph = 2.0 * math.pi * np.outer(t, fi) / N2
    cosm = np.cos(ph) * valid[None, :]
    sinm = -np.sin(ph) * valid[None, :]
    def fwd_layout(m):
        return m.reshape(ntc, 128, nj, 128).transpose(2, 1, 0, 3)
    dfwd = np.concatenate([fwd_layout(cosm), fwd_layout(sinm)], axis=0).astype(ml_dtypes.bfloat16)
    wf = np.where((fi == 0) | (fi == n), 1.0, 2.0) * valid / N2
    icos = (np.cos(ph) * wf[None, :]).T
    isin = (-np.sin(ph) * wf[None, :]).T
    def inv_layout(m):
        return m.reshape(nj, 128, ntc, 128).transpose(2, 1, 0, 3)
    dinv = np.concatenate([inv_layout(icos), inv_layout(isin)], axis=2).astype(ml_dtypes.bfloat16)
    return (np.ascontiguousarray(z.T.astype(np.float32)), np.ascontiguousarray(decay.astype(np.float32)),
            np.ascontiguousarray(dfwd), np.ascontiguousarray(dinv))


def const_inputs():
    c = {}
    c["iota_e"] = np.ascontiguousarray(np.broadcast_to(np.arange(32, dtype=np.float32)[None, :], (128, 32)))
    c["iota_b"] = np.ascontiguousarray(np.broadcast_to(np.arange(256, dtype=np.float32)[None, :], (128, 256)))
    c["iota_p"] = np.arange(128, dtype=np.float32).reshape(128, 1)
    c["ltri"] = np.triu(np.ones((128, 128), np.float32), k=1).astype(ml_dtypes.bfloat16)
    for nm, n in (("l", NLAT), ("c", NCTX)):
        z, dec, dfwd, dinv = _hy_tables(n)
        c["hy_zpos_" + nm] = z
        c["hy_decay_" + nm] = dec
        c["hy_dfwd_" + nm] = dfwd
        c["hy_dinv_" + nm] = dinv
    c["ident"] = np.eye(128, dtype=np.float32)
    cos, sin = _rope_tables()
    c["rope_cos"] = cos
    c["rope_sin"] = sin
    return c


def prep_inputs(inp):
    cst = const_inputs()
    shared = dict(cst)
    for k in ("ada_w", "ada_b", "norm1_g", "norm2_g", "w_in"):
        shared[k] = np.ascontiguousarray(inp[k], dtype=np.float32)
    def lane(a):
        a = np.asarray(a, np.float32)
        Ld = a.shape[0]
        rest = a.shape[4:]
        a = a.reshape((Ld, 2, 8, 2, 64) + rest)
        nd = a.ndim
        a = a.transpose((0, 3, 4, 1, 2) + tuple(range(5, nd)))
        return np.ascontiguousarray(a.reshape((Ld, 128, 16) + rest))
    shared["s5_lre"] = lane(inp["s5_lambda_re"])
    shared["s5_lim"] = lane(inp["s5_lambda_im"])
    shared["s5_ldt"] = lane(np.broadcast_to(np.asarray(inp["s5_log_dt"], np.float32)[..., None], (DEPTH, 2, 16, 64)))
    shared["s5_bre"] = lane(inp["s5_b_re"])
    shared["s5_bim"] = lane(inp["s5_b_im"])
    shared["s5_cre"] = lane(np.asarray(inp["s5_c_re"], np.float32).transpose(0, 1, 2, 4, 3))
    shared["s5_cim"] = lane(np.asarray(inp["s5_c_im"], np.float32).transpose(0, 1, 2, 4, 3))
    shared["s5_dcol"] = np.ascontiguousarray(np.asarray(inp["s5_d"], np.float32).reshape(DEPTH, 2, 128).transpose(0, 2, 1))
    shared["s5_glu_w"] = np.ascontiguousarray(inp["s5_glu_w"], dtype=np.float32)
    shared["mla_qg"] = np.ascontiguousarray(np.asarray(inp["mla_q_norm_g"], np.float32).reshape(DEPTH, 3, 128).transpose(0, 2, 1))
    shared["mla_kvg"] = np.ascontiguousarray(np.asarray(inp["mla_kv_norm_g"], np.float32).reshape(DEPTH, 2, 128).transpose(0, 2, 1))
    shared["mla_w_uq"] = np.ascontiguousarray(inp["mla_w_uq"], dtype=np.float32)
    shared["mla_w_ukv"] = np.ascontiguousarray(inp["mla_w_ukv"], dtype=np.float32)
    shared["mix_norm_g"] = np.ascontiguousarray(inp["mix_norm_g"], dtype=np.float32)
    shared["hy_sw"] = np.ascontiguousarray(np.asarray(inp["hy_short_w"], np.float32).reshape(DEPTH, 3, 6, 128).transpose(0, 3, 2, 1))
    shared["hy_sb"] = np.ascontiguousarray(np.asarray(inp["hy_short_b"], np.float32).reshape(DEPTH, 6, 128).transpose(0, 2, 1))
    shared["hy_cols"] = np.ascontiguousarray(np.stack([inp["hy_f_b1"], inp["hy_f_b2"], inp["hy_f_freq"][:, 0], inp["hy_f_freq"][:, 1]], axis=-1), dtype=np.float32)
    shared["hy_b3"] = np.ascontiguousarray(np.asarray(inp["hy_f_b3"], np.float32).reshape(DEPTH, 8, 128).transpose(0, 2, 1))
    for k in ("hy_f_w1", "hy_f_w2", "hy_f_w3", "hy_bias"):
        shared[k] = np.ascontiguousarray(inp[k], dtype=np.float32)
    shared["mix_g_s5col"] = np.ascontiguousarray(np.asarray(inp["mix_norm_g"], np.float32)[:, 0:256].reshape(DEPTH, 2, 128).transpose(0, 2, 1))
    shared["w_out"] = np.ascontiguousarray(inp["w_out"], dtype=np.float32)
    shared["moe_wr"] = np.ascontiguousarray(np.concatenate([inp["moe_w_group"], inp["moe_w_expert"]], axis=-1), dtype=np.float32)
    shared["moe_w1"] = np.ascontiguousarray(np.asarray(inp["moe_w1"], np.float32).reshape(DEPTH, 32, 8, 128, 512).transpose(0, 1, 3, 2, 4)).reshape(DEPTH * 32 * 128, 4096)
    shared["moe_w3"] = np.ascontiguousarray(np.asarray(inp["moe_w3"], np.float32).reshape(DEPTH, 32, 8, 128, 512).transpose(0, 1, 3, 2, 4)).reshape(DEPTH * 32 * 128, 4096)
    shared["moe_w2"] = np.ascontiguousarray(np.asarray(inp["moe_w2"], np.float32).reshape(DEPTH, 32, 4, 128, 1024).transpose(0, 1, 3, 2, 4)).reshape(DEPTH * 32 * 128, 4096)
    shared["final_g"] = np.ascontiguousarray(np.asarray(inp["final_g"], np.float32).reshape(1, DM))
    maps = []
    for b in range(8):
        m = dict(shared)
        m["x"] = np.ascontiguousarray(inp["x"][b])
        m["ctx"] = np.ascontiguousarray(inp["ctx"][b])
        cc = np.stack([inp["c"][b], inp["c_ctx"]], axis=0)
        m["cc"] = np.ascontiguousarray(cc.reshape(2, 8, 128).transpose(2, 1, 0))
        maps.append(m)
    return maps


class Prog:
    def __init__(self, debug=(), needed=None):
        self.kb = KB(needed)
        self.nc = self.kb.nc
        self.debug = set(debug)
        self.I = {}
        self.S = {}
        self.outs = []

    def inp(self, name, shape, dt=F32):
        t = self.nc.dram_tensor(name, list(shape), dt, kind="ExternalInput")
        self.I[name] = t
        return t

    def scratch(self, name, shape, dt=F32):
        kind = "ExternalOutput" if name in self.debug else "Internal"
        t = self.nc.dram_tensor(name, list(shape), dt, kind=kind)
        self.S[name] = t
        if kind == "ExternalOutput":
            self.outs.append(name)
        return t

    def declare(self):
        self.inp("x", [NLAT, DM]); self.inp("ctx", [NCTX, DM]); self.inp("cc", [128, 8, 2])
        self.inp("ada_w", [DEPTH, DM, 6 * DM]); self.inp("ada_b", [DEPTH, 6 * DM])
        self.inp("norm1_g", [DEPTH, DM]); self.inp("norm2_g", [DEPTH, DM])
        self.inp("w_in", [DEPTH, DM, P_IN])
        self.inp("ident", [128, 128]); self.inp("rope_cos", [32, T]); self.inp("rope_sin", [32, T])
        for nm in ("s5_lre", "s5_lim", "s5_ldt"):
            self.inp(nm, [DEPTH, 128, 16])
        for nm in ("s5_bre", "s5_bim", "s5_cre", "s5_cim"):
            self.inp(nm, [DEPTH, 128, 16, 16])
        self.inp("s5_dcol", [DEPTH, 128, 2]); self.inp("s5_glu_w", [DEPTH, 256, 256])
        self.inp("mla_qg", [DEPTH, 128, 3]); self.inp("mla_kvg", [DEPTH, 128, 2])
        self.inp("mla_w_uq", [DEPTH, 384, 768]); self.inp("mla_w_ukv", [DEPTH, 256, 1024])
        self.inp("mix_norm_g", [DEPTH, DM])
        self.scratch("MT", [DM, T], BF16)
        if "MLAO" in self.debug:
            self.scratch("MLAO", [T, 512])
        for nm, n in (("l", NLAT), ("c", NCTX)):
            nj = (n + 1 + 127) // 128
            self.inp("hy_zpos_" + nm, [33, n]); self.inp("hy_decay_" + nm, [256, n])
            self.inp("hy_dfwd_" + nm, [2 * nj, 128, n // 128, 128], BF16); self.inp("hy_dinv_" + nm, [n // 128, 128, 2 * nj, 128], BF16)
            self.scratch("KSPEC_" + nm, [nj, 2, 128, 512])
        self.inp("hy_sw", [DEPTH, 128, 6, 3]); self.inp("hy_sb", [DEPTH, 128, 6]); self.inp("hy_cols", [DEPTH, 64, 4]); self.inp("hy_b3", [DEPTH, 128, 8])
        self.inp("hy_f_w1", [DEPTH, 33, 64]); self.inp("hy_f_w2", [DEPTH, 64, 64]); self.inp("hy_f_w3", [DEPTH, 64, 1024]); self.inp("hy_bias", [DEPTH, 2, 256])
        self.scratch("HZTM", [T, 768]); self.scratch("HY1", [T, 256])
        if "HYO" in self.debug:
            self.scratch("HYO", [T, 256])
        self.inp("iota_e", [128, 32]); self.inp("iota_b", [128, 256]); self.inp("iota_p", [128, 1]); self.inp("ltri", [128, 128], BF16)
        self.inp("mix_g_s5col", [DEPTH, 128, 2])
        self.inp("w_out", [DEPTH, DM, DM]); self.inp("moe_wr", [DEPTH, DM, 36])
        for nm_ in ("moe_w1", "moe_w3", "moe_w2"):
            self.inp(nm_, [DEPTH * 32 * 128, 4096])
        self.inp("final_g", [1, DM])
        self.scratch("FB", [T, DM], BF16); self.scratch("XS", [NBLK * 128, DM], BF16); self.scratch("YS", [NBLK * 128, DM])
        if "XMID" in self.debug:
            self.scratch("XMID", [T, DM])
        if "YMOE" in self.debug:
            self.scratch("YMOE", [T, DM])
        if "DBGR" in self.debug:
            self.scratch("DBGR", [128, NT * 6 + 64])
        self.out = self.nc.dram_tensor("out", [NLAT, DM], F32, kind="ExternalOutput")
        self.scratch("S5T", [256, T])
        self.scratch("X", [T, DM])
        self.scratch("MOD", [2, 6 * DM])
        self.scratch("UT", [256, T]); self.scratch("CQT", [384, T]); self.scratch("CKVT", [256, T])
        self.scratch("KRT", [32, T]); self.scratch("HZT", [768, T])
        if "HL" in self.debug:
            self.scratch("HL", [T, DM])

    def stage_init(self):
        kb = self.kb
        X = self.S["X"]
        kb.dma(X.ap()[0:NCTX, :], self.I["ctx"].ap(), writes=[("X", 0), ("X", 1)])
        for j in range(4):
            kb.dma(X.ap()[NCTX + j * 1024: NCTX + (j + 1) * 1024, :], self.I["x"].ap()[j * 1024:(j + 1) * 1024, :],
                   writes=[("X", 2 + 8 * j + i) for i in range(8)])

    def stage_mod(self, L):
        kb = self.kb
        with ExitStack() as es:
            cc = kb.sb(es, [128, 8, 2], F32, "cc")
            act = kb.sb(es, [128, 8, 2], F32, "act")
            bb = kb.sb(es, [2, 6 * DM], F32, "adab")
            mod = kb.sb(es, [2, 6 * DM], F32, "mod")
            wt = [kb.sb(es, [128, 8, 512], F32, "adaw%d" % i) for i in range(2)]
            kb.dma(cc[:], self.I["cc"].ap(), writes=[cc])
            kb.dma(bb[:], self.I["ada_b"].ap()[L:L + 1, :].partition_broadcast(2), writes=[bb])
            kb.act(act[:], cc[:], AF.Silu, reads=[cc], writes=[act])
            wv = self.I["ada_w"].ap()[L].rearrange("(kc p) n -> p kc n", p=128)
            for nb in range(12):
                w = wt[nb % 2]
                kb.dma(w[:], wv[:, :, nb * 512:(nb + 1) * 512], writes=[w])
                ps = kb.bank[nb % 2]
                for kc in range(8):
                    kb.mm(ps[0:2, :], act[:, kc, :], w[:, kc, :], start=(kc == 0), stop=(kc == 7),
                          reads=[act, w], writes=[ps])
                kb.tt("dve", mod[:, nb * 512:(nb + 1) * 512], ps[0:2, :], bb[:, nb * 512:(nb + 1) * 512], OP.add,
                      reads=[ps, bb], writes=[mod])
            kb.dma(self.S["MOD"].ap(), mod[:], reads=[mod], writes=["MOD"])
        kb.barrier()

    def load_mod_bc(self, es_tiles, L, which, idx_g, g_name):
        kb = self.kb
        G, S, tmp = es_tiles
        MOD = self.S["MOD"].ap()
        kb.dma(S[:], MOD[which:which + 1, idx_g * DM:(idx_g + 1) * DM].partition_broadcast(128), reads=["MOD"], writes=[S])
        kb.dma(tmp[:], MOD[which:which + 1, (idx_g + 1) * DM:(idx_g + 2) * DM].partition_broadcast(128), reads=["MOD"], writes=[tmp])
        kb.dma(G[:], self.I[g_name].ap()[L:L + 1, :].partition_broadcast(128), writes=[G])
        kb.stt("dve", G[:], tmp[:], 1.0, G[:], OP.add, OP.mult, reads=[tmp, G], writes=[G])

    def rms_modulate(self, xt, G, S, sq, ss, rstd, tmp, out, out2=None):
        kb = self.kb
        kb.act(sq[:], xt[:], AF.Square, accum_out=ss[:], reads=[xt], writes=[sq, ss])
        kb.ts("dve", rstd[:], ss[:], 1.0 / DM, OP.mult, EPS, OP.add, reads=[ss], writes=[rstd])
        kb.act(rstd[:], rstd[:], AF.Sqrt, reads=[rstd], writes=[rstd])
        kb.op("dve", lambda E: E.reciprocal(rstd[:], rstd[:]), reads=[rstd], writes=[rstd])
        kb.stt("dve", tmp[:], xt[:], rstd[:, 0:1], G[:], OP.mult, OP.mult, reads=[xt, rstd, G], writes=[tmp])
        kb.tt("pool", out[:], tmp[:], S[:], OP.add, reads=[tmp, S], writes=[out])
        if out2 is not None:
            kb.cp("act", out2[:], out[:], reads=[out], writes=[out2])

    def stage_proj(self, L):
        kb = self.kb
        X = self.S["X"].ap()
        with ExitStack() as es:
            ident_f = kb.sb(es, [128, 128], F32, "identf")
            ident = kb.sb(es, [128, 128], BF16, "identb")
            kb.dma(ident_f[:], self.I["ident"].ap(), writes=[ident_f])
            kb.cp("dve", ident[:], ident_f[:], reads=[ident_f], writes=[ident])
            wf = kb.sb(es, [128, 8, P_IN], F32, "winf")
            wb = kb.sb(es, [128, 8, P_IN + 32], BF16, "winb")
            kb.dma(wf[:], self.I["w_in"].ap()[L].rearrange("(kc p) n -> p kc n", p=128), writes=[wf])
            kb.cp("act", wb[:, :, 0:P_IN], wf[:], reads=[wf], writes=[wb])
            K0 = 896
            kb.ts("dve", wb[:, :, P_IN + 0:P_IN + 8], wf[:, :, K0 + 8:K0 + 16], -1.0, OP.mult, reads=[wf], writes=[wb])
            kb.cp("dve", wb[:, :, P_IN + 8:P_IN + 16], wf[:, :, K0 + 0:K0 + 8], reads=[wf], writes=[wb])
            kb.ts("dve", wb[:, :, P_IN + 16:P_IN + 24], wf[:, :, K0 + 24:K0 + 32], -1.0, OP.mult, reads=[wf], writes=[wb])
            kb.cp("dve", wb[:, :, P_IN + 24:P_IN + 32], wf[:, :, K0 + 16:K0 + 24], reads=[wf], writes=[wb])
            rc = kb.sb(es, [32, T], F32, "ropec")
            rs = kb.sb(es, [32, T], F32, "ropes")
            kb.dma(rc[:], self.I["rope_cos"].ap(), writes=[rc])
            kb.dma(rs[:], self.I["rope_sin"].ap(), writes=[rs])
            G = kb.sb(es, [128, DM], F32, "G"); S = kb.sb(es, [128, DM], F32, "S"); tmpb = kb.sb(es, [128, DM], F32, "tmpb")
            xt = [kb.sb(es, [128, DM], F32, "xt%d" % i) for i in range(2)]
            sq2 = [kb.sb(es, [128, DM], BF16, "sq%d" % i) for i in range(2)]
            ss2 = [kb.sb(es, [128, 1], F32, "ss%d" % i) for i in range(2)]; rstd2 = [kb.sb(es, [128, 1], F32, "rstd%d" % i) for i in range(2)]
            tmp2 = [kb.sb(es, [128, DM], F32, "tmp%d" % i) for i in range(2)]
            hb = [kb.sb(es, [128, DM], BF16, "hb%d" % i) for i in range(2)]
            hf = kb.sb(es, [128, DM], F32, "hf") if "HL" in self.debug else None
            hT = [kb.sb(es, [128, 8, 512], BF16, "hT%d" % i) for i in range(2)]
            stg = [kb.sb(es, [128, 512], F32, "stg%d" % i) for i in range(3)]
            kr1 = kb.sb(es, [32, 512], F32, "kr1")
            nstg = 0
            chunks = []
            for i in range(2):
                chunks.append(("UT", i * 128, i * 128, 128))
            for i in range(3):
                chunks.append(("CQT", i * 128, 256 + i * 128, 128))
            for i in range(2):
                chunks.append(("CKVT", i * 128, 640 + i * 128, 128))
            for i in range(6):
                chunks.append(("HZT", i * 128, 928 + i * 128, 128))
            blocks = [(0, 2)] + [(2 + 4 * i, 4) for i in range(8)]
            nmm = 0
            for bi, (t0, ntl) in enumerate(blocks):
                if bi == 0:
                    self.load_mod_bc((G, S, tmpb), L, 1, 0, "norm1_g")
                elif bi == 1:
                    self.load_mod_bc((G, S, tmpb), L, 0, 0, "norm1_g")
                hTb = hT[bi % 2]
                nb = ntl * 128
                c0 = t0 * 128
                for j in range(ntl):
                    t = t0 + j
                    x_ = xt[t % 2]
                    kb.dma(x_[:], X[t * 128:(t + 1) * 128, :], reads=[("X", t)], writes=[x_])
                    h_ = hb[t % 2]
                    sq, ss, rstd, tmp = sq2[t % 2], ss2[t % 2], rstd2[t % 2], tmp2[t % 2]
                    if hf is not None:
                        self.rms_modulate(x_, G, S, sq, ss, rstd, tmp, hf, h_)
                        kb.dma(self.S["HL"].ap()[t * 128:(t + 1) * 128, :], hf[:], reads=[hf], writes=["HL"])
                    else:
                        self.rms_modulate(x_, G, S, sq, ss, rstd, tmp, h_)
                    pb = kb.bank[2 + (t % 2)]
                    pbv = pb[:].bitcast(BF16)
                    for kc in range(8):
                        kb.tr(pbv[:, kc * 128:(kc + 1) * 128], h_[:, kc * 128:(kc + 1) * 128], ident[:],
                              reads=[h_, ident], writes=[pb])
                    kb.cp("dve" if t % 2 else "act", hTb[:, :, j * 128:(j + 1) * 128],
                          pbv.rearrange("p (k n) -> p k n", k=8), reads=[pb], writes=[hTb])
                for (dn, r0, col0, M) in chunks:
                    ps = kb.bank[4 + (nmm % 2)]
                    nmm += 1
                    for kc in range(8):
                        kb.mm(ps[0:M, 0:nb], wb[:, kc, col0:col0 + M], hTb[:, kc, 0:nb], start=(kc == 0), stop=(kc == 7),
                              reads=[wb, hTb], writes=[ps])
                    st = stg[nstg % 3]
                    nstg += 1
                    kb.cp("act" if nstg % 2 else "dve", st[0:M, 0:nb], ps[0:M, 0:nb], reads=[ps], writes=[st])
                    kb.dma(self.S[dn].ap()[r0:r0 + M, c0:c0 + nb], st[0:M, 0:nb], reads=[st], writes=[(dn, bi)], q="pool")
                ps = kb.bank[6]
                ps2 = kb.bank[7]
                for kc in range(8):
                    kb.mm(ps[0:32, 0:nb], wb[:, kc, 896:928], hTb[:, kc, 0:nb], start=(kc == 0), stop=(kc == 7),
                          reads=[wb, hTb], writes=[ps])
                for kc in range(8):
                    kb.mm(ps2[0:32, 0:nb], wb[:, kc, P_IN:P_IN + 32], hTb[:, kc, 0:nb], start=(kc == 0), stop=(kc == 7),
                          reads=[wb, hTb], writes=[ps2])
                st = stg[nstg % 3]
                nstg += 1
                kb.tt("dve", kr1[:, 0:nb], ps[0:32, 0:nb], rc[:, c0:c0 + nb], OP.mult, reads=[ps, rc], writes=[kr1])
                kb.tt("dve", st[0:32, 0:nb], ps2[0:32, 0:nb], rs[:, c0:c0 + nb], OP.mult, reads=[ps2, rs], writes=[st])
                kb.tt("dve", st[0:32, 0:nb], st[0:32, 0:nb], kr1[:, 0:nb], OP.add, reads=[st, kr1], writes=[st])
                kb.dma(self.S["KRT"].ap()[:, c0:c0 + nb], st[0:32, 0:nb], reads=[st], writes=[("KRT", bi)], q="pool")
        kb.barrier()


    def sin_rr(self, e, out, in_, add, t1, t2):
        kb = self.kb
        kb.ts(e, t1, in_, add, OP.add, reads=[in_.tensor], writes=[t1.tensor])
        kb.ts(e, t2, t1, 1.0 / (2 * math.pi), OP.mult, MAGIC, OP.add, reads=[t1.tensor], writes=[t2.tensor])
        kb.ts(e, t2, t2, -MAGIC, OP.add, -2 * math.pi, OP.mult, reads=[t2.tensor], writes=[t2.tensor])
        kb.tt(e, t1, t1, t2, OP.add, reads=[t1.tensor, t2.tensor], writes=[t1.tensor])
        kb.ts(e, t1, t1, math.pi, OP.min, -math.pi, OP.max, reads=[t1.tensor], writes=[t1.tensor])
        kb.act(out, t1, AF.Sin, reads=[t1.tensor], writes=[out.tensor])

    def stage_s5(self, L):
        kb = self.kb
        I = self.I
        blocks = [(0, 256)] + [(256 + 512 * i, 512) for i in range(8)]
        with ExitStack() as es:
            ident_f = kb.sb(es, [128, 128], F32, "identf")
            kb.dma(ident_f[:], I["ident"].ap(), writes=[ident_f])
            sm = {}
            for nm in ("lre", "lim", "ldt"):
                sm[nm] = kb.sb(es, [128, 16], F32, nm)
                kb.dma(sm[nm][:], I["s5_" + nm].ap()[L], writes=[sm[nm]])
            for nm in ("dt", "a", "th", "r", "cs", "sn", "t1", "t2", "rc", "rsn", "den", "cre", "cim", "x1", "x2"):
                sm[nm] = kb.sb(es, [128, 16], F32, nm)
            big = {}
            for nm in ("bre", "bim", "cre", "cim"):
                big[nm] = kb.sb(es, [128, 16, 16], F32, "s5" + nm)
                kb.dma(big[nm][:], I["s5_" + nm].ap()[L], writes=[big[nm]])
            A = lambda nm: sm[nm][:]
            kb.act(A("dt"), A("ldt"), AF.Exp, reads=[sm["ldt"]], writes=[sm["dt"]])
            kb.tt("dve", A("a"), A("lre"), A("dt"), OP.mult, reads=[sm["lre"], sm["dt"]], writes=[sm["a"]])
            kb.tt("dve", A("th"), A("lim"), A("dt"), OP.mult, reads=[sm["lim"], sm["dt"]], writes=[sm["th"]])
            kb.act(A("r"), A("a"), AF.Exp, reads=[sm["a"]], writes=[sm["r"]])
            self.sin_rr("dve", A("sn"), A("th"), 0.0, A("t1"), A("t2"))
            self.sin_rr("dve", A("cs"), A("th"), math.pi / 2, A("t1"), A("t2"))
            kb.tt("dve", A("rc"), A("r"), A("cs"), OP.mult, reads=[sm["r"], sm["cs"]], writes=[sm["rc"]])
            kb.tt("dve", A("rsn"), A("r"), A("sn"), OP.mult, reads=[sm["r"], sm["sn"]], writes=[sm["rsn"]])
            kb.ts("dve", A("rc"), A("rc"), -1.0, OP.add, reads=[sm["rc"]], writes=[sm["rc"]])
            kb.tt("dve", A("den"), A("lre"), A("lre"), OP.mult, reads=[sm["lre"]], writes=[sm["den"]])
            kb.tt("dve", A("x1"), A("lim"), A("lim"), OP.mult, reads=[sm["lim"]], writes=[sm["x1"]])
            kb.tt("dve", A("den"), A("den"), A("x1"), OP.add, reads=[sm["den"], sm["x1"]], writes=[sm["den"]])
            kb.op("dve", lambda E: E.reciprocal(A("den"), A("den")), reads=[sm["den"]], writes=[sm["den"]])
            kb.tt("dve", A("x1"), A("rc"), A("lre"), OP.mult, reads=[sm["rc"], sm["lre"]], writes=[sm["x1"]])
            kb.tt("dve", A("x2"), A("rsn"), A("lim"), OP.mult, reads=[sm["rsn"], sm["lim"]], writes=[sm["x2"]])
            kb.tt("dve", A("x1"), A("x1"), A("x2"), OP.add, reads=[sm["x1"], sm["x2"]], writes=[sm["x1"]])
            kb.tt("dve", A("cre"), A("x1"), A("den"), OP.mult, reads=[sm["x1"], sm["den"]], writes=[sm["cre"]])
            kb.tt("dve", A("x1"), A("rsn"), A("lre"), OP.mult, reads=[sm["rsn"], sm["lre"]], writes=[sm["x1"]])
            kb.tt("dve", A("x2"), A("rc"), A("lim"), OP.mult, reads=[sm["rc"], sm["lim"]], writes=[sm["x2"]])
            kb.tt("dve", A("x1"), A("x1"), A("x2"), OP.subtract, reads=[sm["x1"], sm["x2"]], writes=[sm["x1"]])
            kb.tt("dve", A("cim"), A("x1"), A("den"), OP.mult, reads=[sm["x1"], sm["den"]], writes=[sm["cim"]])
            ub = kb.sb(es, [128, 2, T], BF16, "ub")
            yacc = kb.sb(es, [128, 2, T], F32, "yacc")
            Er = kb.sb(es, [128, T], F32, "Er"); Ei = kb.sb(es, [128, T], F32, "Ei")
            for c, stg_ in enumerate((Er, Ei)):
                kb.dma(stg_[:], self.S["UT"].ap()[c * 128:(c + 1) * 128, :], reads=[("UT", i) for i in range(9)], writes=[stg_])
                kb.cp("act", ub[:, c, :], stg_[:], reads=[stg_], writes=[ub])
            esm = ExitStack()
            Ebr = [kb.sb(esm, [128, T], BF16, "Ebr%d" % i) for i in range(2)]
            Ebi = [kb.sb(esm, [128, T], BF16, "Ebi%d" % i) for i in range(2)]
            bur = kb.sb(esm, [128, T], BF16, "bur"); bui = kb.sb(esm, [128, T], BF16, "bui")
            vr = kb.sb(esm, [128, T], BF16, "vr"); vi = kb.sb(esm, [128, T], BF16, "vi")
            m1 = kb.sb(esm, [128, T], BF16, "m1"); m2 = kb.sb(esm, [128, T], BF16, "m2")
            ZB = [kb.sb(esm, [128, 128], F32, "ZB%d" % i) for i in range(2)]
            LBs = [[kb.sb(esm, [128, 128], BF16, "LB%d_%d" % (p_, i)) for i in range(2)] for p_ in range(2)]
            LCs = [[kb.sb(esm, [128, 128], BF16, "LC%d_%d" % (p_, i)) for i in range(3)] for p_ in range(2)]
            ytmp = [kb.sb(esm, [128, 512], F32, "ytmp%d" % i) for i in range(2)]
            bt = kb.sb(esm, [128, 16], F32, "bt")
            etmp = kb.sb(esm, [128, 2048], F32, "etmp"); etmp2 = kb.sb(esm, [128, 2048], F32, "etmp2")
            NPW = 13
            WR = kb.sb(esm, [128, NPW, 16], F32, "WR"); WI = kb.sb(esm, [128, NPW, 16], F32, "WI"); wt = kb.sb(esm, [128, 16], F32, "wt")
            kb.cp("dve", WR[:, 0, :], sm["cs"][:], reads=[sm["cs"]], writes=[WR])
            kb.cp("dve", WI[:, 0, :], sm["sn"][:], reads=[sm["sn"]], writes=[WI])
            for k in range(1, NPW):
                kb.tt("dve", wt[:], WI[:, k - 1, :], WI[:, k - 1, :], OP.mult, reads=[WI], writes=[wt])
                kb.tt("dve", WI[:, k, :], WR[:, k - 1, :], WI[:, k - 1, :], OP.mult, reads=[WR, WI], writes=[WI])
                kb.ts("dve", WI[:, k, :], WI[:, k, :], 2.0, OP.mult, reads=[WI], writes=[WI])
                kb.tt("dve", WR[:, k, :], WR[:, k - 1, :], WR[:, k - 1, :], OP.mult, reads=[WR], writes=[WR])
                kb.tt("dve", WR[:, k, :], WR[:, k, :], wt[:], OP.subtract, reads=[WR, wt], writes=[WR])
            first_in_chunk = {0: True, 1: True}

            def prep(lt):
                d, gp = lt // 8, lt % 8
                c0 = 32 * (gp % 4)
                LB = LBs[lt % 2]; LC = LCs[lt % 2]
                for ri, (nm_a, nm_b, op2) in enumerate((("bre", "bim", OP.subtract), ("bim", "bre", OP.add))):
                    Z = ZB[ri]
                    kb.memset("pool", Z[:], 0.0, writes=[Z])
                    kb.ts("pool", bt[:], big[nm_b][:, lt, :], sm["cim"][:, lt:lt + 1], OP.mult, reads=[big[nm_b], sm["cim"]], writes=[bt])
                    for gl in range(2):
                        ps_ = slice(gl * 64, gl * 64 + 64)
                        kb.ts("pool", Z[ps_, c0 + gl * 16:c0 + gl * 16 + 16], big[nm_a][ps_, lt, :], sm["cre"][ps_, lt:lt + 1], OP.mult,
                              reads=[big[nm_a], sm["cre"]], writes=[Z])
                        kb.tt("pool", Z[ps_, c0 + gl * 16:c0 + gl * 16 + 16], Z[ps_, c0 + gl * 16:c0 + gl * 16 + 16], bt[ps_, :], op2,
                              reads=[Z, bt], writes=[Z])
                    pb = kb.bank[6 + ri]
                    kb.tr(pb[:, 0:128], Z[:], ident_f[:], reads=[Z, ident_f], writes=[pb])
                    kb.cp("pool" if False else "act", LB[ri][:], pb[:, 0:128], reads=[pb], writes=[LB[ri]])
                for ri, (nm, sgn) in enumerate((("cre", 1.0), ("cim", -1.0), ("cre", -1.0))):
                    kb.memset("pool", LC[ri][:], 0.0, writes=[LC[ri]])
                    for gl in range(2):
                        ps_ = slice(gl * 64, gl * 64 + 64)
                        kb.ts("pool", LC[ri][ps_, c0 + gl * 16:c0 + gl * 16 + 16], big[nm][ps_, lt, :], sgn, OP.mult,
                              reads=[big[nm]], writes=[LC[ri]])

            def tgen(lt):
                d = lt // 8
                lsl = slice(lt, lt + 1)
                kb.memset("pool", Er[:, 0:1], 1.0, writes=[Er])
                kb.memset("pool", Ei[:, 0:1], 0.0, writes=[Ei])
                kb.cp("pool", Er[:, 1:2], sm["cs"][:, lsl], reads=[sm["cs"]], writes=[Er])
                kb.cp("pool", Ei[:, 1:2], sm["sn"][:, lsl], reads=[sm["sn"]], writes=[Ei])
                n = 2
                k = 1
                while n < T:
                    m = min(n, T - n)
                    wr_ = WR[:, k, lsl]; wi_ = WI[:, k, lsl]
                    for o in range(0, m, 2048):
                        mm_ = min(2048, m - o)
                        kb.act(etmp[:, 0:mm_], Ei[:, o:o + mm_], AF.Copy, scale=wi_, reads=[Ei, WI], writes=[etmp])
                        kb.act(etmp2[:, 0:mm_], Er[:, o:o + mm_], AF.Copy, scale=wr_, reads=[Er, WR], writes=[etmp2])
                        kb.tt("pool", Er[:, n + o:n + o + mm_], etmp2[:, 0:mm_], etmp[:, 0:mm_], OP.subtract, reads=[etmp, etmp2, Er], writes=[Er])
                        kb.act(etmp[:, 0:mm_], Ei[:, o:o + mm_], AF.Copy, scale=wr_, reads=[Ei, WR], writes=[etmp])
                        kb.act(etmp2[:, 0:mm_], Er[:, o:o + mm_], AF.Copy, scale=wi_, reads=[Er, WI], writes=[etmp2])
                        kb.tt("pool", Ei[:, n + o:n + o + mm_], etmp2[:, 0:mm_], etmp[:, 0:mm_], OP.add, reads=[etmp, etmp2, Ei], writes=[Ei])
                    n *= 2
                    k += 1
                br_, bi_ = Ebr[lt % 2], Ebi[lt % 2]
                if d == 0:
                    kb.cp("act", br_[:], Er[:], reads=[Er], writes=[br_])
                    kb.cp("act", bi_[:], Ei[:], reads=[Ei], writes=[bi_])
                else:
                    for (Eb, E_) in ((br_, Er), (bi_, Ei)):
                        kb.cp("act", Eb[:, 0:NCTX], E_[:, 0:NCTX][:, ::-1], reads=[E_], writes=[Eb])
                        kb.cp("act", Eb[:, NCTX:T], E_[:, NCTX:T][:, ::-1], reads=[E_], writes=[Eb])

            def drive(lt):
                gp = lt % 8
                ch = gp // 4
                LB = LBs[lt % 2]
                for bi, (s0, nn) in enumerate(blocks):
                    pr = kb.bank[(bi % 2) * 2]
                    pi_ = kb.bank[(bi % 2) * 2 + 1]
                    kb.mm(pr[:, 0:nn], LB[0][:], ub[:, ch, s0:s0 + nn], reads=[LB[0], ub], writes=[pr])
                    kb.mm(pi_[:, 0:nn], LB[1][:], ub[:, ch, s0:s0 + nn], reads=[LB[1], ub], writes=[pi_])
                    kb.cp("act", bur[:, s0:s0 + nn], pr[:, 0:nn], reads=[pr], writes=[bur])
                    kb.cp("act", bui[:, s0:s0 + nn], pi_[:, 0:nn], reads=[pi_], writes=[bui])

            def rot_scan(lt):
                d = lt // 8
                br_, bi_ = Ebr[lt % 2], Ebi[lt % 2]
                kb.tt("dve", m1[:], br_[:], bur[:], OP.mult, reads=[br_, bur], writes=[m1])
                kb.tt("dve", m2[:], bi_[:], bui[:], OP.mult, reads=[bi_, bui], writes=[m2])
                kb.tt("dve", vr[:], m1[:], m2[:], OP.add, reads=[m1, m2], writes=[vr])
                kb.tt("dve", m1[:], br_[:], bui[:], OP.mult, reads=[br_, bui], writes=[m1])
                kb.tt("dve", m2[:], bi_[:], bur[:], OP.mult, reads=[bi_, bur], writes=[m2])
                kb.tt("dve", vi[:], m1[:], m2[:], OP.subtract, reads=[m1, m2], writes=[vi])
                rdec = sm["r"][:, lt:lt + 1]
                for (v_, g_) in ((vr, bur), (vi, bui)):
                    if d == 0:
                        kb.op("dve", lambda E: E.tensor_tensor_scan(g_[:], rdec.to_broadcast([128, T]), v_[:], 0.0, OP.mult, OP.add),
                              reads=[sm["r"], v_], writes=[g_])
                    else:
                        kb.op("dve", lambda E: E.tensor_tensor_scan(g_[:, 0:NCTX][:, ::-1], rdec.to_broadcast([128, NCTX]), v_[:, 0:NCTX][:, ::-1],
                                                                    0.0, OP.mult, OP.add), reads=[sm["r"], v_], writes=[g_])
                        kb.op("dve", lambda E: E.tensor_tensor_scan(g_[:, NCTX:T][:, ::-1], rdec.to_broadcast([128, NLAT]), v_[:, NCTX:T][:, ::-1],
                                                                    g_[:, 0:1], OP.mult, OP.add), reads=[sm["r"], v_, g_], writes=[g_])
                kb.tt("dve", m1[:], br_[:], bur[:], OP.mult, reads=[br_, bur], writes=[m1])
                kb.tt("dve", m2[:], bi_[:], bui[:], OP.mult, reads=[bi_, bui], writes=[m2])
                kb.tt("dve", vr[:], bi_[:], bur[:], OP.mult, reads=[bi_, bur], writes=[vr])
                kb.tt("dve", vi[:], br_[:], bui[:], OP.mult, reads=[br_, bui], writes=[vi])

            def readout(lt):
                gp = lt % 8
                ch = gp // 4
                LC = LCs[lt % 2]
                for bi, (s0, nn) in enumerate(blocks):
                    py = kb.bank[4 + (bi % 2)]
                    kb.mm(py[:, 0:nn], LC[0][:], m1[:, s0:s0 + nn], start=True, stop=False, reads=[LC[0], m1], writes=[py])
                    kb.mm(py[:, 0:nn], LC[2][:], m2[:, s0:s0 + nn], start=False, stop=False, reads=[LC[2], m2], writes=[py])
                    kb.mm(py[:, 0:nn], LC[1][:], vr[:, s0:s0 + nn], start=False, stop=False, reads=[LC[1], vr], writes=[py])
                    kb.mm(py[:, 0:nn], LC[1][:], vi[:, s0:s0 + nn], start=False, stop=True, reads=[LC[1], vi], writes=[py])
                    if first_in_chunk[ch]:
                        kb.cp("act", yacc[:, ch, s0:s0 + nn], py[:, 0:nn], reads=[py], writes=[yacc])
                    else:
                        yt_ = ytmp[bi % 2]
                        kb.cp("act", yt_[:, 0:nn], py[:, 0:nn], reads=[py], writes=[yt_])
                        kb.tt("pool", yacc[:, ch, s0:s0 + nn], yacc[:, ch, s0:s0 + nn], yt_[:, 0:nn], OP.add, reads=[yt_, yacc], writes=[yacc])
                first_in_chunk[ch] = False

            prep(0)
            tgen(0)
            for lt in range(16):
                drive(lt)
                if lt + 1 < 16:
                    prep(lt + 1)
                    tgen(lt + 1)
                rot_scan(lt)
                readout(lt)
            kb.barrier()
            esm.close()
            dcol = kb.sb(es, [128, 2], F32, "dcol")
            kb.dma(dcol[:], I["s5_dcol"].ap()[L], writes=[dcol])
            uf = (Er, Ei)
            mt = [kb.sb(es, [128, 512], F32, "mt%d" % i) for i in range(4)]
            for c in range(2):
                kb.dma(uf[c][:], self.S["UT"].ap()[c * 128:(c + 1) * 128, :], reads=[("UT", i) for i in range(9)], writes=[uf[c]])
            gwf = kb.sb(es, [128, 2, 256], F32, "gwf"); gwb = kb.sb(es, [128, 2, 256], BF16, "gwb")
            kb.dma(gwf[:], I["s5_glu_w"].ap()[L].rearrange("(kc p) n -> p kc n", p=128), writes=[gwf])
            kb.cp("act", gwb[:], gwf[:], reads=[gwf], writes=[gwb])
            yb = kb.sb(es, [128, 2, 512], BF16, "yb")
            yf = kb.sb(es, [128, 2, 512], F32, "yf")
            so = [kb.sb(es, [128, 512], F32, "so%d" % i) for i in range(2)]
            for bi, (s0, nn) in enumerate(blocks):
                m1, m2, m3, m4 = mt
                for c in range(2):
                    kb.stt("dve", yf[:, c, 0:nn], uf[c][:, s0:s0 + nn], dcol[:, c:c + 1], yacc[:, c, s0:s0 + nn], OP.mult, OP.add,
                           reads=[uf[c], dcol, yacc], writes=[yf])
                    kb.tt("pool", m1[:, 0:nn], yf[:, c, 0:nn], yf[:, c, 0:nn], OP.mult, reads=[yf], writes=[m1])
                    kb.ts("pool", m1[:, 0:nn], m1[:, 0:nn], 0.044715, OP.mult, 1.0, OP.add, reads=[m1], writes=[m1])
                    kb.tt("pool", m1[:, 0:nn], m1[:, 0:nn], yf[:, c, 0:nn], OP.mult, reads=[m1, yf], writes=[m1])
                    kb.act(m2[:, 0:nn], m1[:, 0:nn], AF.Sigmoid, scale=1.5957691216057308, reads=[m1], writes=[m2])
                    kb.tt("dve", yf[:, c, 0:nn], yf[:, c, 0:nn], m2[:, 0:nn], OP.mult, reads=[yf, m2], writes=[yf])
                    kb.cp("act", yb[:, c, 0:nn], yf[:, c, 0:nn], reads=[yf], writes=[yb])
                for mo in range(2):
                    pz = kb.bank[mo]
                    for kc in range(2):
                        kb.mm(pz[:, 0:nn], gwb[:, kc, mo * 128:(mo + 1) * 128], yb[:, kc, 0:nn], start=(kc == 0), stop=(kc == 1),
                              reads=[gwb, yb], writes=[pz])
                    kb.act(m3[:, 0:nn], pz[:, 0:nn], AF.Sigmoid, reads=[pz], writes=[m3])
                    so_ = so[mo]
                    kb.tt("dve", so_[:, 0:nn], yf[:, mo, 0:nn], m3[:, 0:nn], OP.mult, reads=[yf, m3], writes=[so_])
                    kb.dma(self.S["S5T"].ap()[mo * 128:(mo + 1) * 128, s0:s0 + nn], so_[:, 0:nn], reads=[so_], writes=[("S5T", bi)], q="pool")
        kb.barrier()


    def stage_mla(self, L, ctx_out):
        kb = self.kb
        I = self.I
        blocks = [(0, 256)] + [(256 + 512 * i, 512) for i in range(8)]
        with ExitStack() as es:
            ident_f = kb.sb(es, [128, 128], F32, "identf")
            ident = kb.sb(es, [128, 128], BF16, "identb")
            ones = kb.sb(es, [128, 128], BF16, "ones")
            kb.dma(ident_f[:], I["ident"].ap(), writes=[ident_f])
            kb.cp("dve", ident[:], ident_f[:], reads=[ident_f], writes=[ident])
            kb.memset("dve", ones[:], 1.0, writes=[ones])
            qg = kb.sb(es, [128, 3], F32, "qg"); kvg = kb.sb(es, [128, 2], F32, "kvg")
            kb.dma(qg[:], I["mla_qg"].ap()[L], writes=[qg]); kb.dma(kvg[:], I["mla_kvg"].ap()[L], writes=[kvg])
            wuq = kb.sb(es, [128, 3, 768], BF16, "wuq")
            wrot = kb.sb(es, [128, 3, 8, 96], BF16, "wrot")
            wukv = kb.sb(es, [128, 2, 1024], BF16, "wukv")
            KT = kb.sb(es, [97, 8, T], BF16, "KT")
            VA = kb.sb(es, [128, NT, 8, 65], BF16, "VA")
            gbc = kb.sb(es, [128, 512], F32, "gbc")
            kb.dma(gbc[:], I["mix_norm_g"].ap()[L:L + 1, 256:768].partition_broadcast(128), writes=[gbc])
            es2 = ExitStack()
            wst = kb.sb(es2, [128, 3, 1024], F32, "wst")
            kb.dma(wst[:, :, 0:768], I["mla_w_uq"].ap()[L].rearrange("(kc p) n -> p kc n", p=128), writes=[wst])
            kb.cp("act", wuq[:], wst[:, :, 0:768], reads=[wst], writes=[wuq])
            kb.memset("pool", wrot[:], 0.0, writes=[wrot])
            wv4 = wst[:, :, 0:768].rearrange("p k (h x) -> p k h x", x=96)
            kb.ts("dve", wrot[:, :, :, 64:72], wv4[:, :, :, 72:80], -1.0, OP.mult, reads=[wst], writes=[wrot])
            kb.cp("dve", wrot[:, :, :, 72:80], wv4[:, :, :, 64:72], reads=[wst], writes=[wrot])
            kb.ts("dve", wrot[:, :, :, 80:88], wv4[:, :, :, 88:96], -1.0, OP.mult, reads=[wst], writes=[wrot])
            kb.cp("dve", wrot[:, :, :, 88:96], wv4[:, :, :, 80:88], reads=[wst], writes=[wrot])
            kb.dma(wst[:, 0:2, :], I["mla_w_ukv"].ap()[L].rearrange("(kc p) n -> p kc n", p=128), reads=[wst], writes=[wst])
            kb.cp("act", wukv[:], wst[:, 0:2, :], reads=[wst], writes=[wukv])
            kb.memset("pool", KT[96:97, :, :], 1.0, writes=[KT])
            kb.memset("pool", VA[:, :, :, 64:65], 1.0, writes=[VA])
            krf = kb.sb(es2, [32, T], F32, "krf"); krb = kb.sb(es2, [32, T], BF16, "krb")
            kb.dma(krf[:], self.S["KRT"].ap(), reads=[("KRT", i) for i in range(9)], writes=[krf])
            kb.cp("act", krb[:], krf[:], reads=[krf], writes=[krb])
            for h in range(8):
                kb.dma(KT[64:96, h, :], krb[:], reads=[krb], writes=[KT])
            kb.barrier()
            es2.close()
            if "mla_stop1" in self.debug:
                return
            rcb = [kb.sb(es, [96, 512], F32, "rcb%d" % i) for i in range(2)]
            rsb = [kb.sb(es, [96, 512], F32, "rsb%d" % i) for i in range(2)]
            xin = [kb.sb(es, [128, 3, 512], F32, "xin%d" % i) for i in range(2)]
            sqb = kb.sb(es, [128, 3, 512], BF16, "sqb")
            sqk = [kb.sb(es, [96, 512], BF16, "sqk%d" % i) for i in range(2)]
            rst = kb.sb(es, [128, 512], F32, "rst")
            xn = kb.sb(es, [128, 3, 512], BF16, "xn")
            xnq = [kb.sb(es, [128, 3, 512], BF16, "xnq%d" % i) for i in range(2)]
            kmax2 = kb.sb(es, [128, 8], F32, "kmax2"); kmb = kb.sb(es, [128, 8], F32, "kmb"); negk = kb.sb(es, [128, 8], F32, "negk")
            kb.memset("dve", kmax2[:], 0.0, writes=[kmax2])

            def rmsnorm_fm(src, nch, nfeat, nn, gcol, dst):
                kb.tt("pool", sqb[:, 0:nch, 0:nn], src[:, 0:nch, 0:nn], src[:, 0:nch, 0:nn], OP.mult, reads=[src], writes=[sqb])
                pn = kb.bank[6]
                for c in range(nch):
                    kb.mm(pn[:, 0:nn], ones[:], sqb[:, c, 0:nn], start=(c == 0), stop=(c == nch - 1), reads=[ones, sqb], writes=[pn])
                kb.ts("dve", rst[:, 0:nn], pn[:, 0:nn], 1.0 / nfeat, OP.mult, EPS, OP.add, reads=[pn], writes=[rst])
                kb.act(rst[:, 0:nn], rst[:, 0:nn], AF.Sqrt, reads=[rst], writes=[rst])
                kb.op("dve", lambda E: E.reciprocal(rst[:, 0:nn], rst[:, 0:nn]), reads=[rst], writes=[rst])
                for c in range(nch):
                    kb.stt("dve", dst[:, c, 0:nn], src[:, c, 0:nn], gcol[:, c:c + 1], rst[:, 0:nn], OP.mult, OP.mult,
                           reads=[src, gcol, rst], writes=[dst])

            for bi, (s0, nn) in enumerate(blocks):
                x_ = xin[bi % 2]
                kb.dma(x_[:, 0:2, 0:nn], self.S["CKVT"].ap().rearrange("(c p) t -> p c t", p=128)[:, :, s0:s0 + nn],
                       reads=[("CKVT", bi)], writes=[x_])
                rmsnorm_fm(x_, 2, 256, nn, kvg, xn)
                for h in range(8):
                    pk = kb.bank[h % 2]
                    for kc in range(2):
                        kb.mm(pk[0:64, 0:nn], wukv[:, kc, h * 128:h * 128 + 64], xn[:, kc, 0:nn], start=(kc == 0), stop=(kc == 1),
                              reads=[wukv, xn], writes=[pk])
                    kb.cp("dve", KT[0:64, h, s0:s0 + nn], pk[0:64, 0:nn], reads=[pk], writes=[KT])
                    kb.tt("pool", sqk[h % 2][0:96, 0:nn], KT[0:96, h, s0:s0 + nn], KT[0:96, h, s0:s0 + nn], OP.mult, reads=[KT], writes=[sqk[h % 2]])
                    pn = kb.bank[6]
                    kb.mm(pn[:, 0:nn], ones[0:96, :], sqk[h % 2][0:96, 0:nn], reads=[ones, sqk[h % 2]], writes=[pn])
                    kb.op("dve", lambda E: E.reduce_max(kmb[:, h:h + 1], pn[:, 0:nn], AX.X), reads=[pn], writes=[kmb])
                    kb.tt("dve", kmax2[:, h:h + 1], kmax2[:, h:h + 1], kmb[:, h:h + 1], OP.max, reads=[kmb, kmax2], writes=[kmax2])
                for j in range(nn // 128):
                    ti = s0 // 128 + j
                    pv = kb.bank[2 + (j % 2)]
                    for kc in range(2):
                        kb.mm(pv[:, :].rearrange("p (h x) -> p h x", x=64), xn[:, kc, j * 128:(j + 1) * 128],
                              wukv[:, kc, :].rearrange("p (h x) -> p h x", x=128)[:, :, 64:128], start=(kc == 0), stop=(kc == 1),
                              reads=[wukv, xn], writes=[pv])
                    kb.cp("act" if j % 2 else "dve", VA[:, ti, :, 0:64], pv[:, :].rearrange("p (h x) -> p h x", x=64), reads=[pv], writes=[VA])
            if "mla_stop2" in self.debug:
                kb.barrier()
                return
            kb.act(negk[:], kmax2[:], AF.Sqrt, reads=[kmax2], writes=[negk])
            kb.ts("dve", negk[:], negk[:], -1.02, OP.mult, reads=[negk], writes=[negk])
            QT = [kb.sb(es, [97, 8, 512], BF16, "QT%d" % i) for i in range(2)]
            PT = [kb.sb(es, [128, 512], BF16, "PT%d" % i) for i in range(6)]
            tq = kb.sb(es, [96, 512], F32, "tq"); tq2 = kb.sb(es, [96, 512], F32, "tq2")
            qn1 = kb.sb(es, [97, 512], F32, "qn1")
            yt = kb.sb(es, [128, 4, 512], F32, "yt")
            ytb = kb.sb(es, [128, 4, 512], BF16, "ytb")
            rec = kb.sb(es, [128, 4], F32, "rec")
            ss4 = kb.sb(es, [128, 4], F32, "ss4")
            mts = kb.sb(es, [128, 4, 512], BF16, "mts")
            npt = 0
            qblocks = [(bi, s0, nn) for bi, (s0, nn) in enumerate(blocks) if not (bi == 0 and not ctx_out)]

            def qphase(bi, s0, nn):
                x_ = xin[bi % 2]
                xq = xnq[bi % 2]
                kb.dma(x_[:, 0:3, 0:nn], self.S["CQT"].ap().rearrange("(c p) t -> p c t", p=128)[:, :, s0:s0 + nn],
                       reads=[("CQT", bi)], writes=[x_])
                rmsnorm_fm(x_, 3, 384, nn, qg, xq)
                Q = QT[bi % 2]
                rc = rcb[bi % 2]; rs = rsb[bi % 2]
                kb.dma(rc[64:96, 0:nn], I["rope_cos"].ap()[:, s0:s0 + nn], writes=[rc])
                kb.dma(rs[64:96, 0:nn], I["rope_sin"].ap()[:, s0:s0 + nn], writes=[rs])
                for h in range(8):
                    pa = kb.bank[6]
                    pb = kb.bank[7]
                    for kc in range(3):
                        kb.mm(pa[0:96, 0:nn], wuq[:, kc, h * 96:(h + 1) * 96], xq[:, kc, 0:nn], start=(kc == 0), stop=(kc == 2),
                              reads=[wuq, xq], writes=[pa])
                    for kc in range(3):
                        kb.mm(pb[0:96, 0:nn], wrot[:, kc, h, :], xq[:, kc, 0:nn], start=(kc == 0), stop=(kc == 2),
                              reads=[wrot, xq], writes=[pb])
                    kb.cp("dve", Q[0:64, h, 0:nn], pa[0:64, 0:nn], reads=[pa], writes=[Q])
                    kb.tt("dve", tq[64:96, 0:nn], pb[64:96, 0:nn], rs[64:96, 0:nn], OP.mult, reads=[pb, rs], writes=[tq])
                    kb.tt("dve", tq2[64:96, 0:nn], pa[64:96, 0:nn], rc[64:96, 0:nn], OP.mult, reads=[pa, rc], writes=[tq2])
                    kb.tt("dve", Q[64:96, h, 0:nn], tq[64:96, 0:nn], tq2[64:96, 0:nn], OP.add, reads=[tq, tq2], writes=[Q])
                    sk = sqk[h % 2]
                    kb.tt("pool", sk[0:96, 0:nn], Q[0:96, h, 0:nn], Q[0:96, h, 0:nn], OP.mult, reads=[Q], writes=[sk])
                    pn = kb.bank[6]
                    kb.mm(pn[:, 0:nn], ones[0:96, :], sk[0:96, 0:nn], reads=[ones, sk], writes=[pn])
                    kb.act(qn1[96:97, 0:nn], pn[96:97, 0:nn], AF.Sqrt, reads=[pn], writes=[qn1])
                    kb.ts("dve", Q[96:97, h, 0:nn], qn1[96:97, 0:nn], negk[96:97, h:h + 1], OP.mult, reads=[qn1, negk], writes=[Q])

            qphase(*qblocks[0])
            for qi, (bi, s0, nn) in enumerate(qblocks):
                nj = nn // 128
                kchunks = list(range(2)) if bi == 0 else list(range(NT))
                Q = QT[bi % 2]
                if qi + 1 < len(qblocks):
                    qphase(*qblocks[qi + 1])
                if "mla_stop3" in self.debug:
                    continue
                SB = [0, 1, 3, 4, 5]
                DEPTH_P = 4
                for h in range(8):
                    pend = []
                    po = kb.bank[2]

                    def emit_pv(ci_, kc_, P__):
                        for j in range(nj):
                            kb.mm(po[:, j * 128:j * 128 + 65], P__[:, j * 128:(j + 1) * 128], VA[:, kc_, h, :], start=(ci_ == 0 and j == 0),
                                  stop=(ci_ == len(kchunks) - 1 and j == nj - 1), reads=[P__, VA], writes=[po], skip_group_check=True)
                    for ci, kc in enumerate(kchunks):
                        pst = kb.bank[SB[ci % 5]]
                        kb.mm(pst[:, 0:nn], KT[0:97, h, kc * 128:(kc + 1) * 128], Q[0:97, h, 0:nn], reads=[KT, Q], writes=[pst])
                        P_ = PT[npt % 6]
                        npt += 1
                        kb.act(P_[:, 0:nn], pst[:, 0:nn], AF.Exp, scale=MLA_SCALE, reads=[pst], writes=[P_])
                        pend.append((ci, kc, P_))
                        if len(pend) >= DEPTH_P:
                            emit_pv(*pend.pop(0))
                    while pend:
                        emit_pv(*pend.pop(0))
                    for j in range(nj):
                        kb.op("dve", lambda E: E.reciprocal(rec[:, j:j + 1], po[:, j * 128 + 64:j * 128 + 65]), reads=[po], writes=[rec])
                        kb.ts("dve", yt[:, j, h * 64:(h + 1) * 64], po[:, j * 128:j * 128 + 64], rec[:, j:j + 1], OP.mult, reads=[po, rec], writes=[yt])
                for j in range(nj):
                    if "MLAO" in self.debug:
                        kb.dma(self.S["MLAO"].ap()[s0 + j * 128:s0 + (j + 1) * 128, :], yt[:, j, :], reads=[yt], writes=["MLAO"], q="pool")
                    kb.act(mts[:, j, :], yt[:, j, :], AF.Square, accum_out=ss4[:, j:j + 1], reads=[yt], writes=[mts, ss4])
                kb.ts("dve", ss4[:, 0:nj], ss4[:, 0:nj], 1.0 / 512, OP.mult, EPS, OP.add, reads=[ss4], writes=[ss4])
                kb.act(ss4[:, 0:nj], ss4[:, 0:nj], AF.Sqrt, reads=[ss4], writes=[ss4])
                kb.op("dve", lambda E: E.reciprocal(ss4[:, 0:nj], ss4[:, 0:nj]), reads=[ss4], writes=[ss4])
                for j in range(nj):
                    kb.stt("dve", ytb[:, j, :], yt[:, j, :], ss4[:, j:j + 1], gbc[:], OP.mult, OP.mult, reads=[yt, ss4, gbc], writes=[ytb])
                    pt_ = kb.bank[6 + (j % 2)]
                    ptv = pt_[:].bitcast(BF16)
                    for fc in range(4):
                        kb.tr(ptv[:, fc * 128:(fc + 1) * 128], ytb[:, j, fc * 128:(fc + 1) * 128], ident[:], reads=[ytb, ident], writes=[pt_])
                    kb.cp("act", mts[:, :, j * 128:(j + 1) * 128], ptv[:, 0:512].rearrange("p (f q) -> p f q", f=4), reads=[pt_], writes=[mts])
                kb.dma(self.S["MT"].ap()[256:768, s0:s0 + nn].rearrange("(f p) t -> p f t", p=128), mts[:, :, 0:nn], reads=[mts],
                       writes=[("MT_mla", bi)], q="pool")
        kb.barrier()


    def stage_hyena(self, L, part):
        kb = self.kb
        I = self.I
        nm = part
        n = NLAT if part == "l" else NCTX
        r0 = NCTX if part == "l" else 0
        ntc = n // 128
        nj = (n + 1 + 127) // 128
        lagblocks = [(i * 512, min(512, n - i * 512)) for i in range((n + 511) // 512)]
        KSP = self.S["KSPEC_" + nm]
        with ExitStack() as es:
            ident_f = kb.sb(es, [128, 128], F32, "identf")
            kb.dma(ident_f[:], I["ident"].ap(), writes=[ident_f])
            w1 = kb.sb(es, [33, 64], F32, "w1"); w2 = kb.sb(es, [64, 64], F32, "w2"); w3 = kb.sb(es, [64, 1024], F32, "w3")
            cols = kb.sb(es, [64, 4], F32, "cols"); bf = kb.sb(es, [64, 2], F32, "bf"); b3 = kb.sb(es, [128, 8], F32, "b3")
            kb.dma(w1[:], I["hy_f_w1"].ap()[L], writes=[w1]); kb.dma(w2[:], I["hy_f_w2"].ap()[L], writes=[w2]); kb.dma(w3[:], I["hy_f_w3"].ap()[L], writes=[w3])
            kb.dma(cols[:], I["hy_cols"].ap()[L], writes=[cols]); kb.dma(b3[:], I["hy_b3"].ap()[L], writes=[b3])
            kb.tt("dve", bf[:, 0:1], cols[:, 0:1], cols[:, 2:3], OP.mult, reads=[cols], writes=[bf])
            kb.tt("dve", bf[:, 1:2], cols[:, 1:2], cols[:, 3:4], OP.mult, reads=[cols], writes=[bf])
            FS = kb.sb(es, [128, ntc, 512], BF16, "FS"); FD = kb.sb(es, [128, ntc, 512], BF16, "FD")
            zp = kb.sb(es, [33, 512], F32, "zp")
            h1 = kb.sb(es, [64, 512], F32, "h1"); h2 = kb.sb(es, [64, 512], F32, "h2")
            t1 = kb.sb(es, [64, 512], F32, "t1"); t2 = kb.sb(es, [64, 512], F32, "t2"); t0 = kb.sb(es, [64, 512], F32, "t0")
            dec = kb.sb(es, [128, 2, 512], F32, "dec")
            ff = kb.sb(es, [128, 512], F32, "ff"); fb = kb.sb(es, [128, 512], F32, "fb")
            fs_ = kb.sb(es, [128, 512], F32, "fs"); fd_ = kb.sb(es, [128, 512], F32, "fd")
            for (l0, ln) in lagblocks:
                kb.dma(zp[:, 0:ln], I["hy_zpos_" + nm].ap()[:, l0:l0 + ln], writes=[zp])
                kb.dma(dec[:, :, 0:ln], I["hy_decay_" + nm].ap().rearrange("(c p) t -> p c t", p=128)[:, :, l0:l0 + ln], writes=[dec])
                p1 = kb.bank[0]
                kb.mm(p1[0:64, 0:ln], w1[:], zp[:, 0:ln], reads=[w1, zp], writes=[p1])
                kb.ts("dve", t0[:, 0:ln], p1[0:64, 0:ln], cols[:, 2:3], OP.mult, bf[:, 0:1], OP.add, reads=[p1, cols, bf], writes=[t0])
                self.sin_rr("dve", h1[:, 0:ln], t0[:, 0:ln], 0.0, t1[:, 0:ln], t2[:, 0:ln])
                p2 = kb.bank[1]
                kb.mm(p2[0:64, 0:ln], w2[:], h1[:, 0:ln], reads=[w2, h1], writes=[p2])
                kb.ts("dve", t0[:, 0:ln], p2[0:64, 0:ln], cols[:, 3:4], OP.mult, bf[:, 1:2], OP.add, reads=[p2, cols, bf], writes=[t0])
                self.sin_rr("dve", h2[:, 0:ln], t0[:, 0:ln], 0.0, t1[:, 0:ln], t2[:, 0:ln])
                for o in range(2):
                    for chf in range(2):
                        jf = o * 2 + chf
                        jb = 4 + o * 2 + chf
                        pf = kb.bank[2]; pbk = kb.bank[3]
                        kb.mm(pf[:, 0:ln], w3[:, jf * 128:(jf + 1) * 128], h2[:, 0:ln], reads=[w3, h2], writes=[pf])
                        kb.mm(pbk[:, 0:ln], w3[:, jb * 128:(jb + 1) * 128], h2[:, 0:ln], reads=[w3, h2], writes=[pbk])
                        kb.stt("dve", ff[:, 0:ln], pf[:, 0:ln], b3[:, jf:jf + 1], dec[:, chf, 0:ln], OP.add, OP.mult, reads=[pf, b3, dec], writes=[ff])
                        kb.stt("dve", fb[:, 0:ln], pbk[:, 0:ln], b3[:, jb:jb + 1], dec[:, chf, 0:ln], OP.add, OP.mult, reads=[pbk, b3, dec], writes=[fb])
                        if l0 == 0:
                            kb.memset("dve", fb[:, 0:1], 0.0, writes=[fb])
                        kb.tt("dve", fs_[:, 0:ln], ff[:, 0:ln], fb[:, 0:ln], OP.add, reads=[ff, fb], writes=[fs_])
                        kb.tt("pool", fd_[:, 0:ln], ff[:, 0:ln], fb[:, 0:ln], OP.subtract, reads=[ff, fb], writes=[fd_])
                        for (src, dst, pbank) in ((fs_, FS, 4), (fd_, FD, 5)):
                            pt_ = kb.bank[pbank]
                            nq = ln // 128
                            for q in range(nq):
                                kb.tr(pt_[:, q * 128:(q + 1) * 128], src[:, q * 128:(q + 1) * 128], ident_f[:], reads=[src, ident_f], writes=[pt_])
                            tc0 = l0 // 128
                            kb.cp("act", dst[:, tc0:tc0 + nq, o * 256 + chf * 128:o * 256 + (chf + 1) * 128],
                                  pt_[:, 0:nq * 128].rearrange("p (q c) -> p q c", c=128), reads=[pt_], writes=[dst])
            Dt = [kb.sb(es, [128, ntc, 128], BF16, "Dt%d" % i) for i in range(4)]
            ks = [kb.sb(es, [128, 2, 512], F32, "ks%d" % i) for i in range(2)]
            nd = 0
            for j in range(nj):
                for ri, (fc, src) in enumerate(((j, FS), (nj + j, FD))):
                    D_ = Dt[nd % 4]
                    nd += 1
                    kb.dma(D_[:], I["hy_dfwd_" + nm].ap()[fc], writes=[D_])
                    pk = kb.bank[ri]
                    for tc in range(ntc):
                        kb.mm(pk[:, :], D_[:, tc, :], src[:, tc, :], start=(tc == 0), stop=(tc == ntc - 1), reads=[D_, src], writes=[pk])
                    kb.cp("act" if ri else "dve", ks[j % 2][:, ri, :], pk[:, :], reads=[pk], writes=[ks[j % 2]])
                kb.dma(KSP.ap()[j].rearrange("r p c -> p r c"), ks[j % 2][:], reads=[ks[j % 2]], writes=["KSPEC_" + nm], q="pool")
        kb.barrier()
        with ExitStack() as es:
            ident_f = kb.sb(es, [128, 128], F32, "identf")
            kb.dma(ident_f[:], I["ident"].ap(), writes=[ident_f])
            sw = kb.sb(es, [128, 6, 3], F32, "sw"); sbb = kb.sb(es, [128, 6], F32, "sbb")
            kb.dma(sw[:], I["hy_sw"].ap()[L], writes=[sw]); kb.dma(sbb[:], I["hy_sb"].ap()[L], writes=[sbb])
            xin = [kb.sb(es, [128, n], F32, "hzx%d" % i) for i in range(2)]
            zz = [kb.sb(es, [128, n], F32, "hzz%d" % i) for i in range(2)]
            tm = [kb.sb(es, [128, 4, 128], F32, "tm%d" % i) for i in range(2)]
            ntm = 0
            for c in range(6):
                x_ = xin[c % 2]; z_ = zz[c % 2]
                kb.dma(x_[:], self.S["HZT"].ap()[c * 128:(c + 1) * 128, r0:r0 + n], reads=[("HZT", i) for i in range(9)], writes=[x_])
                kb.ts("dve", z_[:], x_[:], sw[:, c, 1:2], OP.mult, sbb[:, c:c + 1], OP.add, reads=[x_, sw, sbb], writes=[z_])
                kb.stt("dve", z_[:, 1:n], x_[:, 0:n - 1], sw[:, c, 0:1], z_[:, 1:n], OP.mult, OP.add, reads=[x_, sw, z_], writes=[z_])
                kb.stt("dve", z_[:, 0:n - 1], x_[:, 1:n], sw[:, c, 2:3], z_[:, 0:n - 1], OP.mult, OP.add, reads=[x_, sw, z_], writes=[z_])
                for tg in range(0, ntc, 4):
                    ng = min(4, ntc - tg)
                    pt_ = kb.bank[ntm % 2]
                    t_ = tm[ntm % 2]
                    ntm += 1
                    for q in range(ng):
                        kb.tr(pt_[:, q * 128:(q + 1) * 128], z_[:, (tg + q) * 128:(tg + q + 1) * 128], ident_f[:], reads=[z_, ident_f], writes=[pt_])
                    kb.cp("act" if ntm % 2 else "dve", t_[:, 0:ng, :], pt_[:, 0:ng * 128].rearrange("p (q c) -> p q c", c=128), reads=[pt_], writes=[t_])
                    kb.dma(self.S["HZTM"].ap()[r0 + tg * 128:r0 + (tg + ng) * 128, c * 128:(c + 1) * 128].rearrange("(q p) c -> p q c", p=128),
                           t_[:, 0:ng, :], reads=[t_], writes=["HZTM"], q="pool")
        kb.barrier()
        with ExitStack() as es:
            ident = kb.sb(es, [128, 128], BF16, "identb")
            idf = kb.sb(es, [128, 128], F32, "identf")
            kb.dma(idf[:], I["ident"].ap(), writes=[idf])
            kb.cp("dve", ident[:], idf[:], reads=[idf], writes=[ident])
            Yb = [kb.sb(es, [128, ntc, 256], BF16, "Yb%d" % i) for i in range(2)]
            Z = kb.sb(es, [128, 2 * nj, 256], BF16, "Zs")
            Dt = [kb.sb(es, [128, ntc, 128], BF16, "Df%d" % i) for i in range(4)]
            Di = [kb.sb(es, [128, 2 * nj, 128], BF16, "Di%d" % i) for i in range(2)]
            kt = [kb.sb(es, [128, 2, 256], F32, "kt%d" % i) for i in range(2)]
            cm = [kb.sb(es, [128, 256], F32, "cm%d" % i) for i in range(4)]
            yo = [kb.sb(es, [128, 256], F32, "yo%d" % i) for i in range(2)]
            gt = [kb.sb(es, [128, 256], F32, "gt%d" % i) for i in range(2)]
            yn = [kb.sb(es, [128, 256], F32, "yn%d" % i) for i in range(2)]
            ynb = kb.sb(es, [128, 256], BF16, "ynb"); sqj = kb.sb(es, [128, 256], BF16, "sqj")
            ss = kb.sb(es, [128, 1], F32, "ss")
            mts = [kb.sb(es, [128, 2, 128], BF16, "mts%d" % i) for i in range(2)]
            bias_bc = kb.sb(es, [128, 2, 256], F32, "biasbc")
            gbc = kb.sb(es, [128, 256], F32, "gbc")
            kb.dma(bias_bc[:].rearrange("p o c -> p (o c)"), I["hy_bias"].ap()[L:L + 1].rearrange("a o c -> a (o c)").partition_broadcast(128), writes=[bias_bc])
            kb.dma(gbc[:], I["mix_norm_g"].ap()[L:L + 1, 768:1024].partition_broadcast(128), writes=[gbc])
            HZ = self.S["HZTM"].ap()
            for tc in range(ntc):
                y_ = yo[tc % 2]
                kb.dma(y_[:], HZ[r0 + tc * 128:r0 + (tc + 1) * 128, 0:256], reads=["HZTM"], writes=[y_])
                kb.cp("act" if tc % 2 else "dve", Yb[0][:, tc, :], y_[:], reads=[y_], writes=[Yb[0]])
            nd = 0
            for o in range(2):
                Yin = Yb[o]
                for j in range(nj):
                    k_ = kt[j % 2]
                    kb.dma(k_[:], KSP.ap()[j].rearrange("r p c -> p r c")[:, :, o * 256:(o + 1) * 256], reads=["KSPEC_" + nm], writes=[k_])
                    pr = kb.bank[(j % 2) * 2]; pi_ = kb.bank[(j % 2) * 2 + 1]
                    for ri, (fc, pk) in enumerate(((j, pr), (nj + j, pi_))):
                        D_ = Dt[nd % 4]
                        nd += 1
                        kb.dma(D_[:], I["hy_dfwd_" + nm].ap()[fc], writes=[D_])
                        for tc in range(ntc):
                            kb.mm(pk[:, 0:256], D_[:, tc, :], Yin[:, tc, :], start=(tc == 0), stop=(tc == ntc - 1), reads=[D_, Yin], writes=[pk])
                    c1, c2, c3, c4 = cm
                    kb.tt("dve", c1[:], pr[:, 0:256], k_[:, 0, :], OP.mult, reads=[pr, k_], writes=[c1])
                    kb.tt("dve", c2[:], pi_[:, 0:256], k_[:, 1, :], OP.mult, reads=[pi_, k_], writes=[c2])
                    kb.tt("pool", Z[:, j, :], c1[:], c2[:], OP.subtract, reads=[c1, c2], writes=[Z])
                    kb.tt("dve", c3[:], pr[:, 0:256], k_[:, 1, :], OP.mult, reads=[pr, k_], writes=[c3])
                    kb.tt("dve", c4[:], pi_[:, 0:256], k_[:, 0, :], OP.mult, reads=[pi_, k_], writes=[c4])
                    kb.tt("pool", Z[:, nj + j, :], c3[:], c4[:], OP.add, reads=[c3, c4], writes=[Z])
                for tc in range(ntc):
                    D_ = Di[tc % 2]
                    kb.dma(D_[:], I["hy_dinv_" + nm].ap()[tc], writes=[D_])
                    y_ = yo[tc % 2]; g_ = gt[tc % 2]; o_ = yn[tc % 2]
                    rows = slice(r0 + tc * 128, r0 + (tc + 1) * 128)
                    if o == 0:
                        kb.dma(y_[:], HZ[rows, 0:256], reads=["HZTM"], writes=[y_])
                    else:
                        kb.dma(y_[:], self.S["HY1"].ap()[rows, :], reads=[("HY1", tc)], writes=[y_])
                    kb.dma(g_[:], HZ[rows, 256 * (o + 1):256 * (o + 2)], reads=["HZTM"], writes=[g_])
                    pc = kb.bank[4 + (tc % 2)]
                    for fc in range(2 * nj):
                        kb.mm(pc[:, 0:256], D_[:, fc, :], Z[:, fc, :], start=(fc == 0), stop=(fc == 2 * nj - 1), reads=[D_, Z], writes=[pc])
                    kb.tt("pool", y_[:], y_[:], bias_bc[:, o, :], OP.mult, reads=[y_, bias_bc], writes=[y_])
                    kb.tt("dve", o_[:], pc[:, 0:256], y_[:], OP.add, reads=[pc, y_], writes=[o_])
                    kb.tt("dve", o_[:], o_[:], g_[:], OP.mult, reads=[o_, g_], writes=[o_])
                    if o == 0:
                        kb.cp("act", Yb[1][:, tc, :], o_[:], reads=[o_], writes=[Yb[1]])
                        kb.dma(self.S["HY1"].ap()[rows, :], o_[:], reads=[o_], writes=[("HY1", tc)], q="pool")
                    else:
                        if "HYO" in self.debug:
                            kb.dma(self.S["HYO"].ap()[rows, :], o_[:], reads=[o_], writes=["HYO"], q="pool")
                        kb.act(sqj[:], o_[:], AF.Square, accum_out=ss[:], reads=[o_], writes=[sqj, ss])
                        kb.ts("dve", ss[:], ss[:], 1.0 / 256, OP.mult, EPS, OP.add, reads=[ss], writes=[ss])
                        kb.act(ss[:], ss[:], AF.Sqrt, reads=[ss], writes=[ss])
                        kb.op("dve", lambda E: E.reciprocal(ss[:], ss[:]), reads=[ss], writes=[ss])
                        kb.stt("dve", ynb[:], o_[:], ss[:, 0:1], gbc[:], OP.mult, OP.mult, reads=[o_, ss, gbc], writes=[ynb])
                        pt_ = kb.bank[6 + (tc % 2)]
                        ptv = pt_[:].bitcast(BF16)
                        for fcx in range(2):
                            kb.tr(ptv[:, fcx * 128:(fcx + 1) * 128], ynb[:, fcx * 128:(fcx + 1) * 128], ident[:], reads=[ynb, ident], writes=[pt_])
                        m_ = mts[tc % 2]
                        kb.cp("act", m_[:], ptv[:, 0:256].rearrange("p (f q) -> p f q", f=2), reads=[pt_], writes=[m_])
                        kb.dma(self.S["MT"].ap()[768:1024, r0 + tc * 128:r0 + (tc + 1) * 128].rearrange("(f p) t -> p f t", p=128), m_[:],
                               reads=[m_], writes=[("MT_hy", part, tc)], q="pool")
        kb.barrier()


    def stage_s5merge(self, L):
        kb = self.kb
        blocks = [(0, 256)] + [(256 + 512 * i, 512) for i in range(8)]
        with ExitStack() as es:
            ones = kb.sb(es, [128, 128], BF16, "ones")
            kb.memset("dve", ones[:], 1.0, writes=[ones])
            gcol = kb.sb(es, [128, 2], F32, "gcol")
            kb.dma(gcol[:], self.I["mix_g_s5col"].ap()[L], writes=[gcol])
            xin = [kb.sb(es, [128, 2, 512], F32, "s5x%d" % i) for i in range(2)]
            sqb = kb.sb(es, [128, 2, 512], BF16, "sqb"); rst = kb.sb(es, [128, 512], F32, "rst")
            ob = [kb.sb(es, [128, 2, 512], BF16, "s5o%d" % i) for i in range(2)]
            for bi, (s0, nn) in enumerate(blocks):
                x_ = xin[bi % 2]; o_ = ob[bi % 2]
                kb.dma(x_[:, :, 0:nn], self.S["S5T"].ap().rearrange("(c p) t -> p c t", p=128)[:, :, s0:s0 + nn], reads=[("S5T", bi)], writes=[x_])
                kb.act(sqb[:, :, 0:nn], x_[:, :, 0:nn], AF.Square, reads=[x_], writes=[sqb])
                pn = kb.bank[bi % 2]
                for c in range(2):
                    kb.mm(pn[:, 0:nn], ones[:], sqb[:, c, 0:nn], start=(c == 0), stop=(c == 1), reads=[ones, sqb], writes=[pn])
                kb.ts("dve", rst[:, 0:nn], pn[:, 0:nn], 1.0 / 256, OP.mult, EPS, OP.add, reads=[pn], writes=[rst])
                kb.act(rst[:, 0:nn], rst[:, 0:nn], AF.Sqrt, reads=[rst], writes=[rst])
                kb.op("dve", lambda E: E.reciprocal(rst[:, 0:nn], rst[:, 0:nn]), reads=[rst], writes=[rst])
                for c in range(2):
                    kb.stt("dve", o_[:, c, 0:nn], x_[:, c, 0:nn], gcol[:, c:c + 1], rst[:, 0:nn], OP.mult, OP.mult, reads=[x_, gcol, rst], writes=[o_])
                kb.dma(self.S["MT"].ap()[0:256, s0:s0 + nn].rearrange("(c p) t -> p c t", p=128), o_[:, :, 0:nn], reads=[o_], writes=[("MT_s5", bi)], q="pool")
        kb.barrier()

    def stage_out_moe(self, L, last):
        kb = self.kb
        I = self.I
        S = self.S
        tiles = list(range(2, NT)) if last else list(range(NT))
        ntl = len(tiles)
        nblk = 2 * ((2 * ntl * 128 + 255) // 256 + 32)
        X = S["X"].ap()
        with ExitStack() as es:
            ident_f = kb.sb(es, [128, 128], F32, "identf")
            ident = kb.sb(es, [128, 128], BF16, "identb")
            kb.dma(ident_f[:], I["ident"].ap(), writes=[ident_f])
            kb.cp("dve", ident[:], ident_f[:], reads=[ident_f], writes=[ident])
            iota_e = kb.sb(es, [128, 32], F32, "iotae"); iota_b = kb.sb(es, [128, 256], F32, "iotab"); iota_p = kb.sb(es, [128, 1], F32, "iotap")
            ltri = kb.sb(es, [128, 128], BF16, "ltri"); ones = kb.sb(es, [128, 128], BF16, "ones")
            kb.dma(iota_e[:], I["iota_e"].ap(), writes=[iota_e]); kb.dma(iota_b[:], I["iota_b"].ap(), writes=[iota_b])
            kb.dma(iota_p[:], I["iota_p"].ap(), writes=[iota_p]); kb.dma(ltri[:], I["ltri"].ap(), writes=[ltri])
            kb.memset("dve", ones[:], 1.0, writes=[ones])
            GW = kb.sb(es, [128, NT, 2], F32, "GW"); EID = kb.sb(es, [128, NT, 2], F32, "EID"); RANK = kb.sb(es, [128, NT, 2], F32, "RANK")
            DEST = kb.sb(es, [128, NT, 2], F32, "DEST"); DESTI = kb.sb(es, [128, NT, 2], I32, "DESTI")
            base = kb.sb(es, [128, 32], F32, "base")
            BE = kb.sb(es, [128, 256], F32, "BE"); IDXW = kb.sb(es, [128, 2, 256], I32, "IDXW")
            CHG = kb.sb(es, [128, 256], F32, "CHG"); IDXF = kb.sb(es, [128, 256], F32, "IDXF")
            kb.memset("dve", base[:], 0.0, writes=[base])
            kb.memset("dve", RANK[:], 0.0, writes=[RANK])
            kb.memset("dve", DEST[:], 0.0, writes=[DEST])
            es2 = ExitStack()
            wo = kb.sb(es2, [128, 8, DM], BF16, "wo")
            wr = kb.sb(es2, [128, 8, 36], F32, "wr")
            kb.dma(wr[:], I["moe_wr"].ap()[L].rearrange("(kc p) n -> p kc n", p=128), writes=[wr])
            gate1 = kb.sb(es2, [128, DM], F32, "gate1"); G2 = kb.sb(es2, [128, DM], F32, "G2"); S2 = kb.sb(es2, [128, DM], F32, "S2")
            tmpb = kb.sb(es2, [128, DM], F32, "tmpb")
            es3 = ExitStack()
            wst = kb.sb(es3, [128, 8, DM], F32, "wst")
            kb.dma(wst[:], I["w_out"].ap()[L].rearrange("(kc p) n -> p kc n", p=128), writes=[wst])
            kb.cp("act", wo[:, 0:4, :], wst[:, 0:4, :], reads=[wst], writes=[wo])
            kb.cp("dve", wo[:, 4:8, :], wst[:, 4:8, :], reads=[wst], writes=[wo])
            kb.barrier()
            es3.close()
            mT = [kb.sb(es2, [128, 8, 512], BF16, "mT%d" % i) for i in range(2)]
            xt = [kb.sb(es2, [128, DM], F32, "xt%d" % i) for i in range(2)]
            xn = [kb.sb(es2, [128, DM], F32, "xn%d" % i) for i in range(2)]
            sq2 = [kb.sb(es2, [128, DM], BF16, "sq%d" % i) for i in range(2)]
            ss2 = [kb.sb(es2, [128, 1], F32, "ss%d" % i) for i in range(2)]; rstd2 = [kb.sb(es2, [128, 1], F32, "rstd%d" % i) for i in range(2)]
            tmp2 = [kb.sb(es2, [128, DM], F32, "tmp%d" % i) for i in range(2)]
            tmpw = [kb.sb(es2, [128, DM], F32, "tmpw%d" % i) for i in range(2)]
            ff = [kb.sb(es2, [128, DM], F32, "ff%d" % i) for i in range(2)]
            fbt = [kb.sb(es2, [128, DM], BF16, "fbt%d" % i) for i in range(2)]
            fT = kb.sb(es2, [128, 8, 128], F32, "fT")
            LG = kb.sb(es2, [128, NT, 36], F32, "LG")
            sm = {k: kb.sb(es2, shp, F32, k) for k, shp in (("gmax", [128, 1]), ("ngmax", [128, 1]), ("ohg", [128, 4]), ("pen", [128, 4]), ("ex4", [128, 4]),
                                                            ("sume", [128, 1]), ("gw", [128, 1]), ("msk", [128, 32]), ("top8", [128, 8]), ("nv2", [128, 1]),
                                                            ("p1", [128, 1]), ("p2", [128, 1]), ("a0", [128, 32]), ("junk", [128, 32]), ("csum", [128, 32]))}
            idx8 = kb.sb(es2, [128, 8], U32, "idx8")
            E2 = kb.sb(es2, [128, 64], F32, "E2"); E2b = kb.sb(es2, [128, 64], BF16, "E2b")
            blocks = [(0, 2)] + [(2 + 4 * i, 4) for i in range(8)]
            if last:
                blocks = blocks[1:]
            for bi, (t0, nt_) in enumerate(blocks):
                which = 1 if t0 == 0 else 0
                if t0 in (0, 2):
                    MOD = S["MOD"].ap()
                    kb.dma(gate1[:], MOD[which:which + 1, 2 * DM:3 * DM].partition_broadcast(128), reads=["MOD"], writes=[gate1])
                    self.load_mod_bc((G2, S2, tmpb), L, which, 3, "norm2_g")
                m_ = mT[bi % 2]
                nb = nt_ * 128
                c0 = t0 * 128
                kb.dma(m_[:, :, 0:nb], S["MT"].ap().rearrange("(kc p) t -> p kc t", p=128)[:, :, c0:c0 + nb],
                       reads=[("MT_s5", i) for i in range(9)] + [("MT_mla", i) for i in range(9)] + [("MT_hy", "l", i) for i in range(32)] + [("MT_hy", "c", i) for i in range(2)],
                       writes=[m_])
                for j in range(nt_):
                    t = t0 + j
                    x_ = xt[t % 2]; xo = xn[t % 2]
                    sq, ss, rstd, tmp = sq2[t % 2], ss2[t % 2], rstd2[t % 2], tmp2[t % 2]
                    tw = tmpw[t % 2]
                    kb.dma(x_[:], X[t * 128:(t + 1) * 128, :], reads=[("X", t)], writes=[x_])
                    for nh in range(2):
                        po = kb.bank[nh]
                        for kc in range(8):
                            kb.mm(po[:, :], m_[:, kc, j * 128:(j + 1) * 128], wo[:, kc, nh * 512:(nh + 1) * 512], start=(kc == 0), stop=(kc == 7),
                                  reads=[m_, wo], writes=[po])
                        kb.tt("dve", tw[:, nh * 512:(nh + 1) * 512], po[:, :], gate1[:, nh * 512:(nh + 1) * 512], OP.mult, reads=[po, gate1], writes=[tw])
                    kb.tt("pool", xo[:], tw[:], x_[:], OP.add, reads=[tw, x_], writes=[xo])
                    kb.dma(X[t * 128:(t + 1) * 128, :], xo[:], reads=[xo], writes=[("X", t)], q="pool")
                    if "XMID" in self.debug:
                        kb.dma(S["XMID"].ap()[t * 128:(t + 1) * 128, :], xo[:], reads=[xo], writes=["XMID"], q="pool")
                    f_ = ff[t % 2]; fb_ = fbt[t % 2]
                    self.rms_modulate(xo, G2, S2, sq, ss, rstd, tmp, f_, fb_)
                    kb.dma(S["FB"].ap()[t * 128:(t + 1) * 128, :], fb_[:], reads=[fb_], writes=[("FB", t)], q="pool")
                    for half in range(2):
                        pt_ = kb.bank[2 + half]
                        for q in range(4):
                            kc = half * 4 + q
                            kb.tr(pt_[:, q * 128:(q + 1) * 128], f_[:, kc * 128:(kc + 1) * 128], ident_f[:], reads=[f_, ident_f], writes=[pt_])
                        kb.cp("act" if half else "dve", fT[:, half * 4:(half + 1) * 4, :], pt_[:, :].rearrange("p (q c) -> p q c", c=128), reads=[pt_], writes=[fT])
                    pl = kb.bank[4]
                    for kc in range(8):
                        kb.mm(pl[:, 0:36], fT[:, kc, :], wr[:, kc, :], start=(kc == 0), stop=(kc == 7), reads=[fT, wr], writes=[pl])
                    kb.cp("act", LG[:, t, :], pl[:, 0:36], reads=[pl], writes=[LG])
            tl = tiles[0]
            NTL = len(tiles)
            TS = slice(tl, NT)

            def bcl(ap2, n_):
                return ap2.unsqueeze(2).to_broadcast([128, NTL, n_])
            R = {k: kb.sb(es2, shp, F32, k) for k, shp in (("gm", [128, NT]), ("d4", [128, NT, 4]), ("ohg", [128, NT, 4]), ("sume", [128, NT]), ("gwv", [128, NT]),
                                                           ("pen", [128, NT, 4]), ("msk", [128, NT, 32]), ("msk2", [128, NT, 32]), ("v1", [128, NT]), ("v2", [128, NT]),
                                                           ("p1", [128, NT]), ("EE", [128, NT, 2, 32]), ("PRE", [128, NT, 64]), ("CNT", [128, NT, 64]),
                                                           ("CNTS", [128, NT, 32]), ("BASEI", [128, NT, 32]), ("A0", [128, NT, 32]), ("onesT", [128, NT]))}
            E2b = kb.sb(es2, [128, NT, 64], BF16, "E2b")
            LG4 = LG[:, TS, 0:4]
            kb.op("dve", lambda E: E.tensor_reduce(R["gm"][:, TS], LG4, AX.X, OP.max), reads=[LG], writes=[R["gm"]])
            kb.tt("dve", R["ohg"][:, TS, :], LG4, bcl(R["gm"][:, TS], 4), OP.is_equal, reads=[LG, R["gm"]], writes=[R["ohg"]])
            kb.tt("dve", R["d4"][:, TS, :], LG4, bcl(R["gm"][:, TS], 4), OP.subtract, reads=[LG, R["gm"]], writes=[R["d4"]])
            kb.act(R["d4"][:, TS, :], R["d4"][:, TS, :], AF.Exp, reads=[R["d4"]], writes=[R["d4"]])
            kb.op("dve", lambda E: E.tensor_reduce(R["sume"][:, TS], R["d4"][:, TS, :], AX.X, OP.add), reads=[R["d4"]], writes=[R["sume"]])
            kb.op("dve", lambda E: E.reciprocal(R["gwv"][:, TS], R["sume"][:, TS]), reads=[R["sume"]], writes=[R["gwv"]])
            kb.ts("dve", R["pen"][:, TS, :], R["ohg"][:, TS, :], 1.0e30, OP.mult, -1.0e30, OP.add, reads=[R["ohg"]], writes=[R["pen"]])
            kb.tt("dve", R["msk"][:, TS, :].rearrange("p t (g e) -> p t g e", g=4), LG[:, TS, 4:36].rearrange("p t (g e) -> p t g e", g=4),
                  R["pen"][:, TS, :].unsqueeze(3).to_broadcast([128, NTL, 4, 8]), OP.add, reads=[LG, R["pen"]], writes=[R["msk"]])
            kb.op("dve", lambda E: E.tensor_reduce(R["v1"][:, TS], R["msk"][:, TS, :], AX.X, OP.max), reads=[R["msk"]], writes=[R["v1"]])
            kb.tt("dve", R["EE"][:, TS, 0, :], R["msk"][:, TS, :], bcl(R["v1"][:, TS], 32), OP.is_equal, reads=[R["msk"], R["v1"]], writes=[R["EE"]])
            kb.stt("dve", R["msk2"][:, TS, :], R["EE"][:, TS, 0, :], -2.0e30, R["msk"][:, TS, :], OP.mult, OP.add, reads=[R["EE"], R["msk"]], writes=[R["msk2"]])
            kb.op("dve", lambda E: E.tensor_reduce(R["v2"][:, TS], R["msk2"][:, TS, :], AX.X, OP.max), reads=[R["msk2"]], writes=[R["v2"]])
            kb.tt("dve", R["EE"][:, TS, 1, :], R["msk2"][:, TS, :], bcl(R["v2"][:, TS], 32), OP.is_equal, reads=[R["msk2"], R["v2"]], writes=[R["EE"]])
            kb.tt("dve", R["p1"][:, TS], R["v1"][:, TS], R["v2"][:, TS], OP.subtract, reads=[R["v1"], R["v2"]], writes=[R["p1"]])
            kb.act(R["p1"][:, TS], R["p1"][:, TS], AF.Sigmoid, reads=[R["p1"]], writes=[R["p1"]])
            kb.tt("dve", GW[:, TS, 0], R["p1"][:, TS], R["gwv"][:, TS], OP.mult, reads=[R["p1"], R["gwv"]], writes=[GW])
            kb.ts("dve", R["p1"][:, TS], R["p1"][:, TS], -1.0, OP.mult, 1.0, OP.add, reads=[R["p1"]], writes=[R["p1"]])
            kb.tt("dve", GW[:, TS, 1], R["p1"][:, TS], R["gwv"][:, TS], OP.mult, reads=[R["p1"], R["gwv"]], writes=[GW])
            kb.cp("act", E2b[:, TS, :], R["EE"][:, TS, :, :].rearrange("p t k e -> p t (k e)"), reads=[R["EE"]], writes=[E2b])
            for g0 in range(tl, NT, 8):
                g1 = min(NT, g0 + 8)
                ppre = kb.bank[5]; pcnt = kb.bank[6]
                for t in range(g0, g1):
                    kb.mm(ppre[:, (t - g0) * 64:(t - g0 + 1) * 64], ltri[:], E2b[:, t, :], reads=[ltri, E2b], writes=[ppre])
                    kb.mm(pcnt[:, (t - g0) * 64:(t - g0 + 1) * 64], ones[:], E2b[:, t, :], reads=[ones, E2b], writes=[pcnt])
                kb.cp("act", R["PRE"][:, g0:g1, :], ppre[:, 0:(g1 - g0) * 64].rearrange("p (t c) -> p t c", c=64), reads=[ppre], writes=[R["PRE"]])
                kb.cp("dve", R["CNT"][:, g0:g1, :], pcnt[:, 0:(g1 - g0) * 64].rearrange("p (t c) -> p t c", c=64), reads=[pcnt], writes=[R["CNT"]])
            kb.tt("dve", R["CNTS"][:, TS, :], R["CNT"][:, TS, 0:32], R["CNT"][:, TS, 32:64], OP.add, reads=[R["CNT"]], writes=[R["CNTS"]])
            kb.memset("dve", R["onesT"][:], 1.0, writes=[R["onesT"]])
            for e in range(32):
                kb.op("dve", lambda E: E.tensor_tensor_scan(R["BASEI"][:, TS, e], R["onesT"][:, TS], R["CNTS"][:, TS, e], 0.0, OP.mult, OP.add),
                      reads=[R["onesT"], R["CNTS"]], writes=[R["BASEI"]])
            kb.cp("dve", base[:], R["BASEI"][:, NT - 1, :], reads=[R["BASEI"]], writes=[base])
            kb.tt("dve", R["BASEI"][:, TS, :], R["BASEI"][:, TS, :], R["CNTS"][:, TS, :], OP.subtract, reads=[R["BASEI"], R["CNTS"]], writes=[R["BASEI"]])
            kb.tt("dve", R["A0"][:, TS, :], R["PRE"][:, TS, 0:32], R["BASEI"][:, TS, :], OP.add, reads=[R["PRE"], R["BASEI"]], writes=[R["A0"]])
            kb.tt("dve", R["A0"][:, TS, :], R["A0"][:, TS, :], R["EE"][:, TS, 0, :], OP.mult, reads=[R["A0"], R["EE"]], writes=[R["A0"]])
            kb.op("dve", lambda E: E.tensor_reduce(RANK[:, TS, 0], R["A0"][:, TS, :], AX.X, OP.add), reads=[R["A0"]], writes=[RANK])
            kb.tt("dve", R["A0"][:, TS, :], R["PRE"][:, TS, 32:64], R["BASEI"][:, TS, :], OP.add, reads=[R["PRE"], R["BASEI"]], writes=[R["A0"]])
            kb.tt("dve", R["A0"][:, TS, :], R["A0"][:, TS, :], R["CNT"][:, TS, 0:32], OP.add, reads=[R["A0"], R["CNT"]], writes=[R["A0"]])
            kb.tt("dve", R["A0"][:, TS, :], R["A0"][:, TS, :], R["EE"][:, TS, 1, :], OP.mult, reads=[R["A0"], R["EE"]], writes=[R["A0"]])
            kb.op("dve", lambda E: E.tensor_reduce(RANK[:, TS, 1], R["A0"][:, TS, :], AX.X, OP.add), reads=[R["A0"]], writes=[RANK])
            pb_ = kb.sb(es2, [128, 32], F32, "padb"); pend = kb.sb(es2, [128, 32], F32, "pend"); pst = kb.sb(es2, [128, 32], F32, "pstart")
            one32 = kb.sb(es2, [128, 32], F32, "one32")
            kb.memset("dve", one32[:], 1.0, writes=[one32])
            kb.ts("dve", pb_[:], base[:], 1.0 / 256, OP.mult, (255.0 / 256 - 0.5 + 1.0 / 512), OP.add, reads=[base], writes=[pb_])
            kb.ts("dve", pb_[:], pb_[:], MAGIC, OP.add, reads=[pb_], writes=[pb_])
            kb.ts("dve", pb_[:], pb_[:], -MAGIC, OP.add, 2.0, OP.mult, reads=[pb_], writes=[pb_])
            kb.op("dve", lambda E: E.tensor_tensor_scan(pend[:], one32[:], pb_[:], 0.0, OP.mult, OP.add), reads=[one32, pb_], writes=[pend])
            kb.tt("dve", pst[:], pend[:], pb_[:], OP.subtract, reads=[pend, pb_], writes=[pst])
            kb.ts("dve", pst[:], pst[:], 128.0, OP.mult, reads=[pst], writes=[pst])
            kb.memset("dve", BE[:], 0.0, writes=[BE])
            for e in range(32):
                kb.stt("dve", BE[:], iota_b[:], pend[:, e:e + 1], BE[:], OP.is_ge, OP.add, reads=[iota_b, pend, BE], writes=[BE])
            NS = 4
            per = nblk // NS
            assert nblk % NS == 0
            BIGI = 1.0e6
            kb.ts("dve", BE[:], BE[:], 31.0, OP.min, reads=[BE], writes=[BE])
            kb.memset("dve", CHG[:], 1.0, writes=[CHG])
            kb.tt("dve", CHG[:, 1:nblk], BE[:, 1:nblk], BE[:, 0:nblk - 1], OP.not_equal, reads=[BE], writes=[CHG])
            for q in range(NS):
                kb.memset("dve", CHG[:, q * per:q * per + 1], 1.0, writes=[CHG])
            kb.ts("dve", BE[:], BE[:], float(L * 32), OP.add, 128.0, OP.mult, reads=[BE], writes=[BE])
            kb.ts("dve", BE[:], BE[:], iota_p[:, 0:1], OP.add, 2.0, OP.mult, reads=[BE, iota_p], writes=[BE])
            for half in range(2):
                kb.ts("dve", IDXF[:], BE[:], float(half) - BIGI, OP.add, reads=[BE], writes=[IDXF])
                kb.tt("dve", IDXF[:], IDXF[:], CHG[:], OP.mult, reads=[IDXF, CHG], writes=[IDXF])
                kb.ts("dve", IDXF[:], IDXF[:], BIGI, OP.add, reads=[IDXF], writes=[IDXF])
                kb.cp("dve", IDXW[:, half, :], IDXF[:], reads=[IDXF], writes=[IDXW])
            for k in range(2):
                kb.tt("dve", R["A0"][:, TS, :], R["EE"][:, TS, k, :], pst[:, :].unsqueeze(1).to_broadcast([128, NTL, 32]), OP.mult,
                      reads=[R["EE"], pst], writes=[R["A0"]])
                kb.op("dve", lambda E: E.tensor_reduce(DEST[:, TS, k], R["A0"][:, TS, :], AX.X, OP.add), reads=[R["A0"]], writes=[DEST])
            kb.tt("dve", DEST[:], DEST[:], RANK[:], OP.add, reads=[DEST, RANK], writes=[DEST])
            kb.ts("dve", DEST[:], DEST[:], float(nblk * 128 - 1), OP.min, 0.0, OP.max, reads=[DEST], writes=[DEST])
            if "DBGR" in self.debug:
                kb.dma(S["DBGR"].ap()[:, 0:NT * 2], DEST[:].rearrange("p t k -> p (t k)"), reads=[DEST], writes=["DBGR"])
                kb.dma(S["DBGR"].ap()[:, NT * 2:NT * 4], RANK[:].rearrange("p t k -> p (t k)"), reads=[RANK], writes=["DBGR"])
                kb.dma(S["DBGR"].ap()[:, NT * 4:NT * 6], GW[:].rearrange("p t k -> p (t k)"), reads=[GW], writes=["DBGR"])
                kb.dma(S["DBGR"].ap()[:, NT * 6:NT * 6 + 32], base[:], reads=[base], writes=["DBGR"])
                kb.dma(S["DBGR"].ap()[:, NT * 6 + 32:NT * 6 + 64], pst[:], reads=[pst], writes=["DBGR"])
            kb.cp("dve", DESTI[:], DEST[:], reads=[DEST], writes=[DESTI])
            for t in tiles:
                fb_ = fbt[t % 2]
                kb.dma(fb_[:], S["FB"].ap()[t * 128:(t + 1) * 128, :], reads=[("FB", t)], writes=[fb_])
                for k in range(2):
                    kb.dma(None, None, reads=[fb_, DESTI], writes=["XS"], q="pool",
                           fn=lambda E: E.indirect_dma_start(out=S["XS"].ap(), out_offset=bass.IndirectOffsetOnAxis(DESTI[:, t, k:k + 1], 0),
                                                             in_=fb_[:], in_offset=None))
            kb.barrier()
            es2.close()
            es4 = ExitStack()
            WQ = [kb.sb(es4, [128, 3, 4096], BF16, "WQ%d" % i) for i in range(NS)]
            xs = [kb.sb(es4, [128, DM], BF16, "xs%d" % i) for i in range(2)]
            XT = [kb.sb(es4, [128, 8, 128], BF16, "XT%d" % i) for i in range(2)]
            h1s = [kb.sb(es4, [128, 512], F32, "h1s%d" % i) for i in range(2)]
            aT = [kb.sb(es4, [128, 4, 128], BF16, "aT%d" % i) for i in range(2)]
            ysb = [kb.sb(es4, [128, DM], F32, "ysb%d" % i) for i in range(2)]
            nrows2 = 2 * DEPTH * 32 * 128
            def slot_blk(slot):
                return (slot % NS) * per + slot // NS

            def Gs(slot):
                b = slot_blk(slot)
                if (b % 2 == 1) and (b % per != 0):
                    return
                wb = WQ[slot % NS]
                for wi, wn in enumerate(("moe_w1", "moe_w3", "moe_w2")):
                    for half in range(2):
                        kb.dma(None, None, reads=[IDXW], writes=[wb], q="pool",
                               fn=lambda E: E.indirect_dma_start(out=wb[:, wi, half * 2048:(half + 1) * 2048], out_offset=None,
                                                                 in_=I[wn].ap().rearrange("r (h c) -> (r h) c", h=2),
                                                                 in_offset=bass.IndirectOffsetOnAxis(IDXW[:, half, b:b + 1], 0),
                                                                 bounds_check=kb.bnd_reg, oob_is_err=False))

            def Ts(slot):
                b = slot_blk(slot)
                x_ = xs[slot % 2]
                kb.dma(x_[:], S["XS"].ap()[b * 128:(b + 1) * 128, :], reads=["XS"], writes=[x_])
                pt_ = kb.bank[0 if slot % 2 == 0 else 7]
                ptv = pt_[:].bitcast(BF16)
                for kc in range(8):
                    kb.tr(ptv[:, kc * 128:(kc + 1) * 128], x_[:, kc * 128:(kc + 1) * 128], ident[:], reads=[x_, ident], writes=[pt_])
                XT_ = XT[slot % 2]
                kb.cp("dve", XT_[:], ptv.rearrange("p (k n) -> p k n", k=8), reads=[pt_], writes=[XT_])

            def Hs(slot):
                wb = WQ[slot % NS]
                XT_ = XT[slot % 2]
                ph1 = kb.bank[1 + 2 * (slot % 2)]; ph3 = kb.bank[2 + 2 * (slot % 2)]
                for (pp, wi) in ((ph1, 0), (ph3, 1)):
                    for hc in range(4):
                        for kc in range(8):
                            kb.mm(pp[:, hc * 128:(hc + 1) * 128], wb[:, wi, kc * 512 + hc * 128:kc * 512 + (hc + 1) * 128], XT_[:, kc, :],
                                  start=(kc == 0), stop=(kc == 7), reads=[wb, XT_], writes=[pp])
                h_ = h1s[slot % 2]
                kb.act(h_[:], ph1[:, :], AF.Silu, reads=[ph1], writes=[h_])
                a_ = aT[slot % 2]
                kb.tt("dve", a_[:].rearrange("p h r -> p (h r)"), h_[:], ph3[:, :], OP.mult, reads=[h_, ph3], writes=[a_])

            def Ys(slot):
                b = slot_blk(slot)
                wb = WQ[slot % NS]
                a_ = aT[slot % 2]
                y_ = ysb[slot % 2]
                for nh in range(2):
                    py = kb.bank[5 + nh]
                    for hc in range(4):
                        kb.mm(py[:, :], a_[:, hc, :], wb[:, 2, hc * 1024 + nh * 512:hc * 1024 + (nh + 1) * 512], start=(hc == 0), stop=(hc == 3),
                              reads=[a_, wb], writes=[py])
                    kb.cp("act", y_[:, nh * 512:(nh + 1) * 512], py[:, :], reads=[py], writes=[y_])
                kb.dma(S["YS"].ap()[b * 128:(b + 1) * 128, :], y_[:], reads=[y_], writes=["YS"])

            if "moe_noB" not in self.debug:
                for s_ in range(min(NS, nblk)):
                    Gs(s_)
                Ts(0); Hs(0); Ts(1)
                for s_ in range(nblk):
                    if s_ + 1 < nblk:
                        Hs(s_ + 1)
                    Ys(s_)
                    if s_ + NS < nblk:
                        Gs(s_ + NS)
                    if s_ + 2 < nblk:
                        Ts(s_ + 2)
            kb.barrier()
            es4.close()
            es5 = ExitStack()
            gate2 = kb.sb(es5, [128, DM], F32, "gate2")
            xt = [kb.sb(es5, [128, DM], F32, "xt%d" % i) for i in range(2)]
            y0 = [kb.sb(es5, [128, DM], F32, "y0%d" % i) for i in range(2)]
            y1 = [kb.sb(es5, [128, DM], F32, "y1%d" % i) for i in range(2)]
            xo = [kb.sb(es5, [128, DM], F32, "xo%d" % i) for i in range(2)]
            if last:
                fg = kb.sb(es5, [128, DM], F32, "fg")
                kb.dma(fg[:], I["final_g"].ap().partition_broadcast(128), writes=[fg])
                sq = kb.sb(es5, [128, DM], BF16, "sq"); ss = kb.sb(es5, [128, 1], F32, "ss")
            for t in tiles:
                if "moe_noC" in self.debug:
                    break
                which = 1 if t < 2 else 0
                if t in (0, 2):
                    kb.dma(gate2[:], S["MOD"].ap()[which:which + 1, 5 * DM:6 * DM].partition_broadcast(128), reads=["MOD"], writes=[gate2])
                x_ = xt[t % 2]; a_ = y0[t % 2]; b_ = y1[t % 2]; o_ = xo[t % 2]
                kb.dma(x_[:], X[t * 128:(t + 1) * 128, :], reads=[("X", t)], writes=[x_])
                for k, yy in enumerate((a_, b_)):
                    kb.dma(None, None, reads=[DESTI, "YS"], writes=[yy], q="pool",
                           fn=lambda E: E.indirect_dma_start(out=yy[:], out_offset=None, in_=S["YS"].ap(),
                                                             in_offset=bass.IndirectOffsetOnAxis(DESTI[:, t, k:k + 1], 0)))
                kb.ts("dve", a_[:], a_[:], GW[:, t, 0:1], OP.mult, reads=[a_, GW], writes=[a_])
                kb.stt("dve", a_[:], b_[:], GW[:, t, 1:2], a_[:], OP.mult, OP.add, reads=[b_, GW, a_], writes=[a_])
                if "YMOE" in self.debug:
                    kb.dma(S["YMOE"].ap()[t * 128:(t + 1) * 128, :], a_[:], reads=[a_], writes=["YMOE"], q="pool")
                kb.tt("pool", a_[:], a_[:], gate2[:], OP.mult, reads=[a_, gate2], writes=[a_])
                kb.tt("dve", o_[:], a_[:], x_[:], OP.add, reads=[a_, x_], writes=[o_])
                if not last:
                    kb.dma(X[t * 128:(t + 1) * 128, :], o_[:], reads=[o_], writes=[("X", t)], q="pool")
                else:
                    kb.act(sq[:], o_[:], AF.Square, accum_out=ss[:], reads=[o_], writes=[sq, ss])
                    kb.ts("dve", ss[:], ss[:], 1.0 / DM, OP.mult, EPS, OP.add, reads=[ss], writes=[ss])
                    kb.act(ss[:], ss[:], AF.Sqrt, reads=[ss], writes=[ss])
                    kb.op("dve", lambda E: E.reciprocal(ss[:], ss[:]), reads=[ss], writes=[ss])
                    kb.stt("dve", o_[:], o_[:], ss[:, 0:1], fg[:], OP.mult, OP.mult, reads=[o_, ss, fg], writes=[o_])
                    kb.dma(self.out.ap()[(t - 2) * 128:(t - 1) * 128, :], o_[:], reads=[o_], writes=["OUT"], q="pool")
            kb.barrier()
            es5.close()
        kb.barrier()

    def finish(self, out_keys):
        kb = self.kb
        kb._waits("sp", out_keys, ())
        kb.barrier()


def build(debug=(), layers=DEPTH, stages=("mod", "proj")):
    P1 = _build(debug, layers, stages, None)
    return _build(debug, layers, stages, P1.kb.waited)


def _build(debug, layers, stages, needed):
    P = Prog(debug, needed)
    P.declare()
    P.stage_init()
    P.kb.barrier()
    for L in range(layers):
        if "mod" in stages:
            P.stage_mod(L)
        if "proj" in stages:
            P.stage_proj(L)
        if "s5" in stages:
            P.stage_s5(L)
        if "mla" in stages:
            P.stage_mla(L, L < DEPTH - 1)
        if "hy" in stages:
            P.stage_hyena(L, "l")
            if L < DEPTH - 1:
                P.stage_hyena(L, "c")
        if "moe" in stages:
            P.stage_s5merge(L)
            P.stage_out_moe(L, L == DEPTH - 1)
    P.finish(["OUT"])
    return P


def kernel(**inputs):
    inp = {k: np.asarray(v) for k, v in inputs.items()}
    P = build(debug=(), layers=DEPTH, stages=("mod", "proj", "s5", "mla", "hy", "moe"))
    maps = prep_inputs(inp)
    names = set(P.I.keys())
    in_maps = [{k: v for k, v in m.items() if k in names} for m in maps]
    res = run_bass_kernel_spmd(P.nc, in_maps, core_ids=list(range(8)))
    return np.stack([np.asarray(r["out"], dtype=np.float32) for r in res.results], axis=0)
```
